# Optimizing a Trainium2 kernel written in Bass

```python
import jax, jax.numpy as jnp
from jax import lax
import numpy as np

D_MODEL = 1024
BATCH = 8
SEQ = 2048
DEPTH = 4

N_BRANCH = 4
BRANCH_W = D_MODEL // 2
RW_N = 64
RW_H = BRANCH_W // RW_N
RW_DECAY_RANK = 64
RW_A_RANK = 64
RW_GN_EPS = 64e-5
RW_SHIFT_W = 3 * BRANCH_W + RW_DECAY_RANK + RW_A_RANK
GLA_H = 4
GLA_DV = BRANCH_W // GLA_H
GLA_DK = GLA_DV // 2
GLA_GATE_RANK = 16
GLA_LOGIT_NORM = 16.0
GLA_CHUNK = 32
ML_H = 4
ML_DV = BRANCH_W // ML_H
ML_DQK = ML_DV // 2
ML_CONV = 4
ML_CHUNK = 64
STAB_INIT = -1e30
HG_H = 4
HG_DK = BRANCH_W // HG_H
HG_DV = BRANCH_W // HG_H
HG_CHUNK = 32
NORM_EPS = 1e-6

COL_SIZES = (
    RW_SHIFT_W, BRANCH_W,
    GLA_H * GLA_DK, GLA_H * GLA_DK, BRANCH_W, GLA_GATE_RANK, BRANCH_W,
    2 * ML_H * ML_DQK, BRANCH_W, ML_H, ML_H, BRANCH_W,
    HG_H * HG_DK, HG_H * HG_DK, BRANCH_W, BRANCH_W,
    N_BRANCH * D_MODEL,
)
N_COLS = sum(COL_SIZES)

kernel_name = 'hybrid_rwkv7_gla_mlstm_hgrn2_gated_merge'


def _rms_norm(x, g, eps=NORM_EPS):
    xf = x.astype(jnp.float32)
    y = xf * lax.rsqrt(jnp.mean(xf * xf, axis=-1, keepdims=True) + eps)
    return (y * g.astype(jnp.float32)).astype(x.dtype)


def _heads(t, h):
    return t.reshape(t.shape[:-1] + (h, t.shape[-1] // h))


def _merge_heads(t):
    return t.reshape(t.shape[:-2] + (t.shape[-2] * t.shape[-1],))


def _head_rms(y, g, eps=NORM_EPS):
    y = y * lax.rsqrt(jnp.mean(y * y, axis=-1, keepdims=True) + eps)
    return _merge_heads(y) * g


def _head_ln(y, eps):
    mu = jnp.mean(y, axis=-1, keepdims=True)
    d = y - mu
    return _merge_heads(d * lax.rsqrt(jnp.mean(d * d, axis=-1, keepdims=True) + eps))


def _token_shift(t):
    return jnp.pad(t[:, :-1], ((0, 0), (1, 0), (0, 0)))


def _causal_conv(t, w):
    k, ch = w.shape
    return lax.conv_general_dilated(t, w[:, None, :].astype(t.dtype), window_strides=(1,),
                                    padding=[(k - 1, 0)], dimension_numbers=('NWC', 'WIO', 'NWC'),
                                    feature_group_count=ch)


def _to_chunks(t, c):
    b, s, h, x = t.shape
    return t.reshape(b, s // c, c, h, x).transpose(1, 0, 3, 2, 4)


def _from_chunks(t):
    n, b, h, c, x = t.shape
    return t.transpose(1, 0, 3, 2, 4).reshape(b, n * c, h, x)


def _chunk_gla(q, k, v, log_g, chunk):
    bsz, _, h, dk = q.shape
    dv = v.shape[-1]
    causal = jnp.tril(jnp.ones((chunk, chunk), dtype=bool))

    def step(state, blk):
        qc, kc, vc, gc = blk
        b = jnp.cumsum(gc, axis=2)
        rel = jnp.where(causal[:, :, None], b[:, :, :, None, :] - b[:, :, None, :, :], -jnp.inf)
        attn = jnp.einsum('bhik,bhjk,bhijk->bhij', qc, kc, jnp.exp(rel))
        o = attn @ vc + jnp.einsum('bhik,bhkv->bhiv', qc * jnp.exp(b), state)
        b_last = b[:, :, -1]
        state = (state * jnp.exp(b_last)[..., None]
                 + jnp.einsum('bhjk,bhjv->bhkv', kc * jnp.exp(b_last[:, :, None] - b), vc))
        return state, o

    init = jnp.zeros((bsz, h, dk, dv), q.dtype)
    _, o = lax.scan(step, init, tuple(_to_chunks(t, chunk) for t in (q, k, v, log_g)))
    return _from_chunks(o)


def _chunk_mlstm(q, k, v, i_pre, log_f, chunk):
    bsz, _, h, dk = q.shape
    dv = v.shape[-1]
    causal = jnp.tril(jnp.ones((chunk, chunk), dtype=bool))

    def step(carry, blk):
        c_mat, n_vec, m = carry
        qc, kc, vc, ic, fc = blk
        cf = jnp.cumsum(fc, axis=-1)
        log_d = jnp.where(causal, cf[..., :, None] - cf[..., None, :] + ic[..., None, :], -jnp.inf)
        log_inter = cf + m[..., None]
        m_t = jnp.maximum(log_inter, jnp.max(log_d, axis=-1))
        s = jnp.einsum('bhik,bhjk->bhij', qc, kc) * jnp.exp(log_d - m_t[..., None])
        w_inter = jnp.exp(log_inter - m_t)
        num = s @ vc + w_inter[..., None] * jnp.einsum('bhik,bhkv->bhiv', qc, c_mat)
        den = jnp.sum(s, axis=-1) + w_inter * jnp.einsum('bhik,bhk->bhi', qc, n_vec)
        o = num / jnp.maximum(jnp.abs(den), jnp.exp(-m_t))[..., None]
        m_new = m_t[..., -1]
        w_carry = jnp.exp(cf[..., -1] + m - m_new)
        kw = kc * jnp.exp(cf[..., -1:] - cf + ic - m_new[..., None])[..., None]
        c_mat = w_carry[..., None, None] * c_mat + jnp.einsum('bhjk,bhjv->bhkv', kw, vc)
        n_vec = w_carry[..., None] * n_vec + jnp.sum(kw, axis=2)
        return (c_mat, n_vec, m_new), o

    init = (jnp.zeros((bsz, h, dk, dv), q.dtype), jnp.zeros((bsz, h, dk), q.dtype),
            jnp.full((bsz, h), STAB_INIT, q.dtype))
    blks = (_to_chunks(q, chunk), _to_chunks(k, chunk), _to_chunks(v, chunk),
            _to_chunks(i_pre[..., None], chunk)[..., 0], _to_chunks(log_f[..., None], chunk)[..., 0])
    _, o = lax.scan(step, init, blks)
    return _from_chunks(o)


def _rwkv7(pre, mu, w0, w_up, a0, a_up, k_k, k_a, r_k, ln_g, ln_b):
    pre = pre + (_token_shift(pre) - pre) * mu
    r, k, v, w_code, a_code = jnp.split(
        pre, np.cumsum([BRANCH_W, BRANCH_W, BRANCH_W, RW_DECAY_RANK]), axis=-1)
    w = -jax.nn.softplus(-(w0 + jnp.tanh(w_code) @ w_up)) - 0.5
    decay = jnp.exp(-jnp.exp(w))
    a = jax.nn.sigmoid(a0 + a_code @ a_up)
    kk = _heads(k * k_k, RW_H)
    kk = kk / jnp.maximum(jnp.sqrt(jnp.sum(kk * kk, axis=-1, keepdims=True)), 1e-12)
    k = k * (1 + (a - 1) * k_a)
    r, k, v, a, decay = (_heads(t, RW_H) for t in (r, k, v, a, decay))

    def step(state, inp):
        r_t, w_t, k_t, v_t, ak_t, bk_t = inp
        sa = jnp.einsum('bhvk,bhk->bhv', state, ak_t)
        state = (state * w_t[:, :, None, :] + sa[..., None] * bk_t[:, :, None, :]
                 + v_t[..., None] * k_t[:, :, None, :])
        return state, jnp.einsum('bhvk,bhk->bhv', state, r_t)

    tm = lambda t: jnp.moveaxis(t, 1, 0)
    bsz = pre.shape[0]
    init = jnp.zeros((bsz, RW_H, RW_N, RW_N), pre.dtype)
    _, y = lax.scan(step, init, (tm(r), tm(decay), tm(k), tm(v), tm(-kk), tm(kk * a)))
    y = _head_ln(jnp.moveaxis(y, 0, 1), RW_GN_EPS) * ln_g + ln_b
    bonus = jnp.sum(r * k * _heads(r_k, RW_H), axis=-1, keepdims=True) * v
    return y + _merge_heads(bonus)


def _gla(q, k, v, g_code, gk_up, gk_b, norm_g):
    log_a = jax.nn.log_sigmoid(g_code @ gk_up + gk_b) / GLA_LOGIT_NORM
    o = _chunk_gla(_heads(q, GLA_H) * GLA_DK ** -0.5, _heads(k, GLA_H), _heads(v, GLA_H),
                   _heads(log_a, GLA_H), GLA_CHUNK)
    return _head_rms(o, norm_g)


def _mlstm(qk, v, i_raw, f_raw, conv_w, i_b, f_b, norm_g):
    qk = jax.nn.silu(_causal_conv(qk, conv_w))
    q, k = jnp.split(qk, 2, axis=-1)
    o = _chunk_mlstm(_heads(q, ML_H), _heads(k * ML_DQK ** -0.5, ML_H), _heads(v, ML_H),
                     i_raw + i_b, jax.nn.log_sigmoid(f_raw + f_b), ML_CHUNK)
    return _head_ln(o, NORM_EPS) * norm_g


def _hgrn2(q, f_raw, i, lb, norm_g):
    g = lb + (1 - lb) * jax.nn.sigmoid(f_raw)
    o = _chunk_gla(_heads(jax.nn.silu(q), HG_H), _heads(1 - g, HG_H), _heads(i, HG_H),
                   _heads(jnp.log(g), HG_H), HG_CHUNK)
    return _head_rms(o, norm_g)


def setup_inputs(seed: int = 0) -> dict:
    key = jax.random.key(seed)
    ks = iter(jax.random.split(key, 40))
    f32 = jnp.float32
    L, D, BW = DEPTH, D_MODEL, BRANCH_W
    nrm = lambda shape, s: s * jax.random.normal(next(ks), shape, f32)
    uni = lambda shape, lo, hi: jax.random.uniform(next(ks), shape, f32, lo, hi)
    return {
        'x': nrm((BATCH, SEQ, D), 1.0),
        'c': nrm((BATCH, D), 1.0),
        'norm_g': 1.0 + nrm((L, D), 0.02),
        'ada_w': nrm((L, D, 3 * D), D ** -0.5),
        'ada_b': nrm((L, 3 * D), 0.02),
        'w_in': nrm((L, D, N_COLS), D ** -0.5),
        'rw_mu': uni((L, RW_SHIFT_W), 0.0, 1.0),
        'rw_w0': uni((L, BW), -3.0, 1.0),
        'rw_w_up': nrm((L, RW_DECAY_RANK, BW), 0.1),
        'rw_a0': nrm((L, BW), 0.1),
        'rw_a_up': nrm((L, RW_A_RANK, BW), RW_A_RANK ** -0.5),
        'rw_k_k': 0.85 + nrm((L, BW), 0.02),
        'rw_k_a': 1.0 + nrm((L, BW), 0.02),
        'rw_r_k': nrm((L, BW), 0.1),
        'rw_ln_g': 1.0 + nrm((L, BW), 0.02),
        'rw_ln_b': nrm((L, BW), 0.02),
        'gla_gk_up': nrm((L, GLA_GATE_RANK, GLA_H * GLA_DK), GLA_GATE_RANK ** -0.5),
        'gla_gk_b': nrm((L, GLA_H * GLA_DK), 0.1),
        'gla_norm_g': 1.0 + nrm((L, BW), 0.02),
        'ml_conv_w': nrm((L, ML_CONV, 2 * ML_H * ML_DQK), ML_CONV ** -0.5),
        'ml_i_b': nrm((L, ML_H), 0.1),
        'ml_f_b': jnp.linspace(3.0, 6.0, ML_H, dtype=f32)[None, :] + nrm((L, ML_H), 0.1),
        'ml_norm_g': 1.0 + nrm((L, BW), 0.02),
        'hg_lb_logits': nrm((L, HG_H * HG_DK), 0.5),
        'hg_norm_g': 1.0 + nrm((L, BW), 0.02),
        'w_branch': nrm((L, N_BRANCH, BW, D), BW ** -0.5),
        'w_out': nrm((L, D, D), D ** -0.5),
        'final_g': 1.0 + nrm((D,), 0.02),
    }


def reference(x, c, norm_g, ada_w, ada_b, w_in, rw_mu, rw_w0, rw_w_up, rw_a0, rw_a_up,
              rw_k_k, rw_k_a, rw_r_k, rw_ln_g, rw_ln_b, gla_gk_up, gla_gk_b, gla_norm_g,
              ml_conv_w, ml_i_b, ml_f_b, ml_norm_g, hg_lb_logits, hg_norm_g, w_branch, w_out,
              final_g):
    dt = x.dtype
    f32 = jnp.float32
    lb_p = jax.nn.softmax(hg_lb_logits.astype(f32), axis=0)
    lower_bounds = jnp.cumsum(lb_p, axis=0) - lb_p[0]
    cond = jax.nn.silu(c)
    split_at = np.cumsum(COL_SIZES)[:-1]
    for l in range(DEPTH):
        shift, scale, gate = jnp.split(cond @ ada_w[l] + ada_b[l], 3, axis=-1)
        u = _rms_norm(x, norm_g[l]) * (1 + scale[:, None]) + shift[:, None]
        p = u @ w_in[l]
        (rw_pre, rw_z, gq, gk, gv, g_code, gz, mqk, mv, mi, mf, mz,
         hq, hf, hi, hz, merge_logits) = [t.astype(f32) for t in jnp.split(p, split_at, axis=-1)]
        y_a = _rwkv7(rw_pre, rw_mu[l], rw_w0[l], rw_w_up[l], rw_a0[l], rw_a_up[l], rw_k_k[l],
                     rw_k_a[l], rw_r_k[l], rw_ln_g[l], rw_ln_b[l])
        y_b = _gla(gq, gk, gv, g_code, gla_gk_up[l], gla_gk_b[l], gla_norm_g[l])
        y_c = _mlstm(mqk, mv, mi, mf, ml_conv_w[l], ml_i_b[l], ml_f_b[l], ml_norm_g[l])
        y_d = _hgrn2(hq, hf, hi, lower_bounds[l], hg_norm_g[l])
        ys = jnp.stack([y_a * jax.nn.silu(rw_z), y_b * jax.nn.silu(gz),
                        y_c * jax.nn.silu(mz), y_d * jax.nn.silu(hz)], axis=2).astype(dt)
        branch = jnp.einsum('bsmv,mvd->bsmd', ys, w_branch[l])
        gates = jax.nn.sigmoid(merge_logits).reshape(merge_logits.shape[:-1] + (N_BRANCH, D_MODEL))
        merged = jnp.einsum('bsmd,bsmd->bsd', gates.astype(dt), branch)
        x = x + gate[:, None] * (merged @ w_out[l])
    return _rms_norm(x, final_g)
```

```python
import contextlib
import numpy as np
import ml_dtypes
import concourse.bass as bass
import concourse.mybir as mybir
from concourse.bass_utils import run_bass_kernel_spmd

F32 = mybir.dt.float32
BF16 = mybir.dt.bfloat16
AF = mybir.ActivationFunctionType
ALU = mybir.AluOpType

D = 1024
SEQ = 2048
DEPTH = 4
NCOLS = 11416
TS = 512
NST = SEQ // TS
ENGS = ("pe", "dve", "act", "pool", "sp")

C_RW = 0; C_RWZ = 1664
C_GQ = 2176; C_GK = 2432; C_GV = 2688; C_GC = 3200; C_GZ = 3216
C_MQK = 3728; C_MV = 4240; C_MI = 4752; C_MF = 4756; C_MZ = 4760
C_HQ = 5272; C_HF = 5784; C_HI = 6296; C_HZ = 6808
C_MG = 7320

PK = {}
_o = 0
for _n, _w in (("norm_g", 8), ("ada_b", 24), ("mu", 12), ("mu_wc", 1), ("mu_ac", 1), ("w0", 4), ("a0", 4),
               ("k_k", 4), ("k_a", 4), ("r_k", 4), ("ln_g", 4), ("ln_b", 4), ("gk_b", 2), ("gla_g", 4),
               ("conv", 16), ("ml_g", 4), ("lb", 16), ("hg_g", 4), ("i_b", 2), ("f_b", 2)):
    PK[_n] = (_o, _w)
    _o += _w
NPK = _o


class Op:
    __slots__ = ("eng", "fn", "deps", "idx", "dma_stream", "signal", "cnt")

    def __init__(self, eng, fn):
        self.eng = eng
        self.fn = fn
        self.deps = []
        self.dma_stream = None
        self.signal = False
        self.cnt = 0


class Sched:
    def __init__(self, nc):
        self.nc = nc
        self.ops = []
        self.last_w = {}
        self.readers = {}
        self.streams = {}
        self.record = False

    def op(self, eng, fn, reads=(), writes=(), dma=None):
        if self.record:
            return None
        o = Op(eng, fn)
        o.idx = len(self.ops)
        deps = {}
        for k in reads:
            w = self.last_w.get(k)
            if w is not None:
                deps[w.idx] = w
        for k in writes:
            w = self.last_w.get(k)
            if w is not None:
                deps[w.idx] = w
            for r in self.readers.get(k, ()):
                deps[r.idx] = r
        if dma is not None:
            o.dma_stream = dma
            n = self.streams.get(dma, 0) + 1
            self.streams[dma] = n
            o.cnt = n
        o.deps = [deps[i] for i in sorted(deps)]
        for k in reads:
            self.readers.setdefault(k, []).append(o)
        for k in writes:
            self.last_w[k] = o
            self.readers[k] = []
        self.ops.append(o)
        return o

    def emit(self, final_wait_ops=()):
        nc = self.nc
        for o in self.ops:
            nd = []
            for d in o.deps:
                if d.dma_stream is None and o.dma_stream is None and d.eng == o.eng and o.eng == "pe":
                    continue
                nd.append(d)
            o.deps = nd
            for d in nd:
                d.signal = True
        for o in final_wait_ops:
            o.signal = True
        cnt = {e: 0 for e in ENGS}
        for o in self.ops:
            if o.dma_stream is None and o.signal:
                cnt[o.eng] += 1
                o.cnt = cnt[o.eng]
        EP, DEP = 10 ** 9, 10 ** 9
        with contextlib.ExitStack() as es:
            esem = {}
            for e in ENGS:
                for ep in range(max(1, (cnt[e] + EP - 1) // EP)):
                    esem[(e, ep)] = es.enter_context(nc.semaphore("c_%s%d" % (e, ep)))
            ssem = {}
            for i, (s_, n_) in enumerate(self.streams.items()):
                for ep in range(max(1, (n_ + DEP - 1) // DEP)):
                    ssem[(s_, ep)] = es.enter_context(nc.semaphore("d%d_%d" % (i, ep)))
            block = es.enter_context(nc.Block())
            per = {e: [o for o in self.ops if o.eng == e] for e in ENGS}

            def semval(d):
                if d.dma_stream is not None:
                    return ssem[(d.dma_stream, (d.cnt - 1) // DEP)], 16 * ((d.cnt - 1) % DEP + 1)
                return esem[(d.eng, (d.cnt - 1) // EP)], (d.cnt - 1) % EP + 1

            def runner(e):
                def body(engine):
                    waited = {}
                    for o in per[e]:
                        for d in o.deps:
                            sem, val = semval(d)
                            key = id(sem)
                            if waited.get(key, 0) >= val:
                                continue
                            waited[key] = val
                            engine.wait_ge(sem, val)
                        ins = o.fn(engine)
                        if o.dma_stream is not None:
                            ins.then_inc(semval(o)[0], 16)
                        elif o.signal:
                            ins.then_inc(semval(o)[0], 1)
                    if e == "sp":
                        for o in final_wait_ops:
                            sem, val = semval(o)
                            engine.wait_ge(sem, val)
                return body

            block.tensor(runner("pe"))
            block.vector(runner("dve"))
            block.scalar(runner("act"))
            block.gpsimd(runner("pool"))
            block.sync(runner("sp"))
        return cnt


def _host_consts():
    c = {}
    i128 = np.eye(128, dtype=np.float32)
    c["ident"] = i128
    s = np.arange(128)[:, None]
    t = np.arange(128)[None, :]
    m_incl = (s <= t).astype(np.float32)
    m_blk = ((s <= t) & ((s // 64) == (t // 64))).astype(np.float32)
    c["m128"] = np.tile(m_incl[:, None, :], (1, 4, 1)).reshape(128, 512)
    c["m64b"] = np.tile(m_blk[:, None, :], (1, 4, 1)).reshape(128, 512)
    m_b32 = ((s <= t) & ((s // 32) == (t // 32))).astype(np.float32)
    c["m32b"] = np.tile(m_b32[:, None, :], (1, 4, 1)).reshape(128, 512)
    m96 = np.zeros((128, 128), np.float32); m96[96:] = 1.0
    c["m96"] = m96
    s6 = np.arange(64)[:, None]
    t6 = np.arange(64)[None, :]
    z = np.zeros((128, 512), np.float32)
    a = z.copy(); a[:64] = np.tile((t6 < s6).astype(np.float32)[:, None, :], (1, 8, 1)).reshape(64, 512)
    c["r_ts"] = a
    a = z.copy(); a[:64] = np.tile((s6 < t6).astype(np.float32)[:, None, :], (1, 8, 1)).reshape(64, 512)
    c["r_st"] = a
    a = z.copy(); a[:64] = np.tile((s6 <= t6).astype(np.float32)[:, None, :], (1, 8, 1)).reshape(64, 512)
    c["r_sti"] = a
    a = z.copy(); a[:64] = np.tile(np.eye(64, dtype=np.float32)[:, None, :], (1, 8, 1)).reshape(64, 512)
    c["i8"] = a
    c["ones"] = np.ones((128, 128), np.float32)
    bd = np.zeros((128, 128), np.float32); bd[:64, :64] = 1; bd[64:, 64:] = 1
    c["bd"] = bd
    names = ["ident", "m128", "m64b", "m32b", "r_ts", "r_st", "r_sti", "i8", "ones", "bd", "m96"]
    offs = {}
    o = 0
    for n in names:
        offs[n] = (o, c[n].shape[1]); o += c[n].shape[1]
    return np.concatenate([c[n] for n in names], axis=1), offs


CONSTS, COFF = _host_consts()
NCONST = CONSTS.shape[1]
CFO = {"ident": 0, "ones": 128, "bd": 256, "m96": 384}
NCF = 512
CSTF = np.concatenate([CONSTS[:, COFF[n][0]:COFF[n][0] + 128] for n in ("ident", "ones", "bd", "m96")], axis=1)


def _pack_params(inp, l):
    P = np.zeros((128, NPK), np.float32)

    def put(name, arr):
        o, w = PK[name]
        P[:arr.shape[0], o:o + arr.shape[1]] = arr

    fm = lambda v: np.ascontiguousarray(v.reshape(-1, 128).T)
    put("norm_g", fm(inp["norm_g"][l]))
    put("ada_b", fm(inp["ada_b"][l]))
    mu = inp["rw_mu"][l]
    put("mu", fm(mu[:1536]))
    put("mu_wc", mu[1536:1600].reshape(64, 1))
    put("mu_ac", mu[1600:1664].reshape(64, 1))
    put("w0", fm(inp["rw_w0"][l])); put("a0", fm(inp["rw_a0"][l]))
    put("k_k", fm(inp["rw_k_k"][l])); put("k_a", fm(inp["rw_k_a"][l])); put("r_k", fm(inp["rw_r_k"][l]))
    put("ln_g", fm(inp["rw_ln_g"][l])); put("ln_b", fm(inp["rw_ln_b"][l]))
    put("gk_b", fm(inp["gla_gk_b"][l])); put("gla_g", fm(inp["gla_norm_g"][l]))
    cw = inp["ml_conv_w"][l]
    put("conv", np.concatenate([fm(cw[j]) for j in range(4)], axis=1))
    put("ml_g", fm(inp["ml_norm_g"][l]))
    put("lb", np.concatenate([fm(inp["hg_lb_logits"][j]) for j in range(4)], axis=1))
    put("hg_g", fm(inp["hg_norm_g"][l]))
    put("i_b", fm(np.repeat(inp["ml_i_b"][l], 64)))
    put("f_b", fm(np.repeat(inp["ml_f_b"][l], 64)))
    return P


def build_nc(n_layers=DEPTH, branches=(0, 1, 2, 3), debug=False, stage=99):
    nc = bass.Bass("TRN2", target_bir_lowering=False)
    dram = lambda n, s, k="ExternalInput": nc.dram_tensor(n, list(s), F32, kind=k).ap()
    xT_d = dram("xT", [D, SEQ])
    c_d = dram("cT", [128, 8])
    pk_d = dram("pk", [DEPTH, 128, NPK])
    fg_d = dram("fg", [128, 8])
    cst_d = dram("cst", [128, NCONST])
    cstf_d = dram("cstf", [128, NCF])
    adaw_d = dram("ada_w", [DEPTH, D, 3 * D])
    win_d = dram("w_in", [DEPTH, D, NCOLS])
    wup_d = dram("rw_w_up", [DEPTH, 64, 512])
    aup_d = dram("rw_a_up", [DEPTH, 64, 512])
    gup_d = dram("gla_gk_up", [DEPTH, 16, 256])
    wbr_d = dram("w_branch", [DEPTH, 4, 512, D])
    wout_d = dram("w_out", [DEPTH, D, D])
    out_d = dram("outT", [D, SEQ], "ExternalOutput")
    dbg_d = dram("dbg", [4, 512, SEQ], "ExternalOutput") if debug else None

    es = contextlib.ExitStack()
    with es:
        T = lambda n, s, d=F32: es.enter_context(nc.sbuf_tensor("s_" + n, list(s), d))
        x = T("x", [128, 8, TS])
        uT = T("uT", [128, 8, TS], BF16)
        merged = T("merged", [128, 8, TS])
        mergedb = uT
        ys = T("ys", [128, 4, TS], BF16)
        pk = T("pk", [128, DEPTH, NPK])
        drv = T("drv", [128, 64])
        cst = T("cst", [128, NCF])
        cstb = T("cstb", [128, NCONST], BF16)
        cT = T("cT", [128, 8])
        fg = T("fg", [128, 8])
        mod = T("mod", [128, 24])
        lbt = T("lbt", [128, 16]); lbe = T("lbe", [128, 16]); lbs = T("lbs", [128, 4])
        NWB = 6
        wb = [T("wb%d" % i, [128, 8, 128], BF16) for i in range(NWB)]
        wv = [T("wv%d" % i, [128, 8, 512], BF16) for i in range(1)]
        wup = T("wup", [64, 512], BF16); aup = T("aup", [64, 512], BF16); gup = T("gup", [16, 256], BF16)
        NF = 16
        fs = [T("fs%d" % i, [128, TS + 4]) for i in range(NF)]
        LL = [T("ll%d" % i, [128, TS]) for i in range(8)]
        NB = 6
        bs = [T("bs%d" % i, [128, TS], BF16) for i in range(NB)]
        BL = [T("bl%d" % i, [128, TS], BF16) for i in range(20)]
        prc = T("prc", [128, 16])
        cqc = T("cqc", [128, 4, 4])
        gnb = [T("gn%d" % i, [128, TS + 1]) for i in range(4)]
        nref = T("nref", [128, 4, 16])
        ktz = T("ktz", [128, 512], BF16)
        vtok = T("vtok", [128, 4, 512], BF16)
        ktok = T("ktok", [128, 4, 512], BF16)
        rv = {n: T("rv_" + n, [64, 512], BF16) for n in
              ("vt", "bt", "kt", "N", "NT", "N2", "NT2", "Y", "Y2", "Aak", "Arb", "Ark", "X", "U")}
        rw_sf = T("rw_sf", [64, 8, 64]); rw_sb = T("rw_sb", [64, 8, 64], BF16)
        la_sf = [T("la_sf%d" % i, [128, 4, 128]) for i in range(4)]
        la_sb = [T("la_sb%d" % i, [128, 4, 128], BF16) for i in range(4)]
        gamh = T("gamh", [64, 8, 16])
        gam = T("gam", [128, 4, 16])
        yT = merged
        wcac = T("wcac", [64, 2, TS])
        wcacb = T("wcacb", [64, 2, TS], BF16)
        ps = [es.enter_context(nc.psum_tensor("ps%d" % i, [128, 512], F32)) for i in range(8)]

        S = Sched(nc)
        st = {"bank": 0, "w": 0, "fsi": 0, "bsi": 0, "wai": 0}

        def bank():
            i = st["bank"]; st["bank"] = (i + 1) % 6
            return ps[i], "ps%d" % i

        CS = lambda n: cst[:, CFO[n]:CFO[n] + 128]
        CB = lambda n: cstb[:, COFF[n][0]:COFF[n][0] + COFF[n][1]]
        PKc = lambda l, n, j=0, w=1: pk[:, l, PK[n][0] + j:PK[n][0] + j + w]

        def mm(out, lhsT, rhs, start, stop, r, w):
            S.op("pe", lambda e: e.matmul(out, lhsT, rhs, start=start, stop=stop), reads=r, writes=w)

        def tr(out, in_, ident, r, w):
            S.op("pe", lambda e: e.transpose(out, in_, ident), reads=r, writes=w)

        def act(out, in_, func, r, w, bias=None, scale=None):
            kw = {}
            if bias is not None:
                kw["bias"] = bias
            if scale is not None:
                kw["scale"] = scale
            S.op("act", lambda e: e.activation(out=out, in_=in_, func=func, **kw), reads=r, writes=w)

        def tt(out, a, b, op, r, w, eng="dve"):
            S.op(eng, lambda e: e.tensor_tensor(out=out, in0=a, in1=b, op=op), reads=r, writes=w)

        def tsc(out, a, s1, s2, op0, op1, r, w, eng="dve"):
            if op1 is None:
                S.op(eng, lambda e: e.tensor_scalar(out=out, in0=a, scalar1=s1, scalar2=None, op0=op0), reads=r, writes=w)
            else:
                S.op(eng, lambda e: e.tensor_scalar(out=out, in0=a, scalar1=s1, scalar2=s2, op0=op0, op1=op1),
                     reads=r, writes=w)

        def stt(out, a, sc, b, op0, op1, r, w, eng="dve"):
            S.op(eng, lambda e: e.scalar_tensor_tensor(out=out, in0=a, scalar=sc, in1=b, op0=op0, op1=op1),
                 reads=r, writes=w)

        def cp(out, in_, r, w, eng="dve"):
            S.op(eng, lambda e: e.tensor_copy(out=out, in_=in_), reads=r, writes=w)

        def rsq(out, in_, scale, bias, r, w):
            act(out, in_, AF.Ln, r, w, bias=bias, scale=scale)
            act(out, out, AF.Exp, w, w, scale=-0.5)

        def recip(out, in_, r, w):
            S.op("dve", lambda e: e.reciprocal(out=out, in_=in_), reads=r, writes=w)

        def mset(ap, val, w):
            S.op("dve", lambda e: e.memset(ap, val), writes=w)

        def dma(eng, out, in_, r, w, stream):
            return S.op(eng, lambda e: e.dma_start(out=out, in_=in_), reads=r, writes=w, dma=stream)

        def cumsum(ct, src, srck):
            S.op("dve", lambda e: e.tensor_tensor_scan(out=gnb[ct][:, 1:TS + 1], data0=src, data1=src, initial=0.0,
                                                       op0=ALU.add, op1=ALU.max), reads=[srck], writes=["gn%d" % ct])

        def wchunk(src, n, kc=8):
            i = st["w"]; st["w"] = (i + 1) % NWB
            key = "wb%d" % i
            dma("pool", wb[i][:, 0:kc, 0:n], src.rearrange("(k p) c -> p k c", p=128), [], [key], key)
            return wb[i], key

        def proj(l, c0, n):
            w, wk = wchunk(win_d[l, :, c0:c0 + n], n)
            b, bk = bank()
            for k in range(8):
                mm(b[0:n, :], w[:, k, 0:n], uT[:, k, :], k == 0, k == 7, [wk, "uT"], [bk])
            return b, bk

        def proj_tok(l, c0):
            for h in range(2):
                dma("pool", wv[0][:, :, h * 256:(h + 1) * 256],
                    win_d[l, :, c0 + h * 256:c0 + (h + 1) * 256].rearrange("(k p) c -> p k c", p=128),
                    [], ["wv_%d" % h], "wv_%d" % h)
            for tI in range(TS // 128):
                b, bk = bank()
                for k in range(8):
                    mm(b[:, :], uT[:, k, tI * 128:(tI + 1) * 128], wv[0][:, k, :], k == 0, k == 7,
                       ["wv_0", "wv_1", "uT"], [bk])
                if tI % 2 == 0:
                    act(vtok[:, tI, :], b[:, :], AF.Copy, [bk], ["vtok%d" % tI])
                else:
                    cp(vtok[:, tI, :], b[:, :], [bk], ["vtok%d" % tI])

        def fsa():
            i = st["fsi"]; st["fsi"] = (i + 1) % NF
            return fs[i], "fs%d" % i

        def bsa():
            i = st["bsi"]; st["bsi"] = (i + 1) % NB
            return bs[i], "bs%d" % i

        XK = ["x%d" % k for k in range(8)]

        def rms_to(sl_src, emit):
            b, bk = bank()
            for k in range(8):
                sq, sqk = fsa()
                act(sq[:, 0:TS], x[:, k, :], AF.Square, [XK[k]], [sqk])
                mm(b[:, :], CS("ones"), sq[:, 0:TS], k == 0, k == 7, [sqk, "cst"], [bk])
            rs, rsk = fsa()
            rsq(rs[:, 0:TS], b[:, :], 1.0, 1024.0 * 1e-6, [bk], [rsk])
            for k in range(8):
                t1, t1k = fsa()
                tt(t1[:, 0:TS], x[:, k, :], rs[:, 0:TS], ALU.mult, [XK[k], rsk], [t1k])
                emit(k, t1, t1k)

        def program():
            dma("sp", cst[:, :], cstf_d, [], ["cst"], "cst")
            dma("pool", cstb[:, :], cst_d, [], ["cstb"], "cstb")
            dma("sp", pk[:, :, :], pk_d.rearrange("l p n -> p l n"), [], ["pk"], "pk")
            dma("sp", cT[:, :], c_d, [], ["cT"], "cT")
            dma("sp", fg[:, :], fg_d, [], ["fg"], "fg")
            act(cT[:, :], cT[:, :], AF.Silu, ["cT"], ["cT"])
            o_lb = PK["lb"][0]
            act(lbe[:, :], pk[:, 0, o_lb:o_lb + 16], AF.Exp, ["pk"], ["lbe"])
            tt(lbs[:, :], lbe[:, 0:4], lbe[:, 4:8], ALU.add, ["lbe"], ["lbs"])
            tt(lbs[:, :], lbs[:, :], lbe[:, 8:12], ALU.add, ["lbs", "lbe"], ["lbs"])
            tt(lbs[:, :], lbs[:, :], lbe[:, 12:16], ALU.add, ["lbs", "lbe"], ["lbs"])
            recip(lbs[:, :], lbs[:, :], ["lbs"], ["lbs"])
            mset(lbt[:, 0:4], 0.0, ["lbt"])
            for j in range(1, 4):
                t_, tk_ = fsa()
                tt(t_[:, 0:4], lbe[:, j * 4:(j + 1) * 4], lbs[:, :], ALU.mult, ["lbe", "lbs"], [tk_])
                tt(lbt[:, j * 4:(j + 1) * 4], lbt[:, (j - 1) * 4:j * 4], t_[:, 0:4], ALU.add, [tk_, "lbt"], ["lbt"])
            for i in range(4):
                mset(gnb[i][:, 0:1], 0.0, ["gn%d" % i])

            for l in range(n_layers):
                layer(l)

            outs = []
            for sI in range(NST):
                sl = slice(sI * TS, (sI + 1) * TS)
                for k in range(8):
                    dma("sp", x[:, k, :], (out_d if n_layers > 0 else xT_d)[k * 128:(k + 1) * 128, sl], ["xd%d" % sI], [XK[k]], "xl%d" % k)

                def emit(k, t1, t1k, sI=sI, sl=sl):
                    tsc(t1[:, 0:TS], t1[:, 0:TS], fg[:, k:k + 1], 32.0, ALU.mult, ALU.mult, [t1k, "fg"], [t1k])
                    outs.append(dma("sp", out_d[k * 128:(k + 1) * 128, sl], t1[:, 0:TS], [t1k], ["xd%d" % sI], "o_" + t1k))
                rms_to(sl, emit)
            return outs

        def layer(l):
            b, bk = bank()
            for j in range(24):
                i = st["wai"]; st["wai"] = (i + 1) % 3
                wa_i = merged[:, 2 * i:2 * i + 2, :].rearrange("p a (k c) -> p (a k) c", c=128)
                keys = ["mg%d" % (2 * i), "mg%d" % (2 * i + 1)]
                dma("sp", wa_i, adaw_d[l, :, j * 128:(j + 1) * 128].rearrange("(k p) c -> p k c", p=128),
                    [], keys, "wa%d" % i)
                for k in range(8):
                    mm(b[:, j:j + 1], wa_i[:, k, :], cT[:, k:k + 1], k == 0, k == 7, keys + ["cT"], [bk])
            tt(mod[:, :], b[:, 0:24], pk[:, l, PK["ada_b"][0]:PK["ada_b"][0] + 24], ALU.add, [bk, "pk"], ["mod"])
            stt(drv[:, 0:8], mod[:, 8:16], 1.0, pk[:, l, PK["norm_g"][0]:PK["norm_g"][0] + 8], ALU.add, ALU.mult,
                ["mod", "pk"], ["drv"])
            tsc(drv[:, 0:8], drv[:, 0:8], 32.0, None, ALU.mult, None, ["drv"], ["drv"])
            o_mu = PK["mu"][0]
            tsc(drv[:, 8:22], pk[:, l, o_mu:o_mu + 14], -1.0, 1.0, ALU.mult, ALU.add, ["pk"], ["drv"])
            tsc(drv[:, 22:26], PKc(l, "w0", 0, 4), -1.0, None, ALU.mult, None, ["pk"], ["drv"])
            tsc(drv[:, 26:30], PKc(l, "k_a", 0, 4), -1.0, 1.0, ALU.mult, ALU.add, ["pk"], ["drv"])
            tsc(drv[:, 30:32], PKc(l, "gk_b", 0, 2), -1.0, None, ALU.mult, None, ["pk"], ["drv"])
            tsc(drv[:, 32:34], PKc(l, "f_b", 0, 2), -1.0, None, ALU.mult, None, ["pk"], ["drv"])
            tsc(drv[:, 34:38], lbt[:, l * 4:(l + 1) * 4], -1.0, 1.0, ALU.mult, ALU.add, ["lbt"], ["drv"])
            tsc(drv[:, 38:42], PKc(l, "gla_g", 0, 4), float(np.sqrt(128.0)), None, ALU.mult, None, ["pk"], ["drv"])
            tsc(drv[:, 42:46], PKc(l, "hg_g", 0, 4), float(np.sqrt(128.0)), None, ALU.mult, None, ["pk"], ["drv"])
            dma("pool", wup[:, :], wup_d[l], [], ["wup"], "wup")
            dma("pool", aup[:, :], aup_d[l], [], ["aup"], "aup")
            dma("pool", gup[:, :], gup_d[l], [], ["gup"], "gup")
            mset(rw_sf[:, :, :], 0.0, ["rw_sf"])
            mset(rw_sb[:, :, :], 0.0, ["rw_sb"])
            for i in range(4):
                mset(la_sf[i][:, :, :], 0.0, ["la_sf%d" % i])
                mset(la_sb[i][:, :, :], 0.0, ["la_sb%d" % i])
            mset(prc[:, :], 0.0, ["prc"])
            mset(cqc[:, :, :], 0.0, ["cqc"])

            src_d = xT_d if l == 0 else out_d
            for sI in range(NST):
                sl = slice(sI * TS, (sI + 1) * TS)
                for k in range(8):
                    dma("sp", x[:, k, :], src_d[k * 128:(k + 1) * 128, sl], ["xd%d" % sI], [XK[k]], "xl%d" % k)

                def emit(k, t1, t1k):
                    act(uT[:, k, :], t1[:, 0:TS], AF.Identity, [t1k, "drv", "mod"], ["uT"],
                        bias=mod[:, k:k + 1], scale=drv[:, k:k + 1])
                rms_to(sl, emit)
                for m in range(4):
                    if m in branches:
                        (rwkv, gla, mlstm, hgrn)[m](l, sI)
                    else:
                        mset(ys[:, :, :], 0.0, ["ys"])
                    if debug and l == 0:
                        for k in range(4):
                            dma("pool", dbg_d[m, k * 128:(k + 1) * 128, sl], ys[:, k, :], ["ys"], [], "dbg%d" % k)
                    for fc in range(8):
                        pg, pgk = proj(l, C_MG + m * 1024 + fc * 128, 128)
                        sg, sgk = fsa()
                        act(sg[:, 0:TS], pg[:, :], AF.Sigmoid, [pgk], [sgk])
                        w, wk = wchunk(wbr_d[l, m, :, fc * 128:(fc + 1) * 128], 128, kc=4)
                        pb, pbk = bank()
                        for k in range(4):
                            mm(pb[:, :], w[:, k, :], ys[:, k, :], k == 0, k == 3, [wk, "ys"], [pbk])
                        if m == 0:
                            tt(merged[:, fc, :], pb[:, :], sg[:, 0:TS], ALU.mult, [pbk, sgk], ["mg%d" % fc])
                        else:
                            t2, t2k = fsa()
                            tt(t2[:, 0:TS], pb[:, :], sg[:, 0:TS], ALU.mult, [pbk, sgk], [t2k])
                            tt(merged[:, fc, :], merged[:, fc, :], t2[:, 0:TS], ALU.add, [t2k, "mg%d" % fc], ["mg%d" % fc])
                for fc in range(8):
                    act(mergedb[:, fc, :], merged[:, fc, :], AF.Copy, ["mg%d" % fc], ["uT"])
                for fc in range(8):
                    w, wk = wchunk(wout_d[l, :, fc * 128:(fc + 1) * 128], 128)
                    pb, pbk = bank()
                    for k in range(8):
                        mm(pb[:, :], w[:, k, :], mergedb[:, k, :], k == 0, k == 7, [wk, "uT"], [pbk])
                    stt(x[:, fc, :], pb[:, :], mod[:, 16 + fc:17 + fc], x[:, fc, :], ALU.mult, ALU.add,
                        [pbk, "mod", XK[fc]], [XK[fc]])
                    dma("sp", out_d[fc * 128:(fc + 1) * 128, sl], x[:, fc, :], [XK[fc]], ["xd%d" % sI], "xs%d" % fc)

        def shift_hi(src_ap, skey, dst_ap, dkey, n, fp32=False):
            b, bk = bank()
            idn = (CS if fp32 else CB)("ident")
            mm(b[0:64, 0:n], idn[:, 64:128], src_ap, True, True, [skey, "cst" if fp32 else "cstb"], [bk])
            cp(dst_ap, b[0:64, 0:n], [bk], [dkey])

        def build_gamh(npair, nch):
            for ct in range(npair):
                cp(gamh[:, 2 * ct, 0:nch], gam[0:64, ct, 0:nch], ["gam"], ["gamh"])
                shift_hi(gam[:, ct, 0:nch], "gam", gamh[:, 2 * ct + 1, 0:nch], "gamh", nch, fp32=True)

        def la(Qh, Kh, KTp, C, dk, si, gm, gmk, fin, ni=None):
            mask = {128: CB("m128"), 64: CB("m64b"), 32: CB("m32b")}[C]
            sfk, sbk = "la_sf%d" % si, "la_sb%d" % si
            for tI in range(TS // 128):
                tsl = slice(tI * 128, (tI + 1) * 128)
                bt, btk = bank()
                btb = bt[:, :].bitcast(BF16)
                nkt = len(KTp)
                for ct in range(nkt):
                    tr(btb[:, ct * 128:(ct + 1) * 128], KTp[ct][0][:, tsl], CB("ident"), [KTp[ct][1], "cstb"], [btk])
                cp(ktok[:, tI, 0:nkt * 128], btb[:, 0:nkt * 128], [btk], ["ktok"])
                if stage <= 4:
                    continue
                if C == 32:
                    tsc(ktz[64:128, 0:nkt * 128], ktok[64:128, tI, 0:nkt * 128], CS("m96")[64:128, 0:1], None, ALU.mult, None,
                        ["ktok", "cst"], ["ktz"])
                bsc, bsck = bank()
                for h in range(4):
                    mm(bsc[:, h * 128:(h + 1) * 128], Kh[h][0][:, tsl], Qh[h][0][:, tsl], True, True,
                       [Kh[h][1], Qh[h][1]], [bsck])
                pt, ptk = bsa()
                tt(pt[:, :], bsc[:, :], mask, ALU.mult, [bsck, "cstb"], [ptk])
                if stage <= 5:
                    continue
                po, pok = ps[6], "ps6"
                pd, pdk = (ps[7], "ps7") if ni is not None else (None, None)
                for cc in range(128 // C):
                    cidx = tI * (128 // C) + cc
                    csl = slice(cc * C, (cc + 1) * C)
                    gsl = slice(tI * 128 + cc * C, tI * 128 + (cc + 1) * C)
                    for h in range(4):
                        osl = slice(h * 128 + cc * C, h * 128 + (cc + 1) * C)
                        mm(po[:, osl], vtok[:, tI, h * 128:(h + 1) * 128], pt[:, osl], True, False,
                           ["vtok%d" % tI, ptk], [pok])
                        mm(po[:, osl], la_sb[si][0:dk, h, :], Qh[h][0][:, gsl], False, True, [sbk, Qh[h][1]], [pok])
                        if ni is not None:
                            mm(pd[:, osl], CB("ones"), pt[:, osl], True, False, ["cstb", ptk], [pdk])
                            mm(pd[:, osl], la_sb[ni][0:dk, h, :], Qh[h][0][:, gsl], False, True,
                               ["la_sb%d" % ni, Qh[h][1]], [pdk])
                    if stage <= 6:
                        continue
                    for sidx, isn in ((si, False),) + (((ni, True),) if ni is not None else ()):
                        bu, buk = bank()
                        for h in range(4):
                            if C == 32 and cc == 3:
                                zsl = slice(64, 128)
                                mm(bu[0:dk, h * 128:(h + 1) * 128], ktz[zsl, h * dk:(h + 1) * dk],
                                   vtok[zsl, tI, h * 128:(h + 1) * 128], True, True, ["ktz", "vtok%d" % tI], [buk])
                            else:
                                rhs = CB("ones")[csl, :] if isn else vtok[csl, tI, h * 128:(h + 1) * 128]
                                mm(bu[0:dk, h * 128:(h + 1) * 128], ktok[csl, tI, h * dk:(h + 1) * dk], rhs, True, True,
                                   ["ktok", "vtok%d" % tI, "cstb"], [buk])
                        tmp_, tmpk = fsa()
                        tv = tmp_[0:dk, 0:512].rearrange("p (h v) -> p h v", h=4)
                        tt(tv, bu[0:dk, :].rearrange("p (h v) -> p h v", h=4), la_sf[sidx][0:dk, :, :], ALU.add,
                           [buk, "la_sf%d" % sidx], [tmpk])
                        gb = gm[0:dk, 0:4, cidx:cidx + 1].to_broadcast([dk, 4, 128])
                        tt(la_sf[sidx][0:dk, :, :], tv, gb, ALU.mult, [tmpk, gmk], ["la_sf%d" % sidx])
                        tt(la_sb[sidx][0:dk, :, :], tv, gb, ALU.mult, [tmpk, gmk], ["la_sb%d" % sidx])
                if stage > 7:
                    fin(tI, po, pok, pd, pdk)
            if stage <= 7:
                mset(ys[:, :, :], 0.0, ["ys"])

        def gam_start(ct, C):
            n = TS // C
            d_, dk_ = fsa()
            tt(d_[:, 0:n], gnb[ct][:, C:TS + 1:C], gnb[ct][:, 0:TS:C], ALU.subtract, ["gn%d" % ct], [dk_])
            act(gam[:, ct, 0:n], d_[:, 0:n], AF.Exp, [dk_], ["gam"], scale=-1.0)

        def qk_decay(ct, q_ap, qk_, k_ap, kk_, C, mid, Qd, Kd, kextra=None, clamp=False):
            gk = "gn%d" % ct
            n = TS // C
            off = C // 2 if mid else 0
            tsc(nref[:, ct, 0:n], gnb[ct][:, off:TS:C], -1.0, None, ALU.mult, None, [gk], ["nref"])
            ei, eik = fsa(); ev, evk = fsa()
            for c in range(n):
                c0 = c * C
                rc = c0 + off
                act(ei[:, c0:c0 + C], gnb[ct][:, c0 + 1:c0 + C + 1], AF.Exp, [gk], [eik], bias=gnb[ct][:, rc:rc + 1], scale=-1.0)
                if kextra is None:
                    act(ev[:, c0:c0 + C], gnb[ct][:, c0 + 1:c0 + C + 1], AF.Exp, [gk, "nref"], [evk],
                        bias=nref[:, ct, c:c + 1], scale=1.0)
                else:
                    act(ev[:, c0:c0 + C], kextra[0][:, c0:c0 + C], AF.Exp, [kextra[1], "nref"], [evk],
                        bias=nref[:, ct, c:c + 1], scale=1.0)
            if clamp:
                tsc(ei[:, 0:TS], ei[:, 0:TS], 2.35e17, None, ALU.min, None, [eik], [eik])
                tsc(ev[:, 0:TS], ev[:, 0:TS], 2.35e17, None, ALU.min, None, [evk], [evk])
            tt(Qd[0][:, :], q_ap, ei[:, 0:TS], ALU.mult, [qk_, eik], [Qd[1]])
            tt(Kd[0][:, :], k_ap, ev[:, 0:TS], ALU.mult, [kk_, evk], [Kd[1]])

        BLk = lambda i: (BL[i], "bl%d" % i)
        LLk = lambda i: (LL[i], "ll%d" % i)

        def zgate(l, c0, h, func_scale_ap, skeys):
            pz, pzk = proj(l, c0 + h * 128, 128)
            z, zk = LLk(h)
            act(z[:, :], pz[:, :], AF.Silu, [pzk], [zk])
            if func_scale_ap is not None:
                tsc(z[:, :], z[:, :], func_scale_ap, None, ALU.mult, None, [zk] + skeys, [zk])
            return z, zk

        def rms_fin(GZ):
            def fin(tI, po, pok, pd, pdk):
                sq, sqk = fsa()
                act(sq[:, 0:TS], po[:, :], AF.Square, [pok], [sqk])
                b, bk = bank()
                mm(b[:, :], CS("ones"), sq[:, 0:TS], True, True, [sqk, "cst"], [bk])
                rs, rsk = fsa()
                rsq(rs[:, 0:TS], b[:, :], 1.0, 128.0 * 1e-6, [bk], [rsk])
                t1, t1k = fsa()
                tt(t1[:, 0:TS], po[:, :], rs[:, 0:TS], ALU.mult, [pok, rsk], [t1k])
                for h in range(4):
                    tt(ys[:, h, tI * 128:(tI + 1) * 128], t1[:, h * 128:(h + 1) * 128],
                       GZ[h][0][:, tI * 128:(tI + 1) * 128], ALU.mult, [t1k, GZ[h][1]], ["ys"])
            return fin

        def heads64(QT, KT):
            Qh, Kh = [], []
            for ct in range(2):
                shift_hi(QT[ct][0][:, :], QT[ct][1], BL[16 + ct][0:64, :], "bl%d" % (16 + ct), TS)
                shift_hi(KT[ct][0][:, :], KT[ct][1], BL[18 + ct][0:64, :], "bl%d" % (18 + ct), TS)
                Qh += [(QT[ct][0][0:64, :], QT[ct][1]), (BL[16 + ct][0:64, :], "bl%d" % (16 + ct))]
                Kh += [(KT[ct][0][0:64, :], KT[ct][1]), (BL[18 + ct][0:64, :], "bl%d" % (18 + ct))]
            return Qh, Kh

        def gla(l, sI):
            proj_tok(l, C_GV)
            if stage <= 1:
                mset(ys[:, :, :], 0.0, ["ys"]); return
            pg, pgk = proj(l, C_GC, 16)
            gc, gck = bsa()
            act(gc[0:16, 0:TS], pg[0:16, :], AF.Copy, [pgk], [gck])
            QT, KT = [], []
            for ct in range(2):
                b, bk = bank()
                mm(b[:, :], gup[:, ct * 128:(ct + 1) * 128], gc[0:16, 0:TS], True, True, ["gup", gck], [bk])
                e1, e1k = fsa()
                act(e1[:, 0:TS], b[:, :], AF.Exp, [bk, "drv"], [e1k], bias=drv[:, 30 + ct:31 + ct], scale=-1.0)
                act(e1[:, 0:TS], e1[:, 0:TS], AF.Ln, [e1k], [e1k], bias=1.0)
                tsc(e1[:, 0:TS], e1[:, 0:TS], 1.0 / 16.0, None, ALU.mult, None, [e1k], [e1k])
                cumsum(ct, e1[:, 0:TS], e1k)
                gam_start(ct, 128)
                if stage <= 2:
                    continue
                pq, pqk = proj(l, C_GQ + ct * 128, 128)
                q, qk_ = fsa()
                act(q[:, 0:TS], pq[:, :], AF.Copy, [pqk], [qk_], scale=0.125)
                pk_, pkk = proj(l, C_GK + ct * 128, 128)
                k, kk_ = fsa()
                cp(k[:, 0:TS], pk_[:, :], [pkk], [kk_])
                qk_decay(ct, q[:, 0:TS], qk_, k[:, 0:TS], kk_, 128, False, BLk(ct), BLk(4 + ct))
                QT.append(BLk(ct)); KT.append(BLk(4 + ct))
            if stage <= 3:
                mset(ys[:, :, :], 0.0, ["ys"]); return
            GZ = [zgate(l, C_GZ, h, drv[:, 38 + h:39 + h], ["drv"]) for h in range(4)]
            Qh, Kh = heads64(QT, KT)
            build_gamh(2, 4)
            la(Qh, Kh, KT, 128, 64, 0, gamh, "gamh", rms_fin(GZ))

        def hgrn(l, sI):
            proj_tok(l, C_HI)
            QT, KT = [], []
            for ct in range(4):
                pf, pfk = proj(l, C_HF + ct * 128, 128)
                g, gk_ = fsa()
                act(g[:, 0:TS], pf[:, :], AF.Sigmoid, [pfk], [gk_])
                tsc(g[:, 0:TS], g[:, 0:TS], drv[:, 34 + ct:35 + ct], lbt[:, l * 4 + ct:l * 4 + ct + 1], ALU.mult, ALU.add,
                    [gk_, "drv", "lbt"], [gk_])
                lg, lgk = fsa()
                recip(lg[:, 0:TS], g[:, 0:TS], [gk_], [lgk])
                act(lg[:, 0:TS], lg[:, 0:TS], AF.Ln, [lgk], [lgk])
                cumsum(ct, lg[:, 0:TS], lgk)
                d_, dk_ = fsa()
                tt(d_[:, 0:15], gnb[ct][:, 48:TS:32], gnb[ct][:, 16:TS - 32:32], ALU.subtract, ["gn%d" % ct], [dk_])
                tt(d_[:, 15:16], gnb[ct][:, TS:TS + 1], gnb[ct][:, TS - 16:TS - 15], ALU.subtract, ["gn%d" % ct], [dk_])
                act(gam[:, ct, 0:16], d_[:, 0:16], AF.Exp, [dk_], ["gam"], scale=-1.0)
                fc_, fck = fsa()
                act(fc_[:, 0:1], gnb[ct][:, 16:17], AF.Exp, ["gn%d" % ct], [fck], scale=-1.0)
                tsc(la_sf[3][:, ct, :], la_sf[3][:, ct, :], fc_[:, 0:1], None, ALU.mult, None,
                    [fck, "la_sf3"], ["la_sf3"])
                cp(la_sb[3][:, ct, :], la_sf[3][:, ct, :], ["la_sf3"], ["la_sb3"])
                tsc(g[:, 0:TS], g[:, 0:TS], -1.0, 1.0, ALU.mult, ALU.add, [gk_], [gk_])
                pq, pqk = proj(l, C_HQ + ct * 128, 128)
                q, qk_ = fsa()
                act(q[:, 0:TS], pq[:, :], AF.Silu, [pqk], [qk_])
                qk_decay(ct, q[:, 0:TS], qk_, g[:, 0:TS], gk_, 32, True, BLk(ct), BLk(4 + ct), clamp=True)
                QT.append(BLk(ct)); KT.append(BLk(4 + ct))
            GZ = [zgate(l, C_HZ, h, drv[:, 42 + h:43 + h], ["drv"]) for h in range(4)]
            la(QT, KT, KT, 32, 128, 3, gam, "gam", rms_fin(GZ))

        def mlstm(l, sI):
            proj_tok(l, C_MV)
            w8, w8k = wchunk(win_d[l, :, C_MI:C_MI + 8], 8)
            reps = {}
            for ct in range(2):
                for gI in range(2):
                    for half in range(2):
                        r_, rk_ = BLk(8 + ct * 4 + gI * 2 + half)
                        rv_ = r_[:, :].rearrange("p (k j m) -> p k j m", k=4, j=2, m=64)
                        src = w8[:, half * 4:(half + 1) * 4, gI * 4 + ct * 2:gI * 4 + ct * 2 + 2]
                        cp(rv_, src.unsqueeze(3).to_broadcast([128, 4, 2, 64]), [w8k], [rk_])
                        reps[(ct, gI, half)] = (r_, rk_)
            for ct in range(4):
                pc, pck = proj(l, C_MQK + ct * 128, 128)
                cb, cbk = fsa()
                cp(cb[:, 0:3], cqc[:, ct, 0:3], ["cqc"], [cbk])
                act(cb[:, 3:TS + 3], pc[:, :], AF.Copy, [pck], [cbk])
                a, ak = fsa()
                oc = PK["conv"][0]
                tsc(a[:, 0:TS], cb[:, 0:TS], pk[:, l, oc + ct:oc + ct + 1], None, ALU.mult, None, [cbk, "pk"], [ak])
                for j in range(1, 4):
                    stt(a[:, 0:TS], cb[:, j:j + TS], pk[:, l, oc + j * 4 + ct:oc + j * 4 + ct + 1], a[:, 0:TS], ALU.mult,
                        ALU.add, [cbk, "pk", ak], [ak])
                cp(cqc[:, ct, 0:3], cb[:, TS:TS + 3], [cbk], ["cqc"])
                act(LL[4 + ct][:, :], a[:, 0:TS], AF.Silu, [ak], ["ll%d" % (4 + ct)])
            QT, KT = [], []
            for ct in range(2):
                pis = []
                for gI in range(2):
                    b, bk = bank()
                    for k in range(8):
                        r_, rk_ = reps[(ct, gI, k // 4)]
                        mm(b[:, :], r_[:, (k % 4) * 128:(k % 4 + 1) * 128], uT[:, k, :], k == 0, k == 7, [rk_, "uT"], [bk])
                    pis.append((b, bk))
                e1, e1k = fsa()
                act(e1[:, 0:TS], pis[1][0][:, :], AF.Exp, [pis[1][1], "drv"], [e1k], bias=drv[:, 32 + ct:33 + ct], scale=-1.0)
                act(e1[:, 0:TS], e1[:, 0:TS], AF.Ln, [e1k], [e1k], bias=1.0)
                cumsum(ct, e1[:, 0:TS], e1k)
                gam_start(ct, 128)
                ig, igk = fsa()
                act(ig[:, 0:TS], pis[0][0][:, :], AF.Identity, [pis[0][1], "pk"], [igk], bias=PKc(l, "i_b", ct))
                tt(ig[:, 0:TS], ig[:, 0:TS], gnb[ct][:, 1:TS + 1], ALU.add, [igk, "gn%d" % ct], [igk])
                k, kk_ = fsa()
                tsc(k[:, 0:TS], LL[6 + ct][:, :], 0.125, None, ALU.mult, None, ["ll%d" % (6 + ct)], [kk_])
                qk_decay(ct, LL[4 + ct][:, :], "ll%d" % (4 + ct), k[:, 0:TS], kk_, 128, False, BLk(ct), BLk(4 + ct),
                         kextra=(ig, igk))
                QT.append(BLk(ct)); KT.append(BLk(4 + ct))
            GZ = [zgate(l, C_MZ, h, PKc(l, "ml_g", h), ["pk"]) for h in range(4)]

            def fin(tI, po, pok, pd, pdk):
                den, dnk = fsa()
                act(den[:, 0:TS], pd[:, :], AF.Abs, [pdk], [dnk])
                tsc(den[:, 0:TS], den[:, 0:TS], 1.0, None, ALU.max, None, [dnk], [dnk])
                recip(den[:, 0:TS], den[:, 0:TS], [dnk], [dnk])
                o, ok = fsa()
                tt(o[:, 0:TS], po[:, :], den[:, 0:TS], ALU.mult, [pok, dnk], [ok])
                b, bk = bank()
                mm(b[:, :], CS("ones"), o[:, 0:TS], True, True, [ok, "cst"], [bk])
                d, dk_ = fsa()
                stt(d[:, 0:TS], b[:, :], -1.0 / 128.0, o[:, 0:TS], ALU.mult, ALU.add, [bk, ok], [dk_])
                sq, sqk = fsa()
                act(sq[:, 0:TS], d[:, 0:TS], AF.Square, [dk_], [sqk])
                b2, b2k = bank()
                mm(b2[:, :], CS("ones"), sq[:, 0:TS], True, True, [sqk, "cst"], [b2k])
                rs, rsk = fsa()
                rsq(rs[:, 0:TS], b2[:, :], 1.0 / 128.0, 1e-6, [b2k], [rsk])
                tt(d[:, 0:TS], d[:, 0:TS], rs[:, 0:TS], ALU.mult, [dk_, rsk], [dk_])
                for h in range(4):
                    tt(ys[:, h, tI * 128:(tI + 1) * 128], d[:, h * 128:(h + 1) * 128],
                       GZ[h][0][:, tI * 128:(tI + 1) * 128], ALU.mult, [dk_, GZ[h][1]], ["ys"])

            Qh, Kh = heads64(QT, KT)
            build_gamh(2, 4)
            la(Qh, Kh, KT, 128, 64, 1, gamh, "gamh", fin, ni=2)

        def rwkv(l, sI):
            C = 64
            NCH = TS // C

            def mixed(idx, c0, n, mucol, omcol, dst=None):
                pp, ppk = proj(l, c0, n)
                pb_, pbk_ = fsa()
                cp(pb_[0:n, 0:1], prc[0:n, idx:idx + 1], ["prc"], [pbk_])
                act(pb_[0:n, 1:TS + 1], pp[0:n, :], AF.Copy, [ppk], [pbk_])
                t_, tk_ = fsa()
                tsc(t_[0:n, 0:TS], pb_[0:n, 0:TS], mucol, None, ALU.mult, None, [pbk_, "pk"], [tk_])
                if dst is None:
                    o_, ok_ = fsa()
                    o_ = o_[:, 0:TS]
                else:
                    o_, ok_ = dst
                stt(o_[0:n, :], pb_[0:n, 1:TS + 1], omcol, t_[0:n, 0:TS], ALU.mult, ALU.add, [pbk_, "drv", tk_], [ok_])
                cp(prc[0:n, idx:idx + 1], pb_[0:n, TS:TS + 1], [pbk_], ["prc"])
                return o_, ok_

            WC = mixed(12, C_RW + 1536, 64, pk[0:64, l, PK["mu_wc"][0]:PK["mu_wc"][0] + 1], drv[0:64, 20:21],
                       dst=(wcac[:, 0, :], "wc"))
            AC = mixed(13, C_RW + 1600, 64, pk[0:64, l, PK["mu_ac"][0]:PK["mu_ac"][0] + 1], drv[0:64, 21:22],
                       dst=(wcac[:, 1, :], "ac"))
            act(wcacb[:, 0, :], WC[0][0:64, :], AF.Tanh, ["wc"], ["wcb"])
            cp(wcacb[:, 1, :], AC[0][0:64, :], ["ac"], ["acb"])
            ops = []
            BON = []
            for ct in range(4):
                csl = slice(ct * 128, (ct + 1) * 128)
                Kx, Kxk = mixed(4 + ct, C_RW + 512 + ct * 128, 128, PKc(l, "mu", 4 + ct), drv[:, 12 + ct:13 + ct])
                kk, kkk = fsa()
                tsc(kk[:, 0:TS], Kx[:, :], PKc(l, "k_k", ct), None, ALU.mult, None, [Kxk, "pk"], [kkk])
                sq, sqk = fsa()
                act(sq[:, 0:TS], kk[:, 0:TS], AF.Square, [kkk], [sqk])
                b3, b3k = bank()
                mm(b3[:, :], CS("bd"), sq[:, 0:TS], True, True, ["cst", sqk], [b3k])
                rsq(sq[:, 0:TS], b3[:, :], 1.0, 1e-24, [b3k], [sqk])
                tt(kk[:, 0:TS], kk[:, 0:TS], sq[:, 0:TS], ALU.mult, [kkk, sqk], [kkk])
                b, bk = bank()
                mm(b[:, :], wup[:, csl], wcacb[:, 0, :], True, True, ["wup", "wcb"], [bk])
                e1, e1k = fsa()
                act(e1[:, 0:TS], b[:, :], AF.Exp, [bk, "drv"], [e1k], bias=drv[:, 22 + ct:23 + ct], scale=-1.0)
                act(e1[:, 0:TS], e1[:, 0:TS], AF.Ln, [e1k], [e1k], bias=1.0)
                act(e1[:, 0:TS], e1[:, 0:TS], AF.Exp, [e1k], [e1k], bias=-0.5, scale=-1.0)
                cumsum(ct, e1[:, 0:TS], e1k)
                gk = "gn%d" % ct
                tsc(nref[:, ct, 0:NCH], gnb[ct][:, 0:TS:C], -1.0, None, ALU.mult, None, [gk], ["nref"])
                b2, b2k = bank()
                mm(b2[:, :], aup[:, csl], wcacb[:, 1, :], True, True, ["aup", "acb"], [b2k])
                sg, sgk = fsa()
                act(sg[:, 0:TS], b2[:, :], AF.Sigmoid, [b2k, "pk"], [sgk], bias=PKc(l, "a0", ct))
                t1, t1k = fsa()
                tsc(t1[:, 0:TS], sg[:, 0:TS], PKc(l, "k_a", ct), drv[:, 26 + ct:27 + ct], ALU.mult, ALU.add,
                    [sgk, "pk", "drv"], [t1k])
                tt(Kx[:, :], Kx[:, :], t1[:, 0:TS], ALU.mult, [Kxk, t1k], [Kxk])
                tt(sg[:, 0:TS], kk[:, 0:TS], sg[:, 0:TS], ALU.mult, [kkk, sgk], [sgk])
                ei, eik = fsa(); ee, eek = fsa(); ev, evk = fsa()
                for c in range(NCH):
                    c0 = c * C
                    act(ei[:, c0:c0 + C], gnb[ct][:, c0 + 1:c0 + C + 1], AF.Exp, [gk], [eik], bias=gnb[ct][:, c0:c0 + 1], scale=-1.0)
                    act(ee[:, c0:c0 + C], gnb[ct][:, c0:c0 + C], AF.Exp, [gk], [eek], bias=gnb[ct][:, c0:c0 + 1], scale=-1.0)
                    act(ev[:, c0:c0 + C], gnb[ct][:, c0 + 1:c0 + C + 1], AF.Exp, [gk, "nref"], [evk],
                        bias=nref[:, ct, c:c + 1], scale=1.0)
                rt, at, btl, ktl, vb = [BLk(ct * 5 + j) for j in range(5)]
                stt(at[0][:, :], kk[:, 0:TS], -1.0, ee[:, 0:TS], ALU.mult, ALU.mult, [kkk, eek], [at[1]])
                tt(btl[0][:, :], sg[:, 0:TS], ev[:, 0:TS], ALU.mult, [sgk, evk], [btl[1]])
                tt(ktl[0][:, :], Kx[:, :], ev[:, 0:TS], ALU.mult, [Kxk, evk], [ktl[1]])
                cp(gam[:, ct, 0:NCH], ei[:, C - 1:TS:C], [eik], ["gam"])
                Rx, Rxk = mixed(ct, C_RW + ct * 128, 128, PKc(l, "mu", ct), drv[:, 8 + ct:9 + ct])
                tt(rt[0][:, :], Rx[:, :], ei[:, 0:TS], ALU.mult, [Rxk, eik], [rt[1]])
                rk, rkk = fsa()
                stt(rk[:, 0:TS], Rx[:, :], PKc(l, "r_k", ct), Kx[:, :], ALU.mult, ALU.mult, [Rxk, "pk", Kxk], [rkk])
                b4, b4k = bank()
                mm(b4[:, :], CS("bd"), rk[:, 0:TS], True, True, ["cst", rkk], [b4k])
                Vx, Vxk = mixed(8 + ct, C_RW + 1024 + ct * 128, 128, PKc(l, "mu", 8 + ct), drv[:, 16 + ct:17 + ct])
                cp(vb[0][:, :], Vx[:, :], [Vxk], [vb[1]])
                tt(LL[ct][:, :], b4[:, :], Vx[:, :], ALU.mult, [b4k, Vxk], ["ll%d" % ct])
                BON.append(LLk(ct))
                ops.append(dict(r=rt, a=at, b=btl, k=ktl, v=vb))

            hd = lambda h: (h // 2, (h % 2) * 64)
            SH = {}
            mhb = merged[:, 4:8, :].bitcast(BF16)
            for ct in range(4):
                for ni_, nm in enumerate(("r", "a", "b", "k")):
                    j = ct * 4 + ni_
                    if j < 8:
                        dst, dkey = mhb[:, j // 2, (j % 2) * 512:(j % 2 + 1) * 512], "mg%d" % (4 + j // 2)
                    elif j < 12:
                        dst, dkey = vtok[:, j - 8, :], "vtok%d" % (j - 8)
                    else:
                        dst, dkey = ktok[:, j - 12, :], "ktok"
                    shift_hi(ops[ct][nm][0][:, :], ops[ct][nm][1], dst[0:64, :], dkey, TS)
                    SH[(ct, nm)] = (dst, dkey)
            build_gamh(4, NCH)

            def opn(nm, h):
                ct, par = h // 2, h % 2
                if par == 0:
                    return ops[ct][nm][0][0:64, :], ops[ct][nm][1]
                return SH[(ct, nm)][0][0:64, :], SH[(ct, nm)][1]

            for c in range(NCH):
                c0 = c * C
                cs = slice(c0, c0 + C)
                for nm, key in (("v", "vt"), ("b", "bt"), ("k", "kt")):
                    bt_, btk2 = bank()
                    btb = bt_[:, :].bitcast(BF16)
                    for ct in range(4):
                        tr(btb[0:64, ct * 128:(ct + 1) * 128], ops[ct][nm][0][:, cs], CB("ident"),
                           [ops[ct][nm][1], "cstb"], [btk2])
                    cp(rv[key][:, :], btb[0:64, 0:512], [btk2], ["rv_" + key])

                def scores(lhs, rhs, dst, maskn):
                    b_, bk_ = bank()
                    for h in range(8):
                        L_, R_ = opn(lhs, h), opn(rhs, h)
                        mm(b_[0:64, h * 64:(h + 1) * 64], L_[0][:, cs], R_[0][:, cs], True, True, [L_[1], R_[1]], [bk_])
                    tt(rv[dst][:, :], b_[0:64, :], CB(maskn)[0:64, :], ALU.mult, [bk_, "cstb"], ["rv_" + dst])
                scores("a", "b", "N", "r_ts")
                scores("b", "a", "NT", "r_st")
                scores("k", "a", "Aak", "r_st")
                scores("b", "r", "Arb", "r_sti")
                scores("k", "r", "Ark", "r_sti")
                tt(rv["Y"][:, :], rv["NT"][:, :], CB("i8")[0:64, :], ALU.add, ["rv_NT", "cstb"], ["rv_Y"])
                M, MT, Y = "N", "NT", "Y"
                M2, MT2, Y2 = "N2", "NT2", "Y2"
                for lev in range(5):
                    ba, bak = bank(); bb_, bbk_ = bank()
                    for h in range(8):
                        hs = slice(h * 64, (h + 1) * 64)
                        mm(ba[0:64, hs], rv[MT][:, hs], rv[M][:, hs], True, True, ["rv_" + MT, "rv_" + M], [bak])
                        mm(bb_[0:64, hs], rv[M][:, hs], rv[MT][:, hs], True, True, ["rv_" + MT, "rv_" + M], [bbk_])
                    act(rv[M2][:, :], ba[0:64, :], AF.Copy, [bak], ["rv_" + M2])
                    cp(rv[MT2][:, :], bb_[0:64, :], [bbk_], ["rv_" + MT2])
                    bc, bck = bank()
                    for h in range(8):
                        hs = slice(h * 64, (h + 1) * 64)
                        mm(bc[0:64, hs], rv[M2][:, hs], rv[Y][:, hs], True, False, ["rv_" + M2, "rv_" + Y], [bck])
                        mm(bc[0:64, hs], CB("ident")[0:64, 0:64], rv[Y][:, hs], False, True, ["cstb", "rv_" + Y], [bck])
                    act(rv[Y2][:, :], bc[0:64, :], AF.Copy, [bck], ["rv_" + Y2])
                    M, M2 = M2, M
                    MT, MT2 = MT2, MT
                    Y, Y2 = Y2, Y
                bx, bxk = bank()
                for h in range(8):
                    ct, pb = hd(h)
                    hs = slice(h * 64, (h + 1) * 64)
                    A_ = opn("a", h)
                    mm(bx[0:64, hs], A_[0][:, cs], rw_sb[:, h, :], True, False, [A_[1], "rw_sb"], [bxk])
                    mm(bx[0:64, hs], rv["Aak"][:, hs], rv["vt"][:, hs], False, True, ["rv_Aak", "rv_vt"], [bxk])
                cp(rv["X"][:, :], bx[0:64, :], [bxk], ["rv_X"])
                bu, buk = bank()
                for h in range(8):
                    hs = slice(h * 64, (h + 1) * 64)
                    mm(bu[0:64, hs], rv[Y][:, hs], rv["X"][:, hs], True, True, ["rv_" + Y, "rv_X"], [buk])
                act(rv["U"][:, :], bu[0:64, :], AF.Copy, [buk], ["rv_U"])
                by, byk = bank()
                for h in range(8):
                    ct, pb = hd(h)
                    hs = slice(h * 64, (h + 1) * 64)
                    o_ = by[pb:pb + 64, ct * 64:(ct + 1) * 64]
                    R_ = opn("r", h)
                    mm(o_, rw_sb[:, h, :], R_[0][:, cs], True, False, ["rw_sb", R_[1]], [byk])
                    mm(o_, rv["U"][:, hs], rv["Arb"][:, hs], False, False, ["rv_U", "rv_Arb"], [byk])
                    mm(o_, rv["vt"][:, hs], rv["Ark"][:, hs], False, True, ["rv_vt", "rv_Ark"], [byk])
                cp(yT[:, 0:4, cs], by[:, 0:256].rearrange("p (c t) -> p c t", c=4), [byk], ["mg0", "mg1", "mg2", "mg3"])
                bs_, bsk = bank()
                for h in range(8):
                    hs = slice(h * 64, (h + 1) * 64)
                    mm(bs_[0:64, hs], rv["bt"][:, hs], rv["U"][:, hs], True, False, ["rv_bt", "rv_U"], [bsk])
                    mm(bs_[0:64, hs], rv["kt"][:, hs], rv["vt"][:, hs], False, True, ["rv_kt", "rv_vt"], [bsk])
                tmp_, tmpk = fsa()
                tv = tmp_[0:64, 0:512].rearrange("p (h v) -> p h v", h=8)
                tt(tv, bs_[0:64, :].rearrange("p (h v) -> p h v", h=8), rw_sf[:, :, :], ALU.add, [bsk, "rw_sf"], [tmpk])
                gb = gamh[0:64, 0:8, c:c + 1].to_broadcast([64, 8, 64])
                tt(rw_sf[:, :, :], tv, gb, ALU.mult, [tmpk, "gamh"], ["rw_sf"])
                tt(rw_sb[:, :, :], tv, gb, ALU.mult, [tmpk, "gamh"], ["rw_sb"])
            for ct in range(4):
                mk = "mg%d" % ct
                b, bk = bank()
                mm(b[:, :], CS("bd"), yT[:, ct, :], True, True, ["cst", mk], [bk])
                d, dk_ = fsa()
                stt(d[:, 0:TS], b[:, :], -1.0 / 64.0, yT[:, ct, :], ALU.mult, ALU.add, [bk, mk], [dk_])
                sq, sqk = fsa()
                act(sq[:, 0:TS], d[:, 0:TS], AF.Square, [dk_], [sqk])
                b2, b2k = bank()
                mm(b2[:, :], CS("bd"), sq[:, 0:TS], True, True, ["cst", sqk], [b2k])
                rsq(sq[:, 0:TS], b2[:, :], 1.0 / 64.0, 64e-5, [b2k], [sqk])
                tt(d[:, 0:TS], d[:, 0:TS], sq[:, 0:TS], ALU.mult, [dk_, sqk], [dk_])
                act(d[:, 0:TS], d[:, 0:TS], AF.Identity, [dk_, "pk"], [dk_], bias=PKc(l, "ln_b", ct), scale=PKc(l, "ln_g", ct))
                tt(d[:, 0:TS], d[:, 0:TS], BON[ct][0][:, :], ALU.add, [dk_, BON[ct][1]], [dk_])
                pz, pzk = proj(l, C_RWZ + ct * 128, 128)
                z, zk = fsa()
                act(z[:, 0:TS], pz[:, :], AF.Silu, [pzk], [zk])
                tt(ys[:, ct, :], d[:, 0:TS], z[:, 0:TS], ALU.mult, [dk_, zk], ["ys"])

        outs = program()
        S.emit(final_wait_ops=outs)
    return nc


_NC_CACHE = {}


def make_in_maps(inp, n_cores=8):
    f = lambda a: np.ascontiguousarray(np.asarray(a, dtype=np.float32))
    pkall = np.stack([_pack_params(inp, l) for l in range(DEPTH)], 0)
    fgT = np.ascontiguousarray(f(inp["final_g"]).reshape(8, 128).T)
    shared = {
        "pk": pkall, "fg": fgT, "cst": CONSTS, "cstf": CSTF,
        "ada_w": f(inp["ada_w"]), "w_in": f(inp["w_in"]), "rw_w_up": f(inp["rw_w_up"]), "rw_a_up": f(inp["rw_a_up"]),
        "gla_gk_up": f(inp["gla_gk_up"]), "w_branch": f(inp["w_branch"]), "w_out": f(inp["w_out"]),
    }
    maps = []
    for b in range(n_cores):
        m = dict(shared)
        m["xT"] = np.ascontiguousarray(f(inp["x"][b]).T)
        m["cT"] = np.ascontiguousarray(f(inp["c"][b]).reshape(8, 128).T)
        maps.append(m)
    return maps


def kernel(**inputs):
    inp = {k: np.asarray(v) for k, v in inputs.items()}
    if "full" not in _NC_CACHE:
        _NC_CACHE["full"] = build_nc()
    nc = _NC_CACHE["full"]
    maps = make_in_maps(inp)
    res = run_bass_kernel_spmd(nc, maps, core_ids=list(range(8)))
    out = np.stack([np.ascontiguousarray(r["outT"].T) for r in res.results], 0)
    return out.astype(np.float32)
```

```python
import contextlib
import numpy as np
import ml_dtypes
import concourse.bass as bass
import concourse.mybir as mybir
from concourse.bass_utils import run_bass_kernel_spmd

F32 = mybir.dt.float32
BF16 = mybir.dt.bfloat16
AF = mybir.ActivationFunctionType
ALU = mybir.AluOpType

D = 1024
SEQ = 2048
DEPTH = 4
NCOLS = 11416
TS = 512
NST = SEQ // TS
ENGS = ("pe", "dve", "act", "pool", "sp")

C_RW = 0; C_RWZ = 1664
C_GQ = 2176; C_GK = 2432; C_GV = 2688; C_GC = 3200; C_GZ = 3216
C_MQK = 3728; C_MV = 4240; C_MI = 4752; C_MF = 4756; C_MZ = 4760
C_HQ = 5272; C_HF = 5784; C_HI = 6296; C_HZ = 6808
C_MG = 7320

PK = {}
_o = 0
for _n, _w in (("norm_g", 8), ("ada_b", 24), ("mu", 12), ("mu_wc", 1), ("mu_ac", 1), ("w0", 4), ("a0", 4),
               ("k_k", 4), ("k_a", 4), ("r_k", 4), ("ln_g", 4), ("ln_b", 4), ("gk_b", 2), ("gla_g", 4),
               ("conv", 16), ("ml_g", 4), ("lb", 16), ("hg_g", 4), ("i_b", 2), ("f_b", 2)):
    PK[_n] = (_o, _w)
    _o += _w
NPK = _o


class Op:
    __slots__ = ("eng", "fn", "deps", "idx", "dma_stream", "signal", "cnt")

    def __init__(self, eng, fn):
        self.eng = eng
        self.fn = fn
        self.deps = []
        self.dma_stream = None
        self.signal = False
        self.cnt = 0


class Sched:
    def __init__(self, nc):
        self.nc = nc
        self.ops = []
        self.last_w = {}
        self.readers = {}
        self.streams = {}
        self.record = False

    def op(self, eng, fn, reads=(), writes=(), dma=None):
        if self.record:
            return None
        o = Op(eng, fn)
        o.idx = len(self.ops)
        deps = {}
        for k in reads:
            w = self.last_w.get(k)
            if w is not None:
                deps[w.idx] = w
        for k in writes:
            w = self.last_w.get(k)
            if w is not None:
                deps[w.idx] = w
            for r in self.readers.get(k, ()):
                deps[r.idx] = r
        if dma is not None:
            o.dma_stream = dma
            n = self.streams.get(dma, 0) + 1
            self.streams[dma] = n
            o.cnt = n
        o.deps = [deps[i] for i in sorted(deps)]
        for k in reads:
            self.readers.setdefault(k, []).append(o)
        for k in writes:
            self.last_w[k] = o
            self.readers[k] = []
        self.ops.append(o)
        return o

    def emit(self, final_wait_ops=()):
        nc = self.nc
        for o in self.ops:
            nd = []
            for d in o.deps:
                if d.dma_stream is None and o.dma_stream is None and d.eng == o.eng and o.eng == "pe":
                    continue
                nd.append(d)
            o.deps = nd
            for d in nd:
                d.signal = True
        for o in final_wait_ops:
            o.signal = True
        cnt = {e: 0 for e in ENGS}
        for o in self.ops:
            if o.dma_stream is None and o.signal:
                cnt[o.eng] += 1
                o.cnt = cnt[o.eng]
        EP, DEP = 10 ** 9, 10 ** 9
        with contextlib.ExitStack() as es:
            esem = {}
            for e in ENGS:
                for ep in range(max(1, (cnt[e] + EP - 1) // EP)):
                    esem[(e, ep)] = es.enter_context(nc.semaphore("c_%s%d" % (e, ep)))
            ssem = {}
            for i, (s_, n_) in enumerate(self.streams.items()):
                for ep in range(max(1, (n_ + DEP - 1) // DEP)):
                    ssem[(s_, ep)] = es.enter_context(nc.semaphore("d%d_%d" % (i, ep)))
            block = es.enter_context(nc.Block())
            per = {e: [o for o in self.ops if o.eng == e] for e in ENGS}

            def semval(d):
                if d.dma_stream is not None:
                    return ssem[(d.dma_stream, (d.cnt - 1) // DEP)], 16 * ((d.cnt - 1) % DEP + 1)
                return esem[(d.eng, (d.cnt - 1) // EP)], (d.cnt - 1) % EP + 1

            def runner(e):
                def body(engine):
                    waited = {}
                    for o in per[e]:
                        for d in o.deps:
                            sem, val = semval(d)
                            key = id(sem)
                            if waited.get(key, 0) >= val:
                                continue
                            waited[key] = val
                            engine.wait_ge(sem, val)
                        ins = o.fn(engine)
                        if o.dma_stream is not None:
                            ins.then_inc(semval(o)[0], 16)
                        elif o.signal:
                            ins.then_inc(semval(o)[0], 1)
                    if e == "sp":
                        for o in final_wait_ops:
                            sem, val = semval(o)
                            engine.wait_ge(sem, val)
                return body

            block.tensor(runner("pe"))
            block.vector(runner("dve"))
            block.scalar(runner("act"))
            block.gpsimd(runner("pool"))
            block.sync(runner("sp"))
        return cnt


def _host_consts():
    c = {}
    i128 = np.eye(128, dtype=np.float32)
    c["ident"] = i128
    s = np.arange(128)[:, None]
    t = np.arange(128)[None, :]
    m_incl = (s <= t).astype(np.float32)
    m_blk = ((s <= t) & ((s // 64) == (t // 64))).astype(np.float32)
    c["m128"] = np.tile(m_incl[:, None, :], (1, 4, 1)).reshape(128, 512)
    c["m64b"] = np.tile(m_blk[:, None, :], (1, 4, 1)).reshape(128, 512)
    m_b32 = ((s <= t) & ((s // 32) == (t // 32))).astype(np.float32)
    c["m32b"] = np.tile(m_b32[:, None, :], (1, 4, 1)).reshape(128, 512)
    m96 = np.zeros((128, 128), np.float32); m96[96:] = 1.0
    c["m96"] = m96
    s6 = np.arange(64)[:, None]
    t6 = np.arange(64)[None, :]
    z = np.zeros((128, 512), np.float32)
    a = z.copy(); a[:64] = np.tile((t6 < s6).astype(np.float32)[:, None, :], (1, 8, 1)).reshape(64, 512)
    c["r_ts"] = a
    a = z.copy(); a[:64] = np.tile((s6 < t6).astype(np.float32)[:, None, :], (1, 8, 1)).reshape(64, 512)
    c["r_st"] = a
    a = z.copy(); a[:64] = np.tile((s6 <= t6).astype(np.float32)[:, None, :], (1, 8, 1)).reshape(64, 512)
    c["r_sti"] = a
    a = z.copy(); a[:64] = np.tile(np.eye(64, dtype=np.float32)[:, None, :], (1, 8, 1)).reshape(64, 512)
    c["i8"] = a
    c["ones"] = np.ones((128, 128), np.float32)
    bd = np.zeros((128, 128), np.float32); bd[:64, :64] = 1; bd[64:, 64:] = 1
    c["bd"] = bd
    names = ["ident", "m128", "m32b", "r_ts", "r_st", "r_sti", "i8", "ones", "bd", "m96"]
    offs = {}
    o = 0
    for n in names:
        offs[n] = (o, c[n].shape[1]); o += c[n].shape[1]
    return np.concatenate([c[n] for n in names], axis=1), offs


CONSTS, COFF = _host_consts()
NCONST = CONSTS.shape[1]
CFO = {"ident": 0, "ones": 128, "bd": 256, "m96": 384}
NCF = 512
CSTF = np.concatenate([CONSTS[:, COFF[n][0]:COFF[n][0] + 128] for n in ("ident", "ones", "bd", "m96")], axis=1)


def _pack_params(inp, l):
    P = np.zeros((128, NPK), np.float32)

    def put(name, arr):
        o, w = PK[name]
        P[:arr.shape[0], o:o + arr.shape[1]] = arr

    fm = lambda v: np.ascontiguousarray(v.reshape(-1, 128).T)
    put("norm_g", fm(inp["norm_g"][l]))
    put("ada_b", fm(inp["ada_b"][l]))
    mu = inp["rw_mu"][l]
    put("mu", fm(mu[:1536]))
    put("mu_wc", mu[1536:1600].reshape(64, 1))
    put("mu_ac", mu[1600:1664].reshape(64, 1))
    put("w0", fm(inp["rw_w0"][l])); put("a0", fm(inp["rw_a0"][l]))
    put("k_k", fm(inp["rw_k_k"][l])); put("k_a", fm(inp["rw_k_a"][l])); put("r_k", fm(inp["rw_r_k"][l]))
    put("ln_g", fm(inp["rw_ln_g"][l])); put("ln_b", fm(inp["rw_ln_b"][l]))
    put("gk_b", fm(inp["gla_gk_b"][l])); put("gla_g", fm(inp["gla_norm_g"][l]))
    cw = inp["ml_conv_w"][l]
    put("conv", np.concatenate([fm(cw[j]) for j in range(4)], axis=1))
    put("ml_g", fm(inp["ml_norm_g"][l]))
    put("lb", np.concatenate([fm(inp["hg_lb_logits"][j]) for j in range(4)], axis=1))
    put("hg_g", fm(inp["hg_norm_g"][l]))
    put("i_b", fm(np.repeat(inp["ml_i_b"][l], 64)))
    put("f_b", fm(np.repeat(inp["ml_f_b"][l], 64)))
    return P


def build_nc(n_layers=DEPTH, branches=(0, 1, 2, 3), debug=False, stage=99):
    nc = bass.Bass("TRN2", target_bir_lowering=False)
    dram = lambda n, s, k="ExternalInput": nc.dram_tensor(n, list(s), F32, kind=k).ap()
    xT_d = dram("xT", [D, SEQ])
    c_d = dram("cT", [128, 8])
    pk_d = dram("pk", [DEPTH, 128, NPK])
    fg_d = dram("fg", [128, 8])
    cst_d = dram("cst", [128, NCONST])
    cstf_d = dram("cstf", [128, NCF])
    adaw_d = dram("ada_w", [DEPTH, D, 3 * D])
    win_d = dram("w_in", [DEPTH, D, NCOLS])
    wup_d = dram("rw_w_up", [DEPTH, 64, 512])
    aup_d = dram("rw_a_up", [DEPTH, 64, 512])
    gup_d = dram("gla_gk_up", [DEPTH, 16, 256])
    wbr_d = dram("w_branch", [DEPTH, 4, 512, D])
    wout_d = dram("w_out", [DEPTH, D, D])
    out_d = dram("outT", [D, SEQ], "ExternalOutput")
    dbg_d = dram("dbg", [4, 512, SEQ], "ExternalOutput") if debug else None

    es = contextlib.ExitStack()
    with es:
        T = lambda n, s, d=F32: es.enter_context(nc.sbuf_tensor("s_" + n, list(s), d))
        x = T("x", [128, 8, TS])
        uT = T("uT", [128, 8, TS], BF16)
        merged = T("merged", [128, 8, TS])
        mergedb = uT
        ys = T("ys", [128, 4, TS], BF16)
        pk = T("pk", [128, DEPTH, NPK])
        drv = T("drv", [128, 64])
        cst = T("cst", [128, NCF])
        cstb = T("cstb", [128, NCONST], BF16)
        cT = T("cT", [128, 8])
        fg = T("fg", [128, 8])
        mod = T("mod", [128, 24])
        lbt = T("lbt", [128, 16]); lbe = T("lbe", [128, 16]); lbs = T("lbs", [128, 4])
        NWB = 6
        wb = [T("wb%d" % i, [128, 8, 128], BF16) for i in range(NWB)]
        wv = [T("wv%d" % i, [128, 8, 512], BF16) for i in range(1)]
        wup = T("wup", [64, 512], BF16); aup = T("aup", [64, 512], BF16); gup = T("gup", [16, 256], BF16)
        NF = 16
        fs = [T("fs%d" % i, [128, TS + 4]) for i in range(NF)]
        LL = [T("ll%d" % i, [128, TS]) for i in range(8)]
        NB = 5
        bs = [T("bs%d" % i, [128, TS], BF16) for i in range(NB)]
        BL = [T("bl%d" % i, [128, TS], BF16) for i in range(20)]
        prc = T("prc", [128, 16])
        cqc = T("cqc", [128, 4, 4])
        gnb = [T("gn%d" % i, [128, TS + 1]) for i in range(4)]
        nref = T("nref", [128, 4, 16])
        ktz = T("ktz", [128, 2, 512], BF16)
        vtok = T("vtok", [128, 4, 512], BF16)
        ktok = T("ktok", [128, 4, 512], BF16)
        rv = {n: T("rv_" + n, [64, 512], BF16) for n in
              ("vt", "bt", "kt", "N", "NT", "N2", "NT2", "Y", "Y2", "Aak", "Arb", "Ark", "X", "U", "Yf")}
        rw_sf = T("rw_sf", [64, 8, 64]); rw_sb = T("rw_sb", [64, 8, 64], BF16)
        la_sf = [T("la_sf%d" % i, [128, 4, 128]) for i in range(4)]
        la_sb = [T("la_sb%d" % i, [128, 4, 128], BF16) for i in range(4)]
        gamh = T("gamh", [64, 8, 16])
        gam = T("gam", [128, 4, 16])
        yT = merged
        wcac = T("wcac", [64, 2, TS])
        wcacb = T("wcacb", [64, 2, TS], BF16)
        ps = [es.enter_context(nc.psum_tensor("ps%d" % i, [128, 512], F32)) for i in range(8)]

        S = Sched(nc)
        st = {"bank": 0, "w": 0, "fsi": 0, "bsi": 0, "wai": 0}

        def bank():
            i = st["bank"]; st["bank"] = (i + 1) % 6
            return ps[i], "ps%d" % i

        CS = lambda n: cst[:, CFO[n]:CFO[n] + 128]
        CB = lambda n: cstb[:, COFF[n][0]:COFF[n][0] + COFF[n][1]]
        PKc = lambda l, n, j=0, w=1: pk[:, l, PK[n][0] + j:PK[n][0] + j + w]

        def mm(out, lhsT, rhs, start, stop, r, w):
            S.op("pe", lambda e: e.matmul(out, lhsT, rhs, start=start, stop=stop), reads=r, writes=w)

        def tr(out, in_, ident, r, w):
            S.op("pe", lambda e: e.transpose(out, in_, ident), reads=r, writes=w)

        def act(out, in_, func, r, w, bias=None, scale=None):
            kw = {}
            if bias is not None:
                kw["bias"] = bias
            if scale is not None:
                kw["scale"] = scale
            S.op("act", lambda e: e.activation(out=out, in_=in_, func=func, **kw), reads=r, writes=w)

        def tt(out, a, b, op, r, w, eng="dve"):
            S.op(eng, lambda e: e.tensor_tensor(out=out, in0=a, in1=b, op=op), reads=r, writes=w)

        def tsc(out, a, s1, s2, op0, op1, r, w, eng="dve"):
            if op1 is None:
                S.op(eng, lambda e: e.tensor_scalar(out=out, in0=a, scalar1=s1, scalar2=None, op0=op0), reads=r, writes=w)
            else:
                S.op(eng, lambda e: e.tensor_scalar(out=out, in0=a, scalar1=s1, scalar2=s2, op0=op0, op1=op1),
                     reads=r, writes=w)

        def stt(out, a, sc, b, op0, op1, r, w, eng="dve"):
            S.op(eng, lambda e: e.scalar_tensor_tensor(out=out, in0=a, scalar=sc, in1=b, op0=op0, op1=op1),
                 reads=r, writes=w)

        def cp(out, in_, r, w, eng="dve"):
            S.op(eng, lambda e: e.tensor_copy(out=out, in_=in_), reads=r, writes=w)

        def rsq(out, in_, scale, bias, r, w):
            act(out, in_, AF.Ln, r, w, bias=bias, scale=scale)
            act(out, out, AF.Exp, w, w, scale=-0.5)

        def recip(out, in_, r, w):
            S.op("dve", lambda e: e.reciprocal(out=out, in_=in_), reads=r, writes=w)

        def mset(ap, val, w):
            S.op("dve", lambda e: e.memset(ap, val), writes=w)

        def dma(eng, out, in_, r, w, stream):
            return S.op(eng, lambda e: e.dma_start(out=out, in_=in_), reads=r, writes=w, dma=stream)

        def cumsum(ct, src, srck):
            S.op("dve", lambda e: e.tensor_tensor_scan(out=gnb[ct][:, 1:TS + 1], data0=src, data1=src, initial=0.0,
                                                       op0=ALU.add, op1=ALU.max), reads=[srck], writes=["gn%d" % ct])

        def wchunk(src, n, kc=8):
            i = st["w"]; st["w"] = (i + 1) % NWB
            key = "wb%d" % i
            dma("pool", wb[i][:, 0:kc, 0:n], src.rearrange("(k p) c -> p k c", p=128), [], [key], key)
            return wb[i], key

        def proj(l, c0, n):
            w, wk = wchunk(win_d[l, :, c0:c0 + n], n)
            b, bk = bank()
            for k in range(8):
                mm(b[0:n, :], w[:, k, 0:n], uT[:, k, :], k == 0, k == 7, [wk, "uT"], [bk])
            return b, bk

        def proj_tok(l, c0):
            for h in range(2):
                dma("pool", wv[0][:, :, h * 256:(h + 1) * 256],
                    win_d[l, :, c0 + h * 256:c0 + (h + 1) * 256].rearrange("(k p) c -> p k c", p=128),
                    [], ["wv_%d" % h], "wv_%d" % h)
            for tI in range(TS // 128):
                b, bk = bank()
                for k in range(8):
                    mm(b[:, :], uT[:, k, tI * 128:(tI + 1) * 128], wv[0][:, k, :], k == 0, k == 7,
                       ["wv_0", "wv_1", "uT"], [bk])
                if tI % 2 == 0:
                    act(vtok[:, tI, :], b[:, :], AF.Copy, [bk], ["vtok%d" % tI])
                else:
                    cp(vtok[:, tI, :], b[:, :], [bk], ["vtok%d" % tI])

        def fsa():
            i = st["fsi"]; st["fsi"] = (i + 1) % NF
            return fs[i], "fs%d" % i

        def bsa():
            i = st["bsi"]; st["bsi"] = (i + 1) % NB
            return bs[i], "bs%d" % i

        XK = ["x%d" % k for k in range(8)]

        def rms_to(sl_src, emit):
            b, bk = bank()
            for k in range(8):
                sq, sqk = fsa()
                act(sq[:, 0:TS], x[:, k, :], AF.Square, [XK[k]], [sqk])
                mm(b[:, :], CS("ones"), sq[:, 0:TS], k == 0, k == 7, [sqk, "cst"], [bk])
            rs, rsk = fsa()
            rsq(rs[:, 0:TS], b[:, :], 1.0, 1024.0 * 1e-6, [bk], [rsk])
            for k in range(8):
                t1, t1k = fsa()
                tt(t1[:, 0:TS], x[:, k, :], rs[:, 0:TS], ALU.mult, [XK[k], rsk], [t1k])
                emit(k, t1, t1k)

        def program():
            dma("sp", cst[:, :], cstf_d, [], ["cst"], "cst")
            dma("pool", cstb[:, :], cst_d, [], ["cstb"], "cstb")
            dma("sp", pk[:, :, :], pk_d.rearrange("l p n -> p l n"), [], ["pk"], "pk")
            dma("sp", cT[:, :], c_d, [], ["cT"], "cT")
            dma("sp", fg[:, :], fg_d, [], ["fg"], "fg")
            act(cT[:, :], cT[:, :], AF.Silu, ["cT"], ["cT"])
            o_lb = PK["lb"][0]
            act(lbe[:, :], pk[:, 0, o_lb:o_lb + 16], AF.Exp, ["pk"], ["lbe"])
            tt(lbs[:, :], lbe[:, 0:4], lbe[:, 4:8], ALU.add, ["lbe"], ["lbs"])
            tt(lbs[:, :], lbs[:, :], lbe[:, 8:12], ALU.add, ["lbs", "lbe"], ["lbs"])
            tt(lbs[:, :], lbs[:, :], lbe[:, 12:16], ALU.add, ["lbs", "lbe"], ["lbs"])
            recip(lbs[:, :], lbs[:, :], ["lbs"], ["lbs"])
            mset(lbt[:, 0:4], 0.0, ["lbt"])
            for j in range(1, 4):
                t_, tk_ = fsa()
                tt(t_[:, 0:4], lbe[:, j * 4:(j + 1) * 4], lbs[:, :], ALU.mult, ["lbe", "lbs"], [tk_])
                tt(lbt[:, j * 4:(j + 1) * 4], lbt[:, (j - 1) * 4:j * 4], t_[:, 0:4], ALU.add, [tk_, "lbt"], ["lbt"])
            for i in range(4):
                mset(gnb[i][:, 0:1], 0.0, ["gn%d" % i])

            for l in range(n_layers):
                layer(l)

            outs = []
            for sI in range(NST):
                sl = slice(sI * TS, (sI + 1) * TS)
                for k in range(8):
                    dma("sp", x[:, k, :], (out_d if n_layers > 0 else xT_d)[k * 128:(k + 1) * 128, sl], ["xd%d" % sI], [XK[k]], "xl%d" % k)

                def emit(k, t1, t1k, sI=sI, sl=sl):
                    tsc(t1[:, 0:TS], t1[:, 0:TS], fg[:, k:k + 1], 32.0, ALU.mult, ALU.mult, [t1k, "fg"], [t1k])
                    outs.append(dma("sp", out_d[k * 128:(k + 1) * 128, sl], t1[:, 0:TS], [t1k], ["xd%d" % sI], "o_" + t1k))
                rms_to(sl, emit)
            return outs

        def layer(l):
            b, bk = bank()
            for j in range(24):
                i = st["wai"]; st["wai"] = (i + 1) % 3
                wa_i = merged[:, 2 * i:2 * i + 2, :].rearrange("p a (k c) -> p (a k) c", c=128)
                keys = ["mg%d" % (2 * i), "mg%d" % (2 * i + 1)]
                dma("sp", wa_i, adaw_d[l, :, j * 128:(j + 1) * 128].rearrange("(k p) c -> p k c", p=128),
                    [], keys, "wa%d" % i)
                for k in range(8):
                    mm(b[:, j:j + 1], wa_i[:, k, :], cT[:, k:k + 1], k == 0, k == 7, keys + ["cT"], [bk])
            tt(mod[:, :], b[:, 0:24], pk[:, l, PK["ada_b"][0]:PK["ada_b"][0] + 24], ALU.add, [bk, "pk"], ["mod"])
            stt(drv[:, 0:8], mod[:, 8:16], 1.0, pk[:, l, PK["norm_g"][0]:PK["norm_g"][0] + 8], ALU.add, ALU.mult,
                ["mod", "pk"], ["drv"])
            tsc(drv[:, 0:8], drv[:, 0:8], 32.0, None, ALU.mult, None, ["drv"], ["drv"])
            o_mu = PK["mu"][0]
            tsc(drv[:, 8:22], pk[:, l, o_mu:o_mu + 14], -1.0, 1.0, ALU.mult, ALU.add, ["pk"], ["drv"])
            tsc(drv[:, 22:26], PKc(l, "w0", 0, 4), -1.0, None, ALU.mult, None, ["pk"], ["drv"])
            tsc(drv[:, 26:30], PKc(l, "k_a", 0, 4), -1.0, 1.0, ALU.mult, ALU.add, ["pk"], ["drv"])
            tsc(drv[:, 30:32], PKc(l, "gk_b", 0, 2), -1.0, None, ALU.mult, None, ["pk"], ["drv"])
            tsc(drv[:, 32:34], PKc(l, "f_b", 0, 2), -1.0, None, ALU.mult, None, ["pk"], ["drv"])
            tsc(drv[:, 34:38], lbt[:, l * 4:(l + 1) * 4], -1.0, 1.0, ALU.mult, ALU.add, ["lbt"], ["drv"])
            tsc(drv[:, 38:42], PKc(l, "gla_g", 0, 4), float(np.sqrt(128.0)), None, ALU.mult, None, ["pk"], ["drv"])
            tsc(drv[:, 42:46], PKc(l, "hg_g", 0, 4), float(np.sqrt(128.0)), None, ALU.mult, None, ["pk"], ["drv"])
            dma("pool", wup[:, :], wup_d[l], [], ["wup"], "wup")
            dma("pool", aup[:, :], aup_d[l], [], ["aup"], "aup")
            dma("pool", gup[:, :], gup_d[l], [], ["gup"], "gup")
            mset(rw_sf[:, :, :], 0.0, ["rw_sf"])
            mset(rw_sb[:, :, :], 0.0, ["rw_sb"])
            for i in range(4):
                mset(la_sf[i][:, :, :], 0.0, ["la_sf%d" % i])
                mset(la_sb[i][:, :, :], 0.0, ["la_sb%d" % i])
            mset(prc[:, :], 0.0, ["prc"])
            mset(cqc[:, :, :], 0.0, ["cqc"])

            src_d = xT_d if l == 0 else out_d
            for sI in range(NST):
                sl = slice(sI * TS, (sI + 1) * TS)
                for k in range(8):
                    dma("sp", x[:, k, :], src_d[k * 128:(k + 1) * 128, sl], ["xd%d" % sI], [XK[k]], "xl%d" % k)

                def emit(k, t1, t1k):
                    act(uT[:, k, :], t1[:, 0:TS], AF.Identity, [t1k, "drv", "mod"], ["uT"],
                        bias=mod[:, k:k + 1], scale=drv[:, k:k + 1])
                rms_to(sl, emit)
                for m in range(4):
                    if m in branches:
                        (rwkv, gla, mlstm, hgrn)[m](l, sI)
                    else:
                        mset(ys[:, :, :], 0.0, ["ys"])
                    if debug and l == 0:
                        for k in range(4):
                            dma("pool", dbg_d[m, k * 128:(k + 1) * 128, sl], ys[:, k, :], ["ys"], [], "dbg%d" % k)
                    for fc in range(8):
                        pg, pgk = proj(l, C_MG + m * 1024 + fc * 128, 128)
                        sg, sgk = fsa()
                        act(sg[:, 0:TS], pg[:, :], AF.Sigmoid, [pgk], [sgk])
                        w, wk = wchunk(wbr_d[l, m, :, fc * 128:(fc + 1) * 128], 128, kc=4)
                        pb, pbk = bank()
                        for k in range(4):
                            mm(pb[:, :], w[:, k, :], ys[:, k, :], k == 0, k == 3, [wk, "ys"], [pbk])
                        if m == 0:
                            tt(merged[:, fc, :], pb[:, :], sg[:, 0:TS], ALU.mult, [pbk, sgk], ["mg%d" % fc])
                        else:
                            t2, t2k = fsa()
                            tt(t2[:, 0:TS], pb[:, :], sg[:, 0:TS], ALU.mult, [pbk, sgk], [t2k])
                            tt(merged[:, fc, :], merged[:, fc, :], t2[:, 0:TS], ALU.add, [t2k, "mg%d" % fc], ["mg%d" % fc])
                for fc in range(8):
                    act(mergedb[:, fc, :], merged[:, fc, :], AF.Copy, ["mg%d" % fc], ["uT"])
                for fc in range(8):
                    w, wk = wchunk(wout_d[l, :, fc * 128:(fc + 1) * 128], 128)
                    pb, pbk = bank()
                    for k in range(8):
                        mm(pb[:, :], w[:, k, :], mergedb[:, k, :], k == 0, k == 7, [wk, "uT"], [pbk])
                    stt(x[:, fc, :], pb[:, :], mod[:, 16 + fc:17 + fc], x[:, fc, :], ALU.mult, ALU.add,
                        [pbk, "mod", XK[fc]], [XK[fc]])
                    dma("sp", out_d[fc * 128:(fc + 1) * 128, sl], x[:, fc, :], [XK[fc]], ["xd%d" % sI], "xs%d" % fc)

        def shift_hi(src_ap, skey, dst_ap, dkey, n, fp32=False):
            b, bk = bank()
            idn = (CS if fp32 else CB)("ident")
            mm(b[0:64, 0:n], idn[:, 64:128], src_ap, True, True, [skey, "cst" if fp32 else "cstb"], [bk])
            cp(dst_ap, b[0:64, 0:n], [bk], [dkey])

        def build_gamh(npair, nch):
            for ct in range(npair):
                cp(gamh[:, 2 * ct, 0:nch], gam[0:64, ct, 0:nch], ["gam"], ["gamh"])
                shift_hi(gam[:, ct, 0:nch], "gam", gamh[:, 2 * ct + 1, 0:nch], "gamh", nch, fp32=True)

        def drive(gens):
            gens = [g for g in gens if g is not None]
            while gens:
                for g in list(gens):
                    try:
                        next(g)
                    except StopIteration:
                        gens.remove(g)

        def la(Qh, Kh, KTp, C, dk, si, gm, gmk, fin, ni=None):
            mask = {128: CB("m128"), 32: CB("m32b")}[C]
            sfk, sbk = "la_sf%d" % si, "la_sb%d" % si
            nkt = len(KTp)
            PT = {}

            def phA(tI):
                tsl = slice(tI * 128, (tI + 1) * 128)
                bt, btk = bank()
                btb = bt[:, :].bitcast(BF16)
                for ct in range(nkt):
                    tr(btb[:, ct * 128:(ct + 1) * 128], KTp[ct][0][:, tsl], CB("ident"), [KTp[ct][1], "cstb"], [btk])
                cp(ktok[:, tI, 0:nkt * 128], btb[:, 0:nkt * 128], [btk], ["ktok%d" % tI])
                if C == 32:
                    tsc(ktz[64:128, tI % 2, 0:nkt * 128], ktok[64:128, tI, 0:nkt * 128], CS("m96")[64:128, 0:1], None,
                        ALU.mult, None, ["ktok%d" % tI, "cst"], ["ktz%d" % (tI % 2)])
                yield
                bsc, bsck = bank()
                for h in range(4):
                    mm(bsc[:, h * 128:(h + 1) * 128], Kh[h][0][:, tsl], Qh[h][0][:, tsl], True, True,
                       [Kh[h][1], Qh[h][1]], [bsck])
                pt, ptk = bsa()
                tt(pt[:, :], bsc[:, :], mask, ALU.mult, [bsck, "cstb"], [ptk])
                PT[tI] = (pt, ptk)
                yield

            def phB(tI):
                pt, ptk = PT[tI]
                po, pok = ps[6], "ps6"
                pd, pdk = (ps[7], "ps7") if ni is not None else (None, None)
                for cc in range(128 // C):
                    cidx = tI * (128 // C) + cc
                    csl = slice(cc * C, (cc + 1) * C)
                    gsl = slice(tI * 128 + cc * C, tI * 128 + (cc + 1) * C)
                    for h in range(4):
                        osl = slice(h * 128 + cc * C, h * 128 + (cc + 1) * C)
                        mm(po[:, osl], vtok[:, tI, h * 128:(h + 1) * 128], pt[:, osl], True, False,
                           ["vtok%d" % tI, ptk], [pok])
                        mm(po[:, osl], la_sb[si][0:dk, h, :], Qh[h][0][:, gsl], False, True, [sbk, Qh[h][1]], [pok])
                        if ni is not None:
                            mm(pd[:, osl], CB("ones"), pt[:, osl], True, False, ["cstb", ptk], [pdk])
                            mm(pd[:, osl], la_sb[ni][0:dk, h, :], Qh[h][0][:, gsl], False, True,
                               ["la_sb%d" % ni, Qh[h][1]], [pdk])
                    for sidx, isn in ((si, False),) + (((ni, True),) if ni is not None else ()):
                        bu, buk = bank()
                        for h in range(4):
                            if C == 32 and cc == 3:
                                zsl = slice(64, 128)
                                mm(bu[0:dk, h * 128:(h + 1) * 128], ktz[zsl, tI % 2, h * dk:(h + 1) * dk],
                                   vtok[zsl, tI, h * 128:(h + 1) * 128], True, True,
                                   ["ktz%d" % (tI % 2), "vtok%d" % tI], [buk])
                            else:
                                rhs = CB("ones")[csl, :] if isn else vtok[csl, tI, h * 128:(h + 1) * 128]
                                mm(bu[0:dk, h * 128:(h + 1) * 128], ktok[csl, tI, h * dk:(h + 1) * dk], rhs, True, True,
                                   ["ktok%d" % tI, "vtok%d" % tI, "cstb"], [buk])
                        tmp_, tmpk = fsa()
                        tv = tmp_[0:dk, 0:512].rearrange("p (h v) -> p h v", h=4)
                        tt(tv, bu[0:dk, :].rearrange("p (h v) -> p h v", h=4), la_sf[sidx][0:dk, :, :], ALU.add,
                           [buk, "la_sf%d" % sidx], [tmpk])
                        gb = gm[0:dk, 0:4, cidx:cidx + 1].to_broadcast([dk, 4, 128])
                        tt(la_sb[sidx][0:dk, :, :], tv, gb, ALU.mult, [tmpk, gmk], ["la_sb%d" % sidx])
                        tt(la_sf[sidx][0:dk, :, :], tv, gb, ALU.mult, [tmpk, gmk], ["la_sf%d" % sidx])
                    yield
                fin(tI, po, pok, pd, pdk)
                yield

            NT_ = TS // 128
            drive([phA(0)])
            for tI in range(NT_):
                drive([phB(tI), phA(tI + 1) if tI + 1 < NT_ else None])

        def gam_start(ct, C):
            n = TS // C
            d_, dk_ = fsa()
            tt(d_[:, 0:n], gnb[ct][:, C:TS + 1:C], gnb[ct][:, 0:TS:C], ALU.subtract, ["gn%d" % ct], [dk_])
            act(gam[:, ct, 0:n], d_[:, 0:n], AF.Exp, [dk_], ["gam"], scale=-1.0)

        def qk_decay(ct, q_ap, qk_, k_ap, kk_, C, mid, Qd, Kd, kextra=None, clamp=False):
            gk = "gn%d" % ct
            n = TS // C
            off = C // 2 if mid else 0
            tsc(nref[:, ct, 0:n], gnb[ct][:, off:TS:C], -1.0, None, ALU.mult, None, [gk], ["nref"])
            ei, eik = fsa(); ev, evk = fsa()
            for c in range(n):
                c0 = c * C
                rc = c0 + off
                act(ei[:, c0:c0 + C], gnb[ct][:, c0 + 1:c0 + C + 1], AF.Exp, [gk], [eik], bias=gnb[ct][:, rc:rc + 1], scale=-1.0)
                if kextra is None:
                    act(ev[:, c0:c0 + C], gnb[ct][:, c0 + 1:c0 + C + 1], AF.Exp, [gk, "nref"], [evk],
                        bias=nref[:, ct, c:c + 1], scale=1.0)
                else:
                    act(ev[:, c0:c0 + C], kextra[0][:, c0:c0 + C], AF.Exp, [kextra[1], "nref"], [evk],
                        bias=nref[:, ct, c:c + 1], scale=1.0)
            if clamp:
                tsc(ei[:, 0:TS], ei[:, 0:TS], 2.35e17, None, ALU.min, None, [eik], [eik])
                tsc(ev[:, 0:TS], ev[:, 0:TS], 2.35e17, None, ALU.min, None, [evk], [evk])
            tt(Qd[0][:, :], q_ap, ei[:, 0:TS], ALU.mult, [qk_, eik], [Qd[1]])
            tt(Kd[0][:, :], k_ap, ev[:, 0:TS], ALU.mult, [kk_, evk], [Kd[1]])

        BLk = lambda i: (BL[i], "bl%d" % i)
        LLk = lambda i: (LL[i], "ll%d" % i)

        def zgate(l, c0, h, func_scale_ap, skeys):
            pz, pzk = proj(l, c0 + h * 128, 128)
            z, zk = LLk(h)
            act(z[:, :], pz[:, :], AF.Silu, [pzk], [zk])
            if func_scale_ap is not None:
                tsc(z[:, :], z[:, :], func_scale_ap, None, ALU.mult, None, [zk] + skeys, [zk])
            return z, zk

        def rms_fin(GZ):
            def fin(tI, po, pok, pd, pdk):
                sq, sqk = fsa()
                act(sq[:, 0:TS], po[:, :], AF.Square, [pok], [sqk])
                b, bk = bank()
                mm(b[:, :], CS("ones"), sq[:, 0:TS], True, True, [sqk, "cst"], [bk])
                rs, rsk = fsa()
                rsq(rs[:, 0:TS], b[:, :], 1.0, 128.0 * 1e-6, [bk], [rsk])
                t1, t1k = fsa()
                tt(t1[:, 0:TS], po[:, :], rs[:, 0:TS], ALU.mult, [pok, rsk], [t1k])
                for h in range(4):
                    tt(ys[:, h, tI * 128:(tI + 1) * 128], t1[:, h * 128:(h + 1) * 128],
                       GZ[h][0][:, tI * 128:(tI + 1) * 128], ALU.mult, [t1k, GZ[h][1]], ["ys"])
            return fin

        def heads64(QT, KT):
            Qh, Kh = [], []
            for ct in range(2):
                shift_hi(QT[ct][0][:, :], QT[ct][1], BL[16 + ct][0:64, :], "bl%d" % (16 + ct), TS)
                shift_hi(KT[ct][0][:, :], KT[ct][1], BL[18 + ct][0:64, :], "bl%d" % (18 + ct), TS)
                Qh += [(QT[ct][0][0:64, :], QT[ct][1]), (BL[16 + ct][0:64, :], "bl%d" % (16 + ct))]
                Kh += [(KT[ct][0][0:64, :], KT[ct][1]), (BL[18 + ct][0:64, :], "bl%d" % (18 + ct))]
            return Qh, Kh

        def gla(l, sI):
            proj_tok(l, C_GV)
            if stage <= 1:
                mset(ys[:, :, :], 0.0, ["ys"]); return
            pg, pgk = proj(l, C_GC, 16)
            gc, gck = bsa()
            act(gc[0:16, 0:TS], pg[0:16, :], AF.Copy, [pgk], [gck])
            QT, KT = [], []
            for ct in range(2):
                b, bk = bank()
                mm(b[:, :], gup[:, ct * 128:(ct + 1) * 128], gc[0:16, 0:TS], True, True, ["gup", gck], [bk])
                e1, e1k = fsa()
                act(e1[:, 0:TS], b[:, :], AF.Exp, [bk, "drv"], [e1k], bias=drv[:, 30 + ct:31 + ct], scale=-1.0)
                act(e1[:, 0:TS], e1[:, 0:TS], AF.Ln, [e1k], [e1k], bias=1.0)
                tsc(e1[:, 0:TS], e1[:, 0:TS], 1.0 / 16.0, None, ALU.mult, None, [e1k], [e1k])
                cumsum(ct, e1[:, 0:TS], e1k)
                gam_start(ct, 128)
                if stage <= 2:
                    continue
                pq, pqk = proj(l, C_GQ + ct * 128, 128)
                q, qk_ = fsa()
                act(q[:, 0:TS], pq[:, :], AF.Copy, [pqk], [qk_], scale=0.125)
                pk_, pkk = proj(l, C_GK + ct * 128, 128)
                k, kk_ = fsa()
                cp(k[:, 0:TS], pk_[:, :], [pkk], [kk_])
                qk_decay(ct, q[:, 0:TS], qk_, k[:, 0:TS], kk_, 128, False, BLk(ct), BLk(4 + ct))
                QT.append(BLk(ct)); KT.append(BLk(4 + ct))
            if stage <= 3:
                mset(ys[:, :, :], 0.0, ["ys"]); return
            GZ = [zgate(l, C_GZ, h, drv[:, 38 + h:39 + h], ["drv"]) for h in range(4)]
            Qh, Kh = heads64(QT, KT)
            build_gamh(2, 4)
            la(Qh, Kh, KT, 128, 64, 0, gamh, "gamh", rms_fin(GZ))

        def hgrn(l, sI):
            proj_tok(l, C_HI)
            QT, KT = [], []
            for ct in range(4):
                pf, pfk = proj(l, C_HF + ct * 128, 128)
                g, gk_ = fsa()
                act(g[:, 0:TS], pf[:, :], AF.Sigmoid, [pfk], [gk_])
                tsc(g[:, 0:TS], g[:, 0:TS], drv[:, 34 + ct:35 + ct], lbt[:, l * 4 + ct:l * 4 + ct + 1], ALU.mult, ALU.add,
                    [gk_, "drv", "lbt"], [gk_])
                lg, lgk = fsa()
                recip(lg[:, 0:TS], g[:, 0:TS], [gk_], [lgk])
                act(lg[:, 0:TS], lg[:, 0:TS], AF.Ln, [lgk], [lgk])
                cumsum(ct, lg[:, 0:TS], lgk)
                d_, dk_ = fsa()
                tt(d_[:, 0:15], gnb[ct][:, 48:TS:32], gnb[ct][:, 16:TS - 32:32], ALU.subtract, ["gn%d" % ct], [dk_])
                tt(d_[:, 15:16], gnb[ct][:, TS:TS + 1], gnb[ct][:, TS - 16:TS - 15], ALU.subtract, ["gn%d" % ct], [dk_])
                act(gam[:, ct, 0:16], d_[:, 0:16], AF.Exp, [dk_], ["gam"], scale=-1.0)
                fc_, fck = fsa()
                act(fc_[:, 0:1], gnb[ct][:, 16:17], AF.Exp, ["gn%d" % ct], [fck], scale=-1.0)
                tsc(la_sf[3][:, ct, :], la_sf[3][:, ct, :], fc_[:, 0:1], None, ALU.mult, None,
                    [fck, "la_sf3"], ["la_sf3"])
                cp(la_sb[3][:, ct, :], la_sf[3][:, ct, :], ["la_sf3"], ["la_sb3"])
                tsc(g[:, 0:TS], g[:, 0:TS], -1.0, 1.0, ALU.mult, ALU.add, [gk_], [gk_])
                pq, pqk = proj(l, C_HQ + ct * 128, 128)
                q, qk_ = fsa()
                act(q[:, 0:TS], pq[:, :], AF.Silu, [pqk], [qk_])
                qk_decay(ct, q[:, 0:TS], qk_, g[:, 0:TS], gk_, 32, True, BLk(ct), BLk(4 + ct), clamp=True)
                QT.append(BLk(ct)); KT.append(BLk(4 + ct))
            GZ = [zgate(l, C_HZ, h, drv[:, 42 + h:43 + h], ["drv"]) for h in range(4)]
            la(QT, KT, KT, 32, 128, 3, gam, "gam", rms_fin(GZ))

        def mlstm(l, sI):
            proj_tok(l, C_MV)
            w8, w8k = wchunk(win_d[l, :, C_MI:C_MI + 8], 8)
            reps = {}
            for ct in range(2):
                for gI in range(2):
                    for half in range(2):
                        r_, rk_ = BLk(8 + ct * 4 + gI * 2 + half)
                        rv_ = r_[:, :].rearrange("p (k j m) -> p k j m", k=4, j=2, m=64)
                        src = w8[:, half * 4:(half + 1) * 4, gI * 4 + ct * 2:gI * 4 + ct * 2 + 2]
                        cp(rv_, src.unsqueeze(3).to_broadcast([128, 4, 2, 64]), [w8k], [rk_])
                        reps[(ct, gI, half)] = (r_, rk_)
            for ct in range(4):
                pc, pck = proj(l, C_MQK + ct * 128, 128)
                cb, cbk = fsa()
                cp(cb[:, 0:3], cqc[:, ct, 0:3], ["cqc"], [cbk])
                act(cb[:, 3:TS + 3], pc[:, :], AF.Copy, [pck], [cbk])
                a, ak = fsa()
                oc = PK["conv"][0]
                tsc(a[:, 0:TS], cb[:, 0:TS], pk[:, l, oc + ct:oc + ct + 1], None, ALU.mult, None, [cbk, "pk"], [ak])
                for j in range(1, 4):
                    stt(a[:, 0:TS], cb[:, j:j + TS], pk[:, l, oc + j * 4 + ct:oc + j * 4 + ct + 1], a[:, 0:TS], ALU.mult,
                        ALU.add, [cbk, "pk", ak], [ak])
                cp(cqc[:, ct, 0:3], cb[:, TS:TS + 3], [cbk], ["cqc"])
                act(LL[4 + ct][:, :], a[:, 0:TS], AF.Silu, [ak], ["ll%d" % (4 + ct)])
            QT, KT = [], []
            for ct in range(2):
                pis = []
                for gI in range(2):
                    b, bk = bank()
                    for k in range(8):
                        r_, rk_ = reps[(ct, gI, k // 4)]
                        mm(b[:, :], r_[:, (k % 4) * 128:(k % 4 + 1) * 128], uT[:, k, :], k == 0, k == 7, [rk_, "uT"], [bk])
                    pis.append((b, bk))
                e1, e1k = fsa()
                act(e1[:, 0:TS], pis[1][0][:, :], AF.Exp, [pis[1][1], "drv"], [e1k], bias=drv[:, 32 + ct:33 + ct], scale=-1.0)
                act(e1[:, 0:TS], e1[:, 0:TS], AF.Ln, [e1k], [e1k], bias=1.0)
                cumsum(ct, e1[:, 0:TS], e1k)
                gam_start(ct, 128)
                ig, igk = fsa()
                act(ig[:, 0:TS], pis[0][0][:, :], AF.Identity, [pis[0][1], "pk"], [igk], bias=PKc(l, "i_b", ct))
                tt(ig[:, 0:TS], ig[:, 0:TS], gnb[ct][:, 1:TS + 1], ALU.add, [igk, "gn%d" % ct], [igk])
                k, kk_ = fsa()
                tsc(k[:, 0:TS], LL[6 + ct][:, :], 0.125, None, ALU.mult, None, ["ll%d" % (6 + ct)], [kk_])
                qk_decay(ct, LL[4 + ct][:, :], "ll%d" % (4 + ct), k[:, 0:TS], kk_, 128, False, BLk(ct), BLk(4 + ct),
                         kextra=(ig, igk))
                QT.append(BLk(ct)); KT.append(BLk(4 + ct))
            GZ = [zgate(l, C_MZ, h, PKc(l, "ml_g", h), ["pk"]) for h in range(4)]

            def fin(tI, po, pok, pd, pdk):
                den, dnk = fsa()
                act(den[:, 0:TS], pd[:, :], AF.Abs, [pdk], [dnk])
                tsc(den[:, 0:TS], den[:, 0:TS], 1.0, None, ALU.max, None, [dnk], [dnk])
                recip(den[:, 0:TS], den[:, 0:TS], [dnk], [dnk])
                o, ok = fsa()
                tt(o[:, 0:TS], po[:, :], den[:, 0:TS], ALU.mult, [pok, dnk], [ok])
                b, bk = bank()
                mm(b[:, :], CS("ones"), o[:, 0:TS], True, True, [ok, "cst"], [bk])
                d, dk_ = fsa()
                stt(d[:, 0:TS], b[:, :], -1.0 / 128.0, o[:, 0:TS], ALU.mult, ALU.add, [bk, ok], [dk_])
                sq, sqk = fsa()
                act(sq[:, 0:TS], d[:, 0:TS], AF.Square, [dk_], [sqk])
                b2, b2k = bank()
                mm(b2[:, :], CS("ones"), sq[:, 0:TS], True, True, [sqk, "cst"], [b2k])
                rs, rsk = fsa()
                rsq(rs[:, 0:TS], b2[:, :], 1.0 / 128.0, 1e-6, [b2k], [rsk])
                tt(d[:, 0:TS], d[:, 0:TS], rs[:, 0:TS], ALU.mult, [dk_, rsk], [dk_])
                for h in range(4):
                    tt(ys[:, h, tI * 128:(tI + 1) * 128], d[:, h * 128:(h + 1) * 128],
                       GZ[h][0][:, tI * 128:(tI + 1) * 128], ALU.mult, [dk_, GZ[h][1]], ["ys"])

            Qh, Kh = heads64(QT, KT)
            build_gamh(2, 4)
            la(Qh, Kh, KT, 128, 64, 1, gamh, "gamh", fin, ni=2)

        def rwkv(l, sI):
            C = 64
            NCH = TS // C

            def mixed(idx, c0, n, mucol, omcol, dst=None):
                pp, ppk = proj(l, c0, n)
                pb_, pbk_ = fsa()
                cp(pb_[0:n, 0:1], prc[0:n, idx:idx + 1], ["prc"], [pbk_])
                act(pb_[0:n, 1:TS + 1], pp[0:n, :], AF.Copy, [ppk], [pbk_])
                t_, tk_ = fsa()
                tsc(t_[0:n, 0:TS], pb_[0:n, 0:TS], mucol, None, ALU.mult, None, [pbk_, "pk"], [tk_])
                if dst is None:
                    o_, ok_ = fsa()
                    o_ = o_[:, 0:TS]
                else:
                    o_, ok_ = dst
                stt(o_[0:n, :], pb_[0:n, 1:TS + 1], omcol, t_[0:n, 0:TS], ALU.mult, ALU.add, [pbk_, "drv", tk_], [ok_])
                cp(prc[0:n, idx:idx + 1], pb_[0:n, TS:TS + 1], [pbk_], ["prc"])
                return o_, ok_

            WC = mixed(12, C_RW + 1536, 64, pk[0:64, l, PK["mu_wc"][0]:PK["mu_wc"][0] + 1], drv[0:64, 20:21],
                       dst=(wcac[:, 0, :], "wc"))
            AC = mixed(13, C_RW + 1600, 64, pk[0:64, l, PK["mu_ac"][0]:PK["mu_ac"][0] + 1], drv[0:64, 21:22],
                       dst=(wcac[:, 1, :], "ac"))
            act(wcacb[:, 0, :], WC[0][0:64, :], AF.Tanh, ["wc"], ["wcb"])
            cp(wcacb[:, 1, :], AC[0][0:64, :], ["ac"], ["acb"])
            ops = []
            BON = []
            for ct in range(4):
                csl = slice(ct * 128, (ct + 1) * 128)
                Kx, Kxk = mixed(4 + ct, C_RW + 512 + ct * 128, 128, PKc(l, "mu", 4 + ct), drv[:, 12 + ct:13 + ct])
                kk, kkk = fsa()
                tsc(kk[:, 0:TS], Kx[:, :], PKc(l, "k_k", ct), None, ALU.mult, None, [Kxk, "pk"], [kkk])
                sq, sqk = fsa()
                act(sq[:, 0:TS], kk[:, 0:TS], AF.Square, [kkk], [sqk])
                b3, b3k = bank()
                mm(b3[:, :], CS("bd"), sq[:, 0:TS], True, True, ["cst", sqk], [b3k])
                rsq(sq[:, 0:TS], b3[:, :], 1.0, 1e-24, [b3k], [sqk])
                tt(kk[:, 0:TS], kk[:, 0:TS], sq[:, 0:TS], ALU.mult, [kkk, sqk], [kkk])
                b, bk = bank()
                mm(b[:, :], wup[:, csl], wcacb[:, 0, :], True, True, ["wup", "wcb"], [bk])
                e1, e1k = fsa()
                act(e1[:, 0:TS], b[:, :], AF.Exp, [bk, "drv"], [e1k], bias=drv[:, 22 + ct:23 + ct], scale=-1.0)
                act(e1[:, 0:TS], e1[:, 0:TS], AF.Ln, [e1k], [e1k], bias=1.0)
                act(e1[:, 0:TS], e1[:, 0:TS], AF.Exp, [e1k], [e1k], bias=-0.5, scale=-1.0)
                cumsum(ct, e1[:, 0:TS], e1k)
                gk = "gn%d" % ct
                tsc(nref[:, ct, 0:NCH], gnb[ct][:, 0:TS:C], -1.0, None, ALU.mult, None, [gk], ["nref"])
                b2, b2k = bank()
                mm(b2[:, :], aup[:, csl], wcacb[:, 1, :], True, True, ["aup", "acb"], [b2k])
                sg, sgk = fsa()
                act(sg[:, 0:TS], b2[:, :], AF.Sigmoid, [b2k, "pk"], [sgk], bias=PKc(l, "a0", ct))
                t1, t1k = fsa()
                tsc(t1[:, 0:TS], sg[:, 0:TS], PKc(l, "k_a", ct), drv[:, 26 + ct:27 + ct], ALU.mult, ALU.add,
                    [sgk, "pk", "drv"], [t1k])
                tt(Kx[:, :], Kx[:, :], t1[:, 0:TS], ALU.mult, [Kxk, t1k], [Kxk])
                tt(sg[:, 0:TS], kk[:, 0:TS], sg[:, 0:TS], ALU.mult, [kkk, sgk], [sgk])
                ei, eik = fsa(); ee, eek = fsa(); ev, evk = fsa()
                for c in range(NCH):
                    c0 = c * C
                    act(ei[:, c0:c0 + C], gnb[ct][:, c0 + 1:c0 + C + 1], AF.Exp, [gk], [eik], bias=gnb[ct][:, c0:c0 + 1], scale=-1.0)
                    act(ee[:, c0:c0 + C], gnb[ct][:, c0:c0 + C], AF.Exp, [gk], [eek], bias=gnb[ct][:, c0:c0 + 1], scale=-1.0)
                    act(ev[:, c0:c0 + C], gnb[ct][:, c0 + 1:c0 + C + 1], AF.Exp, [gk, "nref"], [evk],
                        bias=nref[:, ct, c:c + 1], scale=1.0)
                rt, at, btl, ktl, vb = [BLk(ct * 5 + j) for j in range(5)]
                stt(at[0][:, :], kk[:, 0:TS], -1.0, ee[:, 0:TS], ALU.mult, ALU.mult, [kkk, eek], [at[1]])
                tt(btl[0][:, :], sg[:, 0:TS], ev[:, 0:TS], ALU.mult, [sgk, evk], [btl[1]])
                tt(ktl[0][:, :], Kx[:, :], ev[:, 0:TS], ALU.mult, [Kxk, evk], [ktl[1]])
                cp(gam[:, ct, 0:NCH], ei[:, C - 1:TS:C], [eik], ["gam"])
                Rx, Rxk = mixed(ct, C_RW + ct * 128, 128, PKc(l, "mu", ct), drv[:, 8 + ct:9 + ct])
                tt(rt[0][:, :], Rx[:, :], ei[:, 0:TS], ALU.mult, [Rxk, eik], [rt[1]])
                rk, rkk = fsa()
                stt(rk[:, 0:TS], Rx[:, :], PKc(l, "r_k", ct), Kx[:, :], ALU.mult, ALU.mult, [Rxk, "pk", Kxk], [rkk])
                b4, b4k = bank()
                mm(b4[:, :], CS("bd"), rk[:, 0:TS], True, True, ["cst", rkk], [b4k])
                Vx, Vxk = mixed(8 + ct, C_RW + 1024 + ct * 128, 128, PKc(l, "mu", 8 + ct), drv[:, 16 + ct:17 + ct])
                cp(vb[0][:, :], Vx[:, :], [Vxk], [vb[1]])
                tt(LL[ct][:, :], b4[:, :], Vx[:, :], ALU.mult, [b4k, Vxk], ["ll%d" % ct])
                BON.append(LLk(ct))
                ops.append(dict(r=rt, a=at, b=btl, k=ktl, v=vb))

            hd = lambda h: (h // 2, (h % 2) * 64)
            SH = {}
            mhb = merged[:, 4:8, :].bitcast(BF16)
            for ct in range(4):
                for ni_, nm in enumerate(("r", "a", "b", "k")):
                    j = ct * 4 + ni_
                    if j < 8:
                        dst, dkey = mhb[:, j // 2, (j % 2) * 512:(j % 2 + 1) * 512], "mg%d" % (4 + j // 2)
                    elif j < 12:
                        dst, dkey = vtok[:, j - 8, :], "vtok%d" % (j - 8)
                    else:
                        dst, dkey = ktok[:, j - 12, :], "ktok%d" % (j - 12)
                    shift_hi(ops[ct][nm][0][:, :], ops[ct][nm][1], dst[0:64, :], dkey, TS)
                    SH[(ct, nm)] = (dst, dkey)
            build_gamh(4, NCH)

            def opn(nm, h):
                ct, par = h // 2, h % 2
                if par == 0:
                    return ops[ct][nm][0][0:64, :], ops[ct][nm][1]
                return SH[(ct, nm)][0][0:64, :], SH[(ct, nm)][1]

            llb = [LL[4 + i][:, :].bitcast(BF16) for i in range(4)]
            SETB = ("vt", "bt", "kt", "Aak", "Arb", "Ark", "Yf")
            rvs = [dict(rv), dict(rv)]
            rvk = [{n: "rv_" + n for n in rv}, {n: "rv_" + n for n in rv}]
            for i, n in enumerate(SETB):
                rvs[1][n] = llb[i // 2][0:64, (i % 2) * 512:(i % 2 + 1) * 512]
                rvk[1][n] = "ll%d" % (4 + i // 2)

            def phaseA(c):
                R, RK = rvs[c % 2], rvk[c % 2]
                c0 = c * C
                cs = slice(c0, c0 + C)
                for nm, key in (("v", "vt"), ("b", "bt"), ("k", "kt")):
                    bt_, btk2 = bank()
                    btb = bt_[:, :].bitcast(BF16)
                    for ct in range(4):
                        tr(btb[0:64, ct * 128:(ct + 1) * 128], ops[ct][nm][0][:, cs], CB("ident"),
                           [ops[ct][nm][1], "cstb"], [btk2])
                    cp(R[key][:, :], btb[0:64, 0:512], [btk2], [RK[key]])
                yield

                def scores(lhs, rhs, dst, maskn, eng):
                    b_, bk_ = bank()
                    for h in range(8):
                        L_, R_ = opn(lhs, h), opn(rhs, h)
                        mm(b_[0:64, h * 64:(h + 1) * 64], L_[0][:, cs], R_[0][:, cs], True, True, [L_[1], R_[1]], [bk_])
                    tt(R[dst][:, :], b_[0:64, :], CB(maskn)[0:64, :], ALU.mult, [bk_, "cstb"], [RK[dst]])
                scores("a", "b", "N", "r_ts", "dve")
                scores("b", "a", "NT", "r_st", "dve")
                yield
                scores("k", "a", "Aak", "r_st", "dve")
                scores("b", "r", "Arb", "r_sti", "dve")
                scores("k", "r", "Ark", "r_sti", "dve")
                tt(R["Y"][:, :], R["NT"][:, :], CB("i8")[0:64, :], ALU.add, [RK["NT"], "cstb"], [RK["Y"]])
                yield
                M, MT, Y = "N", "NT", "Y"
                M2, MT2, Y2 = "N2", "NT2", "Y2"
                for lev in range(5):
                    last = lev == 4
                    ba, bak = bank()
                    for h in range(8):
                        hs = slice(h * 64, (h + 1) * 64)
                        mm(ba[0:64, hs], R[MT][:, hs], R[M][:, hs], True, True, [RK[MT], RK[M]], [bak])
                    act(R[M2][:, :], ba[0:64, :], AF.Copy, [bak], [RK[M2]])
                    if not last:
                        bb_, bbk_ = bank()
                        for h in range(8):
                            hs = slice(h * 64, (h + 1) * 64)
                            mm(bb_[0:64, hs], R[M][:, hs], R[MT][:, hs], True, True, [RK[MT], RK[M]], [bbk_])
                        cp(R[MT2][:, :], bb_[0:64, :], [bbk_], [RK[MT2]])
                    bc, bck = bank()
                    for h in range(8):
                        hs = slice(h * 64, (h + 1) * 64)
                        mm(bc[0:64, hs], R[M2][:, hs], R[Y][:, hs], True, True, [RK[M2], RK[Y]], [bck])
                    Yd = "Yf" if last else Y2
                    tt(R[Yd][:, :], bc[0:64, :], R[Y][:, :], ALU.add, [bck, RK[Y]], [RK[Yd]])
                    M, M2 = M2, M
                    MT, MT2 = MT2, MT
                    Y, Y2 = Y2, Y
                    yield

            def phaseB(c):
                R, RK = rvs[c % 2], rvk[c % 2]
                c0 = c * C
                cs = slice(c0, c0 + C)
                bx, bxk = bank()
                for h in range(8):
                    hs = slice(h * 64, (h + 1) * 64)
                    A_ = opn("a", h)
                    mm(bx[0:64, hs], A_[0][:, cs], rw_sb[:, h, :], True, False, [A_[1], "rw_sb"], [bxk])
                    mm(bx[0:64, hs], R["Aak"][:, hs], R["vt"][:, hs], False, True, [RK["Aak"], RK["vt"]], [bxk])
                cp(R["X"][:, :], bx[0:64, :], [bxk], [RK["X"]])
                yield
                bu, buk = bank()
                for h in range(8):
                    hs = slice(h * 64, (h + 1) * 64)
                    mm(bu[0:64, hs], R["Yf"][:, hs], R["X"][:, hs], True, True, [RK["Yf"], RK["X"]], [buk])
                act(R["U"][:, :], bu[0:64, :], AF.Copy, [buk], [RK["U"]])
                yield
                bs_, bsk = bank()
                for h in range(8):
                    hs = slice(h * 64, (h + 1) * 64)
                    mm(bs_[0:64, hs], R["bt"][:, hs], R["U"][:, hs], True, False, [RK["bt"], RK["U"]], [bsk])
                    mm(bs_[0:64, hs], R["kt"][:, hs], R["vt"][:, hs], False, True, [RK["kt"], RK["vt"]], [bsk])
                by, byk = bank()
                for h in range(8):
                    ct, pb = hd(h)
                    hs = slice(h * 64, (h + 1) * 64)
                    o_ = by[pb:pb + 64, ct * 64:(ct + 1) * 64]
                    R_ = opn("r", h)
                    mm(o_, rw_sb[:, h, :], R_[0][:, cs], True, False, ["rw_sb", R_[1]], [byk])
                    mm(o_, R["U"][:, hs], R["Arb"][:, hs], False, False, [RK["U"], RK["Arb"]], [byk])
                    mm(o_, R["vt"][:, hs], R["Ark"][:, hs], False, True, [RK["vt"], RK["Ark"]], [byk])
                tmp_, tmpk = fsa()
                tv = tmp_[0:64, 0:512].rearrange("p (h v) -> p h v", h=8)
                tt(tv, bs_[0:64, :].rearrange("p (h v) -> p h v", h=8), rw_sf[:, :, :], ALU.add, [bsk, "rw_sf"], [tmpk])
                gb = gamh[0:64, 0:8, c:c + 1].to_broadcast([64, 8, 64])
                tt(rw_sb[:, :, :], tv, gb, ALU.mult, [tmpk, "gamh"], ["rw_sb"])
                tt(rw_sf[:, :, :], tv, gb, ALU.mult, [tmpk, "gamh"], ["rw_sf"])
                act(yT[:, 0:4, cs], by[:, 0:256].rearrange("p (c t) -> p c t", c=4), AF.Copy, [byk],
                    ["mg0", "mg1", "mg2", "mg3"])
                yield

            drive([phaseA(0)])
            for c in range(NCH):
                drive([phaseB(c), phaseA(c + 1) if c + 1 < NCH else None])
            for ct in range(4):
                mk = "mg%d" % ct
                b, bk = bank()
                mm(b[:, :], CS("bd"), yT[:, ct, :], True, True, ["cst", mk], [bk])
                d, dk_ = fsa()
                stt(d[:, 0:TS], b[:, :], -1.0 / 64.0, yT[:, ct, :], ALU.mult, ALU.add, [bk, mk], [dk_])
                sq, sqk = fsa()
                act(sq[:, 0:TS], d[:, 0:TS], AF.Square, [dk_], [sqk])
                b2, b2k = bank()
                mm(b2[:, :], CS("bd"), sq[:, 0:TS], True, True, ["cst", sqk], [b2k])
                rsq(sq[:, 0:TS], b2[:, :], 1.0 / 64.0, 64e-5, [b2k], [sqk])
                tt(d[:, 0:TS], d[:, 0:TS], sq[:, 0:TS], ALU.mult, [dk_, sqk], [dk_])
                act(d[:, 0:TS], d[:, 0:TS], AF.Identity, [dk_, "pk"], [dk_], bias=PKc(l, "ln_b", ct), scale=PKc(l, "ln_g", ct))
                tt(d[:, 0:TS], d[:, 0:TS], BON[ct][0][:, :], ALU.add, [dk_, BON[ct][1]], [dk_])
                pz, pzk = proj(l, C_RWZ + ct * 128, 128)
                z, zk = fsa()
                act(z[:, 0:TS], pz[:, :], AF.Silu, [pzk], [zk])
                tt(ys[:, ct, :], d[:, 0:TS], z[:, 0:TS], ALU.mult, [dk_, zk], ["ys"])

        outs = program()
        S.emit(final_wait_ops=outs)
    return nc


_NC_CACHE = {}


def make_in_maps(inp, n_cores=8):
    f = lambda a: np.ascontiguousarray(np.asarray(a, dtype=np.float32))
    pkall = np.stack([_pack_params(inp, l) for l in range(DEPTH)], 0)
    fgT = np.ascontiguousarray(f(inp["final_g"]).reshape(8, 128).T)
    shared = {
        "pk": pkall, "fg": fgT, "cst": CONSTS, "cstf": CSTF,
        "ada_w": f(inp["ada_w"]), "w_in": f(inp["w_in"]), "rw_w_up": f(inp["rw_w_up"]), "rw_a_up": f(inp["rw_a_up"]),
        "gla_gk_up": f(inp["gla_gk_up"]), "w_branch": f(inp["w_branch"]), "w_out": f(inp["w_out"]),
    }
    maps = []
    for b in range(n_cores):
        m = dict(shared)
        m["xT"] = np.ascontiguousarray(f(inp["x"][b]).T)
        m["cT"] = np.ascontiguousarray(f(inp["c"][b]).reshape(8, 128).T)
        maps.append(m)
    return maps


def kernel(**inputs):
    inp = {k: np.asarray(v) for k, v in inputs.items()}
    if "full" not in _NC_CACHE:
        _NC_CACHE["full"] = build_nc()
    nc = _NC_CACHE["full"]
    maps = make_in_maps(inp)
    res = run_bass_kernel_spmd(nc, maps, core_ids=list(range(8)))
    out = np.stack([np.ascontiguousarray(r["outT"].T) for r in res.results], 0)
    return out.astype(np.float32)
```

```python
import contextlib
import numpy as np
import ml_dtypes
import concourse.bass as bass
import concourse.mybir as mybir
from concourse.bass_utils import run_bass_kernel_spmd

F32 = mybir.dt.float32
BF16 = mybir.dt.bfloat16
AF = mybir.ActivationFunctionType
ALU = mybir.AluOpType

D = 1024
SEQ = 2048
DEPTH = 4
NCOLS = 11416
TS = 512
NST = SEQ // TS
ENGS = ("pe", "dve", "act", "pool", "sp")

C_RW = 0; C_RWZ = 1664
C_GQ = 2176; C_GK = 2432; C_GV = 2688; C_GC = 3200; C_GZ = 3216
C_MQK = 3728; C_MV = 4240; C_MI = 4752; C_MF = 4756; C_MZ = 4760
C_HQ = 5272; C_HF = 5784; C_HI = 6296; C_HZ = 6808
C_MG = 7320

PK = {}
_o = 0
for _n, _w in (("norm_g", 8), ("ada_b", 24), ("mu", 12), ("mu_wc", 1), ("mu_ac", 1), ("w0", 4), ("a0", 4),
               ("k_k", 4), ("k_a", 4), ("r_k", 4), ("ln_g", 4), ("ln_b", 4), ("gk_b", 2), ("gla_g", 4),
               ("conv", 16), ("ml_g", 4), ("lb", 16), ("hg_g", 4), ("i_b", 2), ("f_b", 2)):
    PK[_n] = (_o, _w)
    _o += _w
NPK = _o


class Op:
    __slots__ = ("eng", "fn", "deps", "idx", "dma_stream", "signal", "cnt")

    def __init__(self, eng, fn):
        self.eng = eng
        self.fn = fn
        self.deps = []
        self.dma_stream = None
        self.signal = False
        self.cnt = 0


class Sched:
    def __init__(self, nc):
        self.nc = nc
        self.ops = []
        self.last_w = {}
        self.readers = {}
        self.streams = {}
        self.record = False

    def op(self, eng, fn, reads=(), writes=(), dma=None):
        if self.record:
            return None
        o = Op(eng, fn)
        o.idx = len(self.ops)
        deps = {}
        for k in reads:
            w = self.last_w.get(k)
            if w is not None:
                deps[w.idx] = w
        for k in writes:
            w = self.last_w.get(k)
            if w is not None:
                deps[w.idx] = w
            for r in self.readers.get(k, ()):
                deps[r.idx] = r
        if dma is not None:
            o.dma_stream = dma
            n = self.streams.get(dma, 0) + 1
            self.streams[dma] = n
            o.cnt = n
        o.deps = [deps[i] for i in sorted(deps)]
        for k in reads:
            self.readers.setdefault(k, []).append(o)
        for k in writes:
            self.last_w[k] = o
            self.readers[k] = []
        self.ops.append(o)
        return o

    def emit(self, final_wait_ops=()):
        nc = self.nc
        for o in self.ops:
            nd = []
            for d in o.deps:
                if d.dma_stream is None and o.dma_stream is None and d.eng == o.eng and o.eng == "pe":
                    continue
                nd.append(d)
            o.deps = nd
            for d in nd:
                d.signal = True
        for o in final_wait_ops:
            o.signal = True
        cnt = {e: 0 for e in ENGS}
        for o in self.ops:
            if o.dma_stream is None and o.signal:
                cnt[o.eng] += 1
                o.cnt = cnt[o.eng]
        EP, DEP = 10 ** 9, 10 ** 9
        with contextlib.ExitStack() as es:
            esem = {}
            for e in ENGS:
                for ep in range(max(1, (cnt[e] + EP - 1) // EP)):
                    esem[(e, ep)] = es.enter_context(nc.semaphore("c_%s%d" % (e, ep)))
            ssem = {}
            for i, (s_, n_) in enumerate(self.streams.items()):
                for ep in range(max(1, (n_ + DEP - 1) // DEP)):
                    ssem[(s_, ep)] = es.enter_context(nc.semaphore("d%d_%d" % (i, ep)))
            block = es.enter_context(nc.Block())
            per = {e: [o for o in self.ops if o.eng == e] for e in ENGS}

            def semval(d):
                if d.dma_stream is not None:
                    return ssem[(d.dma_stream, (d.cnt - 1) // DEP)], 16 * ((d.cnt - 1) % DEP + 1)
                return esem[(d.eng, (d.cnt - 1) // EP)], (d.cnt - 1) % EP + 1

            def runner(e):
                def body(engine):
                    waited = {}
                    for o in per[e]:
                        for d in o.deps:
                            sem, val = semval(d)
                            key = id(sem)
                            if waited.get(key, 0) >= val:
                                continue
                            waited[key] = val
                            engine.wait_ge(sem, val)
                        ins = o.fn(engine)
                        if o.dma_stream is not None:
                            ins.then_inc(semval(o)[0], 16)
                        elif o.signal:
                            ins.then_inc(semval(o)[0], 1)
                    if e == "sp":
                        for o in final_wait_ops:
                            sem, val = semval(o)
                            engine.wait_ge(sem, val)
                return body

            block.tensor(runner("pe"))
            block.vector(runner("dve"))
            block.scalar(runner("act"))
            block.gpsimd(runner("pool"))
            block.sync(runner("sp"))
        return cnt


def _host_consts():
    c = {}
    i128 = np.eye(128, dtype=np.float32)
    c["ident"] = i128
    s = np.arange(128)[:, None]
    t = np.arange(128)[None, :]
    m_incl = (s <= t).astype(np.float32)
    m_blk = ((s <= t) & ((s // 64) == (t // 64))).astype(np.float32)
    c["m128"] = np.tile(m_incl[:, None, :], (1, 4, 1)).reshape(128, 512)
    c["m64b"] = np.tile(m_blk[:, None, :], (1, 4, 1)).reshape(128, 512)
    m_b32 = ((s <= t) & ((s // 32) == (t // 32))).astype(np.float32)
    c["m32b"] = np.tile(m_b32[:, None, :], (1, 4, 1)).reshape(128, 512)
    m96 = np.zeros((128, 128), np.float32); m96[96:] = 1.0
    c["m96"] = m96
    s6 = np.arange(64)[:, None]
    t6 = np.arange(64)[None, :]
    z = np.zeros((128, 512), np.float32)
    a = z.copy(); a[:64] = np.tile((t6 < s6).astype(np.float32)[:, None, :], (1, 8, 1)).reshape(64, 512)
    c["r_ts"] = a
    a = z.copy(); a[:64] = np.tile((s6 < t6).astype(np.float32)[:, None, :], (1, 8, 1)).reshape(64, 512)
    c["r_st"] = a
    a = z.copy(); a[:64] = np.tile((s6 <= t6).astype(np.float32)[:, None, :], (1, 8, 1)).reshape(64, 512)
    c["r_sti"] = a
    a = z.copy(); a[:64] = np.tile(np.eye(64, dtype=np.float32)[:, None, :], (1, 8, 1)).reshape(64, 512)
    c["i8"] = a
    c["ones"] = np.ones((128, 128), np.float32)
    bd = np.zeros((128, 128), np.float32); bd[:64, :64] = 1; bd[64:, 64:] = 1
    c["bd"] = bd
    names = ["ident", "m128", "m32b", "r_ts", "r_st", "r_sti", "i8", "ones", "bd", "m96"]
    offs = {}
    o = 0
    for n in names:
        offs[n] = (o, c[n].shape[1]); o += c[n].shape[1]
    return np.concatenate([c[n] for n in names], axis=1), offs


CONSTS, COFF = _host_consts()
NCONST = CONSTS.shape[1]
CFO = {"ident": 0, "ones": 128, "bd": 256, "m96": 384}
NCF = 512
CSTF = np.concatenate([CONSTS[:, COFF[n][0]:COFF[n][0] + 128] for n in ("ident", "ones", "bd", "m96")], axis=1)


def _pack_params(inp, l):
    P = np.zeros((128, NPK), np.float32)

    def put(name, arr):
        o, w = PK[name]
        P[:arr.shape[0], o:o + arr.shape[1]] = arr

    fm = lambda v: np.ascontiguousarray(v.reshape(-1, 128).T)
    put("norm_g", fm(inp["norm_g"][l]))
    put("ada_b", fm(inp["ada_b"][l]))
    mu = inp["rw_mu"][l]
    put("mu", fm(mu[:1536]))
    put("mu_wc", mu[1536:1600].reshape(64, 1))
    put("mu_ac", mu[1600:1664].reshape(64, 1))
    put("w0", fm(inp["rw_w0"][l])); put("a0", fm(inp["rw_a0"][l]))
    put("k_k", fm(inp["rw_k_k"][l])); put("k_a", fm(inp["rw_k_a"][l])); put("r_k", fm(inp["rw_r_k"][l]))
    put("ln_g", fm(inp["rw_ln_g"][l])); put("ln_b", fm(inp["rw_ln_b"][l]))
    put("gk_b", fm(inp["gla_gk_b"][l])); put("gla_g", fm(inp["gla_norm_g"][l]))
    cw = inp["ml_conv_w"][l]
    put("conv", np.concatenate([fm(cw[j]) for j in range(4)], axis=1))
    put("ml_g", fm(inp["ml_norm_g"][l]))
    put("lb", np.concatenate([fm(inp["hg_lb_logits"][j]) for j in range(4)], axis=1))
    put("hg_g", fm(inp["hg_norm_g"][l]))
    put("i_b", fm(np.repeat(inp["ml_i_b"][l], 64)))
    put("f_b", fm(np.repeat(inp["ml_f_b"][l], 64)))
    return P


def build_nc(n_layers=DEPTH, branches=(0, 1, 2, 3), debug=False, stage=99):
    nc = bass.Bass("TRN2", target_bir_lowering=False)
    dram = lambda n, s, k="ExternalInput": nc.dram_tensor(n, list(s), F32, kind=k).ap()
    xT_d = dram("xT", [D, SEQ])
    c_d = dram("cT", [128, 8])
    pk_d = dram("pk", [DEPTH, 128, NPK])
    fg_d = dram("fg", [128, 8])
    cst_d = dram("cst", [128, NCONST])
    cstf_d = dram("cstf", [128, NCF])
    adaw_d = dram("ada_w", [DEPTH, D, 3 * D])
    win_d = dram("w_in", [DEPTH, D, NCOLS])
    wup_d = dram("rw_w_up", [DEPTH, 64, 512])
    aup_d = dram("rw_a_up", [DEPTH, 64, 512])
    gup_d = dram("gla_gk_up", [DEPTH, 16, 256])
    wbr_d = dram("w_branch", [DEPTH, 4, 512, D])
    wout_d = dram("w_out", [DEPTH, D, D])
    out_d = dram("outT", [D, SEQ], "ExternalOutput")
    dbg_d = dram("dbg", [4, 512, SEQ], "ExternalOutput") if debug else None

    es = contextlib.ExitStack()
    with es:
        T = lambda n, s, d=F32: es.enter_context(nc.sbuf_tensor("s_" + n, list(s), d))
        x = T("x", [128, 8, TS])
        uT = T("uT", [128, 8, TS], BF16)
        merged = T("merged", [128, 8, TS])
        mergedb = uT
        ys = T("ys", [128, 4, TS], BF16)
        pk = T("pk", [128, DEPTH, NPK])
        drv = T("drv", [128, 64])
        cst = T("cst", [128, NCF])
        cstb = T("cstb", [128, NCONST], BF16)
        cT = T("cT", [128, 8])
        fg = T("fg", [128, 8])
        mod = T("mod", [128, 24])
        lbt = T("lbt", [128, 16]); lbe = T("lbe", [128, 16]); lbs = T("lbs", [128, 4])
        NWB = 6
        wb = [T("wb%d" % i, [128, 8, 128], BF16) for i in range(NWB)]
        wv = [T("wv%d" % i, [128, 8, 512], BF16) for i in range(1)]
        wup = T("wup", [64, 512], BF16); aup = T("aup", [64, 512], BF16); gup = T("gup", [16, 256], BF16)
        NF = 14
        mtl = [T("mt%d" % i, [128, TS]) for i in range(2)]
        fs = [T("fs%d" % i, [128, TS + 4]) for i in range(NF)]
        LL = [T("ll%d" % i, [128, TS]) for i in range(8)]
        NB = 5
        bs = [T("bs%d" % i, [128, TS], BF16) for i in range(NB)]
        BL = [T("bl%d" % i, [128, TS], BF16) for i in range(20)]
        prc = T("prc", [128, 16])
        cqc = T("cqc", [128, 4, 4])
        gnb = [T("gn%d" % i, [128, TS + 1]) for i in range(4)]
        nref = T("nref", [128, 4, 16])
        ktz = T("ktz", [128, 2, 512], BF16)
        vtok = T("vtok", [128, 4, 512], BF16)
        ktok = T("ktok", [128, 4, 512], BF16)
        rv = {n: T("rv_" + n, [64, 512], BF16) for n in
              ("vt", "bt", "kt", "N", "NT", "N2", "NT2", "Y", "Y2", "Aak", "Arb", "Ark", "X", "U", "Yf")}
        rw_sf = T("rw_sf", [64, 8, 64]); rw_sb = T("rw_sb", [64, 8, 64], BF16)
        la_sf = [T("la_sf%d" % i, [128, 4, 128]) for i in range(4)]
        la_sb = [T("la_sb%d" % i, [128, 4, 128], BF16) for i in range(4)]
        gamh = T("gamh", [64, 8, 16])
        gam = T("gam", [128, 4, 16])
        yT = merged
        wcac = T("wcac", [64, 2, TS])
        wcacb = T("wcacb", [64, 2, TS], BF16)
        ps = [es.enter_context(nc.psum_tensor("ps%d" % i, [128, 512], F32)) for i in range(8)]

        S = Sched(nc)
        st = {"bank": 0, "w": 0, "fsi": 0, "bsi": 0, "wai": 0}

        def bank():
            i = st["bank"]; st["bank"] = (i + 1) % 6
            return ps[i], "ps%d" % i

        CS = lambda n: cst[:, CFO[n]:CFO[n] + 128]
        CB = lambda n: cstb[:, COFF[n][0]:COFF[n][0] + COFF[n][1]]
        PKc = lambda l, n, j=0, w=1: pk[:, l, PK[n][0] + j:PK[n][0] + j + w]

        def mm(out, lhsT, rhs, start, stop, r, w):
            S.op("pe", lambda e: e.matmul(out, lhsT, rhs, start=start, stop=stop), reads=r, writes=w)

        def tr(out, in_, ident, r, w):
            S.op("pe", lambda e: e.transpose(out, in_, ident), reads=r, writes=w)

        def act(out, in_, func, r, w, bias=None, scale=None):
            kw = {}
            if bias is not None:
                kw["bias"] = bias
            if scale is not None:
                kw["scale"] = scale
            S.op("act", lambda e: e.activation(out=out, in_=in_, func=func, **kw), reads=r, writes=w)

        def tt(out, a, b, op, r, w, eng="dve"):
            S.op(eng, lambda e: e.tensor_tensor(out=out, in0=a, in1=b, op=op), reads=r, writes=w)

        def tsc(out, a, s1, s2, op0, op1, r, w, eng="dve"):
            if op1 is None:
                S.op(eng, lambda e: e.tensor_scalar(out=out, in0=a, scalar1=s1, scalar2=None, op0=op0), reads=r, writes=w)
            else:
                S.op(eng, lambda e: e.tensor_scalar(out=out, in0=a, scalar1=s1, scalar2=s2, op0=op0, op1=op1),
                     reads=r, writes=w)

        def stt(out, a, sc, b, op0, op1, r, w, eng="dve"):
            S.op(eng, lambda e: e.scalar_tensor_tensor(out=out, in0=a, scalar=sc, in1=b, op0=op0, op1=op1),
                 reads=r, writes=w)

        def cp(out, in_, r, w, eng="dve"):
            S.op(eng, lambda e: e.tensor_copy(out=out, in_=in_), reads=r, writes=w)

        def rsq(out, in_, scale, bias, r, w):
            act(out, in_, AF.Ln, r, w, bias=bias, scale=scale)
            act(out, out, AF.Exp, w, w, scale=-0.5)

        def recip(out, in_, r, w):
            S.op("dve", lambda e: e.reciprocal(out=out, in_=in_), reads=r, writes=w)

        def mset(ap, val, w):
            S.op("dve", lambda e: e.memset(ap, val), writes=w)

        def dma(eng, out, in_, r, w, stream):
            return S.op(eng, lambda e: e.dma_start(out=out, in_=in_), reads=r, writes=w, dma=stream)

        def cumsum(ct, src, srck):
            S.op("dve", lambda e: e.tensor_tensor_scan(out=gnb[ct][:, 1:TS + 1], data0=src, data1=src, initial=0.0,
                                                       op0=ALU.add, op1=ALU.max), reads=[srck], writes=["gn%d" % ct])

        def wchunk(src, n, kc=8):
            i = st["w"]; st["w"] = (i + 1) % NWB
            key = "wb%d" % i
            dma("pool", wb[i][:, 0:kc, 0:n], src.rearrange("(k p) c -> p k c", p=128), [], [key], key)
            return wb[i], key

        def proj(l, c0, n):
            w, wk = wchunk(win_d[l, :, c0:c0 + n], n)
            b, bk = bank()
            for k in range(8):
                mm(b[0:n, :], w[:, k, 0:n], uT[:, k, :], k == 0, k == 7, [wk, "uT"], [bk])
            return b, bk

        def proj_tok(l, c0):
            for h in range(2):
                dma("pool", wv[0][:, :, h * 256:(h + 1) * 256],
                    win_d[l, :, c0 + h * 256:c0 + (h + 1) * 256].rearrange("(k p) c -> p k c", p=128),
                    [], ["wv_%d" % h], "wv_%d" % h)
            for tI in range(TS // 128):
                b, bk = bank()
                for k in range(8):
                    mm(b[:, :], uT[:, k, tI * 128:(tI + 1) * 128], wv[0][:, k, :], k == 0, k == 7,
                       ["wv_0", "wv_1", "uT"], [bk])
                if tI % 2 == 0:
                    act(vtok[:, tI, :], b[:, :], AF.Copy, [bk], ["vtok%d" % tI])
                else:
                    cp(vtok[:, tI, :], b[:, :], [bk], ["vtok%d" % tI])

        def fsa():
            i = st["fsi"]; st["fsi"] = (i + 1) % NF
            return fs[i], "fs%d" % i

        def bsa():
            i = st["bsi"]; st["bsi"] = (i + 1) % NB
            return bs[i], "bs%d" % i

        XK = ["x%d" % k for k in range(8)]

        def rms_to(sl_src, emit):
            b, bk = bank()
            for k in range(8):
                sq, sqk = fsa()
                act(sq[:, 0:TS], x[:, k, :], AF.Square, [XK[k]], [sqk])
                mm(b[:, :], CS("ones"), sq[:, 0:TS], k == 0, k == 7, [sqk, "cst"], [bk])
            rs, rsk = fsa()
            rsq(rs[:, 0:TS], b[:, :], 1.0, 1024.0 * 1e-6, [bk], [rsk])
            for k in range(8):
                t1, t1k = fsa()
                tt(t1[:, 0:TS], x[:, k, :], rs[:, 0:TS], ALU.mult, [XK[k], rsk], [t1k])
                emit(k, t1, t1k)

        def program():
            dma("sp", cst[:, :], cstf_d, [], ["cst"], "cst")
            dma("pool", cstb[:, :], cst_d, [], ["cstb"], "cstb")
            dma("sp", pk[:, :, :], pk_d.rearrange("l p n -> p l n"), [], ["pk"], "pk")
            dma("sp", cT[:, :], c_d, [], ["cT"], "cT")
            dma("sp", fg[:, :], fg_d, [], ["fg"], "fg")
            act(cT[:, :], cT[:, :], AF.Silu, ["cT"], ["cT"])
            o_lb = PK["lb"][0]
            act(lbe[:, :], pk[:, 0, o_lb:o_lb + 16], AF.Exp, ["pk"], ["lbe"])
            tt(lbs[:, :], lbe[:, 0:4], lbe[:, 4:8], ALU.add, ["lbe"], ["lbs"])
            tt(lbs[:, :], lbs[:, :], lbe[:, 8:12], ALU.add, ["lbs", "lbe"], ["lbs"])
            tt(lbs[:, :], lbs[:, :], lbe[:, 12:16], ALU.add, ["lbs", "lbe"], ["lbs"])
            recip(lbs[:, :], lbs[:, :], ["lbs"], ["lbs"])
            mset(lbt[:, 0:4], 0.0, ["lbt"])
            for j in range(1, 4):
                t_, tk_ = fsa()
                tt(t_[:, 0:4], lbe[:, j * 4:(j + 1) * 4], lbs[:, :], ALU.mult, ["lbe", "lbs"], [tk_])
                tt(lbt[:, j * 4:(j + 1) * 4], lbt[:, (j - 1) * 4:j * 4], t_[:, 0:4], ALU.add, [tk_, "lbt"], ["lbt"])
            for i in range(4):
                mset(gnb[i][:, 0:1], 0.0, ["gn%d" % i])

            for l in range(n_layers):
                layer(l)

            outs = []
            for sI in range(NST):
                sl = slice(sI * TS, (sI + 1) * TS)
                for k in range(8):
                    dma("sp", x[:, k, :], (out_d if n_layers > 0 else xT_d)[k * 128:(k + 1) * 128, sl], ["xd%d" % sI], [XK[k]], "xl%d" % k)

                def emit(k, t1, t1k, sI=sI, sl=sl):
                    tsc(t1[:, 0:TS], t1[:, 0:TS], fg[:, k:k + 1], 32.0, ALU.mult, ALU.mult, [t1k, "fg"], [t1k])
                    outs.append(dma("sp", out_d[k * 128:(k + 1) * 128, sl], t1[:, 0:TS], [t1k], ["xd%d" % sI], "o_" + t1k))
                rms_to(sl, emit)
            return outs

        def layer(l):
            b, bk = bank()
            for j in range(24):
                i = st["wai"]; st["wai"] = (i + 1) % 3
                wa_i = merged[:, 2 * i:2 * i + 2, :].rearrange("p a (k c) -> p (a k) c", c=128)
                keys = ["mg%d" % (2 * i), "mg%d" % (2 * i + 1)]
                dma("sp", wa_i, adaw_d[l, :, j * 128:(j + 1) * 128].rearrange("(k p) c -> p k c", p=128),
                    [], keys, "wa%d" % i)
                for k in range(8):
                    mm(b[:, j:j + 1], wa_i[:, k, :], cT[:, k:k + 1], k == 0, k == 7, keys + ["cT"], [bk])
            tt(mod[:, :], b[:, 0:24], pk[:, l, PK["ada_b"][0]:PK["ada_b"][0] + 24], ALU.add, [bk, "pk"], ["mod"])
            stt(drv[:, 0:8], mod[:, 8:16], 1.0, pk[:, l, PK["norm_g"][0]:PK["norm_g"][0] + 8], ALU.add, ALU.mult,
                ["mod", "pk"], ["drv"])
            tsc(drv[:, 0:8], drv[:, 0:8], 32.0, None, ALU.mult, None, ["drv"], ["drv"])
            o_mu = PK["mu"][0]
            tsc(drv[:, 8:22], pk[:, l, o_mu:o_mu + 14], -1.0, 1.0, ALU.mult, ALU.add, ["pk"], ["drv"])
            tsc(drv[:, 22:26], PKc(l, "w0", 0, 4), -1.0, None, ALU.mult, None, ["pk"], ["drv"])
            tsc(drv[:, 26:30], PKc(l, "k_a", 0, 4), -1.0, 1.0, ALU.mult, ALU.add, ["pk"], ["drv"])
            tsc(drv[:, 30:32], PKc(l, "gk_b", 0, 2), -1.0, None, ALU.mult, None, ["pk"], ["drv"])
            tsc(drv[:, 32:34], PKc(l, "f_b", 0, 2), -1.0, None, ALU.mult, None, ["pk"], ["drv"])
            tsc(drv[:, 34:38], lbt[:, l * 4:(l + 1) * 4], -1.0, 1.0, ALU.mult, ALU.add, ["lbt"], ["drv"])
            tsc(drv[:, 38:42], PKc(l, "gla_g", 0, 4), float(np.sqrt(128.0)), None, ALU.mult, None, ["pk"], ["drv"])
            tsc(drv[:, 42:46], PKc(l, "hg_g", 0, 4), float(np.sqrt(128.0)), None, ALU.mult, None, ["pk"], ["drv"])
            dma("pool", wup[:, :], wup_d[l], [], ["wup"], "wup")
            dma("pool", aup[:, :], aup_d[l], [], ["aup"], "aup")
            dma("pool", gup[:, :], gup_d[l], [], ["gup"], "gup")
            mset(rw_sf[:, :, :], 0.0, ["rw_sf"])
            mset(rw_sb[:, :, :], 0.0, ["rw_sb"])
            for i in range(4):
                mset(la_sf[i][:, :, :], 0.0, ["la_sf%d" % i])
                mset(la_sb[i][:, :, :], 0.0, ["la_sb%d" % i])
            mset(prc[:, :], 0.0, ["prc"])
            mset(cqc[:, :, :], 0.0, ["cqc"])

            src_d = xT_d if l == 0 else out_d
            for sI in range(NST):
                sl = slice(sI * TS, (sI + 1) * TS)
                for k in range(8):
                    dma("sp", x[:, k, :], src_d[k * 128:(k + 1) * 128, sl], ["xd%d" % sI], [XK[k]], "xl%d" % k)

                def emit(k, t1, t1k):
                    act(uT[:, k, :], t1[:, 0:TS], AF.Identity, [t1k, "drv", "mod"], ["uT"],
                        bias=mod[:, k:k + 1], scale=drv[:, k:k + 1])
                rms_to(sl, emit)
                def merge_gen(m):
                    for fc in range(8):
                        pg, pgk = proj(l, C_MG + m * 1024 + fc * 128, 128)
                        sg, sgk = mtl[fc % 2], "mt%d" % (fc % 2)
                        act(sg[:, :], pg[:, :], AF.Sigmoid, [pgk], [sgk])
                        w, wk = wchunk(wbr_d[l, m, :, fc * 128:(fc + 1) * 128], 128, kc=4)
                        pb, pbk = bank()
                        for k in range(4):
                            mm(pb[:, :], w[:, k, :], ys[:, k, :], k == 0, k == 3, [wk, "ys"], [pbk])
                        if m == 0:
                            tt(merged[:, fc, :], pb[:, :], sg[:, :], ALU.mult, [pbk, sgk], ["mg%d" % fc])
                        else:
                            tt(sg[:, :], pb[:, :], sg[:, :], ALU.mult, [pbk, sgk], [sgk])
                            tt(merged[:, fc, :], merged[:, fc, :], sg[:, :], ALU.add, [sgk, "mg%d" % fc], ["mg%d" % fc])
                        yield

                def zero_gen():
                    mset(ys[:, :, :], 0.0, ["ys"])
                    yield

                g_prev = None
                for m in range(4):
                    gm = (rwkv, gla, mlstm, hgrn)[m](l, sI) if m in branches else zero_gen()
                    drive([gm, g_prev])
                    if debug and l == 0:
                        for k in range(4):
                            dma("pool", dbg_d[m, k * 128:(k + 1) * 128, sl], ys[:, k, :], ["ys"], [], "dbg%d" % k)
                    g_prev = merge_gen(m)
                drive([g_prev])
                for fc in range(8):
                    act(mergedb[:, fc, :], merged[:, fc, :], AF.Copy, ["mg%d" % fc], ["uT"])
                for fc in range(8):
                    w, wk = wchunk(wout_d[l, :, fc * 128:(fc + 1) * 128], 128)
                    pb, pbk = bank()
                    for k in range(8):
                        mm(pb[:, :], w[:, k, :], mergedb[:, k, :], k == 0, k == 7, [wk, "uT"], [pbk])
                    stt(x[:, fc, :], pb[:, :], mod[:, 16 + fc:17 + fc], x[:, fc, :], ALU.mult, ALU.add,
                        [pbk, "mod", XK[fc]], [XK[fc]])
                    dma("sp", out_d[fc * 128:(fc + 1) * 128, sl], x[:, fc, :], [XK[fc]], ["xd%d" % sI], "xs%d" % fc)

        def shift_hi(src_ap, skey, dst_ap, dkey, n, fp32=False):
            b, bk = bank()
            idn = (CS if fp32 else CB)("ident")
            mm(b[0:64, 0:n], idn[:, 64:128], src_ap, True, True, [skey, "cst" if fp32 else "cstb"], [bk])
            cp(dst_ap, b[0:64, 0:n], [bk], [dkey])

        def build_gamh(npair, nch):
            for ct in range(npair):
                cp(gamh[:, 2 * ct, 0:nch], gam[0:64, ct, 0:nch], ["gam"], ["gamh"])
                shift_hi(gam[:, ct, 0:nch], "gam", gamh[:, 2 * ct + 1, 0:nch], "gamh", nch, fp32=True)

        def drive(gens):
            gens = [g for g in gens if g is not None]
            while gens:
                for g in list(gens):
                    try:
                        next(g)
                    except StopIteration:
                        gens.remove(g)

        def la(Qh, Kh, KTp, C, dk, si, gm, gmk, fin, ni=None):
            mask = {128: CB("m128"), 32: CB("m32b")}[C]
            sfk, sbk = "la_sf%d" % si, "la_sb%d" % si
            nkt = len(KTp)
            PT = {}

            def phA(tI):
                tsl = slice(tI * 128, (tI + 1) * 128)
                bt, btk = bank()
                btb = bt[:, :].bitcast(BF16)
                for ct in range(nkt):
                    tr(btb[:, ct * 128:(ct + 1) * 128], KTp[ct][0][:, tsl], CB("ident"), [KTp[ct][1], "cstb"], [btk])
                cp(ktok[:, tI, 0:nkt * 128], btb[:, 0:nkt * 128], [btk], ["ktok%d" % tI])
                if C == 32:
                    tsc(ktz[64:128, tI % 2, 0:nkt * 128], ktok[64:128, tI, 0:nkt * 128], CS("m96")[64:128, 0:1], None,
                        ALU.mult, None, ["ktok%d" % tI, "cst"], ["ktz%d" % (tI % 2)])
                yield
                bsc, bsck = bank()
                for h in range(4):
                    mm(bsc[:, h * 128:(h + 1) * 128], Kh[h][0][:, tsl], Qh[h][0][:, tsl], True, True,
                       [Kh[h][1], Qh[h][1]], [bsck])
                pt, ptk = bsa()
                tt(pt[:, :], bsc[:, :], mask, ALU.mult, [bsck, "cstb"], [ptk])
                PT[tI] = (pt, ptk)
                yield

            def phB(tI):
                pt, ptk = PT[tI]
                po, pok = ps[6], "ps6"
                pd, pdk = (ps[7], "ps7") if ni is not None else (None, None)
                for cc in range(128 // C):
                    cidx = tI * (128 // C) + cc
                    csl = slice(cc * C, (cc + 1) * C)
                    gsl = slice(tI * 128 + cc * C, tI * 128 + (cc + 1) * C)
                    for h in range(4):
                        osl = slice(h * 128 + cc * C, h * 128 + (cc + 1) * C)
                        mm(po[:, osl], vtok[:, tI, h * 128:(h + 1) * 128], pt[:, osl], True, False,
                           ["vtok%d" % tI, ptk], [pok])
                        mm(po[:, osl], la_sb[si][0:dk, h, :], Qh[h][0][:, gsl], False, True, [sbk, Qh[h][1]], [pok])
                        if ni is not None:
                            mm(pd[:, osl], CB("ones"), pt[:, osl], True, False, ["cstb", ptk], [pdk])
                            mm(pd[:, osl], la_sb[ni][0:dk, h, :], Qh[h][0][:, gsl], False, True,
                               ["la_sb%d" % ni, Qh[h][1]], [pdk])
                    for sidx, isn in ((si, False),) + (((ni, True),) if ni is not None else ()):
                        bu, buk = bank()
                        for h in range(4):
                            if C == 32 and cc == 3:
                                zsl = slice(64, 128)
                                mm(bu[0:dk, h * 128:(h + 1) * 128], ktz[zsl, tI % 2, h * dk:(h + 1) * dk],
                                   vtok[zsl, tI, h * 128:(h + 1) * 128], True, True,
                                   ["ktz%d" % (tI % 2), "vtok%d" % tI], [buk])
                            else:
                                rhs = CB("ones")[csl, :] if isn else vtok[csl, tI, h * 128:(h + 1) * 128]
                                mm(bu[0:dk, h * 128:(h + 1) * 128], ktok[csl, tI, h * dk:(h + 1) * dk], rhs, True, True,
                                   ["ktok%d" % tI, "vtok%d" % tI, "cstb"], [buk])
                        tmp_, tmpk = fsa()
                        tv = tmp_[0:dk, 0:512].rearrange("p (h v) -> p h v", h=4)
                        tt(tv, bu[0:dk, :].rearrange("p (h v) -> p h v", h=4), la_sf[sidx][0:dk, :, :], ALU.add,
                           [buk, "la_sf%d" % sidx], [tmpk])
                        gb = gm[0:dk, 0:4, cidx:cidx + 1].to_broadcast([dk, 4, 128])
                        tt(la_sb[sidx][0:dk, :, :], tv, gb, ALU.mult, [tmpk, gmk], ["la_sb%d" % sidx])
                        tt(la_sf[sidx][0:dk, :, :], tv, gb, ALU.mult, [tmpk, gmk], ["la_sf%d" % sidx])
                    yield
                fin(tI, po, pok, pd, pdk)
                yield

            NT_ = TS // 128
            drive([phA(0)])
            for tI in range(NT_):
                drive([phB(tI), phA(tI + 1) if tI + 1 < NT_ else None])

        def gam_start(ct, C):
            n = TS // C
            d_, dk_ = fsa()
            tt(d_[:, 0:n], gnb[ct][:, C:TS + 1:C], gnb[ct][:, 0:TS:C], ALU.subtract, ["gn%d" % ct], [dk_])
            act(gam[:, ct, 0:n], d_[:, 0:n], AF.Exp, [dk_], ["gam"], scale=-1.0)

        def qk_decay(ct, q_ap, qk_, k_ap, kk_, C, mid, Qd, Kd, kextra=None, clamp=False):
            gk = "gn%d" % ct
            n = TS // C
            off = C // 2 if mid else 0
            tsc(nref[:, ct, 0:n], gnb[ct][:, off:TS:C], -1.0, None, ALU.mult, None, [gk], ["nref"])
            ei, eik = fsa(); ev, evk = fsa()
            for c in range(n):
                c0 = c * C
                rc = c0 + off
                act(ei[:, c0:c0 + C], gnb[ct][:, c0 + 1:c0 + C + 1], AF.Exp, [gk], [eik], bias=gnb[ct][:, rc:rc + 1], scale=-1.0)
                if kextra is None:
                    act(ev[:, c0:c0 + C], gnb[ct][:, c0 + 1:c0 + C + 1], AF.Exp, [gk, "nref"], [evk],
                        bias=nref[:, ct, c:c + 1], scale=1.0)
                else:
                    act(ev[:, c0:c0 + C], kextra[0][:, c0:c0 + C], AF.Exp, [kextra[1], "nref"], [evk],
                        bias=nref[:, ct, c:c + 1], scale=1.0)
            if clamp:
                tsc(ei[:, 0:TS], ei[:, 0:TS], 2.35e17, None, ALU.min, None, [eik], [eik])
                tsc(ev[:, 0:TS], ev[:, 0:TS], 2.35e17, None, ALU.min, None, [evk], [evk])
            tt(Qd[0][:, :], q_ap, ei[:, 0:TS], ALU.mult, [qk_, eik], [Qd[1]])
            tt(Kd[0][:, :], k_ap, ev[:, 0:TS], ALU.mult, [kk_, evk], [Kd[1]])

        BLk = lambda i: (BL[i], "bl%d" % i)
        LLk = lambda i: (LL[i], "ll%d" % i)

        def zgate(l, c0, h, func_scale_ap, skeys):
            pz, pzk = proj(l, c0 + h * 128, 128)
            z, zk = LLk(h)
            act(z[:, :], pz[:, :], AF.Silu, [pzk], [zk])
            if func_scale_ap is not None:
                tsc(z[:, :], z[:, :], func_scale_ap, None, ALU.mult, None, [zk] + skeys, [zk])
            return z, zk

        def rms_fin(GZ):
            def fin(tI, po, pok, pd, pdk):
                sq, sqk = fsa()
                act(sq[:, 0:TS], po[:, :], AF.Square, [pok], [sqk])
                b, bk = bank()
                mm(b[:, :], CS("ones"), sq[:, 0:TS], True, True, [sqk, "cst"], [bk])
                rs, rsk = fsa()
                rsq(rs[:, 0:TS], b[:, :], 1.0, 128.0 * 1e-6, [bk], [rsk])
                t1, t1k = fsa()
                tt(t1[:, 0:TS], po[:, :], rs[:, 0:TS], ALU.mult, [pok, rsk], [t1k])
                for h in range(4):
                    tt(ys[:, h, tI * 128:(tI + 1) * 128], t1[:, h * 128:(h + 1) * 128],
                       GZ[h][0][:, tI * 128:(tI + 1) * 128], ALU.mult, [t1k, GZ[h][1]], ["ys"])
            return fin

        def heads64(QT, KT):
            Qh, Kh = [], []
            for ct in range(2):
                shift_hi(QT[ct][0][:, :], QT[ct][1], BL[16 + ct][0:64, :], "bl%d" % (16 + ct), TS)
                shift_hi(KT[ct][0][:, :], KT[ct][1], BL[18 + ct][0:64, :], "bl%d" % (18 + ct), TS)
                Qh += [(QT[ct][0][0:64, :], QT[ct][1]), (BL[16 + ct][0:64, :], "bl%d" % (16 + ct))]
                Kh += [(KT[ct][0][0:64, :], KT[ct][1]), (BL[18 + ct][0:64, :], "bl%d" % (18 + ct))]
            return Qh, Kh

        def gla(l, sI):
            proj_tok(l, C_GV)
            yield
            pg, pgk = proj(l, C_GC, 16)
            gc, gck = bsa()
            act(gc[0:16, 0:TS], pg[0:16, :], AF.Copy, [pgk], [gck])
            yield
            QT, KT = [], []
            for ct in range(2):
                yield
                b, bk = bank()
                mm(b[:, :], gup[:, ct * 128:(ct + 1) * 128], gc[0:16, 0:TS], True, True, ["gup", gck], [bk])
                e1, e1k = fsa()
                act(e1[:, 0:TS], b[:, :], AF.Exp, [bk, "drv"], [e1k], bias=drv[:, 30 + ct:31 + ct], scale=-1.0)
                act(e1[:, 0:TS], e1[:, 0:TS], AF.Ln, [e1k], [e1k], bias=1.0)
                tsc(e1[:, 0:TS], e1[:, 0:TS], 1.0 / 16.0, None, ALU.mult, None, [e1k], [e1k])
                cumsum(ct, e1[:, 0:TS], e1k)
                gam_start(ct, 128)
                yield
                pq, pqk = proj(l, C_GQ + ct * 128, 128)
                q, qk_ = fsa()
                act(q[:, 0:TS], pq[:, :], AF.Copy, [pqk], [qk_], scale=0.125)
                yield
                pk_, pkk = proj(l, C_GK + ct * 128, 128)
                k, kk_ = fsa()
                cp(k[:, 0:TS], pk_[:, :], [pkk], [kk_])
                qk_decay(ct, q[:, 0:TS], qk_, k[:, 0:TS], kk_, 128, False, BLk(ct), BLk(4 + ct))
                QT.append(BLk(ct)); KT.append(BLk(4 + ct))
            GZ = []
            for h in range(4):
                GZ.append(zgate(l, C_GZ, h, drv[:, 38 + h:39 + h], ["drv"]))
                yield
            Qh, Kh = heads64(QT, KT)
            build_gamh(2, 4)
            la(Qh, Kh, KT, 128, 64, 0, gamh, "gamh", rms_fin(GZ))

        def hgrn(l, sI):
            proj_tok(l, C_HI)
            yield
            QT, KT = [], []
            for ct in range(4):
                yield
                pf, pfk = proj(l, C_HF + ct * 128, 128)
                g, gk_ = fsa()
                act(g[:, 0:TS], pf[:, :], AF.Sigmoid, [pfk], [gk_])
                tsc(g[:, 0:TS], g[:, 0:TS], drv[:, 34 + ct:35 + ct], lbt[:, l * 4 + ct:l * 4 + ct + 1], ALU.mult, ALU.add,
                    [gk_, "drv", "lbt"], [gk_])
                lg, lgk = fsa()
                act(lg[:, 0:TS], g[:, 0:TS], AF.Ln, [gk_], [lgk])
                tsc(lg[:, 0:TS], lg[:, 0:TS], -1.0, None, ALU.mult, None, [lgk], [lgk])
                cumsum(ct, lg[:, 0:TS], lgk)
                d_, dk_ = fsa()
                tt(d_[:, 0:15], gnb[ct][:, 48:TS:32], gnb[ct][:, 16:TS - 32:32], ALU.subtract, ["gn%d" % ct], [dk_])
                tt(d_[:, 15:16], gnb[ct][:, TS:TS + 1], gnb[ct][:, TS - 16:TS - 15], ALU.subtract, ["gn%d" % ct], [dk_])
                act(gam[:, ct, 0:16], d_[:, 0:16], AF.Exp, [dk_], ["gam"], scale=-1.0)
                fc_, fck = fsa()
                act(fc_[:, 0:1], gnb[ct][:, 16:17], AF.Exp, ["gn%d" % ct], [fck], scale=-1.0)
                tsc(la_sf[3][:, ct, :], la_sf[3][:, ct, :], fc_[:, 0:1], None, ALU.mult, None,
                    [fck, "la_sf3"], ["la_sf3"])
                cp(la_sb[3][:, ct, :], la_sf[3][:, ct, :], ["la_sf3"], ["la_sb3"])
                tsc(g[:, 0:TS], g[:, 0:TS], -1.0, 1.0, ALU.mult, ALU.add, [gk_], [gk_])
                yield
                pq, pqk = proj(l, C_HQ + ct * 128, 128)
                q, qk_ = fsa()
                act(q[:, 0:TS], pq[:, :], AF.Silu, [pqk], [qk_])
                qk_decay(ct, q[:, 0:TS], qk_, g[:, 0:TS], gk_, 32, True, BLk(ct), BLk(4 + ct), clamp=True)
                QT.append(BLk(ct)); KT.append(BLk(4 + ct))
            GZ = []
            for h in range(4):
                GZ.append(zgate(l, C_HZ, h, drv[:, 42 + h:43 + h], ["drv"]))
                yield
            la(QT, KT, KT, 32, 128, 3, gam, "gam", rms_fin(GZ))

        def mlstm(l, sI):
            proj_tok(l, C_MV)
            yield
            w8, w8k = wchunk(win_d[l, :, C_MI:C_MI + 8], 8)
            reps = {}
            for ct in range(2):
                for gI in range(2):
                    for half in range(2):
                        r_, rk_ = BLk(8 + ct * 4 + gI * 2 + half)
                        rv_ = r_[:, :].rearrange("p (k j m) -> p k j m", k=4, j=2, m=64)
                        src = w8[:, half * 4:(half + 1) * 4, gI * 4 + ct * 2:gI * 4 + ct * 2 + 2]
                        cp(rv_, src.unsqueeze(3).to_broadcast([128, 4, 2, 64]), [w8k], [rk_])
                        reps[(ct, gI, half)] = (r_, rk_)
            for ct in range(4):
                yield
                pc, pck = proj(l, C_MQK + ct * 128, 128)
                cb, cbk = fsa()
                cp(cb[:, 0:3], cqc[:, ct, 0:3], ["cqc"], [cbk])
                act(cb[:, 3:TS + 3], pc[:, :], AF.Copy, [pck], [cbk])
                a, ak = fsa()
                oc = PK["conv"][0]
                tsc(a[:, 0:TS], cb[:, 0:TS], pk[:, l, oc + ct:oc + ct + 1], None, ALU.mult, None, [cbk, "pk"], [ak])
                for j in range(1, 4):
                    stt(a[:, 0:TS], cb[:, j:j + TS], pk[:, l, oc + j * 4 + ct:oc + j * 4 + ct + 1], a[:, 0:TS], ALU.mult,
                        ALU.add, [cbk, "pk", ak], [ak])
                cp(cqc[:, ct, 0:3], cb[:, TS:TS + 3], [cbk], ["cqc"])
                act(LL[4 + ct][:, :], a[:, 0:TS], AF.Silu, [ak], ["ll%d" % (4 + ct)])
            QT, KT = [], []
            for ct in range(2):
                yield
                pis = []
                for gI in range(2):
                    b, bk = bank()
                    for k in range(8):
                        r_, rk_ = reps[(ct, gI, k // 4)]
                        mm(b[:, :], r_[:, (k % 4) * 128:(k % 4 + 1) * 128], uT[:, k, :], k == 0, k == 7, [rk_, "uT"], [bk])
                    pis.append((b, bk))
                e1, e1k = fsa()
                act(e1[:, 0:TS], pis[1][0][:, :], AF.Exp, [pis[1][1], "drv"], [e1k], bias=drv[:, 32 + ct:33 + ct], scale=-1.0)
                act(e1[:, 0:TS], e1[:, 0:TS], AF.Ln, [e1k], [e1k], bias=1.0)
                cumsum(ct, e1[:, 0:TS], e1k)
                gam_start(ct, 128)
                ig, igk = fsa()
                act(ig[:, 0:TS], pis[0][0][:, :], AF.Identity, [pis[0][1], "pk"], [igk], bias=PKc(l, "i_b", ct))
                tt(ig[:, 0:TS], ig[:, 0:TS], gnb[ct][:, 1:TS + 1], ALU.add, [igk, "gn%d" % ct], [igk])
                k, kk_ = fsa()
                tsc(k[:, 0:TS], LL[6 + ct][:, :], 0.125, None, ALU.mult, None, ["ll%d" % (6 + ct)], [kk_])
                qk_decay(ct, LL[4 + ct][:, :], "ll%d" % (4 + ct), k[:, 0:TS], kk_, 128, False, BLk(ct), BLk(4 + ct),
                         kextra=(ig, igk))
                QT.append(BLk(ct)); KT.append(BLk(4 + ct))
            GZ = []
            for h in range(4):
                GZ.append(zgate(l, C_MZ, h, PKc(l, "ml_g", h), ["pk"]))
                yield

            def fin(tI, po, pok, pd, pdk):
                den, dnk = fsa()
                act(den[:, 0:TS], pd[:, :], AF.Abs, [pdk], [dnk])
                tsc(den[:, 0:TS], den[:, 0:TS], 1.0, None, ALU.max, None, [dnk], [dnk])
                recip(den[:, 0:TS], den[:, 0:TS], [dnk], [dnk])
                o, ok = fsa()
                tt(o[:, 0:TS], po[:, :], den[:, 0:TS], ALU.mult, [pok, dnk], [ok])
                b, bk = bank()
                mm(b[:, :], CS("ones"), o[:, 0:TS], True, True, [ok, "cst"], [bk])
                d, dk_ = fsa()
                stt(d[:, 0:TS], b[:, :], -1.0 / 128.0, o[:, 0:TS], ALU.mult, ALU.add, [bk, ok], [dk_])
                sq, sqk = fsa()
                act(sq[:, 0:TS], d[:, 0:TS], AF.Square, [dk_], [sqk])
                b2, b2k = bank()
                mm(b2[:, :], CS("ones"), sq[:, 0:TS], True, True, [sqk, "cst"], [b2k])
                rs, rsk = fsa()
                rsq(rs[:, 0:TS], b2[:, :], 1.0 / 128.0, 1e-6, [b2k], [rsk])
                tt(d[:, 0:TS], d[:, 0:TS], rs[:, 0:TS], ALU.mult, [dk_, rsk], [dk_])
                for h in range(4):
                    tt(ys[:, h, tI * 128:(tI + 1) * 128], d[:, h * 128:(h + 1) * 128],
                       GZ[h][0][:, tI * 128:(tI + 1) * 128], ALU.mult, [dk_, GZ[h][1]], ["ys"])

            Qh, Kh = heads64(QT, KT)
            build_gamh(2, 4)
            la(Qh, Kh, KT, 128, 64, 1, gamh, "gamh", fin, ni=2)

        def rwkv(l, sI):
            C = 64
            NCH = TS // C

            def mixed(idx, c0, n, mucol, omcol, dst=None):
                pp, ppk = proj(l, c0, n)
                pb_, pbk_ = fsa()
                cp(pb_[0:n, 0:1], prc[0:n, idx:idx + 1], ["prc"], [pbk_])
                act(pb_[0:n, 1:TS + 1], pp[0:n, :], AF.Copy, [ppk], [pbk_])
                t_, tk_ = fsa()
                tsc(t_[0:n, 0:TS], pb_[0:n, 0:TS], mucol, None, ALU.mult, None, [pbk_, "pk"], [tk_])
                if dst is None:
                    o_, ok_ = fsa()
                    o_ = o_[:, 0:TS]
                else:
                    o_, ok_ = dst
                stt(o_[0:n, :], pb_[0:n, 1:TS + 1], omcol, t_[0:n, 0:TS], ALU.mult, ALU.add, [pbk_, "drv", tk_], [ok_])
                cp(prc[0:n, idx:idx + 1], pb_[0:n, TS:TS + 1], [pbk_], ["prc"])
                return o_, ok_

            WC = mixed(12, C_RW + 1536, 64, pk[0:64, l, PK["mu_wc"][0]:PK["mu_wc"][0] + 1], drv[0:64, 20:21],
                       dst=(wcac[:, 0, :], "wc"))
            AC = mixed(13, C_RW + 1600, 64, pk[0:64, l, PK["mu_ac"][0]:PK["mu_ac"][0] + 1], drv[0:64, 21:22],
                       dst=(wcac[:, 1, :], "ac"))
            yield
            act(wcacb[:, 0, :], WC[0][0:64, :], AF.Tanh, ["wc"], ["wcb"])
            cp(wcacb[:, 1, :], AC[0][0:64, :], ["ac"], ["acb"])
            ops = []
            BON = []
            for ct in range(4):
                csl = slice(ct * 128, (ct + 1) * 128)
                yield
                Kx, Kxk = mixed(4 + ct, C_RW + 512 + ct * 128, 128, PKc(l, "mu", 4 + ct), drv[:, 12 + ct:13 + ct])
                kk, kkk = fsa()
                tsc(kk[:, 0:TS], Kx[:, :], PKc(l, "k_k", ct), None, ALU.mult, None, [Kxk, "pk"], [kkk])
                sq, sqk = fsa()
                act(sq[:, 0:TS], kk[:, 0:TS], AF.Square, [kkk], [sqk])
                b3, b3k = bank()
                mm(b3[:, :], CS("bd"), sq[:, 0:TS], True, True, ["cst", sqk], [b3k])
                rsq(sq[:, 0:TS], b3[:, :], 1.0, 1e-24, [b3k], [sqk])
                tt(kk[:, 0:TS], kk[:, 0:TS], sq[:, 0:TS], ALU.mult, [kkk, sqk], [kkk])
                b, bk = bank()
                mm(b[:, :], wup[:, csl], wcacb[:, 0, :], True, True, ["wup", "wcb"], [bk])
                e1, e1k = fsa()
                act(e1[:, 0:TS], b[:, :], AF.Exp, [bk, "drv"], [e1k], bias=drv[:, 22 + ct:23 + ct], scale=-1.0)
                act(e1[:, 0:TS], e1[:, 0:TS], AF.Ln, [e1k], [e1k], bias=1.0)
                act(e1[:, 0:TS], e1[:, 0:TS], AF.Exp, [e1k], [e1k], bias=-0.5, scale=-1.0)
                cumsum(ct, e1[:, 0:TS], e1k)
                gk = "gn%d" % ct
                tsc(nref[:, ct, 0:NCH], gnb[ct][:, 0:TS:C], -1.0, None, ALU.mult, None, [gk], ["nref"])
                yield
                b2, b2k = bank()
                mm(b2[:, :], aup[:, csl], wcacb[:, 1, :], True, True, ["aup", "acb"], [b2k])
                sg, sgk = fsa()
                act(sg[:, 0:TS], b2[:, :], AF.Sigmoid, [b2k, "pk"], [sgk], bias=PKc(l, "a0", ct))
                t1, t1k = fsa()
                tsc(t1[:, 0:TS], sg[:, 0:TS], PKc(l, "k_a", ct), drv[:, 26 + ct:27 + ct], ALU.mult, ALU.add,
                    [sgk, "pk", "drv"], [t1k])
                tt(Kx[:, :], Kx[:, :], t1[:, 0:TS], ALU.mult, [Kxk, t1k], [Kxk])
                tt(sg[:, 0:TS], kk[:, 0:TS], sg[:, 0:TS], ALU.mult, [kkk, sgk], [sgk])
                ei, eik = fsa(); ee, eek = fsa(); ev, evk = fsa()
                for c in range(NCH):
                    c0 = c * C
                    act(ei[:, c0:c0 + C], gnb[ct][:, c0 + 1:c0 + C + 1], AF.Exp, [gk], [eik], bias=gnb[ct][:, c0:c0 + 1], scale=-1.0)
                    act(ee[:, c0:c0 + C], gnb[ct][:, c0:c0 + C], AF.Exp, [gk], [eek], bias=gnb[ct][:, c0:c0 + 1], scale=-1.0)
                    act(ev[:, c0:c0 + C], gnb[ct][:, c0 + 1:c0 + C + 1], AF.Exp, [gk, "nref"], [evk],
                        bias=nref[:, ct, c:c + 1], scale=1.0)
                rt, at, btl, ktl, vb = [BLk(ct * 5 + j) for j in range(5)]
                stt(at[0][:, :], kk[:, 0:TS], -1.0, ee[:, 0:TS], ALU.mult, ALU.mult, [kkk, eek], [at[1]])
                tt(btl[0][:, :], sg[:, 0:TS], ev[:, 0:TS], ALU.mult, [sgk, evk], [btl[1]])
                tt(ktl[0][:, :], Kx[:, :], ev[:, 0:TS], ALU.mult, [Kxk, evk], [ktl[1]])
                cp(gam[:, ct, 0:NCH], ei[:, C - 1:TS:C], [eik], ["gam"])
                yield
                Rx, Rxk = mixed(ct, C_RW + ct * 128, 128, PKc(l, "mu", ct), drv[:, 8 + ct:9 + ct])
                tt(rt[0][:, :], Rx[:, :], ei[:, 0:TS], ALU.mult, [Rxk, eik], [rt[1]])
                rk, rkk = fsa()
                stt(rk[:, 0:TS], Rx[:, :], PKc(l, "r_k", ct), Kx[:, :], ALU.mult, ALU.mult, [Rxk, "pk", Kxk], [rkk])
                b4, b4k = bank()
                mm(b4[:, :], CS("bd"), rk[:, 0:TS], True, True, ["cst", rkk], [b4k])
                Vx, Vxk = mixed(8 + ct, C_RW + 1024 + ct * 128, 128, PKc(l, "mu", 8 + ct), drv[:, 16 + ct:17 + ct])
                cp(vb[0][:, :], Vx[:, :], [Vxk], [vb[1]])
                tt(LL[ct][:, :], b4[:, :], Vx[:, :], ALU.mult, [b4k, Vxk], ["ll%d" % ct])
                BON.append(LLk(ct))
                ops.append(dict(r=rt, a=at, b=btl, k=ktl, v=vb))

            hd = lambda h: (h // 2, (h % 2) * 64)
            SH = {}
            mhb = merged[:, 4:8, :].bitcast(BF16)
            for ct in range(4):
                for ni_, nm in enumerate(("r", "a", "b", "k")):
                    j = ct * 4 + ni_
                    if j < 8:
                        dst, dkey = mhb[:, j // 2, (j % 2) * 512:(j % 2 + 1) * 512], "mg%d" % (4 + j // 2)
                    elif j < 12:
                        dst, dkey = vtok[:, j - 8, :], "vtok%d" % (j - 8)
                    else:
                        dst, dkey = ktok[:, j - 12, :], "ktok%d" % (j - 12)
                    shift_hi(ops[ct][nm][0][:, :], ops[ct][nm][1], dst[0:64, :], dkey, TS)
                    SH[(ct, nm)] = (dst, dkey)
            build_gamh(4, NCH)

            def opn(nm, h):
                ct, par = h // 2, h % 2
                if par == 0:
                    return ops[ct][nm][0][0:64, :], ops[ct][nm][1]
                return SH[(ct, nm)][0][0:64, :], SH[(ct, nm)][1]

            llb = [LL[4 + i][:, :].bitcast(BF16) for i in range(4)]
            SETB = ("vt", "bt", "kt", "Aak", "Arb", "Ark", "Yf")
            rvs = [dict(rv), dict(rv)]
            rvk = [{n: "rv_" + n for n in rv}, {n: "rv_" + n for n in rv}]
            for i, n in enumerate(SETB):
                rvs[1][n] = llb[i // 2][0:64, (i % 2) * 512:(i % 2 + 1) * 512]
                rvk[1][n] = "ll%d" % (4 + i // 2)

            def phaseA(c):
                R, RK = rvs[c % 2], rvk[c % 2]
                c0 = c * C
                cs = slice(c0, c0 + C)
                for nm, key in (("v", "vt"), ("b", "bt"), ("k", "kt")):
                    bt_, btk2 = bank()
                    btb = bt_[:, :].bitcast(BF16)
                    for ct in range(4):
                        tr(btb[0:64, ct * 128:(ct + 1) * 128], ops[ct][nm][0][:, cs], CB("ident"),
                           [ops[ct][nm][1], "cstb"], [btk2])
                    cp(R[key][:, :], btb[0:64, 0:512], [btk2], [RK[key]])
                yield

                def scores(lhs, rhs, dst, maskn, eng):
                    b_, bk_ = bank()
                    for h in range(8):
                        L_, R_ = opn(lhs, h), opn(rhs, h)
                        mm(b_[0:64, h * 64:(h + 1) * 64], L_[0][:, cs], R_[0][:, cs], True, True, [L_[1], R_[1]], [bk_])
                    tt(R[dst][:, :], b_[0:64, :], CB(maskn)[0:64, :], ALU.mult, [bk_, "cstb"], [RK[dst]])
                scores("a", "b", "N", "r_ts", "dve")
                scores("b", "a", "NT", "r_st", "dve")
                yield
                scores("k", "a", "Aak", "r_st", "dve")
                scores("b", "r", "Arb", "r_sti", "dve")
                scores("k", "r", "Ark", "r_sti", "dve")
                tt(R["Y"][:, :], R["NT"][:, :], CB("i8")[0:64, :], ALU.add, [RK["NT"], "cstb"], [RK["Y"]])
                yield
                M, MT, Y = "N", "NT", "Y"
                M2, MT2, Y2 = "N2", "NT2", "Y2"
                for lev in range(5):
                    last = lev == 4
                    ba, bak = bank()
                    for h in range(8):
                        hs = slice(h * 64, (h + 1) * 64)
                        mm(ba[0:64, hs], R[MT][:, hs], R[M][:, hs], True, True, [RK[MT], RK[M]], [bak])
                    act(R[M2][:, :], ba[0:64, :], AF.Copy, [bak], [RK[M2]])
                    if not last:
                        bb_, bbk_ = bank()
                        for h in range(8):
                            hs = slice(h * 64, (h + 1) * 64)
                            mm(bb_[0:64, hs], R[M][:, hs], R[MT][:, hs], True, True, [RK[MT], RK[M]], [bbk_])
                        cp(R[MT2][:, :], bb_[0:64, :], [bbk_], [RK[MT2]])
                    bc, bck = bank()
                    for h in range(8):
                        hs = slice(h * 64, (h + 1) * 64)
                        mm(bc[0:64, hs], R[M2][:, hs], R[Y][:, hs], True, True, [RK[M2], RK[Y]], [bck])
                    Yd = "Yf" if last else Y2
                    tt(R[Yd][:, :], bc[0:64, :], R[Y][:, :], ALU.add, [bck, RK[Y]], [RK[Yd]])
                    M, M2 = M2, M
                    MT, MT2 = MT2, MT
                    Y, Y2 = Y2, Y
                    yield

            def phaseB(c):
                R, RK = rvs[c % 2], rvk[c % 2]
                c0 = c * C
                cs = slice(c0, c0 + C)
                bx, bxk = bank()
                for h in range(8):
                    hs = slice(h * 64, (h + 1) * 64)
                    A_ = opn("a", h)
                    mm(bx[0:64, hs], A_[0][:, cs], rw_sb[:, h, :], True, False, [A_[1], "rw_sb"], [bxk])
                    mm(bx[0:64, hs], R["Aak"][:, hs], R["vt"][:, hs], False, True, [RK["Aak"], RK["vt"]], [bxk])
                cp(R["X"][:, :], bx[0:64, :], [bxk], [RK["X"]])
                yield
                bu, buk = bank()
                for h in range(8):
                    hs = slice(h * 64, (h + 1) * 64)
                    mm(bu[0:64, hs], R["Yf"][:, hs], R["X"][:, hs], True, True, [RK["Yf"], RK["X"]], [buk])
                act(R["U"][:, :], bu[0:64, :], AF.Copy, [buk], [RK["U"]])
                yield
                bs_, bsk = bank()
                for h in range(8):
                    hs = slice(h * 64, (h + 1) * 64)
                    mm(bs_[0:64, hs], R["bt"][:, hs], R["U"][:, hs], True, False, [RK["bt"], RK["U"]], [bsk])
                    mm(bs_[0:64, hs], R["kt"][:, hs], R["vt"][:, hs], False, True, [RK["kt"], RK["vt"]], [bsk])
                by, byk = bank()
                for h in range(8):
                    ct, pb = hd(h)
                    hs = slice(h * 64, (h + 1) * 64)
                    o_ = by[pb:pb + 64, ct * 64:(ct + 1) * 64]
                    R_ = opn("r", h)
                    mm(o_, rw_sb[:, h, :], R_[0][:, cs], True, False, ["rw_sb", R_[1]], [byk])
                    mm(o_, R["U"][:, hs], R["Arb"][:, hs], False, False, [RK["U"], RK["Arb"]], [byk])
                    mm(o_, R["vt"][:, hs], R["Ark"][:, hs], False, True, [RK["vt"], RK["Ark"]], [byk])
                tmp_, tmpk = fsa()
                tv = tmp_[0:64, 0:512].rearrange("p (h v) -> p h v", h=8)
                tt(tv, bs_[0:64, :].rearrange("p (h v) -> p h v", h=8), rw_sf[:, :, :], ALU.add, [bsk, "rw_sf"], [tmpk])
                gb = gamh[0:64, 0:8, c:c + 1].to_broadcast([64, 8, 64])
                tt(rw_sb[:, :, :], tv, gb, ALU.mult, [tmpk, "gamh"], ["rw_sb"])
                tt(rw_sf[:, :, :], tv, gb, ALU.mult, [tmpk, "gamh"], ["rw_sf"])
                act(yT[:, 0:4, cs], by[:, 0:256].rearrange("p (c t) -> p c t", c=4), AF.Copy, [byk],
                    ["mg0", "mg1", "mg2", "mg3"])
                yield

            drive([phaseA(0)])
            for c in range(NCH):
                drive([phaseB(c), phaseA(c + 1) if c + 1 < NCH else None])
            for ct in range(4):
                mk = "mg%d" % ct
                b, bk = bank()
                mm(b[:, :], CS("bd"), yT[:, ct, :], True, True, ["cst", mk], [bk])
                d, dk_ = fsa()
                stt(d[:, 0:TS], b[:, :], -1.0 / 64.0, yT[:, ct, :], ALU.mult, ALU.add, [bk, mk], [dk_])
                sq, sqk = fsa()
                act(sq[:, 0:TS], d[:, 0:TS], AF.Square, [dk_], [sqk])
                b2, b2k = bank()
                mm(b2[:, :], CS("bd"), sq[:, 0:TS], True, True, ["cst", sqk], [b2k])
                rsq(sq[:, 0:TS], b2[:, :], 1.0 / 64.0, 64e-5, [b2k], [sqk])
                tt(d[:, 0:TS], d[:, 0:TS], sq[:, 0:TS], ALU.mult, [dk_, sqk], [dk_])
                act(d[:, 0:TS], d[:, 0:TS], AF.Identity, [dk_, "pk"], [dk_], bias=PKc(l, "ln_b", ct), scale=PKc(l, "ln_g", ct))
                tt(d[:, 0:TS], d[:, 0:TS], BON[ct][0][:, :], ALU.add, [dk_, BON[ct][1]], [dk_])
                pz, pzk = proj(l, C_RWZ + ct * 128, 128)
                z, zk = fsa()
                act(z[:, 0:TS], pz[:, :], AF.Silu, [pzk], [zk])
                tt(ys[:, ct, :], d[:, 0:TS], z[:, 0:TS], ALU.mult, [dk_, zk], ["ys"])

        outs = program()
        S.emit(final_wait_ops=outs)
    return nc


_NC_CACHE = {}


def make_in_maps(inp, n_cores=8):
    f = lambda a: np.ascontiguousarray(np.asarray(a, dtype=np.float32))
    pkall = np.stack([_pack_params(inp, l) for l in range(DEPTH)], 0)
    fgT = np.ascontiguousarray(f(inp["final_g"]).reshape(8, 128).T)
    shared = {
        "pk": pkall, "fg": fgT, "cst": CONSTS, "cstf": CSTF,
        "ada_w": f(inp["ada_w"]), "w_in": f(inp["w_in"]), "rw_w_up": f(inp["rw_w_up"]), "rw_a_up": f(inp["rw_a_up"]),
        "gla_gk_up": f(inp["gla_gk_up"]), "w_branch": f(inp["w_branch"]), "w_out": f(inp["w_out"]),
    }
    maps = []
    for b in range(n_cores):
        m = dict(shared)
        m["xT"] = np.ascontiguousarray(f(inp["x"][b]).T)
        m["cT"] = np.ascontiguousarray(f(inp["c"][b]).reshape(8, 128).T)
        maps.append(m)
    return maps


def kernel(**inputs):
    inp = {k: np.asarray(v) for k, v in inputs.items()}
    if "full" not in _NC_CACHE:
        _NC_CACHE["full"] = build_nc()
    nc = _NC_CACHE["full"]
    maps = make_in_maps(inp)
    res = run_bass_kernel_spmd(nc, maps, core_ids=list(range(8)))
    out = np.stack([np.ascontiguousarray(r["outT"].T) for r in res.results], 0)
    return out.astype(np.float32)
```

```python
import contextlib
import numpy as np
import ml_dtypes
import concourse.bass as bass
import concourse.mybir as mybir
from concourse.bass_utils import run_bass_kernel_spmd

F32 = mybir.dt.float32
BF16 = mybir.dt.bfloat16
AF = mybir.ActivationFunctionType
ALU = mybir.AluOpType

D = 1024
SEQ = 2048
DEPTH = 4
NCOLS = 11416
TS = 512
NST = SEQ // TS
ENGS = ("pe", "dve", "act", "pool", "sp")

C_RW = 0; C_RWZ = 1664
C_GQ = 2176; C_GK = 2432; C_GV = 2688; C_GC = 3200; C_GZ = 3216
C_MQK = 3728; C_MV = 4240; C_MI = 4752; C_MF = 4756; C_MZ = 4760
C_HQ = 5272; C_HF = 5784; C_HI = 6296; C_HZ = 6808
C_MG = 7320

PK = {}
_o = 0
for _n, _w in (("norm_g", 8), ("ada_b", 24), ("mu", 12), ("mu_wc", 1), ("mu_ac", 1), ("w0", 4), ("a0", 4),
               ("k_k", 4), ("k_a", 4), ("r_k", 4), ("ln_g", 4), ("ln_b", 4), ("gk_b", 2), ("gla_g", 4),
               ("conv", 16), ("ml_g", 4), ("lb", 16), ("hg_g", 4), ("i_b", 2), ("f_b", 2)):
    PK[_n] = (_o, _w)
    _o += _w
NPK = _o


class Op:
    __slots__ = ("eng", "fn", "deps", "idx", "dma_stream", "signal", "cnt")

    def __init__(self, eng, fn):
        self.eng = eng
        self.fn = fn
        self.deps = []
        self.dma_stream = None
        self.signal = False
        self.cnt = 0


class Sched:
    def __init__(self, nc):
        self.nc = nc
        self.ops = []
        self.last_w = {}
        self.readers = {}
        self.streams = {}
        self.record = False

    def op(self, eng, fn, reads=(), writes=(), dma=None):
        if self.record:
            return None
        o = Op(eng, fn)
        o.idx = len(self.ops)
        deps = {}
        for k in reads:
            w = self.last_w.get(k)
            if w is not None:
                deps[w.idx] = w
        for k in writes:
            w = self.last_w.get(k)
            if w is not None:
                deps[w.idx] = w
            for r in self.readers.get(k, ()):
                deps[r.idx] = r
        if dma is not None:
            o.dma_stream = dma
            n = self.streams.get(dma, 0) + 1
            self.streams[dma] = n
            o.cnt = n
        o.deps = [deps[i] for i in sorted(deps)]
        for k in reads:
            self.readers.setdefault(k, []).append(o)
        for k in writes:
            self.last_w[k] = o
            self.readers[k] = []
        self.ops.append(o)
        return o

    def emit(self, final_wait_ops=()):
        nc = self.nc
        for o in self.ops:
            nd = []
            for d in o.deps:
                if d.dma_stream is None and o.dma_stream is None and d.eng == o.eng and o.eng == "pe":
                    continue
                nd.append(d)
            o.deps = nd
            for d in nd:
                d.signal = True
        for o in final_wait_ops:
            o.signal = True
        cnt = {e: 0 for e in ENGS}
        for o in self.ops:
            if o.dma_stream is None and o.signal:
                cnt[o.eng] += 1
                o.cnt = cnt[o.eng]
        EP, DEP = 10 ** 9, 10 ** 9
        with contextlib.ExitStack() as es:
            esem = {}
            for e in ENGS:
                for ep in range(max(1, (cnt[e] + EP - 1) // EP)):
                    esem[(e, ep)] = es.enter_context(nc.semaphore("c_%s%d" % (e, ep)))
            ssem = {}
            for i, (s_, n_) in enumerate(self.streams.items()):
                for ep in range(max(1, (n_ + DEP - 1) // DEP)):
                    ssem[(s_, ep)] = es.enter_context(nc.semaphore("d%d_%d" % (i, ep)))
            block = es.enter_context(nc.Block())
            per = {e: [o for o in self.ops if o.eng == e] for e in ENGS}

            def semval(d):
                if d.dma_stream is not None:
                    return ssem[(d.dma_stream, (d.cnt - 1) // DEP)], 16 * ((d.cnt - 1) % DEP + 1)
                return esem[(d.eng, (d.cnt - 1) // EP)], (d.cnt - 1) % EP + 1

            def runner(e):
                def body(engine):
                    waited = {}
                    for o in per[e]:
                        for d in o.deps:
                            sem, val = semval(d)
                            key = id(sem)
                            if waited.get(key, 0) >= val:
                                continue
                            waited[key] = val
                            engine.wait_ge(sem, val)
                        ins = o.fn(engine)
                        if o.dma_stream is not None:
                            ins.then_inc(semval(o)[0], 16)
                        elif o.signal:
                            ins.then_inc(semval(o)[0], 1)
                    if e == "sp":
                        for o in final_wait_ops:
                            sem, val = semval(o)
                            engine.wait_ge(sem, val)
                return body

            block.tensor(runner("pe"))
            block.vector(runner("dve"))
            block.scalar(runner("act"))
            block.gpsimd(runner("pool"))
            block.sync(runner("sp"))
        return cnt


def _host_consts():
    c = {}
    i128 = np.eye(128, dtype=np.float32)
    c["ident"] = i128
    s = np.arange(128)[:, None]
    t = np.arange(128)[None, :]
    m_incl = (s <= t).astype(np.float32)
    m_blk = ((s <= t) & ((s // 64) == (t // 64))).astype(np.float32)
    c["m128"] = np.tile(m_incl[:, None, :], (1, 4, 1)).reshape(128, 512)
    c["m64b"] = np.tile(m_blk[:, None, :], (1, 4, 1)).reshape(128, 512)
    m_b32 = ((s <= t) & ((s // 32) == (t // 32))).astype(np.float32)
    c["m32b"] = np.tile(m_b32[:, None, :], (1, 4, 1)).reshape(128, 512)
    m96 = np.zeros((128, 128), np.float32); m96[96:] = 1.0
    c["m96"] = m96
    s6 = np.arange(64)[:, None]
    t6 = np.arange(64)[None, :]
    z = np.zeros((128, 512), np.float32)
    a = z.copy(); a[:64] = np.tile((t6 < s6).astype(np.float32)[:, None, :], (1, 8, 1)).reshape(64, 512)
    c["r_ts"] = a
    a = z.copy(); a[:64] = np.tile((s6 < t6).astype(np.float32)[:, None, :], (1, 8, 1)).reshape(64, 512)
    c["r_st"] = a
    a = z.copy(); a[:64] = np.tile((s6 <= t6).astype(np.float32)[:, None, :], (1, 8, 1)).reshape(64, 512)
    c["r_sti"] = a
    a = z.copy(); a[:64] = np.tile(np.eye(64, dtype=np.float32)[:, None, :], (1, 8, 1)).reshape(64, 512)
    c["i8"] = a
    c["ones"] = np.ones((128, 128), np.float32)
    bd = np.zeros((128, 128), np.float32); bd[:64, :64] = 1; bd[64:, 64:] = 1
    c["bd"] = bd
    names = ["ident", "m128", "m32b", "r_ts", "r_st", "r_sti", "i8", "ones", "bd", "m96"]
    offs = {}
    o = 0
    for n in names:
        offs[n] = (o, c[n].shape[1]); o += c[n].shape[1]
    return np.concatenate([c[n] for n in names], axis=1), offs


CONSTS, COFF = _host_consts()
NCONST = CONSTS.shape[1]
CFO = {"ident": 0, "ones": 128, "bd": 256, "m96": 384}
NCF = 512
CSTF = np.concatenate([CONSTS[:, COFF[n][0]:COFF[n][0] + 128] for n in ("ident", "ones", "bd", "m96")], axis=1)


def _pack_params(inp, l):
    P = np.zeros((128, NPK), np.float32)

    def put(name, arr):
        o, w = PK[name]
        P[:arr.shape[0], o:o + arr.shape[1]] = arr

    fm = lambda v: np.ascontiguousarray(v.reshape(-1, 128).T)
    put("norm_g", fm(inp["norm_g"][l]))
    put("ada_b", fm(inp["ada_b"][l]))
    mu = inp["rw_mu"][l]
    put("mu", fm(mu[:1536]))
    put("mu_wc", mu[1536:1600].reshape(64, 1))
    put("mu_ac", mu[1600:1664].reshape(64, 1))
    put("w0", fm(inp["rw_w0"][l])); put("a0", fm(inp["rw_a0"][l]))
    put("k_k", fm(inp["rw_k_k"][l])); put("k_a", fm(inp["rw_k_a"][l])); put("r_k", fm(inp["rw_r_k"][l]))
    put("ln_g", fm(inp["rw_ln_g"][l])); put("ln_b", fm(inp["rw_ln_b"][l]))
    put("gk_b", fm(inp["gla_gk_b"][l])); put("gla_g", fm(inp["gla_norm_g"][l]))
    cw = inp["ml_conv_w"][l]
    put("conv", np.concatenate([fm(cw[j]) for j in range(4)], axis=1))
    put("ml_g", fm(inp["ml_norm_g"][l]))
    put("lb", np.concatenate([fm(inp["hg_lb_logits"][j]) for j in range(4)], axis=1))
    put("hg_g", fm(inp["hg_norm_g"][l]))
    put("i_b", fm(np.repeat(inp["ml_i_b"][l], 64)))
    put("f_b", fm(np.repeat(inp["ml_f_b"][l], 64)))
    return P


def build_nc(n_layers=DEPTH, branches=(0, 1, 2, 3), debug=False, stage=99):
    nc = bass.Bass("TRN2", target_bir_lowering=False)
    dram = lambda n, s, k="ExternalInput": nc.dram_tensor(n, list(s), F32, kind=k).ap()
    xT_d = dram("xT", [D, SEQ])
    c_d = dram("cT", [128, 8])
    pk_d = dram("pk", [DEPTH, 128, NPK])
    fg_d = dram("fg", [128, 8])
    cst_d = dram("cst", [128, NCONST])
    cstf_d = dram("cstf", [128, NCF])
    adaw_d = dram("ada_w", [DEPTH, D, 3 * D])
    win_d = dram("w_in", [DEPTH, D, NCOLS])
    wup_d = dram("rw_w_up", [DEPTH, 64, 512])
    aup_d = dram("rw_a_up", [DEPTH, 64, 512])
    gup_d = dram("gla_gk_up", [DEPTH, 16, 256])
    wbr_d = dram("w_branch", [DEPTH, 4, 512, D])
    wout_d = dram("w_out", [DEPTH, D, D])
    out_d = dram("outT", [D, SEQ], "ExternalOutput")
    dbg_d = dram("dbg", [4, 512, SEQ], "ExternalOutput") if debug else None

    es = contextlib.ExitStack()
    with es:
        T = lambda n, s, d=F32: es.enter_context(nc.sbuf_tensor("s_" + n, list(s), d))
        x = T("x", [128, 8, TS])
        uT = T("uT", [128, 8, TS], BF16)
        merged = T("merged", [128, 8, TS])
        mergedb = uT
        ys = T("ys", [128, 4, TS], BF16)
        pk = T("pk", [128, DEPTH, NPK])
        drv = T("drv", [128, 64])
        cst = T("cst", [128, NCF])
        cstb = T("cstb", [128, NCONST], BF16)
        cT = T("cT", [128, 8])
        fg = T("fg", [128, 8])
        mod = T("mod", [128, 24])
        lbt = T("lbt", [128, 16]); lbe = T("lbe", [128, 16]); lbs = T("lbs", [128, 4])
        NWB = 6
        wb = [T("wb%d" % i, [128, 8, 128], BF16) for i in range(NWB)]
        wv = [T("wv%d" % i, [128, 8, 512], BF16) for i in range(1)]
        wup = T("wup", [64, 512], BF16); aup = T("aup", [64, 512], BF16); gup = T("gup", [16, 256], BF16)
        NF = 14
        mtl = [T("mt%d" % i, [128, TS]) for i in range(2)]
        fs = [T("fs%d" % i, [128, TS + 4]) for i in range(NF)]
        LL = [T("ll%d" % i, [128, TS]) for i in range(8)]
        NB = 5
        bs = [T("bs%d" % i, [128, TS], BF16) for i in range(NB)]
        BL = [T("bl%d" % i, [128, TS], BF16) for i in range(20)]
        prc = T("prc", [128, 16])
        cqc = T("cqc", [128, 4, 4])
        gnb = [T("gn%d" % i, [128, TS + 1]) for i in range(4)]
        nref = T("nref", [128, 4, 16])
        ktz = T("ktz", [128, 2, 512], BF16)
        vtok = T("vtok", [128, 4, 512], BF16)
        ktok = T("ktok", [128, 4, 512], BF16)
        rv = {n: T("rv_" + n, [64, 512], BF16) for n in
              ("vt", "bt", "kt", "N", "NT", "N2", "NT2", "Y", "Y2", "Aak", "Arb", "Ark", "X", "U", "Yf")}
        rw_sf = T("rw_sf", [64, 8, 64]); rw_sb = T("rw_sb", [64, 8, 64], BF16)
        la_sf = [T("la_sf%d" % i, [128, 4, 128]) for i in range(4)]
        la_sb = [T("la_sb%d" % i, [128, 4, 128], BF16) for i in range(4)]
        gamh = T("gamh", [64, 8, 16])
        gam = T("gam", [128, 4, 16])
        yT = merged
        wcac = T("wcac", [64, 2, TS])
        wcacb = T("wcacb", [64, 2, TS], BF16)
        ps = [es.enter_context(nc.psum_tensor("ps%d" % i, [128, 512], F32)) for i in range(8)]

        S = Sched(nc)
        st = {"bank": 0, "w": 0, "fsi": 0, "bsi": 0, "wai": 0}

        def bank():
            i = st["bank"]; st["bank"] = (i + 1) % 6
            return ps[i], "ps%d" % i

        CS = lambda n: cst[:, CFO[n]:CFO[n] + 128]
        CB = lambda n: cstb[:, COFF[n][0]:COFF[n][0] + COFF[n][1]]
        PKc = lambda l, n, j=0, w=1: pk[:, l, PK[n][0] + j:PK[n][0] + j + w]

        def mm(out, lhsT, rhs, start, stop, r, w):
            S.op("pe", lambda e: e.matmul(out, lhsT, rhs, start=start, stop=stop), reads=r, writes=w)

        def tr(out, in_, ident, r, w):
            S.op("pe", lambda e: e.transpose(out, in_, ident), reads=r, writes=w)

        def act(out, in_, func, r, w, bias=None, scale=None):
            kw = {}
            if bias is not None:
                kw["bias"] = bias
            if scale is not None:
                kw["scale"] = scale
            S.op("act", lambda e: e.activation(out=out, in_=in_, func=func, **kw), reads=r, writes=w)

        def tt(out, a, b, op, r, w, eng="dve"):
            S.op(eng, lambda e: e.tensor_tensor(out=out, in0=a, in1=b, op=op), reads=r, writes=w)

        def tsc(out, a, s1, s2, op0, op1, r, w, eng="dve"):
            if op1 is None:
                S.op(eng, lambda e: e.tensor_scalar(out=out, in0=a, scalar1=s1, scalar2=None, op0=op0), reads=r, writes=w)
            else:
                S.op(eng, lambda e: e.tensor_scalar(out=out, in0=a, scalar1=s1, scalar2=s2, op0=op0, op1=op1),
                     reads=r, writes=w)

        def stt(out, a, sc, b, op0, op1, r, w, eng="dve"):
            S.op(eng, lambda e: e.scalar_tensor_tensor(out=out, in0=a, scalar=sc, in1=b, op0=op0, op1=op1),
                 reads=r, writes=w)

        def cp(out, in_, r, w, eng="dve"):
            S.op(eng, lambda e: e.tensor_copy(out=out, in_=in_), reads=r, writes=w)

        def rsq(out, in_, scale, bias, r, w):
            act(out, in_, AF.Ln, r, w, bias=bias, scale=scale)
            act(out, out, AF.Exp, w, w, scale=-0.5)

        def recip(out, in_, r, w):
            S.op("dve", lambda e: e.reciprocal(out=out, in_=in_), reads=r, writes=w)

        def mset(ap, val, w):
            S.op("dve", lambda e: e.memset(ap, val), writes=w)

        def dma(eng, out, in_, r, w, stream):
            return S.op(eng, lambda e: e.dma_start(out=out, in_=in_), reads=r, writes=w, dma=stream)

        def cumsum(ct, src, srck):
            S.op("dve", lambda e: e.tensor_tensor_scan(out=gnb[ct][:, 1:TS + 1], data0=src, data1=src, initial=0.0,
                                                       op0=ALU.add, op1=ALU.max), reads=[srck], writes=["gn%d" % ct])

        def wchunk(src, n, kc=8):
            i = st["w"]; st["w"] = (i + 1) % NWB
            key = "wb%d" % i
            dma("pool", wb[i][:, 0:kc, 0:n], src.rearrange("(k p) c -> p k c", p=128), [], [key], key)
            return wb[i], key

        def proj(l, c0, n):
            w, wk = wchunk(win_d[l, :, c0:c0 + n], n)
            b, bk = bank()
            for k in range(8):
                mm(b[0:n, :], w[:, k, 0:n], uT[:, k, :], k == 0, k == 7, [wk, "uT"], [bk])
            return b, bk

        def proj_tok(l, c0):
            for h in range(2):
                dma("pool", wv[0][:, :, h * 256:(h + 1) * 256],
                    win_d[l, :, c0 + h * 256:c0 + (h + 1) * 256].rearrange("(k p) c -> p k c", p=128),
                    [], ["wv_%d" % h], "wv_%d" % h)
            for tI in range(TS // 128):
                b, bk = bank()
                for k in range(8):
                    mm(b[:, :], uT[:, k, tI * 128:(tI + 1) * 128], wv[0][:, k, :], k == 0, k == 7,
                       ["wv_0", "wv_1", "uT"], [bk])
                if tI % 2 == 0:
                    act(vtok[:, tI, :], b[:, :], AF.Copy, [bk], ["vtok%d" % tI])
                else:
                    cp(vtok[:, tI, :], b[:, :], [bk], ["vtok%d" % tI])

        def fsa():
            i = st["fsi"]; st["fsi"] = (i + 1) % NF
            return fs[i], "fs%d" % i

        def bsa():
            i = st["bsi"]; st["bsi"] = (i + 1) % NB
            return bs[i], "bs%d" % i

        XK = ["x%d" % k for k in range(8)]

        def rms_to(sl_src, emit):
            b, bk = bank()
            for k in range(8):
                sq, sqk = bsa()
                act(sq[:, :], x[:, k, :], AF.Square, [XK[k]], [sqk])
                mm(b[:, :], CB("ones"), sq[:, :], k == 0, k == 7, [sqk, "cstb"], [bk])
            rs, rsk = fsa()
            rsq(rs[:, 0:TS], b[:, :], 1.0, 1024.0 * 1e-6, [bk], [rsk])
            for k in range(8):
                t1, t1k = fsa()
                tt(t1[:, 0:TS], x[:, k, :], rs[:, 0:TS], ALU.mult, [XK[k], rsk], [t1k])
                emit(k, t1, t1k)

        def program():
            dma("sp", cst[:, :], cstf_d, [], ["cst"], "cst")
            dma("pool", cstb[:, :], cst_d, [], ["cstb"], "cstb")
            dma("sp", pk[:, :, :], pk_d.rearrange("l p n -> p l n"), [], ["pk"], "pk")
            dma("sp", cT[:, :], c_d, [], ["cT"], "cT")
            dma("sp", fg[:, :], fg_d, [], ["fg"], "fg")
            act(cT[:, :], cT[:, :], AF.Silu, ["cT"], ["cT"])
            o_lb = PK["lb"][0]
            act(lbe[:, :], pk[:, 0, o_lb:o_lb + 16], AF.Exp, ["pk"], ["lbe"])
            tt(lbs[:, :], lbe[:, 0:4], lbe[:, 4:8], ALU.add, ["lbe"], ["lbs"])
            tt(lbs[:, :], lbs[:, :], lbe[:, 8:12], ALU.add, ["lbs", "lbe"], ["lbs"])
            tt(lbs[:, :], lbs[:, :], lbe[:, 12:16], ALU.add, ["lbs", "lbe"], ["lbs"])
            recip(lbs[:, :], lbs[:, :], ["lbs"], ["lbs"])
            mset(lbt[:, 0:4], 0.0, ["lbt"])
            for j in range(1, 4):
                t_, tk_ = fsa()
                tt(t_[:, 0:4], lbe[:, j * 4:(j + 1) * 4], lbs[:, :], ALU.mult, ["lbe", "lbs"], [tk_])
                tt(lbt[:, j * 4:(j + 1) * 4], lbt[:, (j - 1) * 4:j * 4], t_[:, 0:4], ALU.add, [tk_, "lbt"], ["lbt"])
            for i in range(4):
                mset(gnb[i][:, 0:1], 0.0, ["gn%d" % i])

            for l in range(n_layers):
                layer(l)

            outs = []
            for sI in range(NST):
                sl = slice(sI * TS, (sI + 1) * TS)
                for k in range(8):
                    dma("sp", x[:, k, :], (out_d if n_layers > 0 else xT_d)[k * 128:(k + 1) * 128, sl], ["xd%d" % sI], [XK[k]], "xl%d" % k)

                def emit(k, t1, t1k, sI=sI, sl=sl):
                    tsc(t1[:, 0:TS], t1[:, 0:TS], fg[:, k:k + 1], 32.0, ALU.mult, ALU.mult, [t1k, "fg"], [t1k])
                    outs.append(dma("sp", out_d[k * 128:(k + 1) * 128, sl], t1[:, 0:TS], [t1k], ["xd%d" % sI], "o_" + t1k))
                rms_to(sl, emit)
            return outs

        def layer(l):
            b, bk = bank()
            for j in range(24):
                i = st["wai"]; st["wai"] = (i + 1) % 3
                wa_i = merged[:, 2 * i:2 * i + 2, :].rearrange("p a (k c) -> p (a k) c", c=128)
                keys = ["mg%d" % (2 * i), "mg%d" % (2 * i + 1)]
                dma("sp", wa_i, adaw_d[l, :, j * 128:(j + 1) * 128].rearrange("(k p) c -> p k c", p=128),
                    [], keys, "wa%d" % i)
                for k in range(8):
                    mm(b[:, j:j + 1], wa_i[:, k, :], cT[:, k:k + 1], k == 0, k == 7, keys + ["cT"], [bk])
            tt(mod[:, :], b[:, 0:24], pk[:, l, PK["ada_b"][0]:PK["ada_b"][0] + 24], ALU.add, [bk, "pk"], ["mod"])
            stt(drv[:, 0:8], mod[:, 8:16], 1.0, pk[:, l, PK["norm_g"][0]:PK["norm_g"][0] + 8], ALU.add, ALU.mult,
                ["mod", "pk"], ["drv"])
            tsc(drv[:, 0:8], drv[:, 0:8], 32.0, None, ALU.mult, None, ["drv"], ["drv"])
            o_mu = PK["mu"][0]
            tsc(drv[:, 8:22], pk[:, l, o_mu:o_mu + 14], -1.0, 1.0, ALU.mult, ALU.add, ["pk"], ["drv"])
            tsc(drv[:, 22:26], PKc(l, "w0", 0, 4), -1.0, None, ALU.mult, None, ["pk"], ["drv"])
            tsc(drv[:, 26:30], PKc(l, "k_a", 0, 4), -1.0, 1.0, ALU.mult, ALU.add, ["pk"], ["drv"])
            tsc(drv[:, 30:32], PKc(l, "gk_b", 0, 2), -1.0, None, ALU.mult, None, ["pk"], ["drv"])
            tsc(drv[:, 32:34], PKc(l, "f_b", 0, 2), -1.0, None, ALU.mult, None, ["pk"], ["drv"])
            tsc(drv[:, 34:38], lbt[:, l * 4:(l + 1) * 4], -1.0, 1.0, ALU.mult, ALU.add, ["lbt"], ["drv"])
            tsc(drv[:, 38:42], PKc(l, "gla_g", 0, 4), float(np.sqrt(128.0)), None, ALU.mult, None, ["pk"], ["drv"])
            tsc(drv[:, 42:46], PKc(l, "hg_g", 0, 4), float(np.sqrt(128.0)), None, ALU.mult, None, ["pk"], ["drv"])
            dma("pool", wup[:, :], wup_d[l], [], ["wup"], "wup")
            dma("pool", aup[:, :], aup_d[l], [], ["aup"], "aup")
            dma("pool", gup[:, :], gup_d[l], [], ["gup"], "gup")
            mset(rw_sf[:, :, :], 0.0, ["rw_sf"])
            mset(rw_sb[:, :, :], 0.0, ["rw_sb"])
            for i in range(4):
                mset(la_sf[i][:, :, :], 0.0, ["la_sf%d" % i])
                mset(la_sb[i][:, :, :], 0.0, ["la_sb%d" % i])
            mset(prc[:, :], 0.0, ["prc"])
            mset(cqc[:, :, :], 0.0, ["cqc"])

            src_d = xT_d if l == 0 else out_d
            for sI in range(NST):
                sl = slice(sI * TS, (sI + 1) * TS)
                for k in range(8):
                    dma("sp", x[:, k, :], src_d[k * 128:(k + 1) * 128, sl], ["xd%d" % sI], [XK[k]], "xl%d" % k)

                def emit(k, t1, t1k):
                    act(uT[:, k, :], t1[:, 0:TS], AF.Identity, [t1k, "drv", "mod"], ["uT"],
                        bias=mod[:, k:k + 1], scale=drv[:, k:k + 1])
                rms_to(sl, emit)
                def merge_gen(m):
                    for fc in range(8):
                        pg, pgk = proj(l, C_MG + m * 1024 + fc * 128, 128)
                        sg, sgk = mtl[fc % 2], "mt%d" % (fc % 2)
                        act(sg[:, :], pg[:, :], AF.Sigmoid, [pgk], [sgk])
                        w, wk = wchunk(wbr_d[l, m, :, fc * 128:(fc + 1) * 128], 128, kc=4)
                        pb, pbk = bank()
                        for k in range(4):
                            mm(pb[:, :], w[:, k, :], ys[:, k, :], k == 0, k == 3, [wk, "ys"], [pbk])
                        if m == 0:
                            tt(merged[:, fc, :], pb[:, :], sg[:, :], ALU.mult, [pbk, sgk], ["mg%d" % fc])
                        else:
                            tt(sg[:, :], pb[:, :], sg[:, :], ALU.mult, [pbk, sgk], [sgk])
                            tt(merged[:, fc, :], merged[:, fc, :], sg[:, :], ALU.add, [sgk, "mg%d" % fc], ["mg%d" % fc])
                        yield

                def zero_gen():
                    for _ in range(9):
                        yield
                    mset(ys[:, :, :], 0.0, ["ys"])
                    yield

                g_prev = None
                for m in range(4):
                    gm = (rwkv, gla, mlstm, hgrn)[m](l, sI) if m in branches else zero_gen()
                    drive([gm, g_prev])
                    if debug and l == 0:
                        for k in range(4):
                            dma("pool", dbg_d[m, k * 128:(k + 1) * 128, sl], ys[:, k, :], ["ys"], [], "dbg%d" % k)
                    g_prev = merge_gen(m)
                drive([g_prev])
                for fc in range(8):
                    act(mergedb[:, fc, :], merged[:, fc, :], AF.Copy, ["mg%d" % fc], ["uT"])
                for fc in range(8):
                    w, wk = wchunk(wout_d[l, :, fc * 128:(fc + 1) * 128], 128)
                    pb, pbk = bank()
                    for k in range(8):
                        mm(pb[:, :], w[:, k, :], mergedb[:, k, :], k == 0, k == 7, [wk, "uT"], [pbk])
                    stt(x[:, fc, :], pb[:, :], mod[:, 16 + fc:17 + fc], x[:, fc, :], ALU.mult, ALU.add,
                        [pbk, "mod", XK[fc]], [XK[fc]])
                    dma("sp", out_d[fc * 128:(fc + 1) * 128, sl], x[:, fc, :], [XK[fc]], ["xd%d" % sI], "xs%d" % fc)

        def shift_hi(src_ap, skey, dst_ap, dkey, n, fp32=False):
            b, bk = bank()
            idn = (CS if fp32 else CB)("ident")
            mm(b[0:64, 0:n], idn[:, 64:128], src_ap, True, True, [skey, "cst" if fp32 else "cstb"], [bk])
            cp(dst_ap, b[0:64, 0:n], [bk], [dkey])

        def build_gamh(npair, nch):
            for ct in range(npair):
                cp(gamh[:, 2 * ct, 0:nch], gam[0:64, ct, 0:nch], ["gam"], ["gamh"])
                shift_hi(gam[:, ct, 0:nch], "gam", gamh[:, 2 * ct + 1, 0:nch], "gamh", nch, fp32=True)

        def drive(gens):
            gens = [g for g in gens if g is not None]
            while gens:
                for g in list(gens):
                    try:
                        next(g)
                    except StopIteration:
                        gens.remove(g)

        def la(Qh, Kh, KTp, C, dk, si, gm, gmk, fin, ni=None):
            mask = {128: CB("m128"), 32: CB("m32b")}[C]
            sfk, sbk = "la_sf%d" % si, "la_sb%d" % si
            nkt = len(KTp)
            PT = {}

            def phA(tI):
                tsl = slice(tI * 128, (tI + 1) * 128)
                bt, btk = bank()
                btb = bt[:, :].bitcast(BF16)
                for ct in range(nkt):
                    tr(btb[:, ct * 128:(ct + 1) * 128], KTp[ct][0][:, tsl], CB("ident"), [KTp[ct][1], "cstb"], [btk])
                cp(ktok[:, tI, 0:nkt * 128], btb[:, 0:nkt * 128], [btk], ["ktok%d" % tI])
                if C == 32:
                    tsc(ktz[64:128, tI % 2, 0:nkt * 128], ktok[64:128, tI, 0:nkt * 128], CS("m96")[64:128, 0:1], None,
                        ALU.mult, None, ["ktok%d" % tI, "cst"], ["ktz%d" % (tI % 2)])
                yield
                bsc, bsck = bank()
                for h in range(4):
                    mm(bsc[:, h * 128:(h + 1) * 128], Kh[h][0][:, tsl], Qh[h][0][:, tsl], True, True,
                       [Kh[h][1], Qh[h][1]], [bsck])
                pt, ptk = bsa()
                tt(pt[:, :], bsc[:, :], mask, ALU.mult, [bsck, "cstb"], [ptk])
                PT[tI] = (pt, ptk)
                yield

            def phB(tI):
                pt, ptk = PT[tI]
                po, pok = ps[6], "ps6"
                pd, pdk = (ps[7], "ps7") if ni is not None else (None, None)
                for cc in range(128 // C):
                    cidx = tI * (128 // C) + cc
                    csl = slice(cc * C, (cc + 1) * C)
                    gsl = slice(tI * 128 + cc * C, tI * 128 + (cc + 1) * C)
                    for h in range(4):
                        osl = slice(h * 128 + cc * C, h * 128 + (cc + 1) * C)
                        mm(po[:, osl], vtok[:, tI, h * 128:(h + 1) * 128], pt[:, osl], True, False,
                           ["vtok%d" % tI, ptk], [pok])
                        mm(po[:, osl], la_sb[si][0:dk, h, :], Qh[h][0][:, gsl], False, True, [sbk, Qh[h][1]], [pok])
                        if ni is not None:
                            mm(pd[:, osl], CB("ones"), pt[:, osl], True, False, ["cstb", ptk], [pdk])
                            mm(pd[:, osl], la_sb[ni][0:dk, h, :], Qh[h][0][:, gsl], False, True,
                               ["la_sb%d" % ni, Qh[h][1]], [pdk])
                    for sidx, isn in ((si, False),) + (((ni, True),) if ni is not None else ()):
                        bu, buk = bank()
                        for h in range(4):
                            if C == 32 and cc == 3:
                                zsl = slice(64, 128)
                                mm(bu[0:dk, h * 128:(h + 1) * 128], ktz[zsl, tI % 2, h * dk:(h + 1) * dk],
                                   vtok[zsl, tI, h * 128:(h + 1) * 128], True, True,
                                   ["ktz%d" % (tI % 2), "vtok%d" % tI], [buk])
                            else:
                                rhs = CB("ones")[csl, :] if isn else vtok[csl, tI, h * 128:(h + 1) * 128]
                                mm(bu[0:dk, h * 128:(h + 1) * 128], ktok[csl, tI, h * dk:(h + 1) * dk], rhs, True, True,
                                   ["ktok%d" % tI, "vtok%d" % tI, "cstb"], [buk])
                        tmp_, tmpk = fsa()
                        tv = tmp_[0:dk, 0:512].rearrange("p (h v) -> p h v", h=4)
                        tt(tv, bu[0:dk, :].rearrange("p (h v) -> p h v", h=4), la_sf[sidx][0:dk, :, :], ALU.add,
                           [buk, "la_sf%d" % sidx], [tmpk])
                        gb = gm[0:dk, 0:4, cidx:cidx + 1].to_broadcast([dk, 4, 128])
                        tt(la_sb[sidx][0:dk, :, :], tv, gb, ALU.mult, [tmpk, gmk], ["la_sb%d" % sidx])
                        tt(la_sf[sidx][0:dk, :, :], tv, gb, ALU.mult, [tmpk, gmk], ["la_sf%d" % sidx])
                    yield
                fin(tI, po, pok, pd, pdk)
                yield

            NT_ = TS // 128
            drive([phA(0)])
            for tI in range(NT_):
                drive([phB(tI), phA(tI + 1) if tI + 1 < NT_ else None])

        def gam_start(ct, C):
            n = TS // C
            d_, dk_ = fsa()
            tt(d_[:, 0:n], gnb[ct][:, C:TS + 1:C], gnb[ct][:, 0:TS:C], ALU.subtract, ["gn%d" % ct], [dk_])
            act(gam[:, ct, 0:n], d_[:, 0:n], AF.Exp, [dk_], ["gam"], scale=-1.0)

        def qk_decay(ct, q_ap, qk_, k_ap, kk_, C, mid, Qd, Kd, kextra=None, clamp=False):
            gk = "gn%d" % ct
            n = TS // C
            off = C // 2 if mid else 0
            tsc(nref[:, ct, 0:n], gnb[ct][:, off:TS:C], -1.0, None, ALU.mult, None, [gk], ["nref"])
            ei, eik = fsa(); ev, evk = fsa()
            for c in range(n):
                c0 = c * C
                rc = c0 + off
                act(ei[:, c0:c0 + C], gnb[ct][:, c0 + 1:c0 + C + 1], AF.Exp, [gk], [eik], bias=gnb[ct][:, rc:rc + 1], scale=-1.0)
                if kextra is None:
                    act(ev[:, c0:c0 + C], gnb[ct][:, c0 + 1:c0 + C + 1], AF.Exp, [gk, "nref"], [evk],
                        bias=nref[:, ct, c:c + 1], scale=1.0)
                else:
                    act(ev[:, c0:c0 + C], kextra[0][:, c0:c0 + C], AF.Exp, [kextra[1], "nref"], [evk],
                        bias=nref[:, ct, c:c + 1], scale=1.0)
            if clamp:
                tsc(ei[:, 0:TS], ei[:, 0:TS], 2.35e17, None, ALU.min, None, [eik], [eik])
                tsc(ev[:, 0:TS], ev[:, 0:TS], 2.35e17, None, ALU.min, None, [evk], [evk])
            tt(Qd[0][:, :], q_ap, ei[:, 0:TS], ALU.mult, [qk_, eik], [Qd[1]])
            tt(Kd[0][:, :], k_ap, ev[:, 0:TS], ALU.mult, [kk_, evk], [Kd[1]])

        BLk = lambda i: (BL[i], "bl%d" % i)
        LLk = lambda i: (LL[i], "ll%d" % i)

        def zgate(l, c0, h, func_scale_ap, skeys):
            pz, pzk = proj(l, c0 + h * 128, 128)
            z, zk = LLk(h)
            act(z[:, :], pz[:, :], AF.Silu, [pzk], [zk])
            if func_scale_ap is not None:
                tsc(z[:, :], z[:, :], func_scale_ap, None, ALU.mult, None, [zk] + skeys, [zk])
            return z, zk

        def rms_fin(GZ):
            def fin(tI, po, pok, pd, pdk):
                sq, sqk = bsa()
                act(sq[:, :], po[:, :], AF.Square, [pok], [sqk])
                b, bk = bank()
                mm(b[:, :], CB("ones"), sq[:, :], True, True, [sqk, "cstb"], [bk])
                rs, rsk = fsa()
                rsq(rs[:, 0:TS], b[:, :], 1.0, 128.0 * 1e-6, [bk], [rsk])
                t1, t1k = fsa()
                tt(t1[:, 0:TS], po[:, :], rs[:, 0:TS], ALU.mult, [pok, rsk], [t1k])
                for h in range(4):
                    tt(ys[:, h, tI * 128:(tI + 1) * 128], t1[:, h * 128:(h + 1) * 128],
                       GZ[h][0][:, tI * 128:(tI + 1) * 128], ALU.mult, [t1k, GZ[h][1]], ["ys"])
            return fin

        def heads64(QT, KT):
            Qh, Kh = [], []
            for ct in range(2):
                shift_hi(QT[ct][0][:, :], QT[ct][1], BL[16 + ct][0:64, :], "bl%d" % (16 + ct), TS)
                shift_hi(KT[ct][0][:, :], KT[ct][1], BL[18 + ct][0:64, :], "bl%d" % (18 + ct), TS)
                Qh += [(QT[ct][0][0:64, :], QT[ct][1]), (BL[16 + ct][0:64, :], "bl%d" % (16 + ct))]
                Kh += [(KT[ct][0][0:64, :], KT[ct][1]), (BL[18 + ct][0:64, :], "bl%d" % (18 + ct))]
            return Qh, Kh

        def gla(l, sI):
            proj_tok(l, C_GV)
            yield
            pg, pgk = proj(l, C_GC, 16)
            gc, gck = bsa()
            act(gc[0:16, 0:TS], pg[0:16, :], AF.Copy, [pgk], [gck])
            yield
            QT, KT = [], []
            for ct in range(2):
                yield
                b, bk = bank()
                mm(b[:, :], gup[:, ct * 128:(ct + 1) * 128], gc[0:16, 0:TS], True, True, ["gup", gck], [bk])
                e1, e1k = fsa()
                act(e1[:, 0:TS], b[:, :], AF.Exp, [bk, "drv"], [e1k], bias=drv[:, 30 + ct:31 + ct], scale=-1.0)
                act(e1[:, 0:TS], e1[:, 0:TS], AF.Ln, [e1k], [e1k], bias=1.0)
                tsc(e1[:, 0:TS], e1[:, 0:TS], 1.0 / 16.0, None, ALU.mult, None, [e1k], [e1k])
                cumsum(ct, e1[:, 0:TS], e1k)
                gam_start(ct, 128)
                yield
                pq, pqk = proj(l, C_GQ + ct * 128, 128)
                q, qk_ = fsa()
                act(q[:, 0:TS], pq[:, :], AF.Copy, [pqk], [qk_], scale=0.125)
                yield
                pk_, pkk = proj(l, C_GK + ct * 128, 128)
                k, kk_ = fsa()
                cp(k[:, 0:TS], pk_[:, :], [pkk], [kk_])
                qk_decay(ct, q[:, 0:TS], qk_, k[:, 0:TS], kk_, 128, False, BLk(ct), BLk(4 + ct))
                QT.append(BLk(ct)); KT.append(BLk(4 + ct))
            GZ = []
            for h in range(4):
                GZ.append(zgate(l, C_GZ, h, drv[:, 38 + h:39 + h], ["drv"]))
                yield
            Qh, Kh = heads64(QT, KT)
            build_gamh(2, 4)
            la(Qh, Kh, KT, 128, 64, 0, gamh, "gamh", rms_fin(GZ))

        def hgrn(l, sI):
            proj_tok(l, C_HI)
            yield
            QT, KT = [], []
            for ct in range(4):
                yield
                pf, pfk = proj(l, C_HF + ct * 128, 128)
                g, gk_ = fsa()
                act(g[:, 0:TS], pf[:, :], AF.Sigmoid, [pfk], [gk_])
                tsc(g[:, 0:TS], g[:, 0:TS], drv[:, 34 + ct:35 + ct], lbt[:, l * 4 + ct:l * 4 + ct + 1], ALU.mult, ALU.add,
                    [gk_, "drv", "lbt"], [gk_])
                lg, lgk = fsa()
                act(lg[:, 0:TS], g[:, 0:TS], AF.Ln, [gk_], [lgk])
                tsc(lg[:, 0:TS], lg[:, 0:TS], -1.0, None, ALU.mult, None, [lgk], [lgk])
                cumsum(ct, lg[:, 0:TS], lgk)
                d_, dk_ = fsa()
                tt(d_[:, 0:15], gnb[ct][:, 48:TS:32], gnb[ct][:, 16:TS - 32:32], ALU.subtract, ["gn%d" % ct], [dk_])
                tt(d_[:, 15:16], gnb[ct][:, TS:TS + 1], gnb[ct][:, TS - 16:TS - 15], ALU.subtract, ["gn%d" % ct], [dk_])
                act(gam[:, ct, 0:16], d_[:, 0:16], AF.Exp, [dk_], ["gam"], scale=-1.0)
                fc_, fck = fsa()
                act(fc_[:, 0:1], gnb[ct][:, 16:17], AF.Exp, ["gn%d" % ct], [fck], scale=-1.0)
                tsc(la_sf[3][:, ct, :], la_sf[3][:, ct, :], fc_[:, 0:1], None, ALU.mult, None,
                    [fck, "la_sf3"], ["la_sf3"])
                cp(la_sb[3][:, ct, :], la_sf[3][:, ct, :], ["la_sf3"], ["la_sb3"])
                tsc(g[:, 0:TS], g[:, 0:TS], -1.0, 1.0, ALU.mult, ALU.add, [gk_], [gk_])
                yield
                pq, pqk = proj(l, C_HQ + ct * 128, 128)
                q, qk_ = fsa()
                act(q[:, 0:TS], pq[:, :], AF.Silu, [pqk], [qk_])
                qk_decay(ct, q[:, 0:TS], qk_, g[:, 0:TS], gk_, 32, True, BLk(ct), BLk(4 + ct), clamp=True)
                QT.append(BLk(ct)); KT.append(BLk(4 + ct))
            GZ = []
            for h in range(4):
                GZ.append(zgate(l, C_HZ, h, drv[:, 42 + h:43 + h], ["drv"]))
                yield
            la(QT, KT, KT, 32, 128, 3, gam, "gam", rms_fin(GZ))

        def mlstm(l, sI):
            proj_tok(l, C_MV)
            yield
            w8, w8k = wchunk(win_d[l, :, C_MI:C_MI + 8], 8)
            reps = {}
            for ct in range(2):
                for gI in range(2):
                    for half in range(2):
                        r_, rk_ = BLk(8 + ct * 4 + gI * 2 + half)
                        rv_ = r_[:, :].rearrange("p (k j m) -> p k j m", k=4, j=2, m=64)
                        src = w8[:, half * 4:(half + 1) * 4, gI * 4 + ct * 2:gI * 4 + ct * 2 + 2]
                        cp(rv_, src.unsqueeze(3).to_broadcast([128, 4, 2, 64]), [w8k], [rk_])
                        reps[(ct, gI, half)] = (r_, rk_)
            for ct in range(4):
                yield
                pc, pck = proj(l, C_MQK + ct * 128, 128)
                cb, cbk = fsa()
                cp(cb[:, 0:3], cqc[:, ct, 0:3], ["cqc"], [cbk])
                act(cb[:, 3:TS + 3], pc[:, :], AF.Copy, [pck], [cbk])
                a, ak = fsa()
                oc = PK["conv"][0]
                tsc(a[:, 0:TS], cb[:, 0:TS], pk[:, l, oc + ct:oc + ct + 1], None, ALU.mult, None, [cbk, "pk"], [ak])
                for j in range(1, 4):
                    stt(a[:, 0:TS], cb[:, j:j + TS], pk[:, l, oc + j * 4 + ct:oc + j * 4 + ct + 1], a[:, 0:TS], ALU.mult,
                        ALU.add, [cbk, "pk", ak], [ak])
                cp(cqc[:, ct, 0:3], cb[:, TS:TS + 3], [cbk], ["cqc"])
                act(LL[4 + ct][:, :], a[:, 0:TS], AF.Silu, [ak], ["ll%d" % (4 + ct)])
            QT, KT = [], []
            for ct in range(2):
                yield
                pis = []
                for gI in range(2):
                    b, bk = bank()
                    for k in range(8):
                        r_, rk_ = reps[(ct, gI, k // 4)]
                        mm(b[:, :], r_[:, (k % 4) * 128:(k % 4 + 1) * 128], uT[:, k, :], k == 0, k == 7, [rk_, "uT"], [bk])
                    pis.append((b, bk))
                e1, e1k = fsa()
                act(e1[:, 0:TS], pis[1][0][:, :], AF.Exp, [pis[1][1], "drv"], [e1k], bias=drv[:, 32 + ct:33 + ct], scale=-1.0)
                act(e1[:, 0:TS], e1[:, 0:TS], AF.Ln, [e1k], [e1k], bias=1.0)
                cumsum(ct, e1[:, 0:TS], e1k)
                gam_start(ct, 128)
                ig, igk = fsa()
                act(ig[:, 0:TS], pis[0][0][:, :], AF.Identity, [pis[0][1], "pk"], [igk], bias=PKc(l, "i_b", ct))
                tt(ig[:, 0:TS], ig[:, 0:TS], gnb[ct][:, 1:TS + 1], ALU.add, [igk, "gn%d" % ct], [igk])
                k, kk_ = fsa()
                tsc(k[:, 0:TS], LL[6 + ct][:, :], 0.125, None, ALU.mult, None, ["ll%d" % (6 + ct)], [kk_])
                qk_decay(ct, LL[4 + ct][:, :], "ll%d" % (4 + ct), k[:, 0:TS], kk_, 128, False, BLk(ct), BLk(4 + ct),
                         kextra=(ig, igk))
                QT.append(BLk(ct)); KT.append(BLk(4 + ct))
            GZ = []
            for h in range(4):
                GZ.append(zgate(l, C_MZ, h, PKc(l, "ml_g", h), ["pk"]))
                yield

            def fin(tI, po, pok, pd, pdk):
                den, dnk = fsa()
                act(den[:, 0:TS], pd[:, :], AF.Abs, [pdk], [dnk])
                tsc(den[:, 0:TS], den[:, 0:TS], 1.0, None, ALU.max, None, [dnk], [dnk])
                recip(den[:, 0:TS], den[:, 0:TS], [dnk], [dnk])
                o, ok = fsa()
                tt(o[:, 0:TS], po[:, :], den[:, 0:TS], ALU.mult, [pok, dnk], [ok])
                b, bk = bank()
                mm(b[:, :], CS("ones"), o[:, 0:TS], True, True, [ok, "cst"], [bk])
                d, dk_ = fsa()
                stt(d[:, 0:TS], b[:, :], -1.0 / 128.0, o[:, 0:TS], ALU.mult, ALU.add, [bk, ok], [dk_])
                sq, sqk = fsa()
                act(sq[:, 0:TS], d[:, 0:TS], AF.Square, [dk_], [sqk])
                b2, b2k = bank()
                mm(b2[:, :], CS("ones"), sq[:, 0:TS], True, True, [sqk, "cst"], [b2k])
                rs, rsk = fsa()
                rsq(rs[:, 0:TS], b2[:, :], 1.0 / 128.0, 1e-6, [b2k], [rsk])
                tt(d[:, 0:TS], d[:, 0:TS], rs[:, 0:TS], ALU.mult, [dk_, rsk], [dk_])
                for h in range(4):
                    tt(ys[:, h, tI * 128:(tI + 1) * 128], d[:, h * 128:(h + 1) * 128],
                       GZ[h][0][:, tI * 128:(tI + 1) * 128], ALU.mult, [dk_, GZ[h][1]], ["ys"])

            Qh, Kh = heads64(QT, KT)
            build_gamh(2, 4)
            la(Qh, Kh, KT, 128, 64, 1, gamh, "gamh", fin, ni=2)

        def rwkv(l, sI):
            C = 64
            NCH = TS // C

            def mixed(idx, c0, n, mucol, omcol, dst=None):
                pp, ppk = proj(l, c0, n)
                pb_, pbk_ = fsa()
                cp(pb_[0:n, 0:1], prc[0:n, idx:idx + 1], ["prc"], [pbk_])
                act(pb_[0:n, 1:TS + 1], pp[0:n, :], AF.Copy, [ppk], [pbk_])
                t_, tk_ = fsa()
                tsc(t_[0:n, 0:TS], pb_[0:n, 0:TS], mucol, None, ALU.mult, None, [pbk_, "pk"], [tk_])
                if dst is None:
                    o_, ok_ = fsa()
                    o_ = o_[:, 0:TS]
                else:
                    o_, ok_ = dst
                stt(o_[0:n, :], pb_[0:n, 1:TS + 1], omcol, t_[0:n, 0:TS], ALU.mult, ALU.add, [pbk_, "drv", tk_], [ok_])
                cp(prc[0:n, idx:idx + 1], pb_[0:n, TS:TS + 1], [pbk_], ["prc"])
                return o_, ok_

            WC = mixed(12, C_RW + 1536, 64, pk[0:64, l, PK["mu_wc"][0]:PK["mu_wc"][0] + 1], drv[0:64, 20:21],
                       dst=(wcac[:, 0, :], "wc"))
            AC = mixed(13, C_RW + 1600, 64, pk[0:64, l, PK["mu_ac"][0]:PK["mu_ac"][0] + 1], drv[0:64, 21:22],
                       dst=(wcac[:, 1, :], "ac"))
            yield
            act(wcacb[:, 0, :], WC[0][0:64, :], AF.Tanh, ["wc"], ["wcb"])
            cp(wcacb[:, 1, :], AC[0][0:64, :], ["ac"], ["acb"])
            ops = []
            BON = []
            for ct in range(4):
                csl = slice(ct * 128, (ct + 1) * 128)
                yield
                Kx, Kxk = mixed(4 + ct, C_RW + 512 + ct * 128, 128, PKc(l, "mu", 4 + ct), drv[:, 12 + ct:13 + ct])
                kk, kkk = fsa()
                tsc(kk[:, 0:TS], Kx[:, :], PKc(l, "k_k", ct), None, ALU.mult, None, [Kxk, "pk"], [kkk])
                sq, sqk = fsa()
                act(sq[:, 0:TS], kk[:, 0:TS], AF.Square, [kkk], [sqk])
                b3, b3k = bank()
                mm(b3[:, :], CS("bd"), sq[:, 0:TS], True, True, ["cst", sqk], [b3k])
                rsq(sq[:, 0:TS], b3[:, :], 1.0, 1e-24, [b3k], [sqk])
                tt(kk[:, 0:TS], kk[:, 0:TS], sq[:, 0:TS], ALU.mult, [kkk, sqk], [kkk])
                b, bk = bank()
                mm(b[:, :], wup[:, csl], wcacb[:, 0, :], True, True, ["wup", "wcb"], [bk])
                e1, e1k = fsa()
                act(e1[:, 0:TS], b[:, :], AF.Exp, [bk, "drv"], [e1k], bias=drv[:, 22 + ct:23 + ct], scale=-1.0)
                act(e1[:, 0:TS], e1[:, 0:TS], AF.Ln, [e1k], [e1k], bias=1.0)
                act(e1[:, 0:TS], e1[:, 0:TS], AF.Exp, [e1k], [e1k], bias=-0.5, scale=-1.0)
                cumsum(ct, e1[:, 0:TS], e1k)
                gk = "gn%d" % ct
                tsc(nref[:, ct, 0:NCH], gnb[ct][:, 0:TS:C], -1.0, None, ALU.mult, None, [gk], ["nref"])
                yield
                b2, b2k = bank()
                mm(b2[:, :], aup[:, csl], wcacb[:, 1, :], True, True, ["aup", "acb"], [b2k])
                sg, sgk = fsa()
                act(sg[:, 0:TS], b2[:, :], AF.Sigmoid, [b2k, "pk"], [sgk], bias=PKc(l, "a0", ct))
                t1, t1k = fsa()
                tsc(t1[:, 0:TS], sg[:, 0:TS], PKc(l, "k_a", ct), drv[:, 26 + ct:27 + ct], ALU.mult, ALU.add,
                    [sgk, "pk", "drv"], [t1k])
                tt(Kx[:, :], Kx[:, :], t1[:, 0:TS], ALU.mult, [Kxk, t1k], [Kxk])
                tt(sg[:, 0:TS], kk[:, 0:TS], sg[:, 0:TS], ALU.mult, [kkk, sgk], [sgk])
                ei, eik = fsa(); ee, eek = fsa(); ev, evk = fsa()
                for c in range(NCH):
                    c0 = c * C
                    act(ei[:, c0:c0 + C], gnb[ct][:, c0 + 1:c0 + C + 1], AF.Exp, [gk], [eik], bias=gnb[ct][:, c0:c0 + 1], scale=-1.0)
                    act(ee[:, c0:c0 + C], gnb[ct][:, c0:c0 + C], AF.Exp, [gk], [eek], bias=gnb[ct][:, c0:c0 + 1], scale=-1.0)
                    act(ev[:, c0:c0 + C], gnb[ct][:, c0 + 1:c0 + C + 1], AF.Exp, [gk, "nref"], [evk],
                        bias=nref[:, ct, c:c + 1], scale=1.0)
                rt, at, btl, ktl, vb = [BLk(ct * 5 + j) for j in range(5)]
                stt(at[0][:, :], kk[:, 0:TS], -1.0, ee[:, 0:TS], ALU.mult, ALU.mult, [kkk, eek], [at[1]])
                tt(btl[0][:, :], sg[:, 0:TS], ev[:, 0:TS], ALU.mult, [sgk, evk], [btl[1]])
                tt(ktl[0][:, :], Kx[:, :], ev[:, 0:TS], ALU.mult, [Kxk, evk], [ktl[1]])
                cp(gam[:, ct, 0:NCH], ei[:, C - 1:TS:C], [eik], ["gam"])
                yield
                Rx, Rxk = mixed(ct, C_RW + ct * 128, 128, PKc(l, "mu", ct), drv[:, 8 + ct:9 + ct])
                tt(rt[0][:, :], Rx[:, :], ei[:, 0:TS], ALU.mult, [Rxk, eik], [rt[1]])
                rk, rkk = fsa()
                stt(rk[:, 0:TS], Rx[:, :], PKc(l, "r_k", ct), Kx[:, :], ALU.mult, ALU.mult, [Rxk, "pk", Kxk], [rkk])
                b4, b4k = bank()
                mm(b4[:, :], CS("bd"), rk[:, 0:TS], True, True, ["cst", rkk], [b4k])
                Vx, Vxk = mixed(8 + ct, C_RW + 1024 + ct * 128, 128, PKc(l, "mu", 8 + ct), drv[:, 16 + ct:17 + ct])
                cp(vb[0][:, :], Vx[:, :], [Vxk], [vb[1]])
                tt(LL[ct][:, :], b4[:, :], Vx[:, :], ALU.mult, [b4k, Vxk], ["ll%d" % ct])
                BON.append(LLk(ct))
                ops.append(dict(r=rt, a=at, b=btl, k=ktl, v=vb))

            hd = lambda h: (h // 2, (h % 2) * 64)
            SH = {}
            mhb = merged[:, 4:8, :].bitcast(BF16)
            for ct in range(4):
                for ni_, nm in enumerate(("r", "a", "b", "k")):
                    j = ct * 4 + ni_
                    if j < 8:
                        dst, dkey = mhb[:, j // 2, (j % 2) * 512:(j % 2 + 1) * 512], "mg%d" % (4 + j // 2)
                    elif j < 12:
                        dst, dkey = vtok[:, j - 8, :], "vtok%d" % (j - 8)
                    else:
                        dst, dkey = ktok[:, j - 12, :], "ktok%d" % (j - 12)
                    shift_hi(ops[ct][nm][0][:, :], ops[ct][nm][1], dst[0:64, :], dkey, TS)
                    SH[(ct, nm)] = (dst, dkey)
            build_gamh(4, NCH)

            def opn(nm, h):
                ct, par = h // 2, h % 2
                if par == 0:
                    return ops[ct][nm][0][0:64, :], ops[ct][nm][1]
                return SH[(ct, nm)][0][0:64, :], SH[(ct, nm)][1]

            llb = [LL[4 + i][:, :].bitcast(BF16) for i in range(4)]
            SETB = ("vt", "bt", "kt", "Aak", "Arb", "Ark", "Yf")
            rvs = [dict(rv), dict(rv)]
            rvk = [{n: "rv_" + n for n in rv}, {n: "rv_" + n for n in rv}]
            for i, n in enumerate(SETB):
                rvs[1][n] = llb[i // 2][0:64, (i % 2) * 512:(i % 2 + 1) * 512]
                rvk[1][n] = "ll%d" % (4 + i // 2)

            def phaseA(c):
                R, RK = rvs[c % 2], rvk[c % 2]
                c0 = c * C
                cs = slice(c0, c0 + C)
                for nm, key in (("v", "vt"), ("b", "bt"), ("k", "kt")):
                    bt_, btk2 = bank()
                    btb = bt_[:, :].bitcast(BF16)
                    for ct in range(4):
                        tr(btb[0:64, ct * 128:(ct + 1) * 128], ops[ct][nm][0][:, cs], CB("ident"),
                           [ops[ct][nm][1], "cstb"], [btk2])
                    cp(R[key][:, :], btb[0:64, 0:512], [btk2], [RK[key]])
                yield

                def scores(lhs, rhs, dst, maskn, eng):
                    b_, bk_ = bank()
                    for h in range(8):
                        L_, R_ = opn(lhs, h), opn(rhs, h)
                        mm(b_[0:64, h * 64:(h + 1) * 64], L_[0][:, cs], R_[0][:, cs], True, True, [L_[1], R_[1]], [bk_])
                    tt(R[dst][:, :], b_[0:64, :], CB(maskn)[0:64, :], ALU.mult, [bk_, "cstb"], [RK[dst]])
                scores("a", "b", "N", "r_ts", "dve")
                scores("b", "a", "NT", "r_st", "dve")
                yield
                scores("k", "a", "Aak", "r_st", "dve")
                scores("b", "r", "Arb", "r_sti", "dve")
                scores("k", "r", "Ark", "r_sti", "dve")
                tt(R["Y"][:, :], R["NT"][:, :], CB("i8")[0:64, :], ALU.add, [RK["NT"], "cstb"], [RK["Y"]])
                yield
                M, MT, Y = "N", "NT", "Y"
                M2, MT2, Y2 = "N2", "NT2", "Y2"
                for lev in range(5):
                    last = lev == 4
                    ba, bak = bank()
                    for h in range(8):
                        hs = slice(h * 64, (h + 1) * 64)
                        mm(ba[0:64, hs], R[MT][:, hs], R[M][:, hs], True, True, [RK[MT], RK[M]], [bak])
                    act(R[M2][:, :], ba[0:64, :], AF.Copy, [bak], [RK[M2]])
                    if not last:
                        bb_, bbk_ = bank()
                        for h in range(8):
                            hs = slice(h * 64, (h + 1) * 64)
                            mm(bb_[0:64, hs], R[M][:, hs], R[MT][:, hs], True, True, [RK[MT], RK[M]], [bbk_])
                        cp(R[MT2][:, :], bb_[0:64, :], [bbk_], [RK[MT2]])
                    bc, bck = bank()
                    for h in range(8):
                        hs = slice(h * 64, (h + 1) * 64)
                        mm(bc[0:64, hs], R[M2][:, hs], R[Y][:, hs], True, True, [RK[M2], RK[Y]], [bck])
                    Yd = "Yf" if last else Y2
                    tt(R[Yd][:, :], bc[0:64, :], R[Y][:, :], ALU.add, [bck, RK[Y]], [RK[Yd]])
                    M, M2 = M2, M
                    MT, MT2 = MT2, MT
                    Y, Y2 = Y2, Y
                    yield

            def phaseB(c):
                R, RK = rvs[c % 2], rvk[c % 2]
                c0 = c * C
                cs = slice(c0, c0 + C)
                bx, bxk = bank()
                for h in range(8):
                    hs = slice(h * 64, (h + 1) * 64)
                    A_ = opn("a", h)
                    mm(bx[0:64, hs], A_[0][:, cs], rw_sb[:, h, :], True, False, [A_[1], "rw_sb"], [bxk])
                    mm(bx[0:64, hs], R["Aak"][:, hs], R["vt"][:, hs], False, True, [RK["Aak"], RK["vt"]], [bxk])
                cp(R["X"][:, :], bx[0:64, :], [bxk], [RK["X"]])
                yield
                bu, buk = bank()
                for h in range(8):
                    hs = slice(h * 64, (h + 1) * 64)
                    mm(bu[0:64, hs], R["Yf"][:, hs], R["X"][:, hs], True, True, [RK["Yf"], RK["X"]], [buk])
                act(R["U"][:, :], bu[0:64, :], AF.Copy, [buk], [RK["U"]])
                yield
                bs_, bsk = bank()
                for h in range(8):
                    hs = slice(h * 64, (h + 1) * 64)
                    mm(bs_[0:64, hs], R["bt"][:, hs], R["U"][:, hs], True, False, [RK["bt"], RK["U"]], [bsk])
                    mm(bs_[0:64, hs], R["kt"][:, hs], R["vt"][:, hs], False, True, [RK["kt"], RK["vt"]], [bsk])
                by, byk = bank()
                for h in range(8):
                    ct, pb = hd(h)
                    hs = slice(h * 64, (h + 1) * 64)
                    o_ = by[pb:pb + 64, ct * 64:(ct + 1) * 64]
                    R_ = opn("r", h)
                    mm(o_, rw_sb[:, h, :], R_[0][:, cs], True, False, ["rw_sb", R_[1]], [byk])
                    mm(o_, R["U"][:, hs], R["Arb"][:, hs], False, False, [RK["U"], RK["Arb"]], [byk])
                    mm(o_, R["vt"][:, hs], R["Ark"][:, hs], False, True, [RK["vt"], RK["Ark"]], [byk])
                tmp_, tmpk = fsa()
                tv = tmp_[0:64, 0:512].rearrange("p (h v) -> p h v", h=8)
                tt(tv, bs_[0:64, :].rearrange("p (h v) -> p h v", h=8), rw_sf[:, :, :], ALU.add, [bsk, "rw_sf"], [tmpk])
                gb = gamh[0:64, 0:8, c:c + 1].to_broadcast([64, 8, 64])
                tt(rw_sb[:, :, :], tv, gb, ALU.mult, [tmpk, "gamh"], ["rw_sb"])
                tt(rw_sf[:, :, :], tv, gb, ALU.mult, [tmpk, "gamh"], ["rw_sf"])
                act(yT[:, 0:4, cs], by[:, 0:256].rearrange("p (c t) -> p c t", c=4), AF.Copy, [byk],
                    ["mg0", "mg1", "mg2", "mg3"])
                yield

            drive([phaseA(0)])
            for c in range(NCH):
                drive([phaseB(c), phaseA(c + 1) if c + 1 < NCH else None])
            for ct in range(4):
                mk = "mg%d" % ct
                b, bk = bank()
                mm(b[:, :], CS("bd"), yT[:, ct, :], True, True, ["cst", mk], [bk])
                d, dk_ = fsa()
                stt(d[:, 0:TS], b[:, :], -1.0 / 64.0, yT[:, ct, :], ALU.mult, ALU.add, [bk, mk], [dk_])
                sq, sqk = fsa()
                act(sq[:, 0:TS], d[:, 0:TS], AF.Square, [dk_], [sqk])
                b2, b2k = bank()
                mm(b2[:, :], CS("bd"), sq[:, 0:TS], True, True, ["cst", sqk], [b2k])
                rsq(sq[:, 0:TS], b2[:, :], 1.0 / 64.0, 64e-5, [b2k], [sqk])
                tt(d[:, 0:TS], d[:, 0:TS], sq[:, 0:TS], ALU.mult, [dk_, sqk], [dk_])
                act(d[:, 0:TS], d[:, 0:TS], AF.Identity, [dk_, "pk"], [dk_], bias=PKc(l, "ln_b", ct), scale=PKc(l, "ln_g", ct))
                tt(d[:, 0:TS], d[:, 0:TS], BON[ct][0][:, :], ALU.add, [dk_, BON[ct][1]], [dk_])
                pz, pzk = proj(l, C_RWZ + ct * 128, 128)
                z, zk = fsa()
                act(z[:, 0:TS], pz[:, :], AF.Silu, [pzk], [zk])
                tt(ys[:, ct, :], d[:, 0:TS], z[:, 0:TS], ALU.mult, [dk_, zk], ["ys"])

        outs = program()
        S.emit(final_wait_ops=outs)
    return nc


_NC_CACHE = {}


def make_in_maps(inp, n_cores=8):
    f = lambda a: np.ascontiguousarray(np.asarray(a, dtype=np.float32))
    pkall = np.stack([_pack_params(inp, l) for l in range(DEPTH)], 0)
    fgT = np.ascontiguousarray(f(inp["final_g"]).reshape(8, 128).T)
    shared = {
        "pk": pkall, "fg": fgT, "cst": CONSTS, "cstf": CSTF,
        "ada_w": f(inp["ada_w"]), "w_in": f(inp["w_in"]), "rw_w_up": f(inp["rw_w_up"]), "rw_a_up": f(inp["rw_a_up"]),
        "gla_gk_up": f(inp["gla_gk_up"]), "w_branch": f(inp["w_branch"]), "w_out": f(inp["w_out"]),
    }
    maps = []
    for b in range(n_cores):
        m = dict(shared)
        m["xT"] = np.ascontiguousarray(f(inp["x"][b]).T)
        m["cT"] = np.ascontiguousarray(f(inp["c"][b]).reshape(8, 128).T)
        maps.append(m)
    return maps


def kernel(**inputs):
    inp = {k: np.asarray(v) for k, v in inputs.items()}
    if "full" not in _NC_CACHE:
        _NC_CACHE["full"] = build_nc()
    nc = _NC_CACHE["full"]
    maps = make_in_maps(inp)
    res = run_bass_kernel_spmd(nc, maps, core_ids=list(range(8)))
    out = np.stack([np.ascontiguousarray(r["outT"].T) for r in res.results], 0)
    return out.astype(np.float32)
```

```python
import contextlib
import numpy as np
import ml_dtypes
import concourse.bass as bass
import concourse.mybir as mybir
from concourse.bass_utils import run_bass_kernel_spmd

F32 = mybir.dt.float32
BF16 = mybir.dt.bfloat16
AF = mybir.ActivationFunctionType
ALU = mybir.AluOpType

D = 1024
SEQ = 2048
DEPTH = 4
NCOLS = 11416
TS = 512
NST = SEQ // TS
ENGS = ("pe", "dve", "act", "pool", "sp")

C_RW = 0; C_RWZ = 1664
C_GQ = 2176; C_GK = 2432; C_GV = 2688; C_GC = 3200; C_GZ = 3216
C_MQK = 3728; C_MV = 4240; C_MI = 4752; C_MF = 4756; C_MZ = 4760
C_HQ = 5272; C_HF = 5784; C_HI = 6296; C_HZ = 6808
C_MG = 7320

PK = {}
_o = 0
for _n, _w in (("norm_g", 8), ("ada_b", 24), ("mu", 12), ("mu_wc", 1), ("mu_ac", 1), ("w0", 4), ("a0", 4),
               ("k_k", 4), ("k_a", 4), ("r_k", 4), ("ln_g", 4), ("ln_b", 4), ("gk_b", 2), ("gla_g", 4),
               ("conv", 16), ("ml_g", 4), ("lb", 16), ("hg_g", 4), ("i_b", 2), ("f_b", 2)):
    PK[_n] = (_o, _w)
    _o += _w
NPK = _o


class Op:
    __slots__ = ("eng", "fn", "deps", "idx", "dma_stream", "signal", "cnt")

    def __init__(self, eng, fn):
        self.eng = eng
        self.fn = fn
        self.deps = []
        self.dma_stream = None
        self.signal = False
        self.cnt = 0


class Sched:
    def __init__(self, nc):
        self.nc = nc
        self.ops = []
        self.last_w = {}
        self.readers = {}
        self.streams = {}
        self.record = False

    def op(self, eng, fn, reads=(), writes=(), dma=None):
        if self.record:
            return None
        o = Op(eng, fn)
        o.idx = len(self.ops)
        deps = {}
        for k in reads:
            w = self.last_w.get(k)
            if w is not None:
                deps[w.idx] = w
        for k in writes:
            w = self.last_w.get(k)
            if w is not None:
                deps[w.idx] = w
            for r in self.readers.get(k, ()):
                deps[r.idx] = r
        if dma is not None:
            o.dma_stream = dma
            n = self.streams.get(dma, 0) + 1
            self.streams[dma] = n
            o.cnt = n
        o.deps = [deps[i] for i in sorted(deps)]
        for k in reads:
            self.readers.setdefault(k, []).append(o)
        for k in writes:
            self.last_w[k] = o
            self.readers[k] = []
        self.ops.append(o)
        return o

    def emit(self, final_wait_ops=()):
        nc = self.nc
        for o in self.ops:
            nd = []
            for d in o.deps:
                if d.dma_stream is None and o.dma_stream is None and d.eng == o.eng and o.eng == "pe":
                    continue
                nd.append(d)
            o.deps = nd
            for d in nd:
                d.signal = True
        for o in final_wait_ops:
            o.signal = True
        cnt = {e: 0 for e in ENGS}
        for o in self.ops:
            if o.dma_stream is None and o.signal:
                cnt[o.eng] += 1
                o.cnt = cnt[o.eng]
        EP, DEP = 10 ** 9, 10 ** 9
        with contextlib.ExitStack() as es:
            esem = {}
            for e in ENGS:
                for ep in range(max(1, (cnt[e] + EP - 1) // EP)):
                    esem[(e, ep)] = es.enter_context(nc.semaphore("c_%s%d" % (e, ep)))
            ssem = {}
            for i, (s_, n_) in enumerate(self.streams.items()):
                for ep in range(max(1, (n_ + DEP - 1) // DEP)):
                    ssem[(s_, ep)] = es.enter_context(nc.semaphore("d%d_%d" % (i, ep)))
            block = es.enter_context(nc.Block())
            per = {e: [o for o in self.ops if o.eng == e] for e in ENGS}

            def semval(d):
                if d.dma_stream is not None:
                    return ssem[(d.dma_stream, (d.cnt - 1) // DEP)], 16 * ((d.cnt - 1) % DEP + 1)
                return esem[(d.eng, (d.cnt - 1) // EP)], (d.cnt - 1) % EP + 1

            def runner(e):
                def body(engine):
                    waited = {}
                    for o in per[e]:
                        for d in o.deps:
                            sem, val = semval(d)
                            key = id(sem)
                            if waited.get(key, 0) >= val:
                                continue
                            waited[key] = val
                            engine.wait_ge(sem, val)
                        ins = o.fn(engine)
                        if o.dma_stream is not None:
                            ins.then_inc(semval(o)[0], 16)
                        elif o.signal:
                            ins.then_inc(semval(o)[0], 1)
                    if e == "sp":
                        for o in final_wait_ops:
                            sem, val = semval(o)
                            engine.wait_ge(sem, val)
                return body

            block.tensor(runner("pe"))
            block.vector(runner("dve"))
            block.scalar(runner("act"))
            block.gpsimd(runner("pool"))
            block.sync(runner("sp"))
        return cnt


def _host_consts():
    c = {}
    i128 = np.eye(128, dtype=np.float32)
    c["ident"] = i128
    s = np.arange(128)[:, None]
    t = np.arange(128)[None, :]
    m_incl = (s <= t).astype(np.float32)
    m_blk = ((s <= t) & ((s // 64) == (t // 64))).astype(np.float32)
    c["m128"] = np.tile(m_incl[:, None, :], (1, 4, 1)).reshape(128, 512)
    c["m64b"] = np.tile(m_blk[:, None, :], (1, 4, 1)).reshape(128, 512)
    m_b32 = ((s <= t) & ((s // 32) == (t // 32))).astype(np.float32)
    c["m32b"] = np.tile(m_b32[:, None, :], (1, 4, 1)).reshape(128, 512)
    m96 = np.zeros((128, 128), np.float32); m96[96:] = 1.0
    c["m96"] = m96
    s6 = np.arange(64)[:, None]
    t6 = np.arange(64)[None, :]
    z = np.zeros((128, 512), np.float32)
    a = z.copy(); a[:64] = np.tile((t6 < s6).astype(np.float32)[:, None, :], (1, 8, 1)).reshape(64, 512)
    c["r_ts"] = a
    a = z.copy(); a[:64] = np.tile((s6 < t6).astype(np.float32)[:, None, :], (1, 8, 1)).reshape(64, 512)
    c["r_st"] = a
    a = z.copy(); a[:64] = np.tile((s6 <= t6).astype(np.float32)[:, None, :], (1, 8, 1)).reshape(64, 512)
    c["r_sti"] = a
    a = z.copy(); a[:64] = np.tile(np.eye(64, dtype=np.float32)[:, None, :], (1, 8, 1)).reshape(64, 512)
    c["i8"] = a
    c["ones"] = np.ones((128, 128), np.float32)
    bd = np.zeros((128, 128), np.float32); bd[:64, :64] = 1; bd[64:, 64:] = 1
    c["bd"] = bd
    names = ["ident", "m128", "m32b", "r_ts", "r_st", "r_sti", "i8", "ones", "bd", "m96"]
    offs = {}
    o = 0
    for n in names:
        offs[n] = (o, c[n].shape[1]); o += c[n].shape[1]
    return np.concatenate([c[n] for n in names], axis=1), offs


CONSTS, COFF = _host_consts()
NCONST = CONSTS.shape[1]
CFO = {"ident": 0, "ones": 128, "bd": 256, "m96": 384}
NCF = 512
CSTF = np.concatenate([CONSTS[:, COFF[n][0]:COFF[n][0] + 128] for n in ("ident", "ones", "bd", "m96")], axis=1)


def _pack_params(inp, l):
    P = np.zeros((128, NPK), np.float32)

    def put(name, arr):
        o, w = PK[name]
        P[:arr.shape[0], o:o + arr.shape[1]] = arr

    fm = lambda v: np.ascontiguousarray(v.reshape(-1, 128).T)
    put("norm_g", fm(inp["norm_g"][l]))
    put("ada_b", fm(inp["ada_b"][l]))
    mu = inp["rw_mu"][l]
    put("mu", fm(mu[:1536]))
    put("mu_wc", mu[1536:1600].reshape(64, 1))
    put("mu_ac", mu[1600:1664].reshape(64, 1))
    put("w0", fm(inp["rw_w0"][l])); put("a0", fm(inp["rw_a0"][l]))
    put("k_k", fm(inp["rw_k_k"][l])); put("k_a", fm(inp["rw_k_a"][l])); put("r_k", fm(inp["rw_r_k"][l]))
    put("ln_g", fm(inp["rw_ln_g"][l])); put("ln_b", fm(inp["rw_ln_b"][l]))
    put("gk_b", fm(inp["gla_gk_b"][l])); put("gla_g", fm(inp["gla_norm_g"][l]))
    cw = inp["ml_conv_w"][l]
    put("conv", np.concatenate([fm(cw[j]) for j in range(4)], axis=1))
    put("ml_g", fm(inp["ml_norm_g"][l]))
    put("lb", np.concatenate([fm(inp["hg_lb_logits"][j]) for j in range(4)], axis=1))
    put("hg_g", fm(inp["hg_norm_g"][l]))
    put("i_b", fm(np.repeat(inp["ml_i_b"][l], 64)))
    put("f_b", fm(np.repeat(inp["ml_f_b"][l], 64)))
    return P


def build_nc(n_layers=DEPTH, branches=(0, 1, 2, 3), debug=False, stage=99):
    nc = bass.Bass("TRN2", target_bir_lowering=False)
    dram = lambda n, s, k="ExternalInput": nc.dram_tensor(n, list(s), F32, kind=k).ap()
    xT_d = dram("xT", [D, SEQ])
    c_d = dram("cT", [128, 8])
    pk_d = dram("pk", [DEPTH, 128, NPK])
    fg_d = dram("fg", [128, 8])
    cst_d = dram("cst", [128, NCONST])
    cstf_d = dram("cstf", [128, NCF])
    adaw_d = dram("ada_w", [DEPTH, D, 3 * D])
    win_d = dram("w_in", [DEPTH, D, NCOLS])
    wup_d = dram("rw_w_up", [DEPTH, 64, 512])
    aup_d = dram("rw_a_up", [DEPTH, 64, 512])
    gup_d = dram("gla_gk_up", [DEPTH, 16, 256])
    wbr_d = dram("w_branch", [DEPTH, 4, 512, D])
    wout_d = dram("w_out", [DEPTH, D, D])
    out_d = dram("outT", [D, SEQ], "ExternalOutput")
    dbg_d = dram("dbg", [4, 512, SEQ], "ExternalOutput") if debug else None

    es = contextlib.ExitStack()
    with es:
        T = lambda n, s, d=F32: es.enter_context(nc.sbuf_tensor("s_" + n, list(s), d))
        x = T("x", [128, 8, TS])
        uT = T("uT", [128, 8, TS], BF16)
        merged = T("merged", [128, 8, TS])
        mergedb = uT
        ys = T("ys", [128, 4, TS], BF16)
        pk = T("pk", [128, DEPTH, NPK])
        drv = T("drv", [128, 64])
        cst = T("cst", [128, NCF])
        cstb = T("cstb", [128, NCONST], BF16)
        cT = T("cT", [128, 8])
        fg = T("fg", [128, 8])
        mod = T("mod", [128, 24])
        lbt = T("lbt", [128, 16]); lbe = T("lbe", [128, 16]); lbs = T("lbs", [128, 4])
        NWB = 6
        wb = [T("wb%d" % i, [128, 8, 128], BF16) for i in range(NWB)]
        wv = [T("wv%d" % i, [128, 8, 512], BF16) for i in range(1)]
        wup = T("wup", [64, 512], BF16); aup = T("aup", [64, 512], BF16); gup = T("gup", [16, 256], BF16)
        NF = 14
        mtl = [T("mt%d" % i, [128, TS]) for i in range(2)]
        fs = [T("fs%d" % i, [128, TS + 4]) for i in range(NF)]
        LL = [T("ll%d" % i, [128, TS]) for i in range(8)]
        NB = 5
        bs = [T("bs%d" % i, [128, TS], BF16) for i in range(NB)]
        BL = [T("bl%d" % i, [128, TS], BF16) for i in range(20)]
        prc = T("prc", [128, 16])
        cqc = T("cqc", [128, 4, 4])
        gnb = [T("gn%d" % i, [128, TS + 1]) for i in range(4)]
        nref = T("nref", [128, 4, 16])
        ktz = T("ktz", [128, 2, 512], BF16)
        vtok = T("vtok", [128, 4, 512], BF16)
        ktok = T("ktok", [128, 4, 512], BF16)
        rv = {n: T("rv_" + n, [64, 512], BF16) for n in
              ("vt", "bt", "kt", "N", "NT", "N2", "NT2", "Y", "Y2", "Aak", "Arb", "Ark", "X", "U", "Yf")}
        rw_sf = T("rw_sf", [64, 8, 64]); rw_sb = T("rw_sb", [64, 8, 64], BF16)
        la_sf = [T("la_sf%d" % i, [128, 4, 128]) for i in range(4)]
        la_sb = [T("la_sb%d" % i, [128, 4, 128], BF16) for i in range(4)]
        gamh = T("gamh", [64, 8, 16])
        gam = T("gam", [128, 4, 16])
        yT = merged
        wcac = T("wcac", [64, 2, TS])
        wcacb = T("wcacb", [64, 2, TS], BF16)
        ps = [es.enter_context(nc.psum_tensor("ps%d" % i, [128, 512], F32)) for i in range(8)]

        S = Sched(nc)
        st = {"bank": 0, "w": 0, "fsi": 0, "bsi": 0, "wai": 0}

        def bank():
            i = st["bank"]; st["bank"] = (i + 1) % 6
            return ps[i], "ps%d" % i

        CS = lambda n: cst[:, CFO[n]:CFO[n] + 128]
        CB = lambda n: cstb[:, COFF[n][0]:COFF[n][0] + COFF[n][1]]
        PKc = lambda l, n, j=0, w=1: pk[:, l, PK[n][0] + j:PK[n][0] + j + w]

        def mm(out, lhsT, rhs, start, stop, r, w):
            S.op("pe", lambda e: e.matmul(out, lhsT, rhs, start=start, stop=stop), reads=r, writes=w)

        def tr(out, in_, ident, r, w):
            S.op("pe", lambda e: e.transpose(out, in_, ident), reads=r, writes=w)

        def act(out, in_, func, r, w, bias=None, scale=None):
            kw = {}
            if bias is not None:
                kw["bias"] = bias
            if scale is not None:
                kw["scale"] = scale
            S.op("act", lambda e: e.activation(out=out, in_=in_, func=func, **kw), reads=r, writes=w)

        def tt(out, a, b, op, r, w, eng="dve"):
            S.op(eng, lambda e: e.tensor_tensor(out=out, in0=a, in1=b, op=op), reads=r, writes=w)

        def tsc(out, a, s1, s2, op0, op1, r, w, eng="dve"):
            if op1 is None:
                S.op(eng, lambda e: e.tensor_scalar(out=out, in0=a, scalar1=s1, scalar2=None, op0=op0), reads=r, writes=w)
            else:
                S.op(eng, lambda e: e.tensor_scalar(out=out, in0=a, scalar1=s1, scalar2=s2, op0=op0, op1=op1),
                     reads=r, writes=w)

        def stt(out, a, sc, b, op0, op1, r, w, eng="dve"):
            S.op(eng, lambda e: e.scalar_tensor_tensor(out=out, in0=a, scalar=sc, in1=b, op0=op0, op1=op1),
                 reads=r, writes=w)

        def cp(out, in_, r, w, eng="dve"):
            S.op(eng, lambda e: e.tensor_copy(out=out, in_=in_), reads=r, writes=w)

        def rsq(out, in_, scale, bias, r, w):
            act(out, in_, AF.Ln, r, w, bias=bias, scale=scale)
            act(out, out, AF.Exp, w, w, scale=-0.5)

        def recip(out, in_, r, w):
            S.op("dve", lambda e: e.reciprocal(out=out, in_=in_), reads=r, writes=w)

        def mset(ap, val, w):
            S.op("dve", lambda e: e.memset(ap, val), writes=w)

        def dma(eng, out, in_, r, w, stream):
            return S.op(eng, lambda e: e.dma_start(out=out, in_=in_), reads=r, writes=w, dma=stream)

        def cumsum(ct, src, srck):
            S.op("dve", lambda e: e.tensor_tensor_scan(out=gnb[ct][:, 1:TS + 1], data0=src, data1=src, initial=0.0,
                                                       op0=ALU.add, op1=ALU.max), reads=[srck], writes=["gn%d" % ct])

        def wchunk(src, n, kc=8):
            i = st["w"]; st["w"] = (i + 1) % NWB
            key = "wb%d" % i
            dma("pool", wb[i][:, 0:kc, 0:n], src.rearrange("(k p) c -> p k c", p=128), [], [key], key)
            return wb[i], key

        def proj(l, c0, n):
            w, wk = wchunk(win_d[l, :, c0:c0 + n], n)
            b, bk = bank()
            for k in range(8):
                mm(b[0:n, :], w[:, k, 0:n], uT[:, k, :], k == 0, k == 7, [wk, "uT"], [bk])
            return b, bk

        def proj_tok(l, c0):
            for h in range(2):
                dma("pool", wv[0][:, :, h * 256:(h + 1) * 256],
                    win_d[l, :, c0 + h * 256:c0 + (h + 1) * 256].rearrange("(k p) c -> p k c", p=128),
                    [], ["wv_%d" % h], "wv_%d" % h)
            for tI in range(TS // 128):
                b, bk = bank()
                for k in range(8):
                    mm(b[:, :], uT[:, k, tI * 128:(tI + 1) * 128], wv[0][:, k, :], k == 0, k == 7,
                       ["wv_0", "wv_1", "uT"], [bk])
                if tI % 2 == 0:
                    act(vtok[:, tI, :], b[:, :], AF.Copy, [bk], ["vtok%d" % tI])
                else:
                    cp(vtok[:, tI, :], b[:, :], [bk], ["vtok%d" % tI])

        def fsa():
            i = st["fsi"]; st["fsi"] = (i + 1) % NF
            return fs[i], "fs%d" % i

        def bsa():
            i = st["bsi"]; st["bsi"] = (i + 1) % NB
            return bs[i], "bs%d" % i

        XK = ["x%d" % k for k in range(8)]

        def rms_to(sl_src, emit):
            b, bk = bank()
            for k in range(8):
                sq, sqk = bsa()
                act(sq[:, :], x[:, k, :], AF.Square, [XK[k]], [sqk])
                mm(b[:, :], CB("ones"), sq[:, :], k == 0, k == 7, [sqk, "cstb"], [bk])
            rs, rsk = fsa()
            rsq(rs[:, 0:TS], b[:, :], 1.0, 1024.0 * 1e-6, [bk], [rsk])
            for k in range(8):
                t1, t1k = fsa()
                tt(t1[:, 0:TS], x[:, k, :], rs[:, 0:TS], ALU.mult, [XK[k], rsk], [t1k])
                emit(k, t1, t1k)

        def program():
            dma("sp", cst[:, :], cstf_d, [], ["cst"], "cst")
            dma("pool", cstb[:, :], cst_d, [], ["cstb"], "cstb")
            dma("sp", pk[:, :, :], pk_d.rearrange("l p n -> p l n"), [], ["pk"], "pk")
            dma("sp", cT[:, :], c_d, [], ["cT"], "cT")
            dma("sp", fg[:, :], fg_d, [], ["fg"], "fg")
            act(cT[:, :], cT[:, :], AF.Silu, ["cT"], ["cT"])
            o_lb = PK["lb"][0]
            act(lbe[:, :], pk[:, 0, o_lb:o_lb + 16], AF.Exp, ["pk"], ["lbe"])
            tt(lbs[:, :], lbe[:, 0:4], lbe[:, 4:8], ALU.add, ["lbe"], ["lbs"])
            tt(lbs[:, :], lbs[:, :], lbe[:, 8:12], ALU.add, ["lbs", "lbe"], ["lbs"])
            tt(lbs[:, :], lbs[:, :], lbe[:, 12:16], ALU.add, ["lbs", "lbe"], ["lbs"])
            recip(lbs[:, :], lbs[:, :], ["lbs"], ["lbs"])
            mset(lbt[:, 0:4], 0.0, ["lbt"])
            for j in range(1, 4):
                t_, tk_ = fsa()
                tt(t_[:, 0:4], lbe[:, j * 4:(j + 1) * 4], lbs[:, :], ALU.mult, ["lbe", "lbs"], [tk_])
                tt(lbt[:, j * 4:(j + 1) * 4], lbt[:, (j - 1) * 4:j * 4], t_[:, 0:4], ALU.add, [tk_, "lbt"], ["lbt"])
            for i in range(4):
                mset(gnb[i][:, 0:1], 0.0, ["gn%d" % i])

            for l in range(n_layers):
                layer(l)

            outs = []
            for sI in range(NST):
                sl = slice(sI * TS, (sI + 1) * TS)
                for k in range(8):
                    dma("sp", x[:, k, :], (out_d if n_layers > 0 else xT_d)[k * 128:(k + 1) * 128, sl], ["xd%d" % sI], [XK[k]], "xl%d" % k)

                def emit(k, t1, t1k, sI=sI, sl=sl):
                    tsc(t1[:, 0:TS], t1[:, 0:TS], fg[:, k:k + 1], 32.0, ALU.mult, ALU.mult, [t1k, "fg"], [t1k])
                    outs.append(dma("sp", out_d[k * 128:(k + 1) * 128, sl], t1[:, 0:TS], [t1k], ["xd%d" % sI], "o_" + t1k))
                rms_to(sl, emit)
            return outs

        def layer(l):
            b, bk = bank()
            for j in range(24):
                i = st["wai"]; st["wai"] = (i + 1) % 3
                wa_i = merged[:, 2 * i:2 * i + 2, :].rearrange("p a (k c) -> p (a k) c", c=128)
                keys = ["mg%d" % (2 * i), "mg%d" % (2 * i + 1)]
                dma("sp", wa_i, adaw_d[l, :, j * 128:(j + 1) * 128].rearrange("(k p) c -> p k c", p=128),
                    [], keys, "wa%d" % i)
                for k in range(8):
                    mm(b[:, j:j + 1], wa_i[:, k, :], cT[:, k:k + 1], k == 0, k == 7, keys + ["cT"], [bk])
            tt(mod[:, :], b[:, 0:24], pk[:, l, PK["ada_b"][0]:PK["ada_b"][0] + 24], ALU.add, [bk, "pk"], ["mod"])
            stt(drv[:, 0:8], mod[:, 8:16], 1.0, pk[:, l, PK["norm_g"][0]:PK["norm_g"][0] + 8], ALU.add, ALU.mult,
                ["mod", "pk"], ["drv"])
            tsc(drv[:, 0:8], drv[:, 0:8], 32.0, None, ALU.mult, None, ["drv"], ["drv"])
            o_mu = PK["mu"][0]
            tsc(drv[:, 8:22], pk[:, l, o_mu:o_mu + 14], -1.0, 1.0, ALU.mult, ALU.add, ["pk"], ["drv"])
            tsc(drv[:, 22:26], PKc(l, "w0", 0, 4), -1.0, None, ALU.mult, None, ["pk"], ["drv"])
            tsc(drv[:, 26:30], PKc(l, "k_a", 0, 4), -1.0, 1.0, ALU.mult, ALU.add, ["pk"], ["drv"])
            tsc(drv[:, 30:32], PKc(l, "gk_b", 0, 2), -1.0, None, ALU.mult, None, ["pk"], ["drv"])
            tsc(drv[:, 32:34], PKc(l, "f_b", 0, 2), -1.0, None, ALU.mult, None, ["pk"], ["drv"])
            tsc(drv[:, 34:38], lbt[:, l * 4:(l + 1) * 4], -1.0, 1.0, ALU.mult, ALU.add, ["lbt"], ["drv"])
            tsc(drv[:, 38:42], PKc(l, "gla_g", 0, 4), float(np.sqrt(128.0)), None, ALU.mult, None, ["pk"], ["drv"])
            tsc(drv[:, 42:46], PKc(l, "hg_g", 0, 4), float(np.sqrt(128.0)), None, ALU.mult, None, ["pk"], ["drv"])
            dma("pool", wup[:, :], wup_d[l], [], ["wup"], "wup")
            dma("pool", aup[:, :], aup_d[l], [], ["aup"], "aup")
            dma("pool", gup[:, :], gup_d[l], [], ["gup"], "gup")
            mset(rw_sf[:, :, :], 0.0, ["rw_sf"])
            mset(rw_sb[:, :, :], 0.0, ["rw_sb"])
            for i in range(4):
                mset(la_sf[i][:, :, :], 0.0, ["la_sf%d" % i])
                mset(la_sb[i][:, :, :], 0.0, ["la_sb%d" % i])
            mset(prc[:, :], 0.0, ["prc"])
            mset(cqc[:, :, :], 0.0, ["cqc"])

            src_d = xT_d if l == 0 else out_d
            for sI in range(NST):
                sl = slice(sI * TS, (sI + 1) * TS)
                for k in range(8):
                    dma("sp", x[:, k, :], src_d[k * 128:(k + 1) * 128, sl], ["xd%d" % sI], [XK[k]], "xl%d" % k)

                def emit(k, t1, t1k):
                    act(uT[:, k, :], t1[:, 0:TS], AF.Identity, [t1k, "drv", "mod"], ["uT"],
                        bias=mod[:, k:k + 1], scale=drv[:, k:k + 1])
                rms_to(sl, emit)
                def merge_gen(m):
                    for fc in range(8):
                        pg, pgk = proj(l, C_MG + m * 1024 + fc * 128, 128)
                        sg, sgk = mtl[fc % 2], "mt%d" % (fc % 2)
                        act(sg[:, :], pg[:, :], AF.Sigmoid, [pgk], [sgk])
                        w, wk = wchunk(wbr_d[l, m, :, fc * 128:(fc + 1) * 128], 128, kc=4)
                        pb, pbk = bank()
                        for k in range(4):
                            mm(pb[:, :], w[:, k, :], ys[:, k, :], k == 0, k == 3, [wk, "ys"], [pbk])
                        if m == 0:
                            tt(merged[:, fc, :], pb[:, :], sg[:, :], ALU.mult, [pbk, sgk], ["mg%d" % fc])
                        else:
                            tt(sg[:, :], pb[:, :], sg[:, :], ALU.mult, [pbk, sgk], [sgk])
                            tt(merged[:, fc, :], merged[:, fc, :], sg[:, :], ALU.add, [sgk, "mg%d" % fc], ["mg%d" % fc])
                        yield

                def zero_gen():
                    for _ in range(9):
                        yield
                    mset(ys[:, :, :], 0.0, ["ys"])
                    yield

                g_prev = None
                for m in range(4):
                    gm = (rwkv, gla, mlstm, hgrn)[m](l, sI) if m in branches else zero_gen()
                    drive([gm, g_prev])
                    if debug and l == 0:
                        for k in range(4):
                            dma("pool", dbg_d[m, k * 128:(k + 1) * 128, sl], ys[:, k, :], ["ys"], [], "dbg%d" % k)
                    g_prev = merge_gen(m)
                drive([g_prev])
                for fc in range(8):
                    act(mergedb[:, fc, :], merged[:, fc, :], AF.Copy, ["mg%d" % fc], ["uT"])
                for fc in range(8):
                    w, wk = wchunk(wout_d[l, :, fc * 128:(fc + 1) * 128], 128)
                    pb, pbk = bank()
                    for k in range(8):
                        mm(pb[:, :], w[:, k, :], mergedb[:, k, :], k == 0, k == 7, [wk, "uT"], [pbk])
                    stt(x[:, fc, :], pb[:, :], mod[:, 16 + fc:17 + fc], x[:, fc, :], ALU.mult, ALU.add,
                        [pbk, "mod", XK[fc]], [XK[fc]])
                    dma("sp", out_d[fc * 128:(fc + 1) * 128, sl], x[:, fc, :], [XK[fc]], ["xd%d" % sI], "xs%d" % fc)

        def shift_hi(src_ap, skey, dst_ap, dkey, n, fp32=False):
            b, bk = bank()
            idn = (CS if fp32 else CB)("ident")
            mm(b[0:64, 0:n], idn[:, 64:128], src_ap, True, True, [skey, "cst" if fp32 else "cstb"], [bk])
            cp(dst_ap, b[0:64, 0:n], [bk], [dkey])

        def build_gamh(npair, nch):
            for ct in range(npair):
                cp(gamh[:, 2 * ct, 0:nch], gam[0:64, ct, 0:nch], ["gam"], ["gamh"])
                shift_hi(gam[:, ct, 0:nch], "gam", gamh[:, 2 * ct + 1, 0:nch], "gamh", nch, fp32=True)

        def drive(gens):
            gens = [g for g in gens if g is not None]
            while gens:
                for g in list(gens):
                    try:
                        next(g)
                    except StopIteration:
                        gens.remove(g)

        def la(Qh, Kh, KTp, C, dk, si, gm, gmk, fin, ni=None):
            mask = {128: CB("m128"), 32: CB("m32b")}[C]
            sfk, sbk = "la_sf%d" % si, "la_sb%d" % si
            nkt = len(KTp)
            PT = {}

            def phA(tI):
                tsl = slice(tI * 128, (tI + 1) * 128)
                bt, btk = bank()
                btb = bt[:, :].bitcast(BF16)
                for ct in range(nkt):
                    tr(btb[:, ct * 128:(ct + 1) * 128], KTp[ct][0][:, tsl], CB("ident"), [KTp[ct][1], "cstb"], [btk])
                cp(ktok[:, tI, 0:nkt * 128], btb[:, 0:nkt * 128], [btk], ["ktok%d" % tI])
                if C == 32:
                    tsc(ktz[64:128, tI % 2, 0:nkt * 128], ktok[64:128, tI, 0:nkt * 128], CS("m96")[64:128, 0:1], None,
                        ALU.mult, None, ["ktok%d" % tI, "cst"], ["ktz%d" % (tI % 2)])
                yield
                bsc, bsck = bank()
                for h in range(4):
                    mm(bsc[:, h * 128:(h + 1) * 128], Kh[h][0][:, tsl], Qh[h][0][:, tsl], True, True,
                       [Kh[h][1], Qh[h][1]], [bsck])
                pt, ptk = bsa()
                tt(pt[:, :], bsc[:, :], mask, ALU.mult, [bsck, "cstb"], [ptk])
                PT[tI] = (pt, ptk)
                yield

            def phB(tI):
                pt, ptk = PT[tI]
                po, pok = ps[6], "ps6"
                pd, pdk = (ps[7], "ps7") if ni is not None else (None, None)
                for cc in range(128 // C):
                    cidx = tI * (128 // C) + cc
                    csl = slice(cc * C, (cc + 1) * C)
                    gsl = slice(tI * 128 + cc * C, tI * 128 + (cc + 1) * C)
                    for h in range(4):
                        osl = slice(h * 128 + cc * C, h * 128 + (cc + 1) * C)
                        mm(po[:, osl], vtok[:, tI, h * 128:(h + 1) * 128], pt[:, osl], True, False,
                           ["vtok%d" % tI, ptk], [pok])
                        mm(po[:, osl], la_sb[si][0:dk, h, :], Qh[h][0][:, gsl], False, True, [sbk, Qh[h][1]], [pok])
                        if ni is not None:
                            mm(pd[:, osl], CB("ones"), pt[:, osl], True, False, ["cstb", ptk], [pdk])
                            mm(pd[:, osl], la_sb[ni][0:dk, h, :], Qh[h][0][:, gsl], False, True,
                               ["la_sb%d" % ni, Qh[h][1]], [pdk])
                    for sidx, isn in ((si, False),) + (((ni, True),) if ni is not None else ()):
                        bu, buk = bank()
                        for h in range(4):
                            if C == 32 and cc == 3:
                                zsl = slice(64, 128)
                                mm(bu[0:dk, h * 128:(h + 1) * 128], ktz[zsl, tI % 2, h * dk:(h + 1) * dk],
                                   vtok[zsl, tI, h * 128:(h + 1) * 128], True, True,
                                   ["ktz%d" % (tI % 2), "vtok%d" % tI], [buk])
                            else:
                                rhs = CB("ones")[csl, :] if isn else vtok[csl, tI, h * 128:(h + 1) * 128]
                                mm(bu[0:dk, h * 128:(h + 1) * 128], ktok[csl, tI, h * dk:(h + 1) * dk], rhs, True, True,
                                   ["ktok%d" % tI, "vtok%d" % tI, "cstb"], [buk])
                        tmp_, tmpk = fsa()
                        tv = tmp_[0:dk, 0:512].rearrange("p (h v) -> p h v", h=4)
                        tt(tv, bu[0:dk, :].rearrange("p (h v) -> p h v", h=4), la_sf[sidx][0:dk, :, :], ALU.add,
                           [buk, "la_sf%d" % sidx], [tmpk])
                        gb = gm[0:dk, 0:4, cidx:cidx + 1].to_broadcast([dk, 4, 128])
                        tt(la_sb[sidx][0:dk, :, :], tv, gb, ALU.mult, [tmpk, gmk], ["la_sb%d" % sidx])
                        tt(la_sf[sidx][0:dk, :, :], tv, gb, ALU.mult, [tmpk, gmk], ["la_sf%d" % sidx])
                    yield
                fin(tI, po, pok, pd, pdk)
                yield

            NT_ = TS // 128
            drive([phA(0)])
            for tI in range(NT_):
                drive([phB(tI), phA(tI + 1) if tI + 1 < NT_ else None])

        def gam_start(ct, C):
            n = TS // C
            d_, dk_ = fsa()
            tt(d_[:, 0:n], gnb[ct][:, C:TS + 1:C], gnb[ct][:, 0:TS:C], ALU.subtract, ["gn%d" % ct], [dk_])
            act(gam[:, ct, 0:n], d_[:, 0:n], AF.Exp, [dk_], ["gam"], scale=-1.0)

        def qk_decay(ct, q_ap, qk_, k_ap, kk_, C, mid, Qd, Kd, kextra=None, clamp=False):
            gk = "gn%d" % ct
            n = TS // C
            off = C // 2 if mid else 0
            v3 = lambda ap: ap.rearrange("p (c t) -> p c t", t=C)
            gref = gnb[ct][:, off:TS:C].unsqueeze(2).to_broadcast([128, n, C])
            d_, dk_ = fsa(); ei, eik = fsa()
            tt(v3(d_[:, 0:TS]), v3(gnb[ct][:, 1:TS + 1]), gref, ALU.subtract, [gk], [dk_])
            act(ei[:, 0:TS], d_[:, 0:TS], AF.Exp, [dk_], [eik], scale=-1.0)
            if kextra is not None:
                tt(v3(d_[:, 0:TS]), v3(kextra[0][:, 0:TS]), gref, ALU.subtract, [kextra[1], gk, dk_], [dk_])
            act(d_[:, 0:TS], d_[:, 0:TS], AF.Exp, [dk_], [dk_])
            ev, evk = d_, dk_
            if clamp:
                tsc(ei[:, 0:TS], ei[:, 0:TS], 2.35e17, None, ALU.min, None, [eik], [eik])
                tsc(ev[:, 0:TS], ev[:, 0:TS], 2.35e17, None, ALU.min, None, [evk], [evk])
            tt(Qd[0][:, :], q_ap, ei[:, 0:TS], ALU.mult, [qk_, eik], [Qd[1]])
            tt(Kd[0][:, :], k_ap, ev[:, 0:TS], ALU.mult, [kk_, evk], [Kd[1]])

        BLk = lambda i: (BL[i], "bl%d" % i)
        LLk = lambda i: (LL[i], "ll%d" % i)

        def zgate(l, c0, h, func_scale_ap, skeys):
            pz, pzk = proj(l, c0 + h * 128, 128)
            z, zk = LLk(h)
            act(z[:, :], pz[:, :], AF.Silu, [pzk], [zk])
            if func_scale_ap is not None:
                tsc(z[:, :], z[:, :], func_scale_ap, None, ALU.mult, None, [zk] + skeys, [zk])
            return z, zk

        def rms_fin(GZ):
            def fin(tI, po, pok, pd, pdk):
                sq, sqk = bsa()
                act(sq[:, :], po[:, :], AF.Square, [pok], [sqk])
                b, bk = bank()
                mm(b[:, :], CB("ones"), sq[:, :], True, True, [sqk, "cstb"], [bk])
                rs, rsk = fsa()
                rsq(rs[:, 0:TS], b[:, :], 1.0, 128.0 * 1e-6, [bk], [rsk])
                t1, t1k = fsa()
                tt(t1[:, 0:TS], po[:, :], rs[:, 0:TS], ALU.mult, [pok, rsk], [t1k])
                for h in range(4):
                    tt(ys[:, h, tI * 128:(tI + 1) * 128], t1[:, h * 128:(h + 1) * 128],
                       GZ[h][0][:, tI * 128:(tI + 1) * 128], ALU.mult, [t1k, GZ[h][1]], ["ys"])
            return fin

        def heads64(QT, KT):
            Qh, Kh = [], []
            for ct in range(2):
                shift_hi(QT[ct][0][:, :], QT[ct][1], BL[16 + ct][0:64, :], "bl%d" % (16 + ct), TS)
                shift_hi(KT[ct][0][:, :], KT[ct][1], BL[18 + ct][0:64, :], "bl%d" % (18 + ct), TS)
                Qh += [(QT[ct][0][0:64, :], QT[ct][1]), (BL[16 + ct][0:64, :], "bl%d" % (16 + ct))]
                Kh += [(KT[ct][0][0:64, :], KT[ct][1]), (BL[18 + ct][0:64, :], "bl%d" % (18 + ct))]
            return Qh, Kh

        def gla(l, sI):
            proj_tok(l, C_GV)
            yield
            pg, pgk = proj(l, C_GC, 16)
            gc, gck = bsa()
            act(gc[0:16, 0:TS], pg[0:16, :], AF.Copy, [pgk], [gck])
            yield
            QT, KT = [], []
            for ct in range(2):
                yield
                b, bk = bank()
                mm(b[:, :], gup[:, ct * 128:(ct + 1) * 128], gc[0:16, 0:TS], True, True, ["gup", gck], [bk])
                e1, e1k = fsa()
                act(e1[:, 0:TS], b[:, :], AF.Exp, [bk, "drv"], [e1k], bias=drv[:, 30 + ct:31 + ct], scale=-1.0)
                act(e1[:, 0:TS], e1[:, 0:TS], AF.Ln, [e1k], [e1k], bias=1.0)
                tsc(e1[:, 0:TS], e1[:, 0:TS], 1.0 / 16.0, None, ALU.mult, None, [e1k], [e1k])
                cumsum(ct, e1[:, 0:TS], e1k)
                gam_start(ct, 128)
                yield
                pq, pqk = proj(l, C_GQ + ct * 128, 128)
                q, qk_ = fsa()
                act(q[:, 0:TS], pq[:, :], AF.Copy, [pqk], [qk_], scale=0.125)
                yield
                pk_, pkk = proj(l, C_GK + ct * 128, 128)
                k, kk_ = fsa()
                cp(k[:, 0:TS], pk_[:, :], [pkk], [kk_])
                qk_decay(ct, q[:, 0:TS], qk_, k[:, 0:TS], kk_, 128, False, BLk(ct), BLk(4 + ct))
                QT.append(BLk(ct)); KT.append(BLk(4 + ct))
            GZ = []
            for h in range(4):
                GZ.append(zgate(l, C_GZ, h, drv[:, 38 + h:39 + h], ["drv"]))
                yield
            Qh, Kh = heads64(QT, KT)
            build_gamh(2, 4)
            la(Qh, Kh, KT, 128, 64, 0, gamh, "gamh", rms_fin(GZ))

        def hgrn(l, sI):
            proj_tok(l, C_HI)
            yield
            QT, KT = [], []
            for ct in range(4):
                yield
                pf, pfk = proj(l, C_HF + ct * 128, 128)
                g, gk_ = fsa()
                act(g[:, 0:TS], pf[:, :], AF.Sigmoid, [pfk], [gk_])
                tsc(g[:, 0:TS], g[:, 0:TS], drv[:, 34 + ct:35 + ct], lbt[:, l * 4 + ct:l * 4 + ct + 1], ALU.mult, ALU.add,
                    [gk_, "drv", "lbt"], [gk_])
                lg, lgk = fsa()
                act(lg[:, 0:TS], g[:, 0:TS], AF.Ln, [gk_], [lgk])
                tsc(lg[:, 0:TS], lg[:, 0:TS], -1.0, None, ALU.mult, None, [lgk], [lgk])
                cumsum(ct, lg[:, 0:TS], lgk)
                d_, dk_ = fsa()
                tt(d_[:, 0:15], gnb[ct][:, 48:TS:32], gnb[ct][:, 16:TS - 32:32], ALU.subtract, ["gn%d" % ct], [dk_])
                tt(d_[:, 15:16], gnb[ct][:, TS:TS + 1], gnb[ct][:, TS - 16:TS - 15], ALU.subtract, ["gn%d" % ct], [dk_])
                act(gam[:, ct, 0:16], d_[:, 0:16], AF.Exp, [dk_], ["gam"], scale=-1.0)
                fc_, fck = fsa()
                act(fc_[:, 0:1], gnb[ct][:, 16:17], AF.Exp, ["gn%d" % ct], [fck], scale=-1.0)
                tsc(la_sf[3][:, ct, :], la_sf[3][:, ct, :], fc_[:, 0:1], None, ALU.mult, None,
                    [fck, "la_sf3"], ["la_sf3"])
                cp(la_sb[3][:, ct, :], la_sf[3][:, ct, :], ["la_sf3"], ["la_sb3"])
                tsc(g[:, 0:TS], g[:, 0:TS], -1.0, 1.0, ALU.mult, ALU.add, [gk_], [gk_])
                yield
                pq, pqk = proj(l, C_HQ + ct * 128, 128)
                q, qk_ = fsa()
                act(q[:, 0:TS], pq[:, :], AF.Silu, [pqk], [qk_])
                qk_decay(ct, q[:, 0:TS], qk_, g[:, 0:TS], gk_, 32, True, BLk(ct), BLk(4 + ct), clamp=True)
                QT.append(BLk(ct)); KT.append(BLk(4 + ct))
            GZ = []
            for h in range(4):
                GZ.append(zgate(l, C_HZ, h, drv[:, 42 + h:43 + h], ["drv"]))
                yield
            la(QT, KT, KT, 32, 128, 3, gam, "gam", rms_fin(GZ))

        def mlstm(l, sI):
            proj_tok(l, C_MV)
            yield
            w8, w8k = wchunk(win_d[l, :, C_MI:C_MI + 8], 8)
            reps = {}
            for ct in range(2):
                for gI in range(2):
                    for half in range(2):
                        r_, rk_ = BLk(8 + ct * 4 + gI * 2 + half)
                        rv_ = r_[:, :].rearrange("p (k j m) -> p k j m", k=4, j=2, m=64)
                        src = w8[:, half * 4:(half + 1) * 4, gI * 4 + ct * 2:gI * 4 + ct * 2 + 2]
                        cp(rv_, src.unsqueeze(3).to_broadcast([128, 4, 2, 64]), [w8k], [rk_])
                        reps[(ct, gI, half)] = (r_, rk_)
            for ct in range(4):
                yield
                pc, pck = proj(l, C_MQK + ct * 128, 128)
                cb, cbk = fsa()
                cp(cb[:, 0:3], cqc[:, ct, 0:3], ["cqc"], [cbk])
                act(cb[:, 3:TS + 3], pc[:, :], AF.Copy, [pck], [cbk])
                a, ak = fsa()
                oc = PK["conv"][0]
                tsc(a[:, 0:TS], cb[:, 0:TS], pk[:, l, oc + ct:oc + ct + 1], None, ALU.mult, None, [cbk, "pk"], [ak])
                for j in range(1, 4):
                    stt(a[:, 0:TS], cb[:, j:j + TS], pk[:, l, oc + j * 4 + ct:oc + j * 4 + ct + 1], a[:, 0:TS], ALU.mult,
                        ALU.add, [cbk, "pk", ak], [ak])
                cp(cqc[:, ct, 0:3], cb[:, TS:TS + 3], [cbk], ["cqc"])
                act(LL[4 + ct][:, :], a[:, 0:TS], AF.Silu, [ak], ["ll%d" % (4 + ct)])
            QT, KT = [], []
            for ct in range(2):
                yield
                pis = []
                for gI in range(2):
                    b, bk = bank()
                    for k in range(8):
                        r_, rk_ = reps[(ct, gI, k // 4)]
                        mm(b[:, :], r_[:, (k % 4) * 128:(k % 4 + 1) * 128], uT[:, k, :], k == 0, k == 7, [rk_, "uT"], [bk])
                    pis.append((b, bk))
                e1, e1k = fsa()
                act(e1[:, 0:TS], pis[1][0][:, :], AF.Exp, [pis[1][1], "drv"], [e1k], bias=drv[:, 32 + ct:33 + ct], scale=-1.0)
                act(e1[:, 0:TS], e1[:, 0:TS], AF.Ln, [e1k], [e1k], bias=1.0)
                cumsum(ct, e1[:, 0:TS], e1k)
                gam_start(ct, 128)
                ig, igk = fsa()
                act(ig[:, 0:TS], pis[0][0][:, :], AF.Identity, [pis[0][1], "pk"], [igk], bias=PKc(l, "i_b", ct))
                tt(ig[:, 0:TS], ig[:, 0:TS], gnb[ct][:, 1:TS + 1], ALU.add, [igk, "gn%d" % ct], [igk])
                k, kk_ = fsa()
                tsc(k[:, 0:TS], LL[6 + ct][:, :], 0.125, None, ALU.mult, None, ["ll%d" % (6 + ct)], [kk_])
                qk_decay(ct, LL[4 + ct][:, :], "ll%d" % (4 + ct), k[:, 0:TS], kk_, 128, False, BLk(ct), BLk(4 + ct),
                         kextra=(ig, igk))
                QT.append(BLk(ct)); KT.append(BLk(4 + ct))
            GZ = []
            for h in range(4):
                GZ.append(zgate(l, C_MZ, h, PKc(l, "ml_g", h), ["pk"]))
                yield

            def fin(tI, po, pok, pd, pdk):
                den, dnk = fsa()
                act(den[:, 0:TS], pd[:, :], AF.Abs, [pdk], [dnk])
                tsc(den[:, 0:TS], den[:, 0:TS], 1.0, None, ALU.max, None, [dnk], [dnk])
                recip(den[:, 0:TS], den[:, 0:TS], [dnk], [dnk])
                o, ok = fsa()
                tt(o[:, 0:TS], po[:, :], den[:, 0:TS], ALU.mult, [pok, dnk], [ok])
                b, bk = bank()
                mm(b[:, :], CS("ones"), o[:, 0:TS], True, True, [ok, "cst"], [bk])
                d, dk_ = fsa()
                stt(d[:, 0:TS], b[:, :], -1.0 / 128.0, o[:, 0:TS], ALU.mult, ALU.add, [bk, ok], [dk_])
                sq, sqk = fsa()
                act(sq[:, 0:TS], d[:, 0:TS], AF.Square, [dk_], [sqk])
                b2, b2k = bank()
                mm(b2[:, :], CS("ones"), sq[:, 0:TS], True, True, [sqk, "cst"], [b2k])
                rs, rsk = fsa()
                rsq(rs[:, 0:TS], b2[:, :], 1.0 / 128.0, 1e-6, [b2k], [rsk])
                tt(d[:, 0:TS], d[:, 0:TS], rs[:, 0:TS], ALU.mult, [dk_, rsk], [dk_])
                for h in range(4):
                    tt(ys[:, h, tI * 128:(tI + 1) * 128], d[:, h * 128:(h + 1) * 128],
                       GZ[h][0][:, tI * 128:(tI + 1) * 128], ALU.mult, [dk_, GZ[h][1]], ["ys"])

            Qh, Kh = heads64(QT, KT)
            build_gamh(2, 4)
            la(Qh, Kh, KT, 128, 64, 1, gamh, "gamh", fin, ni=2)

        def rwkv(l, sI):
            C = 64
            NCH = TS // C

            def mixed(idx, c0, n, mucol, omcol, dst=None):
                pp, ppk = proj(l, c0, n)
                pb_, pbk_ = fsa()
                cp(pb_[0:n, 0:1], prc[0:n, idx:idx + 1], ["prc"], [pbk_])
                act(pb_[0:n, 1:TS + 1], pp[0:n, :], AF.Copy, [ppk], [pbk_])
                t_, tk_ = fsa()
                tsc(t_[0:n, 0:TS], pb_[0:n, 0:TS], mucol, None, ALU.mult, None, [pbk_, "pk"], [tk_])
                if dst is None:
                    o_, ok_ = fsa()
                    o_ = o_[:, 0:TS]
                else:
                    o_, ok_ = dst
                stt(o_[0:n, :], pb_[0:n, 1:TS + 1], omcol, t_[0:n, 0:TS], ALU.mult, ALU.add, [pbk_, "drv", tk_], [ok_])
                cp(prc[0:n, idx:idx + 1], pb_[0:n, TS:TS + 1], [pbk_], ["prc"])
                return o_, ok_

            WC = mixed(12, C_RW + 1536, 64, pk[0:64, l, PK["mu_wc"][0]:PK["mu_wc"][0] + 1], drv[0:64, 20:21],
                       dst=(wcac[:, 0, :], "wc"))
            AC = mixed(13, C_RW + 1600, 64, pk[0:64, l, PK["mu_ac"][0]:PK["mu_ac"][0] + 1], drv[0:64, 21:22],
                       dst=(wcac[:, 1, :], "ac"))
            yield
            act(wcacb[:, 0, :], WC[0][0:64, :], AF.Tanh, ["wc"], ["wcb"])
            cp(wcacb[:, 1, :], AC[0][0:64, :], ["ac"], ["acb"])
            ops = []
            BON = []
            for ct in range(4):
                csl = slice(ct * 128, (ct + 1) * 128)
                yield
                Kx, Kxk = mixed(4 + ct, C_RW + 512 + ct * 128, 128, PKc(l, "mu", 4 + ct), drv[:, 12 + ct:13 + ct])
                kk, kkk = fsa()
                tsc(kk[:, 0:TS], Kx[:, :], PKc(l, "k_k", ct), None, ALU.mult, None, [Kxk, "pk"], [kkk])
                sq, sqk = fsa()
                act(sq[:, 0:TS], kk[:, 0:TS], AF.Square, [kkk], [sqk])
                b3, b3k = bank()
                mm(b3[:, :], CS("bd"), sq[:, 0:TS], True, True, ["cst", sqk], [b3k])
                rsq(sq[:, 0:TS], b3[:, :], 1.0, 1e-24, [b3k], [sqk])
                tt(kk[:, 0:TS], kk[:, 0:TS], sq[:, 0:TS], ALU.mult, [kkk, sqk], [kkk])
                b, bk = bank()
                mm(b[:, :], wup[:, csl], wcacb[:, 0, :], True, True, ["wup", "wcb"], [bk])
                e1, e1k = fsa()
                act(e1[:, 0:TS], b[:, :], AF.Exp, [bk, "drv"], [e1k], bias=drv[:, 22 + ct:23 + ct], scale=-1.0)
                act(e1[:, 0:TS], e1[:, 0:TS], AF.Ln, [e1k], [e1k], bias=1.0)
                act(e1[:, 0:TS], e1[:, 0:TS], AF.Exp, [e1k], [e1k], bias=-0.5, scale=-1.0)
                cumsum(ct, e1[:, 0:TS], e1k)
                gk = "gn%d" % ct
                yield
                b2, b2k = bank()
                mm(b2[:, :], aup[:, csl], wcacb[:, 1, :], True, True, ["aup", "acb"], [b2k])
                sg, sgk = fsa()
                act(sg[:, 0:TS], b2[:, :], AF.Sigmoid, [b2k, "pk"], [sgk], bias=PKc(l, "a0", ct))
                t1, t1k = fsa()
                tsc(t1[:, 0:TS], sg[:, 0:TS], PKc(l, "k_a", ct), drv[:, 26 + ct:27 + ct], ALU.mult, ALU.add,
                    [sgk, "pk", "drv"], [t1k])
                tt(Kx[:, :], Kx[:, :], t1[:, 0:TS], ALU.mult, [Kxk, t1k], [Kxk])
                tt(sg[:, 0:TS], kk[:, 0:TS], sg[:, 0:TS], ALU.mult, [kkk, sgk], [sgk])
                v3 = lambda ap: ap.rearrange("p (c t) -> p c t", t=C)
                gref = gnb[ct][:, 0:TS:C].unsqueeze(2).to_broadcast([128, NCH, C])
                ei, eik = fsa(); ee, eek = fsa(); ev, evk = fsa()
                tt(v3(ev[:, 0:TS]), v3(gnb[ct][:, 1:TS + 1]), gref, ALU.subtract, [gk], [evk])
                tt(v3(ee[:, 0:TS]), v3(gnb[ct][:, 0:TS]), gref, ALU.subtract, [gk], [eek])
                act(ei[:, 0:TS], ev[:, 0:TS], AF.Exp, [evk], [eik], scale=-1.0)
                act(ev[:, 0:TS], ev[:, 0:TS], AF.Exp, [evk], [evk])
                act(ee[:, 0:TS], ee[:, 0:TS], AF.Exp, [eek], [eek], scale=-1.0)
                rt, at, btl, ktl, vb = [BLk(ct * 5 + j) for j in range(5)]
                stt(at[0][:, :], kk[:, 0:TS], -1.0, ee[:, 0:TS], ALU.mult, ALU.mult, [kkk, eek], [at[1]])
                tt(btl[0][:, :], sg[:, 0:TS], ev[:, 0:TS], ALU.mult, [sgk, evk], [btl[1]])
                tt(ktl[0][:, :], Kx[:, :], ev[:, 0:TS], ALU.mult, [Kxk, evk], [ktl[1]])
                cp(gam[:, ct, 0:NCH], ei[:, C - 1:TS:C], [eik], ["gam"])
                yield
                Rx, Rxk = mixed(ct, C_RW + ct * 128, 128, PKc(l, "mu", ct), drv[:, 8 + ct:9 + ct])
                tt(rt[0][:, :], Rx[:, :], ei[:, 0:TS], ALU.mult, [Rxk, eik], [rt[1]])
                rk, rkk = fsa()
                stt(rk[:, 0:TS], Rx[:, :], PKc(l, "r_k", ct), Kx[:, :], ALU.mult, ALU.mult, [Rxk, "pk", Kxk], [rkk])
                b4, b4k = bank()
                mm(b4[:, :], CS("bd"), rk[:, 0:TS], True, True, ["cst", rkk], [b4k])
                Vx, Vxk = mixed(8 + ct, C_RW + 1024 + ct * 128, 128, PKc(l, "mu", 8 + ct), drv[:, 16 + ct:17 + ct])
                cp(vb[0][:, :], Vx[:, :], [Vxk], [vb[1]])
                tt(LL[ct][:, :], b4[:, :], Vx[:, :], ALU.mult, [b4k, Vxk], ["ll%d" % ct])
                BON.append(LLk(ct))
                ops.append(dict(r=rt, a=at, b=btl, k=ktl, v=vb))

            hd = lambda h: (h // 2, (h % 2) * 64)
            SH = {}
            mhb = merged[:, 4:8, :].bitcast(BF16)
            for ct in range(4):
                for ni_, nm in enumerate(("r", "a", "b", "k")):
                    j = ct * 4 + ni_
                    if j < 8:
                        dst, dkey = mhb[:, j // 2, (j % 2) * 512:(j % 2 + 1) * 512], "mg%d" % (4 + j // 2)
                    elif j < 12:
                        dst, dkey = vtok[:, j - 8, :], "vtok%d" % (j - 8)
                    else:
                        dst, dkey = ktok[:, j - 12, :], "ktok%d" % (j - 12)
                    shift_hi(ops[ct][nm][0][:, :], ops[ct][nm][1], dst[0:64, :], dkey, TS)
                    SH[(ct, nm)] = (dst, dkey)
            build_gamh(4, NCH)

            def opn(nm, h):
                ct, par = h // 2, h % 2
                if par == 0:
                    return ops[ct][nm][0][0:64, :], ops[ct][nm][1]
                return SH[(ct, nm)][0][0:64, :], SH[(ct, nm)][1]

            llb = [LL[4 + i][:, :].bitcast(BF16) for i in range(4)]
            SETB = ("vt", "bt", "kt", "Aak", "Arb", "Ark", "Yf")
            rvs = [dict(rv), dict(rv)]
            rvk = [{n: "rv_" + n for n in rv}, {n: "rv_" + n for n in rv}]
            for i, n in enumerate(SETB):
                rvs[1][n] = llb[i // 2][0:64, (i % 2) * 512:(i % 2 + 1) * 512]
                rvk[1][n] = "ll%d" % (4 + i // 2)

            def phaseA(c):
                R, RK = rvs[c % 2], rvk[c % 2]
                c0 = c * C
                cs = slice(c0, c0 + C)
                for nm, key in (("v", "vt"), ("b", "bt"), ("k", "kt")):
                    bt_, btk2 = bank()
                    btb = bt_[:, :].bitcast(BF16)
                    for ct in range(4):
                        tr(btb[0:64, ct * 128:(ct + 1) * 128], ops[ct][nm][0][:, cs], CB("ident"),
                           [ops[ct][nm][1], "cstb"], [btk2])
                    cp(R[key][:, :], btb[0:64, 0:512], [btk2], [RK[key]])
                yield

                def scores(lhs, rhs, dst, maskn, eng):
                    b_, bk_ = bank()
                    for h in range(8):
                        L_, R_ = opn(lhs, h), opn(rhs, h)
                        mm(b_[0:64, h * 64:(h + 1) * 64], L_[0][:, cs], R_[0][:, cs], True, True, [L_[1], R_[1]], [bk_])
                    tt(R[dst][:, :], b_[0:64, :], CB(maskn)[0:64, :], ALU.mult, [bk_, "cstb"], [RK[dst]])
                scores("a", "b", "N", "r_ts", "dve")
                scores("b", "a", "NT", "r_st", "dve")
                yield
                scores("k", "a", "Aak", "r_st", "dve")
                scores("b", "r", "Arb", "r_sti", "dve")
                scores("k", "r", "Ark", "r_sti", "dve")
                tt(R["Y"][:, :], R["NT"][:, :], CB("i8")[0:64, :], ALU.add, [RK["NT"], "cstb"], [RK["Y"]])
                yield
                M, MT, Y = "N", "NT", "Y"
                M2, MT2, Y2 = "N2", "NT2", "Y2"
                for lev in range(5):
                    last = lev == 4
                    ba, bak = bank()
                    for h in range(8):
                        hs = slice(h * 64, (h + 1) * 64)
                        mm(ba[0:64, hs], R[MT][:, hs], R[M][:, hs], True, True, [RK[MT], RK[M]], [bak])
                    act(R[M2][:, :], ba[0:64, :], AF.Copy, [bak], [RK[M2]])
                    if not last:
                        bb_, bbk_ = bank()
                        for h in range(8):
                            hs = slice(h * 64, (h + 1) * 64)
                            mm(bb_[0:64, hs], R[M][:, hs], R[MT][:, hs], True, True, [RK[MT], RK[M]], [bbk_])
                        cp(R[MT2][:, :], bb_[0:64, :], [bbk_], [RK[MT2]])
                    bc, bck = bank()
                    for h in range(8):
                        hs = slice(h * 64, (h + 1) * 64)
                        mm(bc[0:64, hs], R[M2][:, hs], R[Y][:, hs], True, True, [RK[M2], RK[Y]], [bck])
                    Yd = "Yf" if last else Y2
                    tt(R[Yd][:, :], bc[0:64, :], R[Y][:, :], ALU.add, [bck, RK[Y]], [RK[Yd]])
                    M, M2 = M2, M
                    MT, MT2 = MT2, MT
                    Y, Y2 = Y2, Y
                    yield

            def phaseB(c):
                R, RK = rvs[c % 2], rvk[c % 2]
                c0 = c * C
                cs = slice(c0, c0 + C)
                bx, bxk = bank()
                for h in range(8):
                    hs = slice(h * 64, (h + 1) * 64)
                    A_ = opn("a", h)
                    mm(bx[0:64, hs], A_[0][:, cs], rw_sb[:, h, :], True, False, [A_[1], "rw_sb"], [bxk])
                    mm(bx[0:64, hs], R["Aak"][:, hs], R["vt"][:, hs], False, True, [RK["Aak"], RK["vt"]], [bxk])
                cp(R["X"][:, :], bx[0:64, :], [bxk], [RK["X"]])
                yield
                bu, buk = bank()
                for h in range(8):
                    hs = slice(h * 64, (h + 1) * 64)
                    mm(bu[0:64, hs], R["Yf"][:, hs], R["X"][:, hs], True, True, [RK["Yf"], RK["X"]], [buk])
                act(R["U"][:, :], bu[0:64, :], AF.Copy, [buk], [RK["U"]])
                yield
                bs_, bsk = bank()
                for h in range(8):
                    hs = slice(h * 64, (h + 1) * 64)
                    mm(bs_[0:64, hs], R["bt"][:, hs], R["U"][:, hs], True, False, [RK["bt"], RK["U"]], [bsk])
                    mm(bs_[0:64, hs], R["kt"][:, hs], R["vt"][:, hs], False, True, [RK["kt"], RK["vt"]], [bsk])
                by, byk = bank()
                for h in range(8):
                    ct, pb = hd(h)
                    hs = slice(h * 64, (h + 1) * 64)
                    o_ = by[pb:pb + 64, ct * 64:(ct + 1) * 64]
                    R_ = opn("r", h)
                    mm(o_, rw_sb[:, h, :], R_[0][:, cs], True, False, ["rw_sb", R_[1]], [byk])
                    mm(o_, R["U"][:, hs], R["Arb"][:, hs], False, False, [RK["U"], RK["Arb"]], [byk])
                    mm(o_, R["vt"][:, hs], R["Ark"][:, hs], False, True, [RK["vt"], RK["Ark"]], [byk])
                tmp_, tmpk = fsa()
                tv = tmp_[0:64, 0:512].rearrange("p (h v) -> p h v", h=8)
                tt(tv, bs_[0:64, :].rearrange("p (h v) -> p h v", h=8), rw_sf[:, :, :], ALU.add, [bsk, "rw_sf"], [tmpk])
                gb = gamh[0:64, 0:8, c:c + 1].to_broadcast([64, 8, 64])
                tt(rw_sb[:, :, :], tv, gb, ALU.mult, [tmpk, "gamh"], ["rw_sb"])
                tt(rw_sf[:, :, :], tv, gb, ALU.mult, [tmpk, "gamh"], ["rw_sf"])
                act(yT[:, 0:4, cs], by[:, 0:256].rearrange("p (c t) -> p c t", c=4), AF.Copy, [byk],
                    ["mg0", "mg1", "mg2", "mg3"])
                yield

            drive([phaseA(0)])
            for c in range(NCH):
                drive([phaseB(c), phaseA(c + 1) if c + 1 < NCH else None])
            for ct in range(4):
                mk = "mg%d" % ct
                b, bk = bank()
                mm(b[:, :], CS("bd"), yT[:, ct, :], True, True, ["cst", mk], [bk])
                d, dk_ = fsa()
                stt(d[:, 0:TS], b[:, :], -1.0 / 64.0, yT[:, ct, :], ALU.mult, ALU.add, [bk, mk], [dk_])
                sq, sqk = fsa()
                act(sq[:, 0:TS], d[:, 0:TS], AF.Square, [dk_], [sqk])
                b2, b2k = bank()
                mm(b2[:, :], CS("bd"), sq[:, 0:TS], True, True, ["cst", sqk], [b2k])
                rsq(sq[:, 0:TS], b2[:, :], 1.0 / 64.0, 64e-5, [b2k], [sqk])
                tt(d[:, 0:TS], d[:, 0:TS], sq[:, 0:TS], ALU.mult, [dk_, sqk], [dk_])
                act(d[:, 0:TS], d[:, 0:TS], AF.Identity, [dk_, "pk"], [dk_], bias=PKc(l, "ln_b", ct), scale=PKc(l, "ln_g", ct))
                tt(d[:, 0:TS], d[:, 0:TS], BON[ct][0][:, :], ALU.add, [dk_, BON[ct][1]], [dk_])
                pz, pzk = proj(l, C_RWZ + ct * 128, 128)
                z, zk = fsa()
                act(z[:, 0:TS], pz[:, :], AF.Silu, [pzk], [zk])
                tt(ys[:, ct, :], d[:, 0:TS], z[:, 0:TS], ALU.mult, [dk_, zk], ["ys"])

        outs = program()
        S.emit(final_wait_ops=outs)
    return nc


_NC_CACHE = {}


def make_in_maps(inp, n_cores=8):
    f = lambda a: np.ascontiguousarray(np.asarray(a, dtype=np.float32))
    pkall = np.stack([_pack_params(inp, l) for l in range(DEPTH)], 0)
    fgT = np.ascontiguousarray(f(inp["final_g"]).reshape(8, 128).T)
    shared = {
        "pk": pkall, "fg": fgT, "cst": CONSTS, "cstf": CSTF,
        "ada_w": f(inp["ada_w"]), "w_in": f(inp["w_in"]), "rw_w_up": f(inp["rw_w_up"]), "rw_a_up": f(inp["rw_a_up"]),
        "gla_gk_up": f(inp["gla_gk_up"]), "w_branch": f(inp["w_branch"]), "w_out": f(inp["w_out"]),
    }
    maps = []
    for b in range(n_cores):
        m = dict(shared)
        m["xT"] = np.ascontiguousarray(f(inp["x"][b]).T)
        m["cT"] = np.ascontiguousarray(f(inp["c"][b]).reshape(8, 128).T)
        maps.append(m)
    return maps


def kernel(**inputs):
    inp = {k: np.asarray(v) for k, v in inputs.items()}
    if "full" not in _NC_CACHE:
        _NC_CACHE["full"] = build_nc()
    nc = _NC_CACHE["full"]
    maps = make_in_maps(inp)
    res = run_bass_kernel_spmd(nc, maps, core_ids=list(range(8)))
    out = np.stack([np.ascontiguousarray(r["outT"].T) for r in res.results], 0)
    return out.astype(np.float32)
```

```python
import contextlib
import numpy as np
import ml_dtypes
import concourse.bass as bass
import concourse.mybir as mybir
from concourse.bass_utils import run_bass_kernel_spmd

F32 = mybir.dt.float32
BF16 = mybir.dt.bfloat16
AF = mybir.ActivationFunctionType
ALU = mybir.AluOpType

D = 1024
SEQ = 2048
DEPTH = 4
NCOLS = 11416
TS = 512
NST = SEQ // TS
ENGS = ("pe", "dve", "act", "pool", "sp")

C_RW = 0; C_RWZ = 1664
C_GQ = 2176; C_GK = 2432; C_GV = 2688; C_GC = 3200; C_GZ = 3216
C_MQK = 3728; C_MV = 4240; C_MI = 4752; C_MF = 4756; C_MZ = 4760
C_HQ = 5272; C_HF = 5784; C_HI = 6296; C_HZ = 6808
C_MG = 7320

PK = {}
_o = 0
for _n, _w in (("norm_g", 8), ("ada_b", 24), ("mu", 12), ("mu_wc", 1), ("mu_ac", 1), ("w0", 4), ("a0", 4),
               ("k_k", 4), ("k_a", 4), ("r_k", 4), ("ln_g", 4), ("ln_b", 4), ("gk_b", 2), ("gla_g", 4),
               ("conv", 16), ("ml_g", 4), ("lb", 16), ("hg_g", 4), ("i_b", 2), ("f_b", 2)):
    PK[_n] = (_o, _w)
    _o += _w
NPK = _o


class Op:
    __slots__ = ("eng", "fn", "deps", "idx", "dma_stream", "signal", "cnt")

    def __init__(self, eng, fn):
        self.eng = eng
        self.fn = fn
        self.deps = []
        self.dma_stream = None
        self.signal = False
        self.cnt = 0


class Sched:
    def __init__(self, nc):
        self.nc = nc
        self.ops = []
        self.last_w = {}
        self.readers = {}
        self.streams = {}
        self.record = False

    def op(self, eng, fn, reads=(), writes=(), dma=None):
        if self.record:
            return None
        o = Op(eng, fn)
        o.idx = len(self.ops)
        deps = {}
        for k in reads:
            w = self.last_w.get(k)
            if w is not None:
                deps[w.idx] = w
        for k in writes:
            w = self.last_w.get(k)
            if w is not None:
                deps[w.idx] = w
            for r in self.readers.get(k, ()):
                deps[r.idx] = r
        if dma is not None:
            o.dma_stream = dma
            n = self.streams.get(dma, 0) + 1
            self.streams[dma] = n
            o.cnt = n
        o.deps = [deps[i] for i in sorted(deps)]
        for k in reads:
            self.readers.setdefault(k, []).append(o)
        for k in writes:
            self.last_w[k] = o
            self.readers[k] = []
        self.ops.append(o)
        return o

    def emit(self, final_wait_ops=()):
        nc = self.nc
        for o in self.ops:
            nd = []
            for d in o.deps:
                if d.dma_stream is None and o.dma_stream is None and d.eng == o.eng and o.eng == "pe":
                    continue
                nd.append(d)
            o.deps = nd
            for d in nd:
                d.signal = True
        for o in final_wait_ops:
            o.signal = True
        cnt = {e: 0 for e in ENGS}
        for o in self.ops:
            if o.dma_stream is None and o.signal:
                cnt[o.eng] += 1
                o.cnt = cnt[o.eng]
        EP, DEP = 10 ** 9, 10 ** 9
        with contextlib.ExitStack() as es:
            esem = {}
            for e in ENGS:
                for ep in range(max(1, (cnt[e] + EP - 1) // EP)):
                    esem[(e, ep)] = es.enter_context(nc.semaphore("c_%s%d" % (e, ep)))
            ssem = {}
            for i, (s_, n_) in enumerate(self.streams.items()):
                for ep in range(max(1, (n_ + DEP - 1) // DEP)):
                    ssem[(s_, ep)] = es.enter_context(nc.semaphore("d%d_%d" % (i, ep)))
            block = es.enter_context(nc.Block())
            per = {e: [o for o in self.ops if o.eng == e] for e in ENGS}

            def semval(d):
                if d.dma_stream is not None:
                    return ssem[(d.dma_stream, (d.cnt - 1) // DEP)], 16 * ((d.cnt - 1) % DEP + 1)
                return esem[(d.eng, (d.cnt - 1) // EP)], (d.cnt - 1) % EP + 1

            def runner(e):
                def body(engine):
                    waited = {}
                    for o in per[e]:
                        for d in o.deps:
                            sem, val = semval(d)
                            key = id(sem)
                            if waited.get(key, 0) >= val:
                                continue
                            waited[key] = val
                            engine.wait_ge(sem, val)
                        ins = o.fn(engine)
                        if o.dma_stream is not None:
                            ins.then_inc(semval(o)[0], 16)
                        elif o.signal:
                            ins.then_inc(semval(o)[0], 1)
                    if e == "sp":
                        for o in final_wait_ops:
                            sem, val = semval(o)
                            engine.wait_ge(sem, val)
                return body

            block.tensor(runner("pe"))
            block.vector(runner("dve"))
            block.scalar(runner("act"))
            block.gpsimd(runner("pool"))
            block.sync(runner("sp"))
        return cnt


def _host_consts():
    c = {}
    i128 = np.eye(128, dtype=np.float32)
    c["ident"] = i128
    s = np.arange(128)[:, None]
    t = np.arange(128)[None, :]
    m_incl = (s <= t).astype(np.float32)
    m_blk = ((s <= t) & ((s // 64) == (t // 64))).astype(np.float32)
    c["m128"] = np.tile(m_incl[:, None, :], (1, 4, 1)).reshape(128, 512)
    c["m64b"] = np.tile(m_blk[:, None, :], (1, 4, 1)).reshape(128, 512)
    m_b32 = ((s <= t) & ((s // 32) == (t // 32))).astype(np.float32)
    c["m32b"] = np.tile(m_b32[:, None, :], (1, 4, 1)).reshape(128, 512)
    m96 = np.zeros((128, 128), np.float32); m96[96:] = 1.0
    c["m96"] = m96
    s6 = np.arange(64)[:, None]
    t6 = np.arange(64)[None, :]
    z = np.zeros((128, 512), np.float32)
    a = z.copy(); a[:64] = np.tile((t6 < s6).astype(np.float32)[:, None, :], (1, 8, 1)).reshape(64, 512)
    c["r_ts"] = a
    a = z.copy(); a[:64] = np.tile((s6 < t6).astype(np.float32)[:, None, :], (1, 8, 1)).reshape(64, 512)
    c["r_st"] = a
    a = z.copy(); a[:64] = np.tile((s6 <= t6).astype(np.float32)[:, None, :], (1, 8, 1)).reshape(64, 512)
    c["r_sti"] = a
    a = z.copy(); a[:64] = np.tile(np.eye(64, dtype=np.float32)[:, None, :], (1, 8, 1)).reshape(64, 512)
    c["i8"] = a
    c["ones"] = np.ones((128, 128), np.float32)
    bd = np.zeros((128, 128), np.float32); bd[:64, :64] = 1; bd[64:, 64:] = 1
    c["bd"] = bd
    names = ["ident", "m128", "m32b", "r_ts", "r_st", "r_sti", "i8", "ones", "bd", "m96"]
    offs = {}
    o = 0
    for n in names:
        offs[n] = (o, c[n].shape[1]); o += c[n].shape[1]
    return np.concatenate([c[n] for n in names], axis=1), offs


CONSTS, COFF = _host_consts()
NCONST = CONSTS.shape[1]
CFO = {"ident": 0, "ones": 128, "bd": 256, "m96": 384}
NCF = 512
CSTF = np.concatenate([CONSTS[:, COFF[n][0]:COFF[n][0] + 128] for n in ("ident", "ones", "bd", "m96")], axis=1)


def _pack_params(inp, l):
    P = np.zeros((128, NPK), np.float32)

    def put(name, arr):
        o, w = PK[name]
        P[:arr.shape[0], o:o + arr.shape[1]] = arr

    fm = lambda v: np.ascontiguousarray(v.reshape(-1, 128).T)
    put("norm_g", fm(inp["norm_g"][l]))
    put("ada_b", fm(inp["ada_b"][l]))
    mu = inp["rw_mu"][l]
    put("mu", fm(mu[:1536]))
    put("mu_wc", mu[1536:1600].reshape(64, 1))
    put("mu_ac", mu[1600:1664].reshape(64, 1))
    put("w0", fm(inp["rw_w0"][l])); put("a0", fm(inp["rw_a0"][l]))
    put("k_k", fm(inp["rw_k_k"][l])); put("k_a", fm(inp["rw_k_a"][l])); put("r_k", fm(inp["rw_r_k"][l]))
    put("ln_g", fm(inp["rw_ln_g"][l])); put("ln_b", fm(inp["rw_ln_b"][l]))
    put("gk_b", fm(inp["gla_gk_b"][l])); put("gla_g", fm(inp["gla_norm_g"][l]))
    cw = inp["ml_conv_w"][l]
    put("conv", np.concatenate([fm(cw[j]) for j in range(4)], axis=1))
    put("ml_g", fm(inp["ml_norm_g"][l]))
    put("lb", np.concatenate([fm(inp["hg_lb_logits"][j]) for j in range(4)], axis=1))
    put("hg_g", fm(inp["hg_norm_g"][l]))
    put("i_b", fm(np.repeat(inp["ml_i_b"][l], 64)))
    put("f_b", fm(np.repeat(inp["ml_f_b"][l], 64)))
    return P


def build_nc(n_layers=DEPTH, branches=(0, 1, 2, 3), debug=False, stage=99):
    nc = bass.Bass("TRN2", target_bir_lowering=False)
    dram = lambda n, s, k="ExternalInput": nc.dram_tensor(n, list(s), F32, kind=k).ap()
    xT_d = dram("xT", [D, SEQ])
    c_d = dram("cT", [128, 8])
    pk_d = dram("pk", [DEPTH, 128, NPK])
    fg_d = dram("fg", [128, 8])
    cst_d = dram("cst", [128, NCONST])
    cstf_d = dram("cstf", [128, NCF])
    adaw_d = dram("ada_w", [DEPTH, D, 3 * D])
    win_d = dram("w_in", [DEPTH, D, NCOLS])
    wup_d = dram("rw_w_up", [DEPTH, 64, 512])
    aup_d = dram("rw_a_up", [DEPTH, 64, 512])
    gup_d = dram("gla_gk_up", [DEPTH, 16, 256])
    wbr_d = dram("w_branch", [DEPTH, 4, 512, D])
    wout_d = dram("w_out", [DEPTH, D, D])
    out_d = dram("outT", [D, SEQ], "ExternalOutput")
    dbg_d = dram("dbg", [4, 512, SEQ], "ExternalOutput") if debug else None

    es = contextlib.ExitStack()
    with es:
        T = lambda n, s, d=F32: es.enter_context(nc.sbuf_tensor("s_" + n, list(s), d))
        x = T("x", [128, 8, TS])
        uT = T("uT", [128, 8, TS], BF16)
        merged = T("merged", [128, 8, TS])
        mergedb = uT
        ys = T("ys", [128, 4, TS], BF16)
        pk = T("pk", [128, DEPTH, NPK])
        drv = T("drv", [128, 64])
        cst = T("cst", [128, NCF])
        cstb = T("cstb", [128, NCONST], BF16)
        cT = T("cT", [128, 8])
        fg = T("fg", [128, 8])
        mod = T("mod", [128, 24])
        lbt = T("lbt", [128, 16]); lbe = T("lbe", [128, 16]); lbs = T("lbs", [128, 4])
        NWB = 6
        wb = [T("wb%d" % i, [128, 8, 128], BF16) for i in range(NWB)]
        wv = [T("wv%d" % i, [128, 8, 512], BF16) for i in range(1)]
        wup = T("wup", [64, 512], BF16); aup = T("aup", [64, 512], BF16); gup = T("gup", [16, 256], BF16)
        NF = 14
        mtl = [T("mt%d" % i, [128, TS]) for i in range(2)]
        fs = [T("fs%d" % i, [128, TS + 4]) for i in range(NF)]
        LL = [T("ll%d" % i, [128, TS]) for i in range(8)]
        NB = 5
        bs = [T("bs%d" % i, [128, TS], BF16) for i in range(NB)]
        BL = [T("bl%d" % i, [128, TS], BF16) for i in range(20)]
        prc = T("prc", [128, 16])
        cqc = T("cqc", [128, 4, 4])
        gnb = [T("gn%d" % i, [128, TS + 1]) for i in range(4)]
        nref = T("nref", [128, 4, 16])
        ktz = T("ktz", [128, 2, 512], BF16)
        vtok = T("vtok", [128, 4, 512], BF16)
        ktok = T("ktok", [128, 4, 512], BF16)
        rv = {n: T("rv_" + n, [64, 512], BF16) for n in
              ("vt", "bt", "kt", "N", "NT", "N2", "NT2", "Y", "Y2", "Aak", "Arb", "Ark", "X", "U", "Yf")}
        rw_sf = T("rw_sf", [64, 8, 64]); rw_sb = T("rw_sb", [64, 8, 64], BF16)
        la_sf = [T("la_sf%d" % i, [128, 4, 128]) for i in range(4)]
        la_sb = [T("la_sb%d" % i, [128, 4, 128], BF16) for i in range(4)]
        gamh = T("gamh", [64, 8, 16])
        gam = T("gam", [128, 4, 16])
        yT = merged
        wcac = T("wcac", [64, 2, TS])
        wcacb = T("wcacb", [64, 2, TS], BF16)
        ps = [es.enter_context(nc.psum_tensor("ps%d" % i, [128, 512], F32)) for i in range(8)]

        S = Sched(nc)
        st = {"bank": 0, "w": 0, "fsi": 0, "bsi": 0, "wai": 0}

        def bank():
            i = st["bank"]; st["bank"] = (i + 1) % 6
            return ps[i], "ps%d" % i

        CS = lambda n: cst[:, CFO[n]:CFO[n] + 128]
        CB = lambda n: cstb[:, COFF[n][0]:COFF[n][0] + COFF[n][1]]
        PKc = lambda l, n, j=0, w=1: pk[:, l, PK[n][0] + j:PK[n][0] + j + w]

        def mm(out, lhsT, rhs, start, stop, r, w):
            S.op("pe", lambda e: e.matmul(out, lhsT, rhs, start=start, stop=stop), reads=r, writes=w)

        def tr(out, in_, ident, r, w):
            S.op("pe", lambda e: e.transpose(out, in_, ident), reads=r, writes=w)

        def act(out, in_, func, r, w, bias=None, scale=None):
            kw = {}
            if bias is not None:
                kw["bias"] = bias
            if scale is not None:
                kw["scale"] = scale
            S.op("act", lambda e: e.activation(out=out, in_=in_, func=func, **kw), reads=r, writes=w)

        def tt(out, a, b, op, r, w, eng="dve"):
            S.op(eng, lambda e: e.tensor_tensor(out=out, in0=a, in1=b, op=op), reads=r, writes=w)

        def tsc(out, a, s1, s2, op0, op1, r, w, eng="dve"):
            if op1 is None:
                S.op(eng, lambda e: e.tensor_scalar(out=out, in0=a, scalar1=s1, scalar2=None, op0=op0), reads=r, writes=w)
            else:
                S.op(eng, lambda e: e.tensor_scalar(out=out, in0=a, scalar1=s1, scalar2=s2, op0=op0, op1=op1),
                     reads=r, writes=w)

        def stt(out, a, sc, b, op0, op1, r, w, eng="dve"):
            S.op(eng, lambda e: e.scalar_tensor_tensor(out=out, in0=a, scalar=sc, in1=b, op0=op0, op1=op1),
                 reads=r, writes=w)

        def cp(out, in_, r, w, eng="dve"):
            S.op(eng, lambda e: e.tensor_copy(out=out, in_=in_), reads=r, writes=w)

        def rsq(out, in_, scale, bias, r, w):
            act(out, in_, AF.Ln, r, w, bias=bias, scale=scale)
            act(out, out, AF.Exp, w, w, scale=-0.5)

        def recip(out, in_, r, w):
            S.op("dve", lambda e: e.reciprocal(out=out, in_=in_), reads=r, writes=w)

        def mset(ap, val, w):
            S.op("dve", lambda e: e.memset(ap, val), writes=w)

        def dma(eng, out, in_, r, w, stream):
            return S.op(eng, lambda e: e.dma_start(out=out, in_=in_), reads=r, writes=w, dma=stream)

        def cumsum(ct, src, srck):
            S.op("dve", lambda e: e.tensor_tensor_scan(out=gnb[ct][:, 1:TS + 1], data0=src, data1=src, initial=0.0,
                                                       op0=ALU.add, op1=ALU.max), reads=[srck], writes=["gn%d" % ct])

        def wchunk(src, n, kc=8):
            i = st["w"]; st["w"] = (i + 1) % NWB
            key = "wb%d" % i
            dma("pool", wb[i][:, 0:kc, 0:n], src.rearrange("(k p) c -> p k c", p=128), [], [key], key)
            return wb[i], key

        def proj(l, c0, n):
            w, wk = wchunk(win_d[l, :, c0:c0 + n], n)
            b, bk = bank()
            for k in range(8):
                mm(b[0:n, :], w[:, k, 0:n], uT[:, k, :], k == 0, k == 7, [wk, "uT"], [bk])
            return b, bk

        def proj_tok(l, c0):
            for h in range(2):
                dma("pool", wv[0][:, :, h * 256:(h + 1) * 256],
                    win_d[l, :, c0 + h * 256:c0 + (h + 1) * 256].rearrange("(k p) c -> p k c", p=128),
                    [], ["wv_%d" % h], "wv_%d" % h)
            for tI in range(TS // 128):
                b, bk = bank()
                for k in range(8):
                    mm(b[:, :], uT[:, k, tI * 128:(tI + 1) * 128], wv[0][:, k, :], k == 0, k == 7,
                       ["wv_0", "wv_1", "uT"], [bk])
                if tI % 2 == 0:
                    act(vtok[:, tI, :], b[:, :], AF.Copy, [bk], ["vtok%d" % tI])
                else:
                    cp(vtok[:, tI, :], b[:, :], [bk], ["vtok%d" % tI])

        def fsa():
            i = st["fsi"]; st["fsi"] = (i + 1) % NF
            return fs[i], "fs%d" % i

        def bsa():
            i = st["bsi"]; st["bsi"] = (i + 1) % NB
            return bs[i], "bs%d" % i

        XK = ["x%d" % k for k in range(8)]
        FINAL_OUTS = []

        def rms_to(sl_src, emit):
            b, bk = bank()
            for k in range(8):
                sq, sqk = bsa()
                act(sq[:, :], x[:, k, :], AF.Square, [XK[k]], [sqk])
                mm(b[:, :], CB("ones"), sq[:, :], k == 0, k == 7, [sqk, "cstb"], [bk])
            rs, rsk = fsa()
            rsq(rs[:, 0:TS], b[:, :], 1.0, 1024.0 * 1e-6, [bk], [rsk])
            for k in range(8):
                t1, t1k = fsa()
                tt(t1[:, 0:TS], x[:, k, :], rs[:, 0:TS], ALU.mult, [XK[k], rsk], [t1k])
                emit(k, t1, t1k)

        def program():
            dma("sp", cst[:, :], cstf_d, [], ["cst"], "cst")
            dma("pool", cstb[:, :], cst_d, [], ["cstb"], "cstb")
            dma("sp", pk[:, :, :], pk_d.rearrange("l p n -> p l n"), [], ["pk"], "pk")
            dma("sp", cT[:, :], c_d, [], ["cT"], "cT")
            dma("sp", fg[:, :], fg_d, [], ["fg"], "fg")
            act(cT[:, :], cT[:, :], AF.Silu, ["cT"], ["cT"])
            o_lb = PK["lb"][0]
            act(lbe[:, :], pk[:, 0, o_lb:o_lb + 16], AF.Exp, ["pk"], ["lbe"])
            tt(lbs[:, :], lbe[:, 0:4], lbe[:, 4:8], ALU.add, ["lbe"], ["lbs"])
            tt(lbs[:, :], lbs[:, :], lbe[:, 8:12], ALU.add, ["lbs", "lbe"], ["lbs"])
            tt(lbs[:, :], lbs[:, :], lbe[:, 12:16], ALU.add, ["lbs", "lbe"], ["lbs"])
            recip(lbs[:, :], lbs[:, :], ["lbs"], ["lbs"])
            mset(lbt[:, 0:4], 0.0, ["lbt"])
            for j in range(1, 4):
                t_, tk_ = fsa()
                tt(t_[:, 0:4], lbe[:, j * 4:(j + 1) * 4], lbs[:, :], ALU.mult, ["lbe", "lbs"], [tk_])
                tt(lbt[:, j * 4:(j + 1) * 4], lbt[:, (j - 1) * 4:j * 4], t_[:, 0:4], ALU.add, [tk_, "lbt"], ["lbt"])
            for i in range(4):
                mset(gnb[i][:, 0:1], 0.0, ["gn%d" % i])

            for l in range(n_layers):
                layer(l)

            outs = FINAL_OUTS
            if n_layers == 0:
                for sI in range(NST):
                    sl = slice(sI * TS, (sI + 1) * TS)
                    for k in range(8):
                        dma("sp", x[:, k, :], xT_d[k * 128:(k + 1) * 128, sl], ["xd%d" % sI], [XK[k]], "xl%d" % k)

                    def emit(k, t1, t1k, sI=sI, sl=sl):
                        tsc(t1[:, 0:TS], t1[:, 0:TS], fg[:, k:k + 1], 32.0, ALU.mult, ALU.mult, [t1k, "fg"], [t1k])
                        outs.append(dma("sp", out_d[k * 128:(k + 1) * 128, sl], t1[:, 0:TS], [t1k], ["xd%d" % sI], "o_" + t1k))
                    rms_to(sl, emit)
            return outs

        def layer(l):
            b, bk = bank()
            for j in range(24):
                i = st["wai"]; st["wai"] = (i + 1) % 3
                wa_i = merged[:, 2 * i:2 * i + 2, :].rearrange("p a (k c) -> p (a k) c", c=128)
                keys = ["mg%d" % (2 * i), "mg%d" % (2 * i + 1)]
                dma("sp", wa_i, adaw_d[l, :, j * 128:(j + 1) * 128].rearrange("(k p) c -> p k c", p=128),
                    [], keys, "wa%d" % i)
                for k in range(8):
                    mm(b[:, j:j + 1], wa_i[:, k, :], cT[:, k:k + 1], k == 0, k == 7, keys + ["cT"], [bk])
            tt(mod[:, :], b[:, 0:24], pk[:, l, PK["ada_b"][0]:PK["ada_b"][0] + 24], ALU.add, [bk, "pk"], ["mod"])
            stt(drv[:, 0:8], mod[:, 8:16], 1.0, pk[:, l, PK["norm_g"][0]:PK["norm_g"][0] + 8], ALU.add, ALU.mult,
                ["mod", "pk"], ["drv"])
            tsc(drv[:, 0:8], drv[:, 0:8], 32.0, None, ALU.mult, None, ["drv"], ["drv"])
            o_mu = PK["mu"][0]
            tsc(drv[:, 8:22], pk[:, l, o_mu:o_mu + 14], -1.0, 1.0, ALU.mult, ALU.add, ["pk"], ["drv"])
            tsc(drv[:, 22:26], PKc(l, "w0", 0, 4), -1.0, None, ALU.mult, None, ["pk"], ["drv"])
            tsc(drv[:, 26:30], PKc(l, "k_a", 0, 4), -1.0, 1.0, ALU.mult, ALU.add, ["pk"], ["drv"])
            tsc(drv[:, 30:32], PKc(l, "gk_b", 0, 2), -1.0, None, ALU.mult, None, ["pk"], ["drv"])
            tsc(drv[:, 32:34], PKc(l, "f_b", 0, 2), -1.0, None, ALU.mult, None, ["pk"], ["drv"])
            tsc(drv[:, 34:38], lbt[:, l * 4:(l + 1) * 4], -1.0, 1.0, ALU.mult, ALU.add, ["lbt"], ["drv"])
            tsc(drv[:, 38:42], PKc(l, "gla_g", 0, 4), float(np.sqrt(128.0)), None, ALU.mult, None, ["pk"], ["drv"])
            tsc(drv[:, 42:46], PKc(l, "hg_g", 0, 4), float(np.sqrt(128.0)), None, ALU.mult, None, ["pk"], ["drv"])
            dma("pool", wup[:, :], wup_d[l], [], ["wup"], "wup")
            dma("pool", aup[:, :], aup_d[l], [], ["aup"], "aup")
            dma("pool", gup[:, :], gup_d[l], [], ["gup"], "gup")
            mset(rw_sf[:, :, :], 0.0, ["rw_sf"])
            mset(rw_sb[:, :, :], 0.0, ["rw_sb"])
            for i in range(4):
                mset(la_sf[i][:, :, :], 0.0, ["la_sf%d" % i])
                mset(la_sb[i][:, :, :], 0.0, ["la_sb%d" % i])
            mset(prc[:, :], 0.0, ["prc"])
            mset(cqc[:, :, :], 0.0, ["cqc"])

            src_d = xT_d if l == 0 else out_d
            for sI in range(NST):
                sl = slice(sI * TS, (sI + 1) * TS)
                for k in range(8):
                    dma("sp", x[:, k, :], src_d[k * 128:(k + 1) * 128, sl], ["xd%d" % sI], [XK[k]], "xl%d" % k)

                def emit(k, t1, t1k):
                    act(uT[:, k, :], t1[:, 0:TS], AF.Identity, [t1k, "drv", "mod"], ["uT"],
                        bias=mod[:, k:k + 1], scale=drv[:, k:k + 1])
                rms_to(sl, emit)
                def merge_gen(m):
                    for fc in range(8):
                        pg, pgk = proj(l, C_MG + m * 1024 + fc * 128, 128)
                        sg, sgk = mtl[fc % 2], "mt%d" % (fc % 2)
                        act(sg[:, :], pg[:, :], AF.Sigmoid, [pgk], [sgk])
                        w, wk = wchunk(wbr_d[l, m, :, fc * 128:(fc + 1) * 128], 128, kc=4)
                        pb, pbk = bank()
                        for k in range(4):
                            mm(pb[:, :], w[:, k, :], ys[:, k, :], k == 0, k == 3, [wk, "ys"], [pbk])
                        if m == 0:
                            tt(merged[:, fc, :], pb[:, :], sg[:, :], ALU.mult, [pbk, sgk], ["mg%d" % fc])
                        else:
                            tt(sg[:, :], pb[:, :], sg[:, :], ALU.mult, [pbk, sgk], [sgk])
                            tt(merged[:, fc, :], merged[:, fc, :], sg[:, :], ALU.add, [sgk, "mg%d" % fc], ["mg%d" % fc])
                        yield

                def zero_gen():
                    for _ in range(9):
                        yield
                    mset(ys[:, :, :], 0.0, ["ys"])
                    yield

                g_prev = None
                for m in range(4):
                    gm = (rwkv, gla, mlstm, hgrn)[m](l, sI) if m in branches else zero_gen()
                    drive([gm, g_prev])
                    if debug and l == 0:
                        for k in range(4):
                            dma("pool", dbg_d[m, k * 128:(k + 1) * 128, sl], ys[:, k, :], ["ys"], [], "dbg%d" % k)
                    g_prev = merge_gen(m)
                drive([g_prev])
                for fc in range(8):
                    act(mergedb[:, fc, :], merged[:, fc, :], AF.Copy, ["mg%d" % fc], ["uT"])
                for fc in range(8):
                    w, wk = wchunk(wout_d[l, :, fc * 128:(fc + 1) * 128], 128)
                    pb, pbk = bank()
                    for k in range(8):
                        mm(pb[:, :], w[:, k, :], mergedb[:, k, :], k == 0, k == 7, [wk, "uT"], [pbk])
                    stt(x[:, fc, :], pb[:, :], mod[:, 16 + fc:17 + fc], x[:, fc, :], ALU.mult, ALU.add,
                        [pbk, "mod", XK[fc]], [XK[fc]])
                    if l != n_layers - 1:
                        dma("sp", out_d[fc * 128:(fc + 1) * 128, sl], x[:, fc, :], [XK[fc]], ["xd%d" % sI], "xs%d" % fc)
                if l == n_layers - 1:
                    def emit_fin(k, t1, t1k, sI=sI, sl=sl):
                        tsc(t1[:, 0:TS], t1[:, 0:TS], fg[:, k:k + 1], 32.0, ALU.mult, ALU.mult, [t1k, "fg"], [t1k])
                        FINAL_OUTS.append(dma("sp", out_d[k * 128:(k + 1) * 128, sl], t1[:, 0:TS], [t1k], ["xd%d" % sI],
                                              "o_" + t1k))
                    rms_to(sl, emit_fin)

        def shift_hi(src_ap, skey, dst_ap, dkey, n, fp32=False):
            b, bk = bank()
            idn = (CS if fp32 else CB)("ident")
            mm(b[0:64, 0:n], idn[:, 64:128], src_ap, True, True, [skey, "cst" if fp32 else "cstb"], [bk])
            cp(dst_ap, b[0:64, 0:n], [bk], [dkey])

        def build_gamh(npair, nch):
            for ct in range(npair):
                cp(gamh[:, 2 * ct, 0:nch], gam[0:64, ct, 0:nch], ["gam"], ["gamh"])
                shift_hi(gam[:, ct, 0:nch], "gam", gamh[:, 2 * ct + 1, 0:nch], "gamh", nch, fp32=True)

        def drive(gens):
            gens = [g for g in gens if g is not None]
            while gens:
                for g in list(gens):
                    try:
                        next(g)
                    except StopIteration:
                        gens.remove(g)

        def la(Qh, Kh, KTp, C, dk, si, gm, gmk, fin, ni=None):
            mask = {128: CB("m128"), 32: CB("m32b")}[C]
            sfk, sbk = "la_sf%d" % si, "la_sb%d" % si
            nkt = len(KTp)
            PT = {}

            def phA(tI):
                tsl = slice(tI * 128, (tI + 1) * 128)
                bt, btk = bank()
                btb = bt[:, :].bitcast(BF16)
                for ct in range(nkt):
                    tr(btb[:, ct * 128:(ct + 1) * 128], KTp[ct][0][:, tsl], CB("ident"), [KTp[ct][1], "cstb"], [btk])
                cp(ktok[:, tI, 0:nkt * 128], btb[:, 0:nkt * 128], [btk], ["ktok%d" % tI])
                if C == 32:
                    tsc(ktz[64:128, tI % 2, 0:nkt * 128], ktok[64:128, tI, 0:nkt * 128], CS("m96")[64:128, 0:1], None,
                        ALU.mult, None, ["ktok%d" % tI, "cst"], ["ktz%d" % (tI % 2)])
                yield
                bsc, bsck = bank()
                for h in range(4):
                    mm(bsc[:, h * 128:(h + 1) * 128], Kh[h][0][:, tsl], Qh[h][0][:, tsl], True, True,
                       [Kh[h][1], Qh[h][1]], [bsck])
                pt, ptk = bsa()
                tt(pt[:, :], bsc[:, :], mask, ALU.mult, [bsck, "cstb"], [ptk])
                PT[tI] = (pt, ptk)
                yield

            def phB(tI):
                pt, ptk = PT[tI]
                po, pok = ps[6], "ps6"
                pd, pdk = (ps[7], "ps7") if ni is not None else (None, None)
                for cc in range(128 // C):
                    cidx = tI * (128 // C) + cc
                    csl = slice(cc * C, (cc + 1) * C)
                    gsl = slice(tI * 128 + cc * C, tI * 128 + (cc + 1) * C)
                    for h in range(4):
                        osl = slice(h * 128 + cc * C, h * 128 + (cc + 1) * C)
                        mm(po[:, osl], vtok[:, tI, h * 128:(h + 1) * 128], pt[:, osl], True, False,
                           ["vtok%d" % tI, ptk], [pok])
                        mm(po[:, osl], la_sb[si][0:dk, h, :], Qh[h][0][:, gsl], False, True, [sbk, Qh[h][1]], [pok])
                        if ni is not None:
                            mm(pd[:, osl], CB("ones"), pt[:, osl], True, False, ["cstb", ptk], [pdk])
                            mm(pd[:, osl], la_sb[ni][0:dk, h, :], Qh[h][0][:, gsl], False, True,
                               ["la_sb%d" % ni, Qh[h][1]], [pdk])
                    for sidx, isn in ((si, False),) + (((ni, True),) if ni is not None else ()):
                        bu, buk = bank()
                        for h in range(4):
                            if C == 32 and cc == 3:
                                zsl = slice(64, 128)
                                mm(bu[0:dk, h * 128:(h + 1) * 128], ktz[zsl, tI % 2, h * dk:(h + 1) * dk],
                                   vtok[zsl, tI, h * 128:(h + 1) * 128], True, True,
                                   ["ktz%d" % (tI % 2), "vtok%d" % tI], [buk])
                            else:
                                rhs = CB("ones")[csl, :] if isn else vtok[csl, tI, h * 128:(h + 1) * 128]
                                mm(bu[0:dk, h * 128:(h + 1) * 128], ktok[csl, tI, h * dk:(h + 1) * dk], rhs, True, True,
                                   ["ktok%d" % tI, "vtok%d" % tI, "cstb"], [buk])
                        tmp_, tmpk = fsa()
                        tv = tmp_[0:dk, 0:512].rearrange("p (h v) -> p h v", h=4)
                        tt(tv, bu[0:dk, :].rearrange("p (h v) -> p h v", h=4), la_sf[sidx][0:dk, :, :], ALU.add,
                           [buk, "la_sf%d" % sidx], [tmpk])
                        gb = gm[0:dk, 0:4, cidx:cidx + 1].to_broadcast([dk, 4, 128])
                        tt(la_sb[sidx][0:dk, :, :], tv, gb, ALU.mult, [tmpk, gmk], ["la_sb%d" % sidx])
                        tt(la_sf[sidx][0:dk, :, :], tv, gb, ALU.mult, [tmpk, gmk], ["la_sf%d" % sidx])
                    yield
                fin(tI, po, pok, pd, pdk)
                yield

            NT_ = TS // 128
            drive([phA(0)])
            for tI in range(NT_):
                drive([phB(tI), phA(tI + 1) if tI + 1 < NT_ else None])

        def gam_start(ct, C):
            n = TS // C
            d_, dk_ = fsa()
            tt(d_[:, 0:n], gnb[ct][:, C:TS + 1:C], gnb[ct][:, 0:TS:C], ALU.subtract, ["gn%d" % ct], [dk_])
            act(gam[:, ct, 0:n], d_[:, 0:n], AF.Exp, [dk_], ["gam"], scale=-1.0)

        def qk_decay(ct, q_ap, qk_, k_ap, kk_, C, mid, Qd, Kd, kextra=None, clamp=False):
            gk = "gn%d" % ct
            n = TS // C
            off = C // 2 if mid else 0
            v3 = lambda ap: ap.rearrange("p (c t) -> p c t", t=C)
            gref = gnb[ct][:, off:TS:C].unsqueeze(2).to_broadcast([128, n, C])
            d_, dk_ = fsa(); ei, eik = fsa()
            tt(v3(d_[:, 0:TS]), v3(gnb[ct][:, 1:TS + 1]), gref, ALU.subtract, [gk], [dk_])
            act(ei[:, 0:TS], d_[:, 0:TS], AF.Exp, [dk_], [eik], scale=-1.0)
            if kextra is not None:
                tt(v3(d_[:, 0:TS]), v3(kextra[0][:, 0:TS]), gref, ALU.subtract, [kextra[1], gk, dk_], [dk_])
            act(d_[:, 0:TS], d_[:, 0:TS], AF.Exp, [dk_], [dk_])
            ev, evk = d_, dk_
            if clamp:
                tsc(ei[:, 0:TS], ei[:, 0:TS], 2.35e17, None, ALU.min, None, [eik], [eik])
                tsc(ev[:, 0:TS], ev[:, 0:TS], 2.35e17, None, ALU.min, None, [evk], [evk])
            tt(Qd[0][:, :], q_ap, ei[:, 0:TS], ALU.mult, [qk_, eik], [Qd[1]])
            tt(Kd[0][:, :], k_ap, ev[:, 0:TS], ALU.mult, [kk_, evk], [Kd[1]])

        BLk = lambda i: (BL[i], "bl%d" % i)
        LLk = lambda i: (LL[i], "ll%d" % i)

        def zgate(l, c0, h, func_scale_ap, skeys):
            pz, pzk = proj(l, c0 + h * 128, 128)
            z, zk = LLk(h)
            act(z[:, :], pz[:, :], AF.Silu, [pzk], [zk])
            if func_scale_ap is not None:
                tsc(z[:, :], z[:, :], func_scale_ap, None, ALU.mult, None, [zk] + skeys, [zk])
            return z, zk

        def rms_fin(GZ):
            def fin(tI, po, pok, pd, pdk):
                sq, sqk = bsa()
                act(sq[:, :], po[:, :], AF.Square, [pok], [sqk])
                b, bk = bank()
                mm(b[:, :], CB("ones"), sq[:, :], True, True, [sqk, "cstb"], [bk])
                rs, rsk = fsa()
                rsq(rs[:, 0:TS], b[:, :], 1.0, 128.0 * 1e-6, [bk], [rsk])
                t1, t1k = fsa()
                tt(t1[:, 0:TS], po[:, :], rs[:, 0:TS], ALU.mult, [pok, rsk], [t1k])
                for h in range(4):
                    tt(ys[:, h, tI * 128:(tI + 1) * 128], t1[:, h * 128:(h + 1) * 128],
                       GZ[h][0][:, tI * 128:(tI + 1) * 128], ALU.mult, [t1k, GZ[h][1]], ["ys"])
            return fin

        def heads64(QT, KT):
            Qh, Kh = [], []
            for ct in range(2):
                shift_hi(QT[ct][0][:, :], QT[ct][1], BL[16 + ct][0:64, :], "bl%d" % (16 + ct), TS)
                shift_hi(KT[ct][0][:, :], KT[ct][1], BL[18 + ct][0:64, :], "bl%d" % (18 + ct), TS)
                Qh += [(QT[ct][0][0:64, :], QT[ct][1]), (BL[16 + ct][0:64, :], "bl%d" % (16 + ct))]
                Kh += [(KT[ct][0][0:64, :], KT[ct][1]), (BL[18 + ct][0:64, :], "bl%d" % (18 + ct))]
            return Qh, Kh

        def gla(l, sI):
            proj_tok(l, C_GV)
            yield
            pg, pgk = proj(l, C_GC, 16)
            gc, gck = bsa()
            act(gc[0:16, 0:TS], pg[0:16, :], AF.Copy, [pgk], [gck])
            yield
            QT, KT = [], []
            for ct in range(2):
                yield
                b, bk = bank()
                mm(b[:, :], gup[:, ct * 128:(ct + 1) * 128], gc[0:16, 0:TS], True, True, ["gup", gck], [bk])
                e1, e1k = fsa()
                act(e1[:, 0:TS], b[:, :], AF.Exp, [bk, "drv"], [e1k], bias=drv[:, 30 + ct:31 + ct], scale=-1.0)
                act(e1[:, 0:TS], e1[:, 0:TS], AF.Ln, [e1k], [e1k], bias=1.0)
                tsc(e1[:, 0:TS], e1[:, 0:TS], 1.0 / 16.0, None, ALU.mult, None, [e1k], [e1k])
                cumsum(ct, e1[:, 0:TS], e1k)
                gam_start(ct, 128)
                yield
                pq, pqk = proj(l, C_GQ + ct * 128, 128)
                q, qk_ = fsa()
                act(q[:, 0:TS], pq[:, :], AF.Copy, [pqk], [qk_], scale=0.125)
                yield
                pk_, pkk = proj(l, C_GK + ct * 128, 128)
                k, kk_ = fsa()
                cp(k[:, 0:TS], pk_[:, :], [pkk], [kk_])
                qk_decay(ct, q[:, 0:TS], qk_, k[:, 0:TS], kk_, 128, False, BLk(ct), BLk(4 + ct))
                QT.append(BLk(ct)); KT.append(BLk(4 + ct))
            GZ = []
            for h in range(4):
                GZ.append(zgate(l, C_GZ, h, drv[:, 38 + h:39 + h], ["drv"]))
                yield
            Qh, Kh = heads64(QT, KT)
            build_gamh(2, 4)
            la(Qh, Kh, KT, 128, 64, 0, gamh, "gamh", rms_fin(GZ))

        def hgrn(l, sI):
            proj_tok(l, C_HI)
            yield
            QT, KT = [], []
            for ct in range(4):
                yield
                pf, pfk = proj(l, C_HF + ct * 128, 128)
                g, gk_ = fsa()
                act(g[:, 0:TS], pf[:, :], AF.Sigmoid, [pfk], [gk_])
                tsc(g[:, 0:TS], g[:, 0:TS], drv[:, 34 + ct:35 + ct], lbt[:, l * 4 + ct:l * 4 + ct + 1], ALU.mult, ALU.add,
                    [gk_, "drv", "lbt"], [gk_])
                lg, lgk = fsa()
                act(lg[:, 0:TS], g[:, 0:TS], AF.Ln, [gk_], [lgk])
                tsc(lg[:, 0:TS], lg[:, 0:TS], -1.0, None, ALU.mult, None, [lgk], [lgk])
                cumsum(ct, lg[:, 0:TS], lgk)
                d_, dk_ = fsa()
                tt(d_[:, 0:15], gnb[ct][:, 48:TS:32], gnb[ct][:, 16:TS - 32:32], ALU.subtract, ["gn%d" % ct], [dk_])
                tt(d_[:, 15:16], gnb[ct][:, TS:TS + 1], gnb[ct][:, TS - 16:TS - 15], ALU.subtract, ["gn%d" % ct], [dk_])
                act(gam[:, ct, 0:16], d_[:, 0:16], AF.Exp, [dk_], ["gam"], scale=-1.0)
                fc_, fck = fsa()
                act(fc_[:, 0:1], gnb[ct][:, 16:17], AF.Exp, ["gn%d" % ct], [fck], scale=-1.0)
                tsc(la_sf[3][:, ct, :], la_sf[3][:, ct, :], fc_[:, 0:1], None, ALU.mult, None,
                    [fck, "la_sf3"], ["la_sf3"])
                cp(la_sb[3][:, ct, :], la_sf[3][:, ct, :], ["la_sf3"], ["la_sb3"])
                tsc(g[:, 0:TS], g[:, 0:TS], -1.0, 1.0, ALU.mult, ALU.add, [gk_], [gk_])
                yield
                pq, pqk = proj(l, C_HQ + ct * 128, 128)
                q, qk_ = fsa()
                act(q[:, 0:TS], pq[:, :], AF.Silu, [pqk], [qk_])
                qk_decay(ct, q[:, 0:TS], qk_, g[:, 0:TS], gk_, 32, True, BLk(ct), BLk(4 + ct), clamp=True)
                QT.append(BLk(ct)); KT.append(BLk(4 + ct))
            GZ = []
            for h in range(4):
                GZ.append(zgate(l, C_HZ, h, drv[:, 42 + h:43 + h], ["drv"]))
                yield
            la(QT, KT, KT, 32, 128, 3, gam, "gam", rms_fin(GZ))

        def mlstm(l, sI):
            proj_tok(l, C_MV)
            yield
            w8, w8k = wchunk(win_d[l, :, C_MI:C_MI + 8], 8)
            reps = {}
            for ct in range(2):
                for gI in range(2):
                    for half in range(2):
                        r_, rk_ = BLk(8 + ct * 4 + gI * 2 + half)
                        rv_ = r_[:, :].rearrange("p (k j m) -> p k j m", k=4, j=2, m=64)
                        src = w8[:, half * 4:(half + 1) * 4, gI * 4 + ct * 2:gI * 4 + ct * 2 + 2]
                        cp(rv_, src.unsqueeze(3).to_broadcast([128, 4, 2, 64]), [w8k], [rk_])
                        reps[(ct, gI, half)] = (r_, rk_)
            for ct in range(4):
                yield
                pc, pck = proj(l, C_MQK + ct * 128, 128)
                cb, cbk = fsa()
                cp(cb[:, 0:3], cqc[:, ct, 0:3], ["cqc"], [cbk])
                act(cb[:, 3:TS + 3], pc[:, :], AF.Copy, [pck], [cbk])
                a, ak = fsa()
                oc = PK["conv"][0]
                tsc(a[:, 0:TS], cb[:, 0:TS], pk[:, l, oc + ct:oc + ct + 1], None, ALU.mult, None, [cbk, "pk"], [ak])
                for j in range(1, 4):
                    stt(a[:, 0:TS], cb[:, j:j + TS], pk[:, l, oc + j * 4 + ct:oc + j * 4 + ct + 1], a[:, 0:TS], ALU.mult,
                        ALU.add, [cbk, "pk", ak], [ak])
                cp(cqc[:, ct, 0:3], cb[:, TS:TS + 3], [cbk], ["cqc"])
                act(LL[4 + ct][:, :], a[:, 0:TS], AF.Silu, [ak], ["ll%d" % (4 + ct)])
            QT, KT = [], []
            for ct in range(2):
                yield
                pis = []
                for gI in range(2):
                    b, bk = bank()
                    for k in range(8):
                        r_, rk_ = reps[(ct, gI, k // 4)]
                        mm(b[:, :], r_[:, (k % 4) * 128:(k % 4 + 1) * 128], uT[:, k, :], k == 0, k == 7, [rk_, "uT"], [bk])
                    pis.append((b, bk))
                e1, e1k = fsa()
                act(e1[:, 0:TS], pis[1][0][:, :], AF.Exp, [pis[1][1], "drv"], [e1k], bias=drv[:, 32 + ct:33 + ct], scale=-1.0)
                act(e1[:, 0:TS], e1[:, 0:TS], AF.Ln, [e1k], [e1k], bias=1.0)
                cumsum(ct, e1[:, 0:TS], e1k)
                gam_start(ct, 128)
                ig, igk = fsa()
                act(ig[:, 0:TS], pis[0][0][:, :], AF.Identity, [pis[0][1], "pk"], [igk], bias=PKc(l, "i_b", ct))
                tt(ig[:, 0:TS], ig[:, 0:TS], gnb[ct][:, 1:TS + 1], ALU.add, [igk, "gn%d" % ct], [igk])
                k, kk_ = fsa()
                tsc(k[:, 0:TS], LL[6 + ct][:, :], 0.125, None, ALU.mult, None, ["ll%d" % (6 + ct)], [kk_])
                qk_decay(ct, LL[4 + ct][:, :], "ll%d" % (4 + ct), k[:, 0:TS], kk_, 128, False, BLk(ct), BLk(4 + ct),
                         kextra=(ig, igk))
                QT.append(BLk(ct)); KT.append(BLk(4 + ct))
            GZ = []
            for h in range(4):
                GZ.append(zgate(l, C_MZ, h, PKc(l, "ml_g", h), ["pk"]))
                yield

            def fin(tI, po, pok, pd, pdk):
                den, dnk = fsa()
                act(den[:, 0:TS], pd[:, :], AF.Abs, [pdk], [dnk])
                tsc(den[:, 0:TS], den[:, 0:TS], 1.0, None, ALU.max, None, [dnk], [dnk])
                recip(den[:, 0:TS], den[:, 0:TS], [dnk], [dnk])
                o, ok = fsa()
                tt(o[:, 0:TS], po[:, :], den[:, 0:TS], ALU.mult, [pok, dnk], [ok])
                b, bk = bank()
                mm(b[:, :], CS("ones"), o[:, 0:TS], True, True, [ok, "cst"], [bk])
                d, dk_ = fsa()
                stt(d[:, 0:TS], b[:, :], -1.0 / 128.0, o[:, 0:TS], ALU.mult, ALU.add, [bk, ok], [dk_])
                sq, sqk = bsa()
                act(sq[:, :], d[:, 0:TS], AF.Square, [dk_], [sqk])
                b2, b2k = bank()
                mm(b2[:, :], CB("ones"), sq[:, :], True, True, [sqk, "cstb"], [b2k])
                rs, rsk = fsa()
                rsq(rs[:, 0:TS], b2[:, :], 1.0 / 128.0, 1e-6, [b2k], [rsk])
                tt(d[:, 0:TS], d[:, 0:TS], rs[:, 0:TS], ALU.mult, [dk_, rsk], [dk_])
                for h in range(4):
                    tt(ys[:, h, tI * 128:(tI + 1) * 128], d[:, h * 128:(h + 1) * 128],
                       GZ[h][0][:, tI * 128:(tI + 1) * 128], ALU.mult, [dk_, GZ[h][1]], ["ys"])

            Qh, Kh = heads64(QT, KT)
            build_gamh(2, 4)
            la(Qh, Kh, KT, 128, 64, 1, gamh, "gamh", fin, ni=2)

        def rwkv(l, sI):
            C = 64
            NCH = TS // C

            def mixed(idx, c0, n, mucol, omcol, dst=None):
                pp, ppk = proj(l, c0, n)
                pb_, pbk_ = fsa()
                cp(pb_[0:n, 0:1], prc[0:n, idx:idx + 1], ["prc"], [pbk_])
                act(pb_[0:n, 1:TS + 1], pp[0:n, :], AF.Copy, [ppk], [pbk_])
                t_, tk_ = fsa()
                tsc(t_[0:n, 0:TS], pb_[0:n, 0:TS], mucol, None, ALU.mult, None, [pbk_, "pk"], [tk_])
                if dst is None:
                    o_, ok_ = fsa()
                    o_ = o_[:, 0:TS]
                else:
                    o_, ok_ = dst
                stt(o_[0:n, :], pb_[0:n, 1:TS + 1], omcol, t_[0:n, 0:TS], ALU.mult, ALU.add, [pbk_, "drv", tk_], [ok_])
                cp(prc[0:n, idx:idx + 1], pb_[0:n, TS:TS + 1], [pbk_], ["prc"])
                return o_, ok_

            WC = mixed(12, C_RW + 1536, 64, pk[0:64, l, PK["mu_wc"][0]:PK["mu_wc"][0] + 1], drv[0:64, 20:21],
                       dst=(wcac[:, 0, :], "wc"))
            AC = mixed(13, C_RW + 1600, 64, pk[0:64, l, PK["mu_ac"][0]:PK["mu_ac"][0] + 1], drv[0:64, 21:22],
                       dst=(wcac[:, 1, :], "ac"))
            yield
            act(wcacb[:, 0, :], WC[0][0:64, :], AF.Tanh, ["wc"], ["wcb"])
            cp(wcacb[:, 1, :], AC[0][0:64, :], ["ac"], ["acb"])
            ops = []
            BON = []
            for ct in range(4):
                csl = slice(ct * 128, (ct + 1) * 128)
                yield
                Kx, Kxk = mixed(4 + ct, C_RW + 512 + ct * 128, 128, PKc(l, "mu", 4 + ct), drv[:, 12 + ct:13 + ct])
                kk, kkk = fsa()
                tsc(kk[:, 0:TS], Kx[:, :], PKc(l, "k_k", ct), None, ALU.mult, None, [Kxk, "pk"], [kkk])
                sqb, sqbk = bsa()
                act(sqb[:, :], kk[:, 0:TS], AF.Square, [kkk], [sqbk])
                b3, b3k = bank()
                mm(b3[:, :], CB("bd"), sqb[:, :], True, True, ["cstb", sqbk], [b3k])
                sq, sqk = fsa()
                rsq(sq[:, 0:TS], b3[:, :], 1.0, 1e-24, [b3k], [sqk])
                tt(kk[:, 0:TS], kk[:, 0:TS], sq[:, 0:TS], ALU.mult, [kkk, sqk], [kkk])
                b, bk = bank()
                mm(b[:, :], wup[:, csl], wcacb[:, 0, :], True, True, ["wup", "wcb"], [bk])
                e1, e1k = fsa()
                act(e1[:, 0:TS], b[:, :], AF.Exp, [bk, "drv"], [e1k], bias=drv[:, 22 + ct:23 + ct], scale=-1.0)
                act(e1[:, 0:TS], e1[:, 0:TS], AF.Ln, [e1k], [e1k], bias=1.0)
                act(e1[:, 0:TS], e1[:, 0:TS], AF.Exp, [e1k], [e1k], bias=-0.5, scale=-1.0)
                cumsum(ct, e1[:, 0:TS], e1k)
                gk = "gn%d" % ct
                yield
                b2, b2k = bank()
                mm(b2[:, :], aup[:, csl], wcacb[:, 1, :], True, True, ["aup", "acb"], [b2k])
                sg, sgk = fsa()
                act(sg[:, 0:TS], b2[:, :], AF.Sigmoid, [b2k, "pk"], [sgk], bias=PKc(l, "a0", ct))
                t1, t1k = fsa()
                tsc(t1[:, 0:TS], sg[:, 0:TS], PKc(l, "k_a", ct), drv[:, 26 + ct:27 + ct], ALU.mult, ALU.add,
                    [sgk, "pk", "drv"], [t1k])
                tt(Kx[:, :], Kx[:, :], t1[:, 0:TS], ALU.mult, [Kxk, t1k], [Kxk])
                tt(sg[:, 0:TS], kk[:, 0:TS], sg[:, 0:TS], ALU.mult, [kkk, sgk], [sgk])
                v3 = lambda ap: ap.rearrange("p (c t) -> p c t", t=C)
                gref = gnb[ct][:, 0:TS:C].unsqueeze(2).to_broadcast([128, NCH, C])
                ei, eik = fsa(); ee, eek = fsa(); ev, evk = fsa()
                tt(v3(ev[:, 0:TS]), v3(gnb[ct][:, 1:TS + 1]), gref, ALU.subtract, [gk], [evk])
                tt(v3(ee[:, 0:TS]), v3(gnb[ct][:, 0:TS]), gref, ALU.subtract, [gk], [eek])
                act(ei[:, 0:TS], ev[:, 0:TS], AF.Exp, [evk], [eik], scale=-1.0)
                act(ev[:, 0:TS], ev[:, 0:TS], AF.Exp, [evk], [evk])
                act(ee[:, 0:TS], ee[:, 0:TS], AF.Exp, [eek], [eek], scale=-1.0)
                rt, at, btl, ktl, vb = [BLk(ct * 5 + j) for j in range(5)]
                stt(at[0][:, :], kk[:, 0:TS], -1.0, ee[:, 0:TS], ALU.mult, ALU.mult, [kkk, eek], [at[1]])
                tt(btl[0][:, :], sg[:, 0:TS], ev[:, 0:TS], ALU.mult, [sgk, evk], [btl[1]])
                tt(ktl[0][:, :], Kx[:, :], ev[:, 0:TS], ALU.mult, [Kxk, evk], [ktl[1]])
                cp(gam[:, ct, 0:NCH], ei[:, C - 1:TS:C], [eik], ["gam"])
                yield
                Rx, Rxk = mixed(ct, C_RW + ct * 128, 128, PKc(l, "mu", ct), drv[:, 8 + ct:9 + ct])
                tt(rt[0][:, :], Rx[:, :], ei[:, 0:TS], ALU.mult, [Rxk, eik], [rt[1]])
                rk, rkk = fsa()
                stt(rk[:, 0:TS], Rx[:, :], PKc(l, "r_k", ct), Kx[:, :], ALU.mult, ALU.mult, [Rxk, "pk", Kxk], [rkk])
                b4, b4k = bank()
                mm(b4[:, :], CS("bd"), rk[:, 0:TS], True, True, ["cst", rkk], [b4k])
                Vx, Vxk = mixed(8 + ct, C_RW + 1024 + ct * 128, 128, PKc(l, "mu", 8 + ct), drv[:, 16 + ct:17 + ct])
                cp(vb[0][:, :], Vx[:, :], [Vxk], [vb[1]])
                tt(LL[ct][:, :], b4[:, :], Vx[:, :], ALU.mult, [b4k, Vxk], ["ll%d" % ct])
                BON.append(LLk(ct))
                ops.append(dict(r=rt, a=at, b=btl, k=ktl, v=vb))

            hd = lambda h: (h // 2, (h % 2) * 64)
            SH = {}
            mhb = merged[:, 4:8, :].bitcast(BF16)
            for ct in range(4):
                for ni_, nm in enumerate(("r", "a", "b", "k")):
                    j = ct * 4 + ni_
                    if j < 8:
                        dst, dkey = mhb[:, j // 2, (j % 2) * 512:(j % 2 + 1) * 512], "mg%d" % (4 + j // 2)
                    elif j < 12:
                        dst, dkey = vtok[:, j - 8, :], "vtok%d" % (j - 8)
                    else:
                        dst, dkey = ktok[:, j - 12, :], "ktok%d" % (j - 12)
                    shift_hi(ops[ct][nm][0][:, :], ops[ct][nm][1], dst[0:64, :], dkey, TS)
                    SH[(ct, nm)] = (dst, dkey)
            build_gamh(4, NCH)

            def opn(nm, h):
                ct, par = h // 2, h % 2
                if par == 0:
                    return ops[ct][nm][0][0:64, :], ops[ct][nm][1]
                return SH[(ct, nm)][0][0:64, :], SH[(ct, nm)][1]

            llb = [LL[4 + i][:, :].bitcast(BF16) for i in range(4)]
            SETB = ("vt", "bt", "kt", "Aak", "Arb", "Ark", "Yf")
            rvs = [dict(rv), dict(rv)]
            rvk = [{n: "rv_" + n for n in rv}, {n: "rv_" + n for n in rv}]
            for i, n in enumerate(SETB):
                rvs[1][n] = llb[i // 2][0:64, (i % 2) * 512:(i % 2 + 1) * 512]
                rvk[1][n] = "ll%d" % (4 + i // 2)

            def phaseA(c):
                R, RK = rvs[c % 2], rvk[c % 2]
                c0 = c * C
                cs = slice(c0, c0 + C)
                for nm, key in (("v", "vt"), ("b", "bt"), ("k", "kt")):
                    bt_, btk2 = bank()
                    btb = bt_[:, :].bitcast(BF16)
                    for ct in range(4):
                        tr(btb[0:64, ct * 128:(ct + 1) * 128], ops[ct][nm][0][:, cs], CB("ident"),
                           [ops[ct][nm][1], "cstb"], [btk2])
                    cp(R[key][:, :], btb[0:64, 0:512], [btk2], [RK[key]])
                yield

                def scores(lhs, rhs, dst, maskn, eng):
                    b_, bk_ = bank()
                    for h in range(8):
                        L_, R_ = opn(lhs, h), opn(rhs, h)
                        mm(b_[0:64, h * 64:(h + 1) * 64], L_[0][:, cs], R_[0][:, cs], True, True, [L_[1], R_[1]], [bk_])
                    tt(R[dst][:, :], b_[0:64, :], CB(maskn)[0:64, :], ALU.mult, [bk_, "cstb"], [RK[dst]])
                scores("a", "b", "N", "r_ts", "dve")
                scores("b", "a", "NT", "r_st", "dve")
                yield
                scores("k", "a", "Aak", "r_st", "dve")
                scores("b", "r", "Arb", "r_sti", "dve")
                scores("k", "r", "Ark", "r_sti", "dve")
                tt(R["Y"][:, :], R["NT"][:, :], CB("i8")[0:64, :], ALU.add, [RK["NT"], "cstb"], [RK["Y"]])
                yield
                M, MT, Y = "N", "NT", "Y"
                M2, MT2, Y2 = "N2", "NT2", "Y2"
                for lev in range(5):
                    last = lev == 4
                    ba, bak = bank()
                    for h in range(8):
                        hs = slice(h * 64, (h + 1) * 64)
                        mm(ba[0:64, hs], R[MT][:, hs], R[M][:, hs], True, True, [RK[MT], RK[M]], [bak])
                    act(R[M2][:, :], ba[0:64, :], AF.Copy, [bak], [RK[M2]])
                    if not last:
                        bb_, bbk_ = bank()
                        for h in range(8):
                            hs = slice(h * 64, (h + 1) * 64)
                            mm(bb_[0:64, hs], R[M][:, hs], R[MT][:, hs], True, True, [RK[MT], RK[M]], [bbk_])
                        cp(R[MT2][:, :], bb_[0:64, :], [bbk_], [RK[MT2]])
                    bc, bck = bank()
                    for h in range(8):
                        hs = slice(h * 64, (h + 1) * 64)
                        mm(bc[0:64, hs], R[M2][:, hs], R[Y][:, hs], True, True, [RK[M2], RK[Y]], [bck])
                    Yd = "Yf" if last else Y2
                    tt(R[Yd][:, :], bc[0:64, :], R[Y][:, :], ALU.add, [bck, RK[Y]], [RK[Yd]])
                    M, M2 = M2, M
                    MT, MT2 = MT2, MT
                    Y, Y2 = Y2, Y
                    yield

            def phaseB(c):
                R, RK = rvs[c % 2], rvk[c % 2]
                c0 = c * C
                cs = slice(c0, c0 + C)
                bx, bxk = bank()
                for h in range(8):
                    hs = slice(h * 64, (h + 1) * 64)
                    A_ = opn("a", h)
                    mm(bx[0:64, hs], A_[0][:, cs], rw_sb[:, h, :], True, False, [A_[1], "rw_sb"], [bxk])
                    mm(bx[0:64, hs], R["Aak"][:, hs], R["vt"][:, hs], False, True, [RK["Aak"], RK["vt"]], [bxk])
                cp(R["X"][:, :], bx[0:64, :], [bxk], [RK["X"]])
                yield
                bu, buk = bank()
                for h in range(8):
                    hs = slice(h * 64, (h + 1) * 64)
                    mm(bu[0:64, hs], R["Yf"][:, hs], R["X"][:, hs], True, True, [RK["Yf"], RK["X"]], [buk])
                act(R["U"][:, :], bu[0:64, :], AF.Copy, [buk], [RK["U"]])
                yield
                bs_, bsk = bank()
                for h in range(8):
                    hs = slice(h * 64, (h + 1) * 64)
                    mm(bs_[0:64, hs], R["bt"][:, hs], R["U"][:, hs], True, False, [RK["bt"], RK["U"]], [bsk])
                    mm(bs_[0:64, hs], R["kt"][:, hs], R["vt"][:, hs], False, True, [RK["kt"], RK["vt"]], [bsk])
                by, byk = bank()
                for h in range(8):
                    ct, pb = hd(h)
                    hs = slice(h * 64, (h + 1) * 64)
                    o_ = by[pb:pb + 64, ct * 64:(ct + 1) * 64]
                    R_ = opn("r", h)
                    mm(o_, rw_sb[:, h, :], R_[0][:, cs], True, False, ["rw_sb", R_[1]], [byk])
                    mm(o_, R["U"][:, hs], R["Arb"][:, hs], False, False, [RK["U"], RK["Arb"]], [byk])
                    mm(o_, R["vt"][:, hs], R["Ark"][:, hs], False, True, [RK["vt"], RK["Ark"]], [byk])
                tmp_, tmpk = fsa()
                tv = tmp_[0:64, 0:512].rearrange("p (h v) -> p h v", h=8)
                tt(tv, bs_[0:64, :].rearrange("p (h v) -> p h v", h=8), rw_sf[:, :, :], ALU.add, [bsk, "rw_sf"], [tmpk])
                gb = gamh[0:64, 0:8, c:c + 1].to_broadcast([64, 8, 64])
                tt(rw_sb[:, :, :], tv, gb, ALU.mult, [tmpk, "gamh"], ["rw_sb"])
                tt(rw_sf[:, :, :], tv, gb, ALU.mult, [tmpk, "gamh"], ["rw_sf"])
                act(yT[:, 0:4, cs], by[:, 0:256].rearrange("p (c t) -> p c t", c=4), AF.Copy, [byk],
                    ["mg0", "mg1", "mg2", "mg3"])
                yield

            drive([phaseA(0)])
            for c in range(NCH):
                drive([phaseB(c), phaseA(c + 1) if c + 1 < NCH else None])
            for ct in range(4):
                mk = "mg%d" % ct
                b, bk = bank()
                mm(b[:, :], CS("bd"), yT[:, ct, :], True, True, ["cst", mk], [bk])
                d, dk_ = fsa()
                stt(d[:, 0:TS], b[:, :], -1.0 / 64.0, yT[:, ct, :], ALU.mult, ALU.add, [bk, mk], [dk_])
                sqb, sqbk = bsa()
                act(sqb[:, :], d[:, 0:TS], AF.Square, [dk_], [sqbk])
                b2, b2k = bank()
                mm(b2[:, :], CB("bd"), sqb[:, :], True, True, ["cstb", sqbk], [b2k])
                sq, sqk = fsa()
                rsq(sq[:, 0:TS], b2[:, :], 1.0 / 64.0, 64e-5, [b2k], [sqk])
                tt(d[:, 0:TS], d[:, 0:TS], sq[:, 0:TS], ALU.mult, [dk_, sqk], [dk_])
                act(d[:, 0:TS], d[:, 0:TS], AF.Identity, [dk_, "pk"], [dk_], bias=PKc(l, "ln_b", ct), scale=PKc(l, "ln_g", ct))
                tt(d[:, 0:TS], d[:, 0:TS], BON[ct][0][:, :], ALU.add, [dk_, BON[ct][1]], [dk_])
                pz, pzk = proj(l, C_RWZ + ct * 128, 128)
                z, zk = fsa()
                act(z[:, 0:TS], pz[:, :], AF.Silu, [pzk], [zk])
                tt(ys[:, ct, :], d[:, 0:TS], z[:, 0:TS], ALU.mult, [dk_, zk], ["ys"])

        outs = program()
        S.emit(final_wait_ops=outs)
    return nc


_NC_CACHE = {}


def make_in_maps(inp, n_cores=8):
    f = lambda a: np.ascontiguousarray(np.asarray(a, dtype=np.float32))
    pkall = np.stack([_pack_params(inp, l) for l in range(DEPTH)], 0)
    fgT = np.ascontiguousarray(f(inp["final_g"]).reshape(8, 128).T)
    shared = {
        "pk": pkall, "fg": fgT, "cst": CONSTS, "cstf": CSTF,
        "ada_w": f(inp["ada_w"]), "w_in": f(inp["w_in"]), "rw_w_up": f(inp["rw_w_up"]), "rw_a_up": f(inp["rw_a_up"]),
        "gla_gk_up": f(inp["gla_gk_up"]), "w_branch": f(inp["w_branch"]), "w_out": f(inp["w_out"]),
    }
    maps = []
    for b in range(n_cores):
        m = dict(shared)
        m["xT"] = np.ascontiguousarray(f(inp["x"][b]).T)
        m["cT"] = np.ascontiguousarray(f(inp["c"][b]).reshape(8, 128).T)
        maps.append(m)
    return maps


def kernel(**inputs):
    inp = {k: np.asarray(v) for k, v in inputs.items()}
    if "full" not in _NC_CACHE:
        _NC_CACHE["full"] = build_nc()
    nc = _NC_CACHE["full"]
    maps = make_in_maps(inp)
    res = run_bass_kernel_spmd(nc, maps, core_ids=list(range(8)))
    out = np.stack([np.ascontiguousarray(r["outT"].T) for r in res.results], 0)
    return out.astype(np.float32)
```
